# Optimizing a Trainium2 kernel written in Bass

```python
import functools
import jax, jax.numpy as jnp
from jax import lax
import numpy as np

D_MODEL = 1024
BATCH = 8
SEQ = 2048
DEPTH = 2

GRID_W = 64
CTX_LEN = 256
N_MIXERS = 2
CHUNK = 64
EPS = 1e-6
N_ADA = 6

M_HEADS = 4
M_DV = D_MODEL // M_HEADS
M_DK = M_DV // 2
M_CONV = 3
M_QK = M_HEADS * M_DK
M_V = M_HEADS * M_DV
M_PROJ = 2 * M_QK + 2 * M_V + 4 * M_HEADS

H_HEADS = 8
H_DK = 128
H_DV = D_MODEL // H_HEADS
H_K = H_HEADS * H_DK
H_V = H_HEADS * H_DV
H_PROJ = 3 * H_K + 2 * H_V

N_EXPERTS = 16
N_GROUPS = 4
E_PER_GROUP = N_EXPERTS // N_GROUPS
TOP_K = 2
D_EXPERT = 512

N_MLSTM = (DEPTH + 1) // 2
N_HGRN = DEPTH // 2

kernel_name = 'hybrid_mlstm_hgrn2_moe_dit'


def rmsnorm(x, g):
    xf = x.astype(jnp.float32)
    y = xf * lax.rsqrt(jnp.mean(xf * xf, axis=-1, keepdims=True) + EPS)
    return (y * g.astype(jnp.float32)).astype(x.dtype)


def to_heads(t, n_heads):
    b, l, _ = t.shape
    return t.reshape(b, l, n_heads, -1).transpose(0, 2, 1, 3)


def head_rmsnorm(o, g, n_heads):
    b, _, l, _ = o.shape
    y = o * lax.rsqrt(jnp.mean(o * o, axis=-1, keepdims=True) + EPS)
    y = y * g.astype(jnp.float32).reshape(n_heads, 1, -1)
    return y.transpose(0, 2, 1, 3).reshape(b, l, -1)


def centred_depthwise_conv(x, w, b):
    width, ch = w.shape
    y = lax.conv_general_dilated(x, w[:, None, :].astype(x.dtype), window_strides=(1,),
                                 padding=[(width // 2, width // 2)],
                                 dimension_numbers=('NWC', 'WIO', 'NWC'), feature_group_count=ch)
    return y + b.astype(x.dtype)


def raster_to_column(t):
    b, l, d = t.shape
    rows = l // GRID_W
    return t.reshape(b, rows, GRID_W, d).transpose(0, 2, 1, 3).reshape(b, l, d)


def column_to_raster(t):
    b, l, d = t.shape
    rows = l // GRID_W
    return t.reshape(b, GRID_W, rows, d).transpose(0, 2, 1, 3).reshape(b, l, d)


def to_chunks(t):
    b, h, l = t.shape[:3]
    return jnp.moveaxis(t.reshape(b, h, l // CHUNK, CHUNK, *t.shape[3:]), 2, 0)


def from_chunks(t):
    t = jnp.moveaxis(t, 0, 2)
    return t.reshape(t.shape[0], t.shape[1], -1, *t.shape[4:])


def mlstm_chunk_step(carry, xs, need_out):
    c_mem, n_mem, m_prev = carry
    q, k, v, ig, lf = xs
    b = jnp.cumsum(lf, axis=-1)
    b_last = b[..., -1]
    g = b_last[..., None] - b + ig
    m_new = jnp.maximum(b_last + m_prev, jnp.max(g, axis=-1))
    w_s = jnp.exp(g - m_new[..., None])
    carry_decay = jnp.exp(b_last + m_prev - m_new)
    c_new = carry_decay[..., None, None] * c_mem + jnp.einsum('bhs,bhsv,bhsk->bhvk', w_s, v, k)
    n_new = carry_decay[..., None] * n_mem + jnp.einsum('bhs,bhsk->bhk', w_s, k)
    new_carry = (c_new, n_new, m_new)
    if not need_out:
        return new_carry, None
    prefix = jnp.tril(jnp.ones((CHUNK, CHUNK), dtype=bool))
    log_d = jnp.where(prefix, b[..., :, None] - b[..., None, :] + ig[..., None, :], -jnp.inf)
    log_inter = b + m_prev[..., None]
    m_t = jnp.maximum(log_inter, jnp.max(log_d, axis=-1))
    w_ts = jnp.exp(log_d - m_t[..., None]) * jnp.einsum('bhtk,bhsk->bhts', q, k)
    inter = jnp.exp(log_inter - m_t)
    num = jnp.einsum('bhts,bhsv->bhtv', w_ts, v) + inter[..., None] * jnp.einsum('bhvk,bhtk->bhtv', c_mem, q)
    den = jnp.sum(w_ts, axis=-1) + inter * jnp.einsum('bhk,bhtk->bht', n_mem, q)
    h = num / jnp.maximum(jnp.abs(den), jnp.exp(-m_t))[..., None]
    return new_carry, h


def hgrn2_chunk_step(s_mem, xs, need_out):
    q, k, lf, i = xs
    b = jnp.cumsum(lf, axis=2)
    b_last = b[:, :, -1:, :]
    s_new = jnp.exp(b_last[:, :, 0, :])[..., None] * s_mem + jnp.einsum('bhsk,bhsv->bhkv', k * jnp.exp(b_last - b), i)
    if not need_out:
        return s_new, None
    prefix = jnp.tril(jnp.ones((CHUNK, CHUNK), dtype=bool))[:, :, None]
    decay = jnp.exp(jnp.where(prefix, b[:, :, :, None, :] - b[:, :, None, :, :], -jnp.inf))
    a = jnp.einsum('bhtk,bhsk,bhtsk->bhts', q, k, decay)
    o = jnp.einsum('bhts,bhsv->bhtv', a, i) + jnp.einsum('bhtk,bhkv->bhtv', q * jnp.exp(b), s_mem)
    return s_new, o


def directional_scan(step, init, ctx_xs, lat_xs, reverse, need_ctx_out):
    if reverse:
        ctx_xs = tuple(jnp.flip(t, axis=2) for t in ctx_xs)
        lat_xs = tuple(jnp.flip(t, axis=2) for t in lat_xs)
    ctx_state, ctx_out = lax.scan(functools.partial(step, need_out=need_ctx_out), init,
                                  tuple(to_chunks(t) for t in ctx_xs))
    _, lat_out = lax.scan(functools.partial(step, need_out=True), ctx_state,
                          tuple(to_chunks(t) for t in lat_xs))
    lat_out = from_chunks(lat_out)
    ctx_out = from_chunks(ctx_out) if need_ctx_out else None
    if reverse:
        lat_out = jnp.flip(lat_out, axis=2)
        ctx_out = jnp.flip(ctx_out, axis=2) if need_ctx_out else None
    return lat_out, ctx_out


def mlstm_mixer(h_lat, h_ctx, w_in, conv_w, conv_b, gate_b, head_g, w_out, need_ctx_out):
    f32 = jnp.float32

    def prepare(h):
        b, l, _ = h.shape
        qk, v, o, gates = jnp.split(h @ w_in, [2 * M_QK, 2 * M_QK + M_V, 2 * M_QK + 2 * M_V], axis=-1)
        qk = jax.nn.silu(centred_depthwise_conv(qk, conv_w, conv_b)).astype(f32)
        q = to_heads(qk[..., :M_QK], M_HEADS) * (M_DK ** -0.5)
        k = to_heads(qk[..., M_QK:], M_HEADS)
        v = to_heads(v.astype(f32), M_HEADS)
        gates = (gates.astype(f32) + gate_b.astype(f32)).reshape(b, l, 4, M_HEADS).transpose(2, 0, 3, 1)
        fwd = (q, k, v, gates[0], jax.nn.log_sigmoid(gates[1]))
        bwd = (q, k, v, gates[2], jax.nn.log_sigmoid(gates[3]))
        return (fwd, bwd), o

    lat_dirs, o_lat = prepare(h_lat)
    ctx_dirs, o_ctx = prepare(h_ctx)
    bsz = h_lat.shape[0]
    init = (jnp.zeros((bsz, M_HEADS, M_DV, M_DK), f32), jnp.zeros((bsz, M_HEADS, M_DK), f32),
            jnp.zeros((bsz, M_HEADS), f32))
    lat_f, ctx_f = directional_scan(mlstm_chunk_step, init, ctx_dirs[0], lat_dirs[0], False, need_ctx_out)
    lat_b, ctx_b = directional_scan(mlstm_chunk_step, init, ctx_dirs[1], lat_dirs[1], True, need_ctx_out)

    def read_out(hs, o):
        return (head_rmsnorm(hs, head_g, M_HEADS).astype(o.dtype) * jax.nn.sigmoid(o)) @ w_out

    y_lat = read_out(lat_f + lat_b, o_lat)
    y_ctx = read_out(ctx_f + ctx_b, o_ctx) if need_ctx_out else None
    return y_lat, y_ctx


def hgrn2_mixer(h_lat, h_ctx, w_in, lb, head_g, w_out, need_ctx_out):
    f32 = jnp.float32
    lb = lb.reshape(2, H_HEADS, 1, H_DK)
    log_lb = jnp.log(lb)
    log_1mlb = jnp.log1p(-lb)

    def prepare(h):
        q, z_fw, z_bw, i, g = jnp.split(h @ w_in, [H_K, 2 * H_K, 3 * H_K, 3 * H_K + H_V], axis=-1)
        q = to_heads(jax.nn.silu(q.astype(f32)), H_HEADS)
        i = to_heads(i.astype(f32), H_HEADS)
        dirs = []
        for d, z in enumerate((z_fw, z_bw)):
            z = to_heads(z.astype(f32), H_HEADS)
            log_f = jnp.logaddexp(log_lb[d], log_1mlb[d] + jax.nn.log_sigmoid(z))
            k = jnp.exp(log_1mlb[d]) * jax.nn.sigmoid(-z)
            dirs.append((q, k, log_f, i))
        return dirs, g

    lat_dirs, g_lat = prepare(h_lat)
    ctx_dirs, g_ctx = prepare(h_ctx)
    init = jnp.zeros((h_lat.shape[0], H_HEADS, H_DK, H_DV), f32)
    lat_f, ctx_f = directional_scan(hgrn2_chunk_step, init, ctx_dirs[0], lat_dirs[0], False, need_ctx_out)
    lat_b, ctx_b = directional_scan(hgrn2_chunk_step, init, ctx_dirs[1], lat_dirs[1], True, need_ctx_out)

    def read_out(os, g):
        return (head_rmsnorm(os, head_g, H_HEADS).astype(g.dtype) * jax.nn.silu(g)) @ w_out

    y_lat = read_out(lat_f + lat_b, g_lat)
    y_ctx = read_out(ctx_f + ctx_b, g_ctx) if need_ctx_out else None
    return y_lat, y_ctx


def moe_ffn(h, router_w, router_bias, w_gate, w_up, w_down):
    f32 = jnp.float32
    s = jax.nn.sigmoid((h @ router_w).astype(f32))
    sel = s + router_bias.astype(f32)
    group_score = jnp.sum(lax.top_k(sel.reshape(-1, N_GROUPS, E_PER_GROUP), TOP_K)[0], axis=-1)
    best = jnp.argmax(group_score, axis=-1)
    in_group = (jnp.arange(N_EXPERTS) // E_PER_GROUP)[None, :] == best[:, None]
    _, idx = lax.top_k(jnp.where(in_group, sel, -jnp.inf), TOP_K)
    w = jnp.take_along_axis(s, idx, axis=-1)
    w = w / jnp.sum(w, axis=-1, keepdims=True)
    combine = jnp.sum(jax.nn.one_hot(idx, N_EXPERTS, dtype=f32) * w[..., None], axis=1)
    out = jnp.zeros(h.shape, f32)
    for e in range(N_EXPERTS):
        y = (jax.nn.silu(h @ w_gate[e]) * (h @ w_up[e])) @ w_down[e]
        out = out + combine[:, e:e + 1] * y.astype(f32)
    return out.astype(h.dtype)


def setup_inputs(seed: int = 0) -> dict:
    key = jax.random.key(seed)
    ks = iter(jax.random.split(key, 32))
    f32 = jnp.float32

    def w(shape, fan_in, scale=1.0):
        return (scale * fan_in ** -0.5) * jax.random.normal(next(ks), shape, f32)

    def gain(shape):
        return 1.0 + 0.05 * jax.random.normal(next(ks), shape, f32)

    def small(shape, s=0.02):
        return s * jax.random.normal(next(ks), shape, f32)

    forget_base = jnp.linspace(3.0, 6.0, M_HEADS, dtype=f32)
    zeros_h = jnp.zeros((M_HEADS,), f32)
    gate_base = jnp.concatenate([zeros_h, forget_base, zeros_h, forget_base])
    return {
        'x': jax.random.normal(next(ks), (BATCH, SEQ, D_MODEL), f32),
        'c': jax.random.normal(next(ks), (BATCH, D_MODEL), f32),
        'ctx': jax.random.normal(next(ks), (BATCH, CTX_LEN, D_MODEL), f32),
        'c_ctx': jax.random.normal(next(ks), (D_MODEL,), f32),
        'ada_w': w((DEPTH, D_MODEL, N_ADA * D_MODEL), D_MODEL, 0.5),
        'ada_b': small((DEPTH, N_ADA * D_MODEL)),
        'norm_mix_g': gain((DEPTH, D_MODEL)),
        'norm_ffn_g': gain((DEPTH, D_MODEL)),
        'final_g': gain((D_MODEL,)),
        'm_w_in': w((N_MLSTM, D_MODEL, M_PROJ), D_MODEL),
        'm_conv_w': w((N_MLSTM, M_CONV, 2 * M_QK), M_CONV),
        'm_conv_b': small((N_MLSTM, 2 * M_QK)),
        'm_gate_b': gate_base + small((N_MLSTM, 4 * M_HEADS), 0.1),
        'm_head_g': gain((N_MLSTM, M_V)),
        'm_w_out': w((N_MLSTM, M_V, D_MODEL), M_V),
        'h_w_in': w((N_HGRN, D_MODEL, H_PROJ), D_MODEL),
        'h_lower_bounds': small((DEPTH, 2 * H_K), 0.1),
        'h_head_g': gain((N_HGRN, H_V)),
        'h_w_out': w((N_HGRN, H_V, D_MODEL), H_V),
        'router_w': w((D_MODEL, N_EXPERTS), D_MODEL),
        'router_bias': small((N_EXPERTS,), 0.01),
        'e_w_gate': w((DEPTH, N_EXPERTS, D_MODEL, D_EXPERT), D_MODEL),
        'e_w_up': w((DEPTH, N_EXPERTS, D_MODEL, D_EXPERT), D_MODEL),
        'e_w_down': w((DEPTH, N_EXPERTS, D_EXPERT, D_MODEL), D_EXPERT),
    }


def reference(x, c, ctx, c_ctx, ada_w, ada_b, norm_mix_g, norm_ffn_g, final_g,
              m_w_in, m_conv_w, m_conv_b, m_gate_b, m_head_g, m_w_out,
              h_w_in, h_lower_bounds, h_head_g, h_w_out,
              router_w, router_bias, e_w_gate, e_w_up, e_w_down):
    bsz, seq, d = x.shape
    lbs = jnp.cumsum(jax.nn.softmax(h_lower_bounds.astype(jnp.float32), axis=0), axis=0)
    lbs = lbs - lbs[0]
    s_lat, s_ctx = x, ctx
    for i in range(DEPTH):
        last = i == DEPTH - 1
        j = i // N_MIXERS
        mod = jax.nn.silu(c) @ ada_w[i] + ada_b[i]
        mod_c = jax.nn.silu(c_ctx) @ ada_w[i] + ada_b[i]
        sh1, sc1, g1, sh2, sc2, g2 = [t[:, None, :] for t in jnp.split(mod, N_ADA, axis=-1)]
        csh1, csc1, cg1, csh2, csc2, cg2 = jnp.split(mod_c, N_ADA, axis=-1)
        h_lat = rmsnorm(s_lat, norm_mix_g[i]) * (1 + sc1) + sh1
        h_ctx = rmsnorm(s_ctx, norm_mix_g[i]) * (1 + csc1) + csh1
        if i % N_MIXERS == 0:
            y_lat, y_ctx = mlstm_mixer(h_lat, h_ctx, m_w_in[j], m_conv_w[j], m_conv_b[j], m_gate_b[j],
                                       m_head_g[j], m_w_out[j], not last)
        else:
            y_lat, y_ctx = hgrn2_mixer(raster_to_column(h_lat), h_ctx, h_w_in[j], lbs[i],
                                       h_head_g[j], h_w_out[j], not last)
            y_lat = column_to_raster(y_lat)
        s_lat = s_lat + g1 * y_lat
        h2_lat = rmsnorm(s_lat, norm_ffn_g[i]) * (1 + sc2) + sh2
        if last:
            f_lat = moe_ffn(h2_lat.reshape(-1, d), router_w, router_bias, e_w_gate[i], e_w_up[i], e_w_down[i])
            s_lat = s_lat + g2 * f_lat.reshape(bsz, seq, d)
        else:
            s_ctx = s_ctx + cg1 * y_ctx
            h2_ctx = rmsnorm(s_ctx, norm_ffn_g[i]) * (1 + csc2) + csh2
            tokens = jnp.concatenate([h2_lat.reshape(-1, d), h2_ctx.reshape(-1, d)], axis=0)
            f = moe_ffn(tokens, router_w, router_bias, e_w_gate[i], e_w_up[i], e_w_down[i])
            n_lat = bsz * seq
            s_lat = s_lat + g2 * f[:n_lat].reshape(bsz, seq, d)
            s_ctx = s_ctx + cg2 * f[n_lat:].reshape(s_ctx.shape)
    return rmsnorm(s_lat, final_g)
```

```python
import numpy as np
import concourse.bass as bass
import concourse.mybir as mybir
from contextlib import ExitStack
from concourse.bass_utils import run_bass_kernel_spmd

F32 = mybir.dt.float32
BF16 = mybir.dt.bfloat16
AF = mybir.ActivationFunctionType
ALU = mybir.AluOpType
AX = mybir.AxisListType

D = 1024
SEQ = 2048
CTX = 256
NT = SEQ + CTX
NTILE = NT // 128
EPS = 1e-6
NE = 16
DEXP = 512
ENGS = ("pe", "act", "dve", "pool", "sp")
DEBUG_NT = None


class Op:
    __slots__ = ("eng", "fn", "deps", "signal", "seq", "is_dma", "dsem", "dval", "n_inst", "name", "cost")

    def __init__(self, eng, fn, is_dma, dsem, name):
        self.eng = eng
        self.fn = fn
        self.deps = set()
        self.signal = False
        self.seq = None
        self.is_dma = is_dma
        self.dsem = dsem
        self.dval = None
        self.n_inst = 1
        self.name = name
        self.cost = None


class Prog:
    def __init__(self, nc):
        self.nc = nc
        self.ops = {e: [] for e in ENGS}
        self.last_w = {}
        self.readers = {}
        self.all_ops = []
        self._bar_from = 0
        self._capture = None

    def _add(self, eng, fn, reads, writes, is_dma=False, dsem=None, name=None):
        o = Op(eng, fn, is_dma, dsem, name)
        for k in reads:
            w = self.last_w.get(k)
            if w is not None:
                o.deps.add(w)
        for k in writes:
            w = self.last_w.get(k)
            if w is not None:
                o.deps.add(w)
            for r in self.readers.get(k, ()):
                o.deps.add(r)
        for k in reads:
            self.readers.setdefault(k, []).append(o)
        for k in writes:
            self.last_w[k] = o
            self.readers[k] = []
        o.deps.discard(o)
        self.ops[eng].append(o)
        self.all_ops.append(o)
        return o

    def op(self, eng, fn, reads=(), writes=(), name=None):
        if self._capture is not None:
            self._capture.append(("op", eng, fn, tuple(reads), tuple(writes), None, 1))
            return None
        return self._add(eng, fn, reads, writes, name=name)

    def dma(self, eng, group, fn, reads=(), writes=(), n=1, name=None):
        if self._capture is not None:
            self._capture.append(("dma", eng, fn, tuple(reads), tuple(writes), group, n))
            return None
        o = self._add(eng, fn, reads, writes, is_dma=True, dsem=group, name=name)
        o.n_inst = n
        return o

    def interleave(self, builders):
        streams = []
        for b in builders:
            self._capture = []
            b()
            streams.append(self._capture)
            self._capture = None
        idx = [0] * len(streams)
        while any(idx[i] < len(st) for i, st in enumerate(streams)):
            for i, st in enumerate(streams):
                if idx[i] < len(st):
                    kind, eng, fn, reads, writes, group, n = st[idx[i]]
                    idx[i] += 1
                    if kind == "op":
                        self._add(eng, fn, reads, writes)
                    else:
                        o = self._add(eng, fn, reads, writes, is_dma=True, dsem=group)
                        o.n_inst = n

    def barrier(self):
        self.all_ops.append(None)

    def _schedule(self, seg):
        import heapq
        COST = {"pe": 0.16, "act": 0.42, "dve": 0.45, "pool": 0.6, "sp": 0.1}
        HOP = 0.25
        n = len(seg)
        idx = {id(o): i for i, o in enumerate(seg)}
        cost = [0.0] * n
        lat = [0.0] * n
        for i, o in enumerate(seg):
            c = o.cost if o.cost is not None else COST[o.eng]
            if o.is_dma:
                cost[i] = 0.08 * o.n_inst
                lat[i] = c if o.cost is not None else 2.5
            else:
                cost[i] = c
        deps = [[idx[id(d)] for d in o.deps if id(d) in idx] for o in seg]
        succ = [[] for _ in range(n)]
        for i, dl in enumerate(deps):
            for d in dl:
                succ[d].append(i)
        prio = [0.0] * n
        for i in range(n - 1, -1, -1):
            m = 0.0
            for j in succ[i]:
                if prio[j] > m:
                    m = prio[j]
            prio[i] = m + cost[i] + lat[i] + HOP
        ndep = [len(dl) for dl in deps]
        est = [0.0] * n
        fin = [0.0] * n
        free_at = {e: 0.0 for e in ENGS}
        pend = {e: [] for e in ENGS}
        avail = {e: [] for e in ENGS}
        order = {e: [] for e in ENGS}
        for i in range(n):
            if ndep[i] == 0:
                heapq.heappush(pend[seg[i].eng], (0.0, -prio[i], i))
        left = n
        while left:
            best_e, best_t = None, None
            for e in ENGS:
                if not pend[e] and not avail[e]:
                    continue
                while pend[e] and pend[e][0][0] <= free_at[e]:
                    t_, p_, i_ = heapq.heappop(pend[e])
                    heapq.heappush(avail[e], (p_, i_))
                t = free_at[e] if avail[e] else max(free_at[e], pend[e][0][0])
                if best_t is None or t < best_t:
                    best_e, best_t = e, t
            e = best_e
            if avail[e]:
                p_, i = heapq.heappop(avail[e])
            else:
                t_, p_, i = heapq.heappop(pend[e])
            start = max(free_at[e], est[i])
            free_at[e] = start + cost[i]
            fin[i] = start + cost[i] + lat[i]
            order[e].append(seg[i])
            left -= 1
            for j in succ[i]:
                if fin[i] + HOP > est[j]:
                    est[j] = fin[i] + HOP
                ndep[j] -= 1
                if ndep[j] == 0:
                    heapq.heappush(pend[seg[j].eng], (est[j], -prio[j], j))
        return order

    def emit(self, final_wait_ops=(), schedule=True):
        nc = self.nc
        segs, cur = [], []
        for o in self.all_ops:
            if o is None:
                if cur:
                    segs.append(cur)
                cur = []
            else:
                cur.append(o)
        if cur:
            segs.append(cur)
        seg_orders = []
        for seg in segs:
            if schedule:
                seg_orders.append(self._schedule(seg))
            else:
                od = {e: [] for e in ENGS}
                for o in seg:
                    od[o.eng].append(o)
                seg_orders.append(od)
        bar_deps = [set() for _ in segs]
        last_comp = {}
        for k, od in enumerate(seg_orders):
            if k > 0:
                bar_deps[k] = set(last_comp.values()) | {o for o in segs[k - 1] if o.is_dma}
            for e in ENGS:
                comp = [o for o in od[e] if not o.is_dma]
                if comp:
                    last_comp[e] = comp[-1]
        for o in (x for x in self.all_ops if x is not None):
            for d in o.deps:
                if d.eng == "pe" and o.eng == "pe" and not d.is_dma and not o.is_dma:
                    continue
                d.signal = True
        for bd in bar_deps:
            for d in bd:
                d.signal = True
        for o in final_wait_ops:
            o.signal = True
        with ExitStack() as es:
            esem = {e: es.enter_context(nc.semaphore("c_" + e)) for e in ENGS}
            gsem = {}
            gcount = {}
            for e in ENGS:
                for od in seg_orders:
                    for o in od[e]:
                        if o.is_dma:
                            if o.dsem not in gsem:
                                gsem[o.dsem] = es.enter_context(nc.semaphore("d_" + str(o.dsem)))
                                gcount[o.dsem] = 0
                            gcount[o.dsem] += 16 * o.n_inst
                            o.dval = gcount[o.dsem]
            for e in ENGS:
                c = 0
                for od in seg_orders:
                    for o in od[e]:
                        if not o.is_dma and o.signal:
                            c += 1
                            o.seq = c
            block = es.enter_context(nc.Block())
            engobj = {"pe": "tensor", "act": "scalar", "dve": "vector", "pool": "gpsimd", "sp": "sync"}

            def run(ename, eng):
                waited = {}

                def do_waits(deps, is_pe_compute):
                    need = {}
                    for d in deps:
                        if d.is_dma:
                            key, val, sem = ("g", d.dsem), d.dval, gsem[d.dsem]
                        else:
                            if d.eng == "pe" and ename == "pe" and is_pe_compute:
                                continue
                            if d.eng == ename and ename == "pe":
                                continue
                            key, val, sem = ("e", d.eng), d.seq, esem[d.eng]
                        if waited.get(key, 0) >= val:
                            continue
                        if key not in need or need[key][1] < val:
                            need[key] = (sem, val)
                    for key, (sem, val) in need.items():
                        eng.wait_ge(sem, val)
                        waited[key] = val

                for k, od in enumerate(seg_orders):
                    if bar_deps[k]:
                        do_waits([d for d in bar_deps[k] if not (d.eng == ename and not d.is_dma)], False)
                    for o in od[ename]:
                        do_waits(o.deps, not o.is_dma)
                        r = o.fn(eng)
                        if o.is_dma:
                            insts = r if isinstance(r, (list, tuple)) else [r]
                            assert len(insts) == o.n_inst
                            for ins in insts:
                                ins.then_inc(gsem[o.dsem], 16)
                        elif o.signal:
                            ins = r[-1] if isinstance(r, (list, tuple)) else r
                            ins.then_inc(esem[ename], 1)
                if ename == "sp":
                    for o in final_wait_ops:
                        if o.is_dma:
                            eng.wait_ge(gsem[o.dsem], o.dval)
                        else:
                            eng.wait_ge(esem[o.eng], o.seq)

            for ename in ENGS:
                getattr(block, engobj[ename])(lambda eng, ename=ename: run(ename, eng))


def _consts():
    s = np.arange(128)[:, None]
    t = np.arange(128)[None, :]
    same = (s // 64) == (t // 64)
    c = {}
    c["ident"] = np.eye(128, dtype=np.float32)
    c["ones"] = np.ones((128, 128), np.float32)
    c["triF"] = (same & (s <= t)).astype(np.float32)
    c["triB"] = (same & (s >= t)).astype(np.float32)
    c["sufF"] = (same & (s > t)).astype(np.float32)
    c["sufB"] = (same & (s < t)).astype(np.float32)
    c["sel0"] = np.repeat((s < 64).astype(np.float32), 128, axis=1)
    c["sel1"] = np.repeat((s >= 64).astype(np.float32), 128, axis=1)
    c["tF"] = (s <= t).astype(np.float32)
    c["tB"] = (s >= t).astype(np.float32)
    c["sF"] = (s > t).astype(np.float32)
    c["sB"] = (s < t).astype(np.float32)
    names = ["ident", "ones", "triF", "triB", "sufF", "sufB", "sel0", "sel1", "tF", "tB", "sF", "sB"]
    arr = np.stack([c[n] for n in names], axis=1)
    selE = np.zeros((128, NE, 128), np.float32)
    for e in range(NE):
        selE[e, e, :] = 1.0
    rst = np.ones((128, NT), np.float32)
    rst[:, ::128] = 0.0
    return names, np.ascontiguousarray(arr), selE, rst


CN, CARR, SELE, RST = _consts()
CI = {n: i for i, n in enumerate(CN)}


def _blocks(lo, hi, bs=512):
    out = []
    t = lo
    while t < hi:
        n = min(bs, hi - t)
        if t < CTX:
            n = min(n, CTX - t)
        out.append((t, n, 1 if t < CTX else 0))
        t += n
    return out


def build_program(phases=("mods", "mix0", "moe0", "mix1", "moe1", "final"), dump=None):
    nc = bass.Bass("TRN2", target_bir_lowering=False)
    es = ExitStack()
    P = Prog(nc)

    def din(name, shape, dt=F32):
        return nc.dram_tensor(name, list(shape), dt, kind="ExternalInput").ap()

    d_xT = din("xT", [128, 8, NT])
    d_cT = din("cT", [128, 8, 2])
    d_adaw = din("adaw", [2, 12, 128, 8, 512])
    d_adab = din("adab", [128, 2, 48])
    d_gmix = din("gmix", [128, 2, 8])
    d_gffn = din("gffn", [128, 2, 8])
    d_gfin = din("gfin", [128, 8])
    d_mwin = din("mwin", [128, 8, 3088])
    d_mconv = din("mconv", [128, 8, 4])
    d_mgb = din("mgb", [128, 16])
    d_mhg = din("mhg", [128, D])
    d_mwout = din("mwout", [128, 8, D])
    d_hwin = din("hwin", [128, 8, 5120])
    d_hlb = din("hlb", [128, 2, 16])
    d_hhg = din("hhg", [128, D])
    d_hwout = din("hwout", [128, 8, D])
    d_rw = din("rw", [128, 8, NE])
    d_rb = din("rb", [128, NTILE, NE])
    d_wg = din("ewg", [2, NE, 128, 8, DEXP])
    d_wu = din("ewu", [2, NE, 128, 8, DEXP])
    d_wd = din("ewd", [2, NE, 128, 4, D])
    d_cf = din("cf", [128, 12, 128])
    d_rst = din("rst", [128, NT])
    d_out = nc.dram_tensor("outT", [128, 8, SEQ], F32, kind="ExternalOutput").ap()
    d_dump = None
    if dump is not None:
        d_dump = nc.dram_tensor("dump", [128, 8, NT], F32, kind="ExternalOutput").ap()

    with es:
        def sb(name, shape, dt=F32):
            return es.enter_context(nc.sbuf_tensor("s_" + name, list(shape), dt))

        sT = sb("sT", [128, 8, NT])
        hT = sb("hT", [128, 8, NT], BF16)
        cf = sb("cf", [128, 12, 128])
        cb = sb("cb", [128, 12, 128], BF16)
        maskLE = sb("maskLE", [128, 128], BF16)
        maskGE = sb("maskGE", [128, 128], BF16)
        modT = [sb(f"modT{l}", [128, 48, 2]) for l in range(2)]
        adab = sb("adab", [128, 2, 48])
        gmix = sb("gmix", [128, 2, 8])
        gffn = sb("gffn", [128, 2, 8])
        gfin = sb("gfin", [128, 8])
        cT = sb("cT", [128, 8, 2])
        scT = sb("scT", [128, 8, 2])
        A1 = [sb(f"A1_{l}", [128, 8, 2]) for l in range(2)]
        A2 = [sb(f"A2_{l}", [128, 8, 2]) for l in range(2)]
        pb = [es.enter_context(nc.psum_tensor(f"pb{i}", [128, 512], F32)) for i in range(8)]
        PB = [f"pb{i}" for i in range(8)]

        ARENA = ((nc.sbuf_bytes_remaining - 2048) // 64) * 64
        arena = sb("arena", [128, ARENA // 4], F32)

        class Carver:
            def __init__(self):
                self.off = 0

            def take(self, shape, dt=F32):
                esz = 4 if dt == F32 else 2
                n = int(np.prod(shape[1:]))
                nbytes = ((n * esz + 63) // 64) * 64
                assert self.off + nbytes <= ARENA, (self.off, nbytes, ARENA)
                a = arena[:, self.off // 4:(self.off + nbytes) // 4]
                self.off += nbytes
                if dt != F32:
                    a = a.bitcast(dt)
                a = a[:, 0:n]
                if len(shape) == 3:
                    a = a.rearrange("p (a b) -> p a b", a=shape[1])
                elif len(shape) == 4:
                    a = a.rearrange("p (a b c) -> p a b c", a=shape[1], b=shape[2])
                return a

        dbg_outs = []

        def dbg(name, ap, shape, key, dt=F32):
            if dump is None or dump is True or name not in dump:
                return
            dd = nc.dram_tensor("dbg_" + name, list(shape), dt, kind="ExternalOutput").ap()
            dbg_outs.append(P.dma("sp", "dbg_" + name, lambda e: e.dma_start(out=dd, in_=ap), reads=[key]))

        def mm(out, lhsT, rhs, start, stop):
            return lambda e: e.matmul(out, lhsT=lhsT, rhs=rhs, start=start, stop=stop)

        for c in range(8):
            P.dma("sp", f"sT{c}", lambda e, c=c: e.dma_start(out=sT[:, c, :], in_=d_xT[:, c, :]), writes=[f"sT{c}"])
        P.dma("sp", "cf", lambda e: e.dma_start(out=cf[:], in_=d_cf), writes=["cf"])
        for nm, t, dsrc in (("adab", adab, d_adab), ("gmix", gmix, d_gmix), ("gffn", gffn, d_gffn),
                            ("gfin", gfin, d_gfin), ("cT", cT, d_cT)):
            P.dma("sp", nm, lambda e, t=t, dsrc=dsrc: e.dma_start(out=t[:], in_=dsrc), writes=[nm])
        P.op("dve", lambda e: e.tensor_copy(out=cb[:], in_=cf[:]), reads=["cf"], writes=["cb"])
        P.op("dve", lambda e: e.tensor_copy(out=maskLE[:], in_=cf[:, CI["triF"], :]), reads=["cf"], writes=["masks"])
        P.op("dve", lambda e: e.tensor_copy(out=maskGE[:], in_=cf[:, CI["triB"], :]), reads=["cf"], writes=["masks"])
        ident_b = cb[:, CI["ident"], :]
        ident_f = cf[:, CI["ident"], :]
        ones_f = cf[:, CI["ones"], :]
        SKEY = [f"sT{c}" for c in range(8)]

        MODS_BASE = ((ARENA - (4 * 16384 + 2 * 2048)) // 64) * 64
        if "mods" in phases:
            cv = Carver()
            cv.off = MODS_BASE
            NB_ = 4
            adaw = [cv.take([128, 8, 512]) for _ in range(NB_)]
            modrow = [cv.take([128, 512]) for _ in range(2)]
            P.op("act", lambda e: e.activation(out=scT[:], in_=cT[:], func=AF.Silu), reads=["cT"], writes=["scT"])
            for l in range(2):
                for nb in range(12):
                    bi = nb % NB_
                    mi = nb % 2
                    P.dma("sp" if nb % 2 == 0 else "act", f"adaw{bi}", lambda e, l=l, nb=nb, bi=bi: e.dma_start(out=adaw[bi], in_=d_adaw[l, nb]), writes=[f"arena_adaw{bi}"])
                    for kc in range(8):
                        P.op("pe", mm(pb[mi][0:2, :], scT[:, kc, :], adaw[bi][:, kc, :], kc == 0, kc == 7), reads=[f"arena_adaw{bi}", "scT"], writes=[PB[mi]])
                    P.op("dve", lambda e, mi=mi: e.tensor_copy(out=modrow[mi][0:2, :], in_=pb[mi][0:2, :]), writes=[PB[mi], f"arena_modrow{mi}"])
                    for j in range(4):
                        idx = nb * 4 + j
                        P.op("pe", lambda e, idx=idx, j=j, mi=mi: e.transpose(out=pb[2][:, 2 * idx:2 * idx + 2], in_=modrow[mi][0:2, j * 128:(j + 1) * 128], identity=cf[0:2, CI["ident"], 0:2]),
                             reads=[f"arena_modrow{mi}", "cf"], writes=[PB[2]])
                P.op("dve", lambda e, l=l: e.tensor_tensor(out=modT[l][:], in0=pb[2][:, 0:96].rearrange("p (a b) -> p a b", b=2),
                                                          in1=adab[:, l, :].unsqueeze(2).to_broadcast([128, 48, 2]), op=ALU.add),
                     reads=["adab"], writes=[PB[2], f"modT{l}"])
                for col in range(2):
                    P.op("dve", lambda e, l=l, col=col: e.scalar_tensor_tensor(
                        out=A1[l][:, :, col], in0=modT[l][:, 8:16, col], scalar=1.0, in1=gmix[:, l, :], op0=ALU.add, op1=ALU.mult),
                        reads=[f"modT{l}", "gmix"], writes=[f"A1_{l}"])
                    P.op("dve", lambda e, l=l, col=col: e.scalar_tensor_tensor(
                        out=A2[l][:, :, col], in0=modT[l][:, 32:40, col], scalar=1.0, in1=gffn[:, l, :], op0=ALU.add, op1=ALU.mult),
                        reads=[f"modT{l}", "gffn"], writes=[f"A2_{l}"])

        def SH1(l, c, col): return modT[l][:, 0 + c, col:col + 1]
        def G1(l, c, col): return modT[l][:, 16 + c, col:col + 1]
        def SH2(l, c, col): return modT[l][:, 24 + c, col:col + 1]
        def G2(l, c, col): return modT[l][:, 40 + c, col:col + 1]

        def norm_mod(cv, A, SH, l, lo, hi, out_bf, out_key, out_f32=None, perm_cols=False, after_block=None, bs=512, skey=None):
            if skey is None:
                skey = lambda c, t0: SKEY[c]
            sq = [cv.take([128, bs]) for _ in range(2)]
            rstd = cv.take([128, bs])
            tmp = [cv.take([128, bs]) for _ in range(2)]
            for bix, (t0, n, col) in enumerate(_blocks(lo, hi, bs)):
                o32 = out_f32[bix % 2] if out_f32 is not None else None
                okey = out_key(t0) if callable(out_key) else out_key
                k32 = "nm_h32_0"
                for c in range(8):
                    P.op("act", lambda e, c=c, t0=t0, n=n: e.activation(out=sq[c % 2][:, 0:n], in_=sT[:, c, t0:t0 + n], func=AF.Square),
                         reads=[skey(c, t0)], writes=[f"nm_sq{c % 2}"])
                    P.op("pe", mm(pb[7][:, 0:n], ones_f, sq[c % 2][:, 0:n], c == 0, c == 7), reads=[f"nm_sq{c % 2}", "cf"], writes=[PB[7]])
                P.op("dve", lambda e, n=n: e.tensor_scalar(out=rstd[:, 0:n], in0=pb[7][:, 0:n], scalar1=1.0 / D, scalar2=EPS,
                                                          op0=ALU.mult, op1=ALU.add), reads=[PB[7]], writes=["nm_rstd"])
                P.op("act", lambda e, n=n: e.activation(out=rstd[:, 0:n], in_=rstd[:, 0:n], func=AF.Sqrt), reads=["nm_rstd"], writes=["nm_rstd"])
                P.op("dve", lambda e, n=n: e.reciprocal(out=rstd[:, 0:n], in_=rstd[:, 0:n]), reads=["nm_rstd"], writes=["nm_rstd"])
                for c in range(8):
                    tp = tmp[c % 2]
                    P.op("dve", lambda e, c=c, t0=t0, n=n, tp=tp: e.tensor_tensor(out=tp[:, 0:n], in0=sT[:, c, t0:t0 + n], in1=rstd[:, 0:n], op=ALU.mult),
                         reads=[skey(c, t0), "nm_rstd"], writes=[f"nm_tmp{c % 2}"])
                    src = tp[:, 0:n]
                    if out_f32 is not None:
                        dst, dkey = o32[:, c, 0:n], k32
                    else:
                        dst, dkey = out_bf[:, c, t0:t0 + n], okey
                        if perm_cols and t0 >= CTX:
                            r0, nr = (t0 - CTX) // 64, n // 64
                            dst = out_bf[:, c, CTX:NT].rearrange("p (col row) -> p row col", row=32)[:, r0:r0 + nr, :]
                            src = src.rearrange("p (r c) -> p r c", c=64)
                    P.op("dve", lambda e, c=c, col=col, dst=dst, src=src: e.tensor_scalar(
                        out=dst, in0=src, scalar1=A[l][:, c, col:col + 1], scalar2=SH(l, c, col), op0=ALU.mult, op1=ALU.add),
                        reads=[f"nm_tmp{c % 2}", f"A1_{l}", f"A2_{l}", f"modT{l}"], writes=[dkey])
                    if out_f32 is not None:
                        P.op("act", lambda e, c=c, t0=t0, n=n, o32=o32: e.copy(out=out_bf[:, c, t0:t0 + n], in_=o32[:, c, 0:n]), reads=[k32], writes=[okey])
                if after_block is not None:
                    after_block(t0, n)

        def moe(l, lo, hi):
            P.barrier()
            cv = Carver()
            h32s = [cv.take([128, 8, 256])] * 2
            rw = cv.take([128, 8, NE])
            rb = cv.take([128, NTILE, NE])
            cw_all = cv.take([128, NTILE, NE])
            NTN = NTILE * NE
            rt = [cv.take([128, NTILE, NE]) for _ in range(5)]
            r4 = [cv.take([128, NTILE * 4]) for _ in range(4)]
            r1 = [cv.take([128, NTILE]) for _ in range(2)]
            wgb = [cv.take([128, 8, DEXP], BF16) for _ in range(2)]
            wub = [cv.take([128, 8, DEXP], BF16) for _ in range(2)]
            wdb = [cv.take([128, 4, D], BF16) for _ in range(2)]
            aT = [cv.take([128, 4, 512], BF16) for _ in range(2)]
            sg = [cv.take([128, 512], BF16) for _ in range(2)]
            t1 = [cv.take([128, 512], BF16) for _ in range(2)]
            cwb = [cv.take([128, 512], BF16) for _ in range(2)]
            P.dma("sp", "rw", lambda e: e.dma_start(out=rw, in_=d_rw), writes=["moe_rw"])
            P.dma("sp", "rb", lambda e: e.dma_start(out=rb, in_=d_rb), writes=["moe_rb"])
            T0, T1 = lo // 128, hi // 128
            s_all = rt[0]
            blk_ctr = [0]

            def route(t0, n):
                hb = 0
                for sub in range(n // 128):
                    tix = (t0 + sub * 128) // 128
                    pl, kl = pb[7], PB[7]
                    for c in range(8):
                        P.op("pe", mm(pl[:, 256:256 + NE], h32s[hb][:, c, sub * 128:(sub + 1) * 128], rw[:, c, :], c == 0, c == 7),
                             reads=[f"nm_h32_{hb}", "moe_rw"], writes=[kl])
                    P.op("act", lambda e, tix=tix, pl=pl: e.activation(out=s_all[:, tix, :], in_=pl[:, 256:256 + NE], func=AF.Sigmoid), writes=[kl, f"rt_s{tix}"])

            sel_, sel2_, eq1, eq2 = rt[1:5]
            w_ = sel2_
            m1, m2, gs, geq = r4
            bm, ws = r1
            cw16 = cw_all

            def route_batch(Ta, Tb):
                TS = slice(Ta, Tb)
                nT = Tb - Ta
                K = lambda nm: f"{nm}{Ta}"
                v3 = lambda a: a[:, TS, :].rearrange("p t (g k) -> p (t g) k", k=4)
                g2 = lambda a: a[:, Ta * 4:Tb * 4]
                bc4 = lambda a: g2(a).unsqueeze(2).to_broadcast([128, nT * 4, 4])
                g3 = lambda a: g2(a).rearrange("p (t g) -> p t g", g=4)
                skeys = [f"rt_s{t_}" for t_ in range(Ta, Tb)]
                P.op("dve", lambda e: e.tensor_tensor(out=sel_[:, TS, :], in0=s_all[:, TS, :], in1=rb[:, TS, :], op=ALU.add), reads=skeys + ["moe_rb"], writes=[K("rt_sel")])
                P.op("dve", lambda e: e.tensor_reduce(out=g2(m1), in_=v3(sel_), axis=AX.X, op=ALU.max), reads=[K("rt_sel")], writes=[K("rt_m1")])
                P.op("dve", lambda e: e.tensor_tensor(out=v3(eq1), in0=v3(sel_), in1=bc4(m1), op=ALU.is_equal), reads=[K("rt_sel"), K("rt_m1")], writes=[K("rt_eq1")])
                P.op("dve", lambda e: e.scalar_tensor_tensor(out=sel2_[:, TS, :], in0=eq1[:, TS, :], scalar=-1e9, in1=sel_[:, TS, :], op0=ALU.mult, op1=ALU.add),
                     reads=[K("rt_eq1"), K("rt_sel")], writes=[K("rt_sel2")])
                P.op("dve", lambda e: e.tensor_reduce(out=g2(m2), in_=v3(sel2_), axis=AX.X, op=ALU.max), reads=[K("rt_sel2")], writes=[K("rt_m2")])
                P.op("dve", lambda e: e.tensor_tensor(out=v3(eq2), in0=v3(sel2_), in1=bc4(m2), op=ALU.is_equal), reads=[K("rt_sel2"), K("rt_m2")], writes=[K("rt_eq2")])
                P.op("dve", lambda e: e.tensor_tensor(out=g2(gs), in0=g2(m1), in1=g2(m2), op=ALU.add), reads=[K("rt_m1"), K("rt_m2")], writes=[K("rt_gs")])
                P.op("dve", lambda e: e.tensor_reduce(out=bm[:, TS], in_=g3(gs), axis=AX.X, op=ALU.max), reads=[K("rt_gs")], writes=[K("rt_bm")])
                P.op("dve", lambda e: e.tensor_tensor(out=g3(geq), in0=g3(gs), in1=bm[:, TS].unsqueeze(2).to_broadcast([128, nT, 4]), op=ALU.is_equal),
                     reads=[K("rt_gs"), K("rt_bm")], writes=[K("rt_geq")])
                P.op("dve", lambda e: e.tensor_tensor(out=eq1[:, TS, :], in0=eq1[:, TS, :], in1=eq2[:, TS, :], op=ALU.add), reads=[K("rt_eq2")], writes=[K("rt_eq1")])
                P.op("dve", lambda e: e.tensor_tensor(out=v3(eq1), in0=v3(eq1), in1=bc4(geq), op=ALU.mult), reads=[K("rt_geq")], writes=[K("rt_eq1")])
                P.op("dve", lambda e: e.tensor_tensor(out=w_[:, TS, :], in0=eq1[:, TS, :], in1=s_all[:, TS, :], op=ALU.mult),
                     reads=[K("rt_eq1"), K("rt_eq2"), K("rt_m2")] + skeys, writes=[K("rt_w"), K("rt_sel2")])
                P.op("dve", lambda e: e.tensor_reduce(out=ws[:, TS], in_=w_[:, TS, :], axis=AX.X, op=ALU.add), reads=[K("rt_w")], writes=[K("rt_ws")])
                P.op("dve", lambda e: e.reciprocal(out=ws[:, TS], in_=ws[:, TS]), writes=[K("rt_ws")])
                P.op("dve", lambda e: e.tensor_tensor(out=cw16[:, TS, :], in0=w_[:, TS, :], in1=ws[:, TS].unsqueeze(2).to_broadcast([128, nT, NE]), op=ALU.mult),
                     reads=[K("rt_w"), K("rt_ws")], writes=[f"moe_cw{t_}" for t_ in range(Ta, Tb)])

            def after_blk(t0, n):
                route(t0, n)
                t1_ = t0 + n
                for (bt0, bn, _c) in _blocks(lo, hi):
                    if bt0 + bn == t1_:
                        route_batch(bt0 // 128, (bt0 + bn) // 128)

            norm_mod(cv, A2, SH2, l, lo, hi, out_bf=hT, out_key=lambda t0: f"hT_{t0 // 256}", out_f32=h32s, after_block=after_blk, bs=256,
                     skey=lambda c, t0: f"sT{c}_{t0 // 256}")

            blocks = _blocks(lo, hi)
            items = [(e_, b_) for e_ in range(NE) for b_ in range(len(blocks))]

            def load_w(e_):
                bi = e_ % 2
                P.dma("pool", f"wg{bi}", lambda e: e.dma_start(out=wgb[bi], in_=d_wg[l, e_], max_dma_last_dim=4096), writes=[f"moe_wg{bi}"])
                P.dma("pool", f"wu{bi}", lambda e: e.dma_start(out=wub[bi], in_=d_wu[l, e_], max_dma_last_dim=4096), writes=[f"moe_wu{bi}"])
                P.dma("pool", f"wd{bi}", lambda e: e.dma_start(out=wdb[bi], in_=d_wd[l, e_], max_dma_last_dim=4096), writes=[f"moe_wd{bi}"])

            cwcol = [cv.take([128, 128], BF16) for _ in range(4)]
            cwc_ctr = [0]

            def stage1(i):
                e_, b_ = items[i]
                t0, n, col = blocks[b_]
                bi = e_ % 2
                ai = i % 2
                hkeys = [f"hT_{k_}" for k_ in range(t0 // 256, (t0 + n + 255) // 256)]
                for sub in range(n // 128):
                    tix = (t0 + sub * 128) // 128
                    ci_ = cwc_ctr[0] % 4
                    cwc_ctr[0] += 1
                    P.op("dve", lambda e, tix=tix, ci_=ci_: e.tensor_copy(out=cwcol[ci_], in_=cw16[:, tix, e_:e_ + 1].to_broadcast([128, 128])),
                         reads=[f"moe_cw{tix}"], writes=[f"moe_cwcol{ci_}"])
                    P.op("pe", mm(pb[6][:, sub * 128:(sub + 1) * 128], cwcol[ci_], ident_b, True, True),
                         reads=[f"moe_cwcol{ci_}", "cb"], writes=[PB[6]])
                P.op("act", lambda e: e.copy(out=cwb[ai][:, 0:n], in_=pb[6][:, 0:n]), reads=[PB[6]], writes=[f"moe_cwb{ai}"])
                for fc in range(4):
                    pg, pu = pb[(fc % 2) * 2], pb[(fc % 2) * 2 + 1]
                    kg, ku = PB[(fc % 2) * 2], PB[(fc % 2) * 2 + 1]
                    for c in range(8):
                        P.op("pe", mm(pg[:, 0:n], wgb[bi][:, c, fc * 128:(fc + 1) * 128], hT[:, c, t0:t0 + n], c == 0, c == 7),
                             reads=[f"moe_wg{bi}"] + hkeys, writes=[kg])
                    for c in range(8):
                        P.op("pe", mm(pu[:, 0:n], wub[bi][:, c, fc * 128:(fc + 1) * 128], hT[:, c, t0:t0 + n], c == 0, c == 7),
                             reads=[f"moe_wu{bi}"] + hkeys, writes=[ku])
                    si = fc % 2
                    P.op("act", lambda e, pg=pg, si=si: e.activation(out=sg[si][:, 0:n], in_=pg[:, 0:n], func=AF.Silu), reads=[kg], writes=[f"moe_sg{si}"])
                    P.op("dve", lambda e, pu=pu, si=si: e.tensor_tensor(out=t1[si][:, 0:n], in0=sg[si][:, 0:n], in1=pu[:, 0:n], op=ALU.mult),
                         reads=[f"moe_sg{si}", ku], writes=[f"moe_t1{si}"])
                    P.op("pool", lambda e, si=si, fc=fc: e.tensor_tensor(out=aT[ai][:, fc, 0:n], in0=t1[si][:, 0:n], in1=cwb[ai][:, 0:n], op=ALU.mult),
                         reads=[f"moe_t1{si}", f"moe_cwb{ai}"], writes=[f"moe_aT{ai}"])

            def stage2(i):
                e_, b_ = items[i]
                t0, n, col = blocks[b_]
                bi = e_ % 2
                ai = i % 2
                for dc in range(8):
                    pd, kd = pb[4 + dc % 2], PB[4 + dc % 2]
                    for fc in range(4):
                        P.op("pe", mm(pd[:, 0:n], wdb[bi][:, fc, dc * 128:(dc + 1) * 128], aT[ai][:, fc, 0:n], fc == 0, fc == 3),
                             reads=[f"moe_wd{bi}", f"moe_aT{ai}"], writes=[kd])
                    sk = [f"sT{dc}_{k_}" for k_ in range(t0 // 256, (t0 + n + 255) // 256)]
                    P.op("dve", lambda e, dc=dc, pd=pd: e.scalar_tensor_tensor(
                        out=sT[:, dc, t0:t0 + n], in0=pd[:, 0:n], scalar=G2(l, dc, col), in1=sT[:, dc, t0:t0 + n], op0=ALU.mult, op1=ALU.add),
                        reads=[kd, f"modT{l}"] + sk, writes=sk)

            load_w(0)
            for i in range(len(items)):
                e_, b_ = items[i]
                stage1(i)
                if i > 0:
                    stage2(i - 1)
                if b_ == 0 and e_ + 1 < NE:
                    load_w(e_ + 1)
            stage2(len(items) - 1)

        def mlstm(l):
            cv = Carver()
            wgt = cv.take([128, 8, 16], BF16)
            mgb = cv.take([128, 16])
            mhg = cv.take([128, 256])
            cvw = cv.take([128, 8, 4])
            G = cv.take([128, NTILE, 16])
            Lg = cv.take([128, NTILE, 8])
            EE = cv.take([128, NTILE, 24])
            EB = cv.take([128, NTILE, 8])
            arg = cv.take([128, 24])
            P.dma("pool", "m_wgt", lambda e: e.dma_start(out=wgt, in_=d_mwin[:, :, 3072:3088]), writes=["m_wgt"])
            P.dma("sp", "m_mgb", lambda e: e.dma_start(out=mgb, in_=d_mgb), writes=["m_mgb"])
            P.dma("sp", "m_cvw", lambda e: e.dma_start(out=cvw, in_=d_mconv), writes=["m_cvw"])

            norm_mod(cv, A1, SH1, l, 0, NT, out_bf=hT, out_key="hT", bs=256)

            for tl in range(NTILE):
                ts_ = slice(tl * 128, (tl + 1) * 128)
                for c in range(8):
                    P.op("pe", mm(pb[0][:, 0:16], hT[:, c, ts_], wgt[:, c, :], c == 0, c == 7), reads=["hT", "m_wgt"], writes=[PB[0]])
                P.op("dve", lambda e, tl=tl: e.tensor_tensor(out=G[:, tl, :], in0=pb[0][:, 0:16], in1=mgb, op=ALU.add), reads=[PB[0], "m_mgb"], writes=["m_G"])
                for k, src in enumerate((slice(4, 8), slice(12, 16))):
                    P.op("act", lambda e, tl=tl, k=k, src=src: e.activation(out=Lg[:, tl, 4 * k:4 * k + 4], in_=G[:, tl, src], func=AF.Exp, scale=-1.0),
                         reads=["m_G"], writes=["m_L"])
                P.op("dve", lambda e, tl=tl: e.tensor_scalar(out=Lg[:, tl, :], in0=Lg[:, tl, :], scalar1=1.0, scalar2=None, op0=ALU.add), reads=["m_L"], writes=["m_L"])
                P.op("act", lambda e, tl=tl: e.activation(out=Lg[:, tl, :], in_=Lg[:, tl, :], func=AF.Ln), reads=["m_L"], writes=["m_L"])
                q_ = pb[1]
                for j, (mat, cs) in enumerate((("tF", slice(0, 4)), ("tB", slice(4, 8)), ("sF", slice(0, 4)), ("sB", slice(4, 8)))):
                    P.op("pe", mm(q_[:, 4 * j:4 * j + 4], cf[:, CI[mat], :], Lg[:, tl, cs], True, True), reads=["cf", "m_L"], writes=[PB[1]])
                P.op("pe", mm(q_[:, 16:24], cf[:, CI["ones"], :], Lg[:, tl, :], True, True), reads=["cf", "m_L"], writes=[PB[1]])
                P.op("dve", lambda e, tl=tl: e.tensor_tensor(out=arg[:, 0:4], in0=G[:, tl, 0:4], in1=q_[:, 0:4], op=ALU.add), reads=["m_G"], writes=["m_arg", PB[1]])
                P.op("dve", lambda e, tl=tl: e.tensor_tensor(out=arg[:, 4:8], in0=G[:, tl, 0:4], in1=q_[:, 8:12], op=ALU.subtract), reads=["m_G"], writes=["m_arg", PB[1]])
                P.op("dve", lambda e, tl=tl: e.tensor_tensor(out=arg[:, 8:12], in0=G[:, tl, 8:12], in1=q_[:, 4:8], op=ALU.add), reads=["m_G"], writes=["m_arg", PB[1]])
                P.op("dve", lambda e, tl=tl: e.tensor_tensor(out=arg[:, 12:16], in0=G[:, tl, 8:12], in1=q_[:, 12:16], op=ALU.subtract), reads=["m_G"], writes=["m_arg", PB[1]])
                P.op("dve", lambda e, tl=tl: e.tensor_copy(out=arg[:, 16:24], in_=q_[:, 0:8]), writes=["m_arg", PB[1]])
                P.op("act", lambda e, tl=tl: e.activation(out=EE[:, tl, :], in_=arg, func=AF.Exp), reads=["m_arg"], writes=["m_EE"])
                P.op("act", lambda e, tl=tl: e.activation(out=EB[:, tl, :], in_=q_[:, 16:24], func=AF.Exp, scale=-1.0), writes=["m_EB", PB[1]])

            assert cv.off <= MODS_BASE, (cv.off, MODS_BASE)
            P.barrier()
            wqk = cv.take([128, 8, 2, 128], BF16)
            wv = cv.take([128, 8, 256], BF16)
            wo = cv.take([128, 8, 256], BF16)
            wout = cv.take([128, 2, D], BF16)
            qT = cv.take([128, NT], BF16)
            kT = cv.take([128, NT], BF16)
            vaug = cv.take([128, NTILE, 257], BF16)
            ktok = cv.take([128, NTILE, 128], BF16)
            hfirst = cv.take([128, NTILE, 256], BF16)
            ybuf = [cv.take([128, 512]) for _ in range(2)]
            cvs = cv
            STf = [[cvs.take([128, 128], BF16) for _ in range(2)] for _ in range(2)]
            vt = [[cvs.take([128, 257], BF16) for _ in range(2)] for _ in range(2)]
            vt2 = [[cvs.take([128, 257], BF16) for _ in range(2)] for _ in range(2)]
            X = [cvs.take([128, 257]) for _ in range(2)]
            Xb = [[cvs.take([128, 257], BF16) for _ in range(2)] for _ in range(2)]
            numS = [[cvs.take([128, 257]) for _ in range(2)] for _ in range(2)]
            hs = [cvs.take([128, 256]) for _ in range(2)]
            hn = hs
            hg = [cvs.take([128, 256], BF16) for _ in range(2)]
            sgo = [cvs.take([128, 256]) for _ in range(2)]
            tmpo = [cvs.take([128, 4, 128]) for _ in range(2)]
            sm = [[cvs.take([128, 1]) for _ in range(2)] for _ in range(2)]
            xTt = [cvs.take([128, 2, 128], BF16) for _ in range(2)]
            P.op("pool", lambda e: e.memset(vaug[:, :, 256:257], 1.0), writes=["m_vaug"])
            pb6b = pb[6].bitcast(BF16)

            for h in range(4):
                P.dma("sp", "m_mhg", lambda e, h=h: e.dma_start(out=mhg, in_=d_mhg[:, h * 256:(h + 1) * 256]), writes=["m_mhg"])
                P.dma("pool", "m_wqk", lambda e, h=h: [e.dma_start(out=wqk[:, :, 0, :], in_=d_mwin[:, :, h * 128:(h + 1) * 128]),
                                                       e.dma_start(out=wqk[:, :, 1, :], in_=d_mwin[:, :, 512 + h * 128:512 + (h + 1) * 128])],
                      writes=["m_wqk"], n=2)
                P.dma("pool", "m_wv", lambda e, h=h: e.dma_start(out=wv, in_=d_mwin[:, :, 1024 + h * 256:1024 + (h + 1) * 256]), writes=["m_wv"])
                P.dma("pool", "m_wo", lambda e, h=h: e.dma_start(out=wo, in_=d_mwin[:, :, 2048 + h * 256:2048 + (h + 1) * 256]), writes=["m_wo"])
                P.dma("pool", "m_wout", lambda e, h=h: e.dma_start(out=wout, in_=d_mwout[:, 2 * h:2 * h + 2, :], max_dma_last_dim=4096), writes=["m_wout"])
                qk_blocks = [(0, CTX, 0, CTX)] + [(CTX + 410 * j, min(CTX + 410 * (j + 1), NT), CTX, NT) for j in range(5)]
                cnt_ = [0]
                for which, dstT, dkey in ((0, qT, "m_qT"), (1, kT, "m_kT")):
                    ch = (0 if which == 0 else 4) + h
                    for (a_, b_, A_, B_) in qk_blocks:
                        n = b_ - a_
                        a2, b2 = max(a_ - 1, A_), min(b_ + 1, B_)
                        off = a_ - a2
                        N_ = b2 - a2
                        bi_ = cnt_[0] % 2
                        cnt_[0] += 1
                        pq, kq = pb[bi_], PB[bi_]
                        yb, ky = ybuf[bi_], f"m_y{bi_}"
                        for c in range(8):
                            P.op("pe", mm(pq[:, 0:N_], wqk[:, c, which, :], hT[:, c, a2:b2], c == 0, c == 7), reads=["m_wqk", "hT"], writes=[kq])
                        P.op("dve", lambda e, pq=pq, yb=yb, off=off, n=n, ch=ch: e.tensor_scalar(out=yb[:, 0:n], in0=pq[:, off:off + n], scalar1=cvw[:, ch, 1:2], scalar2=cvw[:, ch, 3:4],
                                                                                       op0=ALU.mult, op1=ALU.add), reads=["m_cvw"], writes=[kq, ky])
                        if off == 1:
                            o0, i0, m0 = 0, 0, n
                        else:
                            o0, i0, m0 = 1, 0, n - 1
                        P.op("dve", lambda e, pq=pq, yb=yb, o0=o0, i0=i0, m0=m0, ch=ch: e.scalar_tensor_tensor(
                            out=yb[:, o0:o0 + m0], in0=pq[:, i0:i0 + m0], scalar=cvw[:, ch, 0:1], in1=yb[:, o0:o0 + m0], op0=ALU.mult, op1=ALU.add),
                            reads=["m_cvw"], writes=[kq, ky])
                        m2 = n if b2 > b_ else n - 1
                        P.op("dve", lambda e, pq=pq, yb=yb, off=off, m2=m2, ch=ch: e.scalar_tensor_tensor(
                            out=yb[:, 0:m2], in0=pq[:, off + 1:off + 1 + m2], scalar=cvw[:, ch, 2:3], in1=yb[:, 0:m2], op0=ALU.mult, op1=ALU.add),
                            reads=["m_cvw"], writes=[kq, ky])
                        if which == 0:
                            P.op("act", lambda e, yb=yb, n=n: e.activation(out=yb[:, 0:n], in_=yb[:, 0:n], func=AF.Silu), writes=[ky])
                            P.op("act", lambda e, yb=yb, n=n, a_=a_, b_=b_: e.activation(out=qT[:, a_:b_], in_=yb[:, 0:n], func=AF.Copy, scale=float(128 ** -0.5)),
                                 reads=[ky], writes=[dkey])
                        else:
                            P.op("act", lambda e, yb=yb, n=n, a_=a_, b_=b_: e.activation(out=kT[:, a_:b_], in_=yb[:, 0:n], func=AF.Silu), reads=[ky], writes=[dkey])
                for tl in range(NTILE):
                    ts_ = slice(tl * 128, (tl + 1) * 128)
                    pv, kv_ = pb[2 + tl % 2], PB[2 + tl % 2]
                    for c in range(8):
                        P.op("pe", mm(pv[:, 0:256], hT[:, c, ts_], wv[:, c, :], c == 0, c == 7), reads=["hT", "m_wv"], writes=[kv_])
                    P.op("act", lambda e, tl=tl, pv=pv: e.copy(out=vaug[:, tl, 0:256], in_=pv[:, 0:256]), reads=[kv_], writes=["m_vaug"])
                    tb = pb[4 + tl % 2].bitcast(BF16)
                    P.op("pe", lambda e, ts_=ts_, tb=tb: e.transpose(out=tb[:, 0:128], in_=kT[:, ts_], identity=ident_b), reads=["m_kT", "cb"], writes=[PB[4 + tl % 2]])
                    P.op("act", lambda e, tl=tl, tb=tb: e.copy(out=ktok[:, tl, :], in_=tb[:, 0:128]), reads=[PB[4 + tl % 2]], writes=["m_ktok"])

                orders = [list(range(NTILE)), [1, 0] + list(range(NTILE - 1, 1, -1))]
                if DEBUG_NT is not None:
                    orders = [o_[:DEBUG_NT] for o_ in orders]
                visited = set()
                for d_ in range(2):
                    P.op("dve", lambda e, d_=d_: e.memset(X[d_], 0.0), writes=[f"m_X{d_}"])
                    P.op("dve", lambda e, d_=d_: e.memset(Xb[d_][0], 0.0), writes=[f"m_Xb{d_}_0"])
                ncomp = [0]

                def step(d_, it, tl):
                    ts_ = slice(tl * 128, (tl + 1) * 128)
                    pi = it % 2
                    eoff = 0 if d_ == 0 else 8
                    doff = 4 if d_ else 0
                    second = tl in visited
                    visited.add(tl)
                    sc_cols = slice(128 * d_, 128 * d_ + 128)
                    P.op("pe", mm(pb[6][:, sc_cols], kT[:, ts_], qT[:, ts_], True, True), reads=["m_kT", "m_qT"], writes=[PB[6]])
                    msk = cb[:, CI["tF"], :] if d_ == 0 else cb[:, CI["tB"], :]
                    P.op("dve", lambda e: e.tensor_tensor(out=STf[d_][pi], in0=pb[6][:, sc_cols], in1=msk, op=ALU.mult),
                         reads=["cb"], writes=[PB[6], f"m_STf{d_}{pi}"])
                    e1 = EE[:, tl, eoff + h:eoff + h + 1]
                    e2 = EE[:, tl, eoff + 4 + h:eoff + 5 + h]
                    P.op("act", lambda e: e.activation(out=vt[d_][pi], in_=vaug[:, tl, :], func=AF.Copy, scale=e1),
                         reads=["m_vaug", "m_EE"], writes=[f"m_vt{d_}{pi}"])
                    P.op("dve", lambda e: e.tensor_scalar(out=vt2[d_][pi], in0=vaug[:, tl, :], scalar1=e2, scalar2=None, op0=ALU.mult),
                         reads=["m_vaug", "m_EE"], writes=[f"m_vt2{d_}{pi}"])
                    if second:
                        for c in range(8):
                            P.op("pe", mm(pb[7][:, 0:256], hT[:, c, ts_], wo[:, c, :], c == 0, c == 7), reads=["hT", "m_wo"], writes=[PB[7]])
                        P.op("act", lambda e: e.activation(out=sgo[d_], in_=pb[7][:, 0:256], func=AF.Exp, scale=-1.0), writes=[PB[7], f"m_sgo{d_}"])
                        P.op("dve", lambda e: e.tensor_scalar(out=sgo[d_], in0=sgo[d_], scalar1=1.0, scalar2=None, op0=ALU.add), writes=[f"m_sgo{d_}"])
                        P.op("act", lambda e: e.activation(out=sgo[d_], in_=sgo[d_], func=AF.Ln), writes=[f"m_sgo{d_}"])
                        P.op("act", lambda e: e.activation(out=sgo[d_], in_=sgo[d_], func=AF.Exp, scale=-1.0), writes=[f"m_sgo{d_}"])
                    num, knum = pb[d_], PB[d_]
                    P.op("pe", mm(num[:, 0:257], STf[d_][pi], vt[d_][pi], True, False), reads=[f"m_STf{d_}{pi}", f"m_vt{d_}{pi}"], writes=[knum])
                    kv, kkv = pb[2 + d_], PB[2 + d_]
                    P.op("pe", mm(kv[:, 0:257], ktok[:, tl, :], vt2[d_][pi], True, True), reads=["m_ktok", f"m_vt2{d_}{pi}"], writes=[kkv])
                    r0, r1 = it % 2, (it + 1) % 2
                    P.op("pe", mm(num[:, 0:257], qT[:, ts_], Xb[d_][r0], False, True), reads=["m_qT", f"m_Xb{d_}_{r0}", knum], writes=[knum])
                    ebc = EB[:, tl, doff + h:doff + h + 1]
                    P.op("dve", lambda e: e.scalar_tensor_tensor(out=X[d_], in0=X[d_], scalar=ebc, in1=kv[:, 0:257], op0=ALU.mult, op1=ALU.add),
                         reads=["m_EB", f"m_X{d_}"], writes=[kkv, f"m_X{d_}"])
                    P.op("act", lambda e: e.copy(out=Xb[d_][r1], in_=X[d_]), reads=[f"m_X{d_}"], writes=[f"m_Xb{d_}_{r1}"])
                    ns, kns = numS[d_][pi], f"m_numS{d_}{pi}"
                    P.op("act", lambda e: e.copy(out=ns, in_=num[:, 0:257]), writes=[knum, kns])
                    thr = EE[:, tl, 16 + doff + h:16 + doff + h + 1]
                    s0, ks0 = sm[d_][0], f"m_sm{d_}0"
                    P.op("act", lambda e: e.activation(out=s0, in_=ns[:, 256:257], func=AF.Abs), reads=[kns], writes=[ks0])
                    P.op("dve", lambda e: e.tensor_tensor(out=s0, in0=s0, in1=thr, op=ALU.max), reads=[ks0, "m_EE"], writes=[ks0])
                    P.op("dve", lambda e: e.reciprocal(out=s0, in_=s0), reads=[ks0], writes=[ks0])
                    if not second:
                        P.op("dve", lambda e: e.tensor_scalar(out=hfirst[:, tl, :], in0=ns[:, 0:256], scalar1=s0[:, 0:1], scalar2=None, op0=ALU.mult),
                             reads=[kns, ks0], writes=[f"m_hf{tl}"])
                        return
                    s1, ks1 = sm[d_][1], f"m_sm{d_}1"
                    P.op("dve", lambda e: e.scalar_tensor_tensor(out=hs[d_], in0=ns[:, 0:256], scalar=s0[:, 0:1], in1=hfirst[:, tl, :], op0=ALU.mult, op1=ALU.add),
                         reads=[kns, ks0, f"m_hf{tl}"], writes=[f"m_hs{d_}"])
                    P.op("act", lambda e: e.activation(out=hg[d_], in_=hs[d_], func=AF.Square, accum_out=s1), reads=[f"m_hs{d_}"], writes=[f"m_hg{d_}", ks1])
                    P.op("dve", lambda e: e.tensor_scalar(out=s1, in0=s1, scalar1=1.0 / 256, scalar2=EPS, op0=ALU.mult, op1=ALU.add), reads=[ks1], writes=[ks1])
                    P.op("act", lambda e: e.activation(out=s1, in_=s1, func=AF.Ln), reads=[ks1], writes=[ks1])
                    P.op("act", lambda e: e.activation(out=s1, in_=s1, func=AF.Exp, scale=-0.5), reads=[ks1], writes=[ks1])
                    P.op("dve", lambda e: e.scalar_tensor_tensor(out=hs[d_], in0=hs[d_], scalar=s1[:, 0:1], in1=mhg, op0=ALU.mult, op1=ALU.mult),
                         reads=[f"m_hs{d_}", ks1, "m_mhg"], writes=[f"m_hs{d_}"])
                    P.op("dve", lambda e: e.tensor_tensor(out=hg[d_], in0=hs[d_], in1=sgo[d_], op=ALU.mult), reads=[f"m_hs{d_}", f"m_sgo{d_}", f"m_hg{d_}"], writes=[f"m_hg{d_}"])
                    for fc in range(2):
                        P.op("pe", lambda e, fc=fc: e.transpose(out=pb6b[:, 512 + fc * 128:512 + (fc + 1) * 128], in_=hg[d_][:, fc * 128:(fc + 1) * 128], identity=ident_b),
                             reads=[f"m_hg{d_}", "cb"], writes=[PB[6]])
                    xi = ncomp[0] % 2
                    ncomp[0] += 1
                    P.op("act", lambda e: e.copy(out=xTt[xi], in_=pb6b[:, 512:768].rearrange("p (a b) -> p a b", a=2)), writes=[PB[6], f"m_xT{xi}"])
                    col = 1 if tl < 2 else 0
                    for half in range(2):
                        po, ko = pb[4 + half], PB[4 + half]
                        for j in range(4):
                            dc = half * 4 + j
                            for fc in range(2):
                                P.op("pe", mm(po[:, j * 128:(j + 1) * 128], wout[:, fc, dc * 128:(dc + 1) * 128], xTt[xi][:, fc, :], fc == 0, fc == 1), reads=["m_wout", f"m_xT{xi}"], writes=[ko])
                        g1b = modT[l][:, 16 + half * 4:16 + half * 4 + 4, col:col + 1].to_broadcast([128, 4, 128])
                        P.op("dve", lambda e, po=po, g1b=g1b, half=half: e.tensor_tensor(out=tmpo[half], in0=po[:, :].rearrange("p (a b) -> p a b", a=4), in1=g1b, op=ALU.mult),
                             reads=[f"modT{l}"], writes=[ko, f"m_tmpo{half}"])
                        keys = SKEY[half * 4:half * 4 + 4]
                        P.op("dve", lambda e, half=half: e.tensor_tensor(out=sT[:, half * 4:half * 4 + 4, ts_], in0=sT[:, half * 4:half * 4 + 4, ts_], in1=tmpo[half], op=ALU.add),
                             reads=[f"m_tmpo{half}"] + keys, writes=keys)

                for it in range(len(orders[0])):
                    step(0, it, orders[0][it])
                    step(1, it, orders[1][it])

        def hgrn(l):
            P.barrier()
            cv = Carver()
            hlb = cv.take([128, 2, 16])
            lbT = cv.take([128, 16])
            omlb = cv.take([128, 16])
            hhg = cv.take([128, 128])
            rst = cv.take([128, 512])
            P.dma("sp", "h_hlb", lambda e: e.dma_start(out=hlb, in_=d_hlb), writes=["h_hlb"])
            P.dma("sp", "h_rst", lambda e: e.dma_start(out=rst, in_=d_rst[:, 0:512]), writes=["h_rst"])
            P.op("dve", lambda e: e.tensor_tensor(out=lbT, in0=hlb[:, 1, :], in1=hlb[:, 0, :], op=ALU.subtract), reads=["h_hlb"], writes=["h_lb"])
            P.op("act", lambda e: e.activation(out=omlb, in_=lbT, func=AF.Exp), reads=["h_lb"], writes=["h_omlb"])
            P.op("act", lambda e: e.activation(out=lbT, in_=lbT, func=AF.Exp, scale=-1.0), reads=["h_lb", "h_omlb"], writes=["h_lb"])
            for t_, k_ in ((omlb, "h_omlb"), (lbT, "h_lb")):
                P.op("dve", lambda e, t_=t_: e.tensor_scalar(out=t_, in0=t_, scalar1=1.0, scalar2=None, op0=ALU.add), writes=[k_])
                P.op("dve", lambda e, t_=t_: e.reciprocal(out=t_, in_=t_), writes=[k_])

            ptmp = [cv.take([128, SEQ]) for _ in range(2)]
            for c in range(8):
                pt = ptmp[c % 2]
                src = sT[:, c, CTX:NT].rearrange("p (row col) -> p col row", col=64)
                P.op("dve" if c % 2 == 0 else "pool", lambda e, pt=pt, src=src: e.tensor_copy(out=pt.rearrange("p (col row) -> p col row", row=32), in_=src),
                     reads=[SKEY[c]], writes=[f"h_ptmp{c % 2}"])
                P.op("act", lambda e, pt=pt, c=c: e.copy(out=sT[:, c, CTX:NT], in_=pt), reads=[f"h_ptmp{c % 2}"], writes=[SKEY[c]])
            P.barrier()
            cv.off -= 2 * SEQ * 4
            norm_base = cv.off
            norm_mod(cv, A1, SH1, l, 0, NT, out_bf=hT, out_key="hT", bs=256)
            norm_end = cv.off
            P.barrier()

            wq = cv.take([128, 8, 128], BF16)
            wz = cv.take([128, 8, 2, 128], BF16)
            wi = cv.take([128, 8, 128], BF16)
            wgg = cv.take([128, 8, 128], BF16)
            wout = cv.take([128, D], BF16)
            qs = cv.take([128, NT], BF16)
            cvn = Carver()
            cvn.off = norm_base
            fTs = [cv.take([128, 512]), cvn.take([128, 512])]
            bTs = [cv.take([128, 512]), cvn.take([128, 512])]
            t32s = [cv.take([128, 512]), cv.take([128, 512])]
            kks = [cv.take([128, 512], BF16), cvn.take([128, 512], BF16)]
            assert cvn.off <= norm_end
            totcs = [cv.take([128, 4]) for _ in range(2)]
            rcs = [cv.take([128, 4]) for _ in range(2)]
            t32 = t32s[0]
            qtl = [cv.take([128, NT], BF16) for _ in range(2)]
            ktl = [cv.take([128, NT], BF16) for _ in range(2)]
            kht = [cv.take([128, NT], BF16) for _ in range(2)]
            ebT = [cv.take([128, NTILE]) for _ in range(2)]
            erT = [cv.take([128, NTILE]) for _ in range(2)]
            itok = cv.take([128, NTILE, 128], BF16)
            sgt = cv.take([128, NTILE, 128], BF16)
            gtmp = cv.take([128, 128])
            ofirst = cv.take([128, NTILE, 128], BF16)
            AT = [[cv.take([128, 128], BF16) for _ in range(2)] for _ in range(2)]
            khtok = [[cv.take([128, 128], BF16) for _ in range(2)] for _ in range(2)]
            S = [cv.take([128, 128]) for _ in range(2)]
            Sb = [[cv.take([128, 128], BF16) for _ in range(2)] for _ in range(2)]
            os_ = [cv.take([128, 128]) for _ in range(2)]
            og = [cv.take([128, 128], BF16) for _ in range(2)]
            sm = [cv.take([128, 1]) for _ in range(2)]
            xTt = [cv.take([128, 128], BF16) for _ in range(2)]
            pb7b = pb[7].bitcast(BF16)
            g1row = cv.take([128, D], BF16)
            for dc in range(8):
                P.op("pe", mm(pb[dc % 2][:, 0:128], modT[l][:, 16 + dc, 0:1].to_broadcast([128, 128]), ident_f, True, True), reads=[f"modT{l}", "cf"], writes=[PB[dc % 2]])
                P.op("act", lambda e, dc=dc: e.copy(out=g1row[:, dc * 128:(dc + 1) * 128], in_=pb[dc % 2][:, 0:128]), writes=[PB[dc % 2], "h_g1row"])
            REF = 64
            pieces = [(0, 512), (512, 512), (1024, 512), (1536, 512), (2048, 256)]

            def v128(a):
                return a.rearrange("p (c k) -> p c k", k=128)

            for h in range(8):
                P.dma("sp", "h_hhg", lambda e, h=h: e.dma_start(out=hhg, in_=d_hhg[:, h * 128:(h + 1) * 128]), writes=["h_hhg"])
                P.dma("pool", "h_wq", lambda e, h=h: e.dma_start(out=wq, in_=d_hwin[:, :, h * 128:(h + 1) * 128]), writes=["h_wq"])
                P.dma("pool", "h_wz", lambda e, h=h: [e.dma_start(out=wz[:, :, 0, :], in_=d_hwin[:, :, 1024 + h * 128:1024 + (h + 1) * 128]),
                                                      e.dma_start(out=wz[:, :, 1, :], in_=d_hwin[:, :, 2048 + h * 128:2048 + (h + 1) * 128])], writes=["h_wz"], n=2)
                P.dma("pool", "h_wi", lambda e, h=h: e.dma_start(out=wi, in_=d_hwin[:, :, 3072 + h * 128:3072 + (h + 1) * 128]), writes=["h_wi"])
                P.dma("pool", "h_wg", lambda e, h=h: e.dma_start(out=wgg, in_=d_hwin[:, :, 4096 + h * 128:4096 + (h + 1) * 128]), writes=["h_wg"])
                P.dma("pool", "h_wout", lambda e, h=h: e.dma_start(out=wout, in_=d_hwout[:, h, :], max_dma_last_dim=4096), writes=["h_wout"])
                P.op("dve", lambda e: e.tensor_tensor(out=wout, in0=wout, in1=g1row, op=ALU.mult), reads=["h_g1row"], writes=["h_wout"])
                for bi_, (t0, n, col) in enumerate(_blocks(0, NT)):
                    pq, kq = pb[bi_ % 2], PB[bi_ % 2]
                    for c in range(8):
                        P.op("pe", mm(pq[:, 0:n], wq[:, c, :], hT[:, c, t0:t0 + n], c == 0, c == 7), reads=["h_wq", "hT"], writes=[kq])
                    P.op("act", lambda e, n=n, pq=pq: e.activation(out=t32[:, 0:n], in_=pq[:, 0:n], func=AF.Exp, scale=-1.0), writes=[kq, "h_t32_0"])
                    P.op("act", lambda e, n=n: e.activation(out=t32[:, 0:n], in_=t32[:, 0:n], func=AF.Ln, bias=1.0), writes=["h_t32_0"])
                    P.op("act", lambda e, n=n: e.activation(out=t32[:, 0:n], in_=t32[:, 0:n], func=AF.Exp, scale=-1.0), writes=["h_t32_0"])
                    P.op("dve", lambda e, t0=t0, n=n, pq=pq: e.tensor_tensor(out=qs[:, t0:t0 + n], in0=t32[:, 0:n], in1=pq[:, 0:n], op=ALU.mult),
                         reads=["h_t32_0"], writes=[kq, "h_qs"])
                for tl in range(NTILE):
                    ts_ = slice(tl * 128, (tl + 1) * 128)
                    pi_, ki_ = pb[2 + tl % 2], PB[2 + tl % 2]
                    for c in range(8):
                        P.op("pe", mm(pi_[:, 0:128], hT[:, c, ts_], wi[:, c, :], c == 0, c == 7), reads=["hT", "h_wi"], writes=[ki_])
                    if tl >= 2:
                        for c in range(8):
                            P.op("pe", mm(pi_[:, 128:256], hT[:, c, ts_], wgg[:, c, :], c == 0, c == 7), reads=["hT", "h_wg"], writes=[ki_])
                    P.op("act", lambda e, tl=tl, pi_=pi_: e.copy(out=itok[:, tl, :], in_=pi_[:, 0:128]), writes=[ki_, "h_itok"])
                    if tl >= 2:
                        P.op("act", lambda e, pi_=pi_: e.activation(out=gtmp, in_=pi_[:, 128:256], func=AF.Exp, scale=-1.0), writes=[ki_, "h_gtmp"])
                        P.op("act", lambda e: e.activation(out=gtmp, in_=gtmp, func=AF.Ln, bias=1.0), writes=["h_gtmp"])
                        P.op("act", lambda e: e.activation(out=gtmp, in_=gtmp, func=AF.Exp, scale=-1.0), writes=["h_gtmp"])
                        P.op("dve", lambda e, tl=tl, pi_=pi_: e.tensor_tensor(out=sgt[:, tl, :], in0=gtmp, in1=pi_[:, 128:256], op=ALU.mult), reads=["h_gtmp"], writes=[ki_, "h_sgt"])

                def precompute(d_, p0, pn, pk):
                    fT, bT, t32, kk, totc, rc = fTs[d_], bTs[d_], t32s[d_], kks[d_], totcs[d_], rcs[d_]
                    kx = f"_{d_}"
                    lbc = lbT[:, d_ * 8 + h:d_ * 8 + h + 1]
                    omc = omlb[:, d_ * 8 + h:d_ * 8 + h + 1]
                    nt = pn // 128
                    tsl = slice(p0 // 128, p0 // 128 + nt)
                    ps_ = slice(p0, p0 + pn)
                    pz, kz = pb[4 + d_], PB[4 + d_]
                    for c in range(8):
                        P.op("pe", mm(pz[:, 0:pn], wz[:, c, d_, :], hT[:, c, ps_], c == 0, c == 7), reads=["h_wz", "hT"], writes=[kz])
                    P.op("act", lambda e: e.activation(out=fT[:, 0:pn], in_=pz[:, 0:pn], func=AF.Exp, scale=-1.0), writes=[kz, "h_fT" + kx])
                    P.op("act", lambda e: e.activation(out=fT[:, 0:pn], in_=fT[:, 0:pn], func=AF.Ln, bias=1.0), writes=["h_fT" + kx])
                    P.op("act", lambda e: e.activation(out=fT[:, 0:pn], in_=fT[:, 0:pn], func=AF.Exp, scale=-1.0), writes=["h_fT" + kx])
                    P.op("act", lambda e: e.activation(out=fT[:, 0:pn], in_=fT[:, 0:pn], func=AF.Identity, scale=omc, bias=lbc),
                         reads=["h_lb", "h_omlb"], writes=["h_fT" + kx])
                    P.op("act", lambda e: e.activation(out=kk[:, 0:pn], in_=fT[:, 0:pn], func=AF.Identity, scale=-1.0, bias=1.0), reads=["h_fT" + kx], writes=["h_kk" + kx])
                    P.op("act", lambda e: e.activation(out=fT[:, 0:pn], in_=fT[:, 0:pn], func=AF.Ln), reads=["h_kk" + kx], writes=["h_fT" + kx])
                    P.op("dve", lambda e: e.tensor_tensor_scan(out=bT[:, 0:pn], data0=rst[:, 0:pn], data1=fT[:, 0:pn], initial=0.0, op0=ALU.mult, op1=ALU.add),
                         reads=["h_rst", "h_fT" + kx], writes=["h_bT" + kx])
                    P.op("dve", lambda e: e.tensor_copy(out=totc[:, 0:nt], in_=v128(bT[:, 0:pn])[:, :, 127]), reads=["h_bT" + kx], writes=["h_totc" + kx])
                    totb = totc[:, 0:nt].unsqueeze(2).to_broadcast([128, nt, 128])
                    P.op("act", lambda e: e.activation(out=ebT[d_][:, tsl], in_=totc[:, 0:nt], func=AF.Exp), reads=["h_totc" + kx], writes=[f"h_ebT{pk}"])
                    if d_ == 0:
                        P.op("dve", lambda e: e.tensor_tensor(out=v128(t32[:, 0:pn]), in0=totb, in1=v128(bT[:, 0:pn]), op=ALU.subtract),
                             reads=["h_bT" + kx, "h_totc" + kx], writes=["h_t32" + kx])
                    else:
                        P.op("dve", lambda e: e.tensor_tensor(out=t32[:, 0:pn], in0=bT[:, 0:pn], in1=fT[:, 0:pn], op=ALU.subtract), reads=["h_bT" + kx, "h_fT" + kx], writes=["h_t32" + kx])
                        P.op("dve", lambda e: e.tensor_tensor(out=v128(bT[:, 0:pn]), in0=totb, in1=v128(t32[:, 0:pn]), op=ALU.subtract),
                             reads=["h_totc" + kx, "h_t32" + kx], writes=["h_bT" + kx])
                    P.op("act", lambda e: e.activation(out=t32[:, 0:pn], in_=t32[:, 0:pn], func=AF.Exp), writes=["h_t32" + kx])
                    P.op("dve", lambda e: e.tensor_tensor(out=kht[d_][:, ps_], in0=t32[:, 0:pn], in1=kk[:, 0:pn], op=ALU.mult),
                         reads=["h_t32" + kx, "h_kk" + kx], writes=[f"h_kht{pk}"])
                    P.op("dve", lambda e: e.tensor_copy(out=rc[:, 0:nt], in_=v128(bT[:, 0:pn])[:, :, REF]), reads=["h_bT" + kx], writes=["h_rc" + kx])
                    rb = rc[:, 0:nt].unsqueeze(2).to_broadcast([128, nt, 128])
                    P.op("act", lambda e: e.activation(out=erT[d_][:, tsl], in_=rc[:, 0:nt], func=AF.Exp), reads=["h_rc" + kx], writes=[f"h_erT{pk}"])
                    P.op("dve", lambda e: e.tensor_tensor(out=v128(bT[:, 0:pn]), in0=v128(bT[:, 0:pn]), in1=rb, op=ALU.subtract), reads=["h_rc" + kx], writes=["h_bT" + kx])
                    P.op("act", lambda e: e.activation(out=t32[:, 0:pn], in_=bT[:, 0:pn], func=AF.Exp), reads=["h_bT" + kx, f"h_kht{pk}"], writes=["h_t32" + kx])
                    P.op("dve", lambda e: e.tensor_tensor(out=qtl[d_][:, ps_], in0=t32[:, 0:pn], in1=qs[:, ps_], op=ALU.mult),
                         reads=["h_t32" + kx, "h_qs"], writes=[f"h_qtl{pk}"])
                    P.op("act", lambda e: e.activation(out=t32[:, 0:pn], in_=bT[:, 0:pn], func=AF.Exp, scale=-1.0), reads=["h_bT" + kx, f"h_qtl{pk}"], writes=["h_t32" + kx])
                    P.op("dve", lambda e: e.tensor_tensor(out=ktl[d_][:, ps_], in0=t32[:, 0:pn], in1=kk[:, 0:pn], op=ALU.mult),
                         reads=["h_t32" + kx, "h_kk" + kx], writes=[f"h_ktl{pk}"])

                orders = [list(range(NTILE)), [1, 0] + list(range(NTILE - 1, 1, -1))]
                piecesD = [[(0, 512), (512, 512), (1024, 512), (1536, 512), (2048, 256)],
                           [(0, 256), (1792, 512), (1280, 512), (768, 512), (256, 512)]]
                pkey = {}
                for d_ in range(2):
                    for k_, (p0, pn) in enumerate(piecesD[d_]):
                        for tl in range(p0 // 128, (p0 + pn) // 128):
                            pkey[(d_, tl)] = f"{d_}_{k_}"
                visited = set()
                for d_ in range(2):
                    P.op("dve", lambda e, d_=d_: e.memset(S[d_], 0.0), writes=[f"h_S{d_}"])
                ncomp = [0]

                def step(d_, it, tl):
                    ts_ = slice(tl * 128, (tl + 1) * 128)
                    pi = it % 2
                    pk = pkey[(d_, tl)]
                    lat = tl >= 2
                    second = tl in visited
                    visited.add(tl)
                    o_, ko = pb[d_], PB[d_]
                    sc_cols = slice(128 * d_, 128 * d_ + 128)
                    if lat:
                        erc = erT[d_][:, tl:tl + 1]
                        P.op("act", lambda e: e.activation(out=Sb[d_][pi], in_=S[d_], func=AF.Copy, scale=erc), reads=[f"h_S{d_}", f"h_erT{pk}"], writes=[f"h_Sb{d_}_{pi}"])
                        P.op("pe", mm(pb[6][:, sc_cols], ktl[d_][:, ts_], qtl[d_][:, ts_], True, True), reads=[f"h_ktl{pk}", f"h_qtl{pk}"], writes=[PB[6]])
                        msk = cb[:, CI["tF"], :] if d_ == 0 else cb[:, CI["tB"], :]
                        P.op("dve", lambda e: e.tensor_tensor(out=AT[d_][pi], in0=pb[6][:, sc_cols], in1=msk, op=ALU.mult), reads=["cb"], writes=[PB[6], f"h_AT{d_}{pi}"])
                        P.op("pe", mm(o_[:, 0:128], AT[d_][pi], itok[:, tl, :], True, False), reads=[f"h_AT{d_}{pi}", "h_itok"], writes=[ko])
                    P.op("pe", lambda e: e.transpose(out=pb7b[:, sc_cols], in_=kht[d_][:, ts_], identity=ident_b), reads=[f"h_kht{pk}", "cb"], writes=[PB[7]])
                    P.op("act", lambda e: e.copy(out=khtok[d_][pi], in_=pb7b[:, sc_cols]), writes=[PB[7], f"h_khtok{d_}{pi}"])
                    kv, kkv = pb[2 + d_], PB[2 + d_]
                    P.op("pe", mm(kv[:, 0:128], khtok[d_][pi], itok[:, tl, :], True, True), reads=[f"h_khtok{d_}{pi}", "h_itok"], writes=[kkv])
                    if lat:
                        P.op("pe", mm(o_[:, 0:128], qtl[d_][:, ts_], Sb[d_][pi], False, True), reads=[f"h_qtl{pk}", f"h_Sb{d_}_{pi}", ko], writes=[ko])
                    ebc = ebT[d_][:, tl:tl + 1]
                    P.op("dve", lambda e: e.scalar_tensor_tensor(out=S[d_], in0=S[d_], scalar=ebc, in1=kv[:, 0:128], op0=ALU.mult, op1=ALU.add),
                         reads=[f"h_ebT{pk}", f"h_S{d_}"], writes=[kkv, f"h_S{d_}"])
                    if not lat:
                        return
                    if not second:
                        P.op("act", lambda e: e.copy(out=ofirst[:, tl, :], in_=o_[:, 0:128]), writes=[ko, f"h_of{tl}"])
                        return
                    s1, ks1 = sm[d_], f"h_sm{d_}"
                    P.op("dve", lambda e: e.tensor_tensor(out=os_[d_], in0=o_[:, 0:128], in1=ofirst[:, tl, :], op=ALU.add), reads=[f"h_of{tl}"], writes=[ko, f"h_os{d_}"])
                    P.op("act", lambda e: e.activation(out=og[d_], in_=os_[d_], func=AF.Square, accum_out=s1), reads=[f"h_os{d_}"], writes=[f"h_og{d_}", ks1])
                    P.op("dve", lambda e: e.tensor_scalar(out=s1, in0=s1, scalar1=1.0 / 128, scalar2=EPS, op0=ALU.mult, op1=ALU.add), writes=[ks1])
                    P.op("act", lambda e: e.activation(out=s1, in_=s1, func=AF.Ln), writes=[ks1])
                    P.op("act", lambda e: e.activation(out=s1, in_=s1, func=AF.Exp, scale=-0.5), writes=[ks1])
                    P.op("dve", lambda e: e.scalar_tensor_tensor(out=os_[d_], in0=os_[d_], scalar=s1[:, 0:1], in1=hhg, op0=ALU.mult, op1=ALU.mult),
                         reads=[ks1, "h_hhg"], writes=[f"h_os{d_}"])
                    P.op("dve", lambda e: e.tensor_tensor(out=og[d_], in0=os_[d_], in1=sgt[:, tl, :], op=ALU.mult), reads=[f"h_os{d_}", "h_sgt"], writes=[f"h_og{d_}"])
                    P.op("pe", lambda e: e.transpose(out=pb7b[:, 256:384], in_=og[d_], identity=ident_b), reads=[f"h_og{d_}", "cb"], writes=[PB[7]])
                    xi = ncomp[0] % 2
                    ncomp[0] += 1
                    P.op("act", lambda e: e.copy(out=xTt[xi], in_=pb7b[:, 256:384]), writes=[PB[7], f"h_xT{xi}"])
                    c0 = (tl * 128 - CTX) // 32
                    for half in range(2):
                        po, kpo = pb[4 + half], PB[4 + half]
                        for j in range(4):
                            dc = half * 4 + j
                            P.op("pe", mm(po[:, j * 128:(j + 1) * 128], wout[:, dc * 128:(dc + 1) * 128], xTt[xi], True, True), reads=["h_wout", f"h_xT{xi}"], writes=[kpo])
                        keys = SKEY[half * 4:half * 4 + 4]
                        dstv = sT[:, half * 4:half * 4 + 4, ts_]
                        srcv = po[:, :].rearrange("p (d t) -> p d t", d=4)
                        P.op("dve", lambda e, dstv=dstv, srcv=srcv: e.tensor_tensor(out=dstv, in0=dstv, in1=srcv, op=ALU.add),
                             reads=keys, writes=[kpo] + keys)

                done = [0, 0]
                for k_ in range(5):
                    P.interleave([lambda d_=d_: precompute(d_, piecesD[d_][k_][0], piecesD[d_][k_][1], f"{d_}_{k_}") for d_ in range(2)])
                    avail = [min(NTILE, 4 * (k_ + 1)) if k_ < 4 else NTILE, min(NTILE, 2 + 4 * k_)]
                    if DEBUG_NT is not None:
                        avail = [min(a_, DEBUG_NT) for a_ in avail]
                    while done[0] < avail[0] or done[1] < avail[1]:
                        for d_ in range(2):
                            if done[d_] < avail[d_]:
                                step(d_, done[d_], orders[d_][done[d_]])
                                done[d_] += 1

        def final():
            P.barrier()
            cv = Carver()
            ob = [cv.take([128, 512]) for _ in range(2)]
            outs = []
            cnt = [0]
            sq = [cv.take([128, 512]) for _ in range(2)]
            rstd = cv.take([128, 512])
            for (t0, n, col) in _blocks(CTX, NT):
                for c in range(8):
                    P.op("act", lambda e, c=c, t0=t0, n=n: e.activation(out=sq[c % 2][:, 0:n], in_=sT[:, c, t0:t0 + n], func=AF.Square), reads=[SKEY[c]], writes=[f"nm_sq{c % 2}"])
                    P.op("pe", mm(pb[7][:, 0:n], ones_f, sq[c % 2][:, 0:n], c == 0, c == 7), reads=[f"nm_sq{c % 2}", "cf"], writes=[PB[7]])
                P.op("dve", lambda e, n=n: e.tensor_scalar(out=rstd[:, 0:n], in0=pb[7][:, 0:n], scalar1=1.0 / D, scalar2=EPS, op0=ALU.mult, op1=ALU.add), reads=[PB[7]], writes=["nm_rstd"])
                P.op("act", lambda e, n=n: e.activation(out=rstd[:, 0:n], in_=rstd[:, 0:n], func=AF.Sqrt), reads=["nm_rstd"], writes=["nm_rstd"])
                P.op("dve", lambda e, n=n: e.reciprocal(out=rstd[:, 0:n], in_=rstd[:, 0:n]), reads=["nm_rstd"], writes=["nm_rstd"])
                for c in range(8):
                    i = cnt[0] % 2
                    cnt[0] += 1
                    P.op("dve", lambda e, c=c, t0=t0, n=n, i=i: e.scalar_tensor_tensor(out=ob[i][:, 0:n], in0=sT[:, c, t0:t0 + n], scalar=gfin[:, c:c + 1], in1=rstd[:, 0:n],
                                                                                 op0=ALU.mult, op1=ALU.mult), reads=[SKEY[c], "gfin", "nm_rstd"], writes=[f"fin_ob{i}"])
                    outs.append(P.dma("sp", f"fin_ob{i}", lambda e, c=c, t0=t0, n=n, i=i: e.dma_start(out=d_out[:, c, t0 - CTX:t0 - CTX + n], in_=ob[i][:, 0:n]),
                                      reads=[f"fin_ob{i}"]))
            return outs

        outs = []
        if "mix0" in phases:
            mlstm(0)
        if "moe0" in phases:
            moe(0, 0, NT)
        if "mix1" in phases:
            hgrn(1)
        if "moe1" in phases:
            moe(1, CTX, NT)
        if "final" in phases:
            outs = final()
        if d_dump is not None:
            P.barrier()
            for c in range(8):
                outs.append(P.dma("sp", f"dump{c}", lambda e, c=c: e.dma_start(out=d_dump[:, c, :], in_=sT[:, c, :]), reads=[SKEY[c]]))
        P.emit(final_wait_ops=outs + dbg_outs)
    return nc


def _fm(v, lead=()):
    v = np.asarray(v, np.float32)
    k = v.shape[-1] // 128
    r = v.reshape(v.shape[:-1] + (k, 128))
    return np.ascontiguousarray(np.moveaxis(r, -1, 0))


def _wl(w):
    w = np.asarray(w, np.float32)
    K, N = w.shape
    return np.ascontiguousarray(w.reshape(K // 128, 128, N).transpose(1, 0, 2))


def prep_shared(x, c, ctx, c_ctx, ada_w, ada_b, norm_mix_g, norm_ffn_g, final_g,
                m_w_in, m_conv_w, m_conv_b, m_gate_b, m_head_g, m_w_out,
                h_w_in, h_lower_bounds, h_head_g, h_w_out,
                router_w, router_bias, e_w_gate, e_w_up, e_w_down):
    sh = {}
    sh["adaw"] = np.ascontiguousarray(np.asarray(ada_w, np.float32).reshape(2, 8, 128, 12, 512).transpose(0, 3, 2, 1, 4))
    sh["adab"] = np.ascontiguousarray(_fm(ada_b))
    sh["gmix"] = _fm(norm_mix_g)
    sh["gffn"] = _fm(norm_ffn_g)
    sh["gfin"] = _fm(final_g)
    sh["mwin"] = _wl(m_w_in[0])
    cw = _fm(m_conv_w[0])
    cbias = _fm(m_conv_b[0])
    sh["mconv"] = np.ascontiguousarray(np.concatenate([cw.transpose(0, 2, 1), cbias[:, :, None]], axis=2))
    sh["mgb"] = np.ascontiguousarray(np.broadcast_to(np.asarray(m_gate_b[0], np.float32)[None, :], (128, 16)))
    sh["mhg"] = np.ascontiguousarray(np.broadcast_to(np.asarray(m_head_g[0], np.float32)[None, :], (128, D)))
    sh["mwout"] = _wl(m_w_out[0])
    sh["hwin"] = _wl(h_w_in[0])
    sh["hlb"] = np.ascontiguousarray(_fm(h_lower_bounds))
    sh["hhg"] = np.ascontiguousarray(np.broadcast_to(np.asarray(h_head_g[0], np.float32)[None, :], (128, D)))
    sh["hwout"] = _wl(h_w_out[0])
    sh["rw"] = _wl(router_w)
    sh["rb"] = np.ascontiguousarray(np.broadcast_to(np.asarray(router_bias, np.float32)[None, None, :], (128, NTILE, NE)))
    sh["ewg"] = np.ascontiguousarray(np.asarray(e_w_gate, np.float32).reshape(2, NE, 8, 128, DEXP).transpose(0, 1, 3, 2, 4))
    sh["ewu"] = np.ascontiguousarray(np.asarray(e_w_up, np.float32).reshape(2, NE, 8, 128, DEXP).transpose(0, 1, 3, 2, 4))
    sh["ewd"] = np.ascontiguousarray(np.asarray(e_w_down, np.float32).reshape(2, NE, 4, 128, D).transpose(0, 1, 3, 2, 4))
    sh["cf"] = CARR
    sh["rst"] = RST
    return sh


def prep_core(b, x, c, ctx, c_ctx, s_override=None):
    if s_override is not None:
        s = s_override
    else:
        s = np.concatenate([np.asarray(ctx[b], np.float32), np.asarray(x[b], np.float32)], axis=0)
    xT = np.ascontiguousarray(s.reshape(NT, 8, 128).transpose(2, 1, 0))
    cc = np.stack([np.asarray(c[b], np.float32), np.asarray(c_ctx, np.float32)], axis=-1)
    cT = np.ascontiguousarray(cc.reshape(8, 128, 2).transpose(1, 0, 2))
    return {"xT": xT, "cT": cT}


_NC_CACHE = {}


def kernel(**inputs):
    x = inputs["x"]
    B = x.shape[0]
    sh = prep_shared(**inputs)
    if "full" not in _NC_CACHE:
        _NC_CACHE["full"] = build_program()
    nc = _NC_CACHE["full"]
    in_maps = []
    for b in range(B):
        m = dict(sh)
        m.update(prep_core(b, inputs["x"], inputs["c"], inputs["ctx"], inputs["c_ctx"]))
        in_maps.append(m)
    res = run_bass_kernel_spmd(nc, in_maps, core_ids=list(range(B)))
    out = np.empty((B, SEQ, D), np.float32)
    for b in range(B):
        oT = np.asarray(res.results[b]["outT"])
        out[b] = oT.transpose(2, 1, 0).reshape(64, 32, D).transpose(1, 0, 2).reshape(SEQ, D)
    return out
```

```python
import numpy as np
import concourse.bass as bass
import concourse.mybir as mybir
from contextlib import ExitStack
from concourse.bass_utils import run_bass_kernel_spmd

F32 = mybir.dt.float32
BF16 = mybir.dt.bfloat16
AF = mybir.ActivationFunctionType
ALU = mybir.AluOpType
AX = mybir.AxisListType

D = 1024
SEQ = 2048
CTX = 256
NT = SEQ + CTX
NTILE = NT // 128
EPS = 1e-6
NE = 16
DEXP = 512
ENGS = ("pe", "act", "dve", "pool", "sp")
DEBUG_NT = None


class Op:
    __slots__ = ("eng", "fn", "deps", "signal", "seq", "is_dma", "dsem", "dval", "n_inst", "name", "cost")

    def __init__(self, eng, fn, is_dma, dsem, name):
        self.eng = eng
        self.fn = fn
        self.deps = set()
        self.signal = False
        self.seq = None
        self.is_dma = is_dma
        self.dsem = dsem
        self.dval = None
        self.n_inst = 1
        self.name = name
        self.cost = None


class Prog:
    def __init__(self, nc):
        self.nc = nc
        self.ops = {e: [] for e in ENGS}
        self.last_w = {}
        self.readers = {}
        self.all_ops = []
        self._bar_from = 0
        self._capture = None

    def _add(self, eng, fn, reads, writes, is_dma=False, dsem=None, name=None):
        o = Op(eng, fn, is_dma, dsem, name)
        for k in reads:
            w = self.last_w.get(k)
            if w is not None:
                o.deps.add(w)
        for k in writes:
            w = self.last_w.get(k)
            if w is not None:
                o.deps.add(w)
            for r in self.readers.get(k, ()):
                o.deps.add(r)
        for k in reads:
            self.readers.setdefault(k, []).append(o)
        for k in writes:
            self.last_w[k] = o
            self.readers[k] = []
        o.deps.discard(o)
        self.ops[eng].append(o)
        self.all_ops.append(o)
        return o

    def op(self, eng, fn, reads=(), writes=(), name=None):
        if self._capture is not None:
            self._capture.append(("op", eng, fn, tuple(reads), tuple(writes), None, 1))
            return None
        return self._add(eng, fn, reads, writes, name=name)

    def dma(self, eng, group, fn, reads=(), writes=(), n=1, name=None):
        if self._capture is not None:
            self._capture.append(("dma", eng, fn, tuple(reads), tuple(writes), group, n))
            return None
        o = self._add(eng, fn, reads, writes, is_dma=True, dsem=group, name=name)
        o.n_inst = n
        return o

    def interleave(self, builders):
        streams = []
        for b in builders:
            self._capture = []
            b()
            streams.append(self._capture)
            self._capture = None
        idx = [0] * len(streams)
        while any(idx[i] < len(st) for i, st in enumerate(streams)):
            for i, st in enumerate(streams):
                if idx[i] < len(st):
                    kind, eng, fn, reads, writes, group, n = st[idx[i]]
                    idx[i] += 1
                    if kind == "op":
                        self._add(eng, fn, reads, writes)
                    else:
                        o = self._add(eng, fn, reads, writes, is_dma=True, dsem=group)
                        o.n_inst = n

    def barrier(self):
        self.all_ops.append(None)

    def _schedule(self, seg):
        import heapq
        COST = {"pe": 0.16, "act": 0.42, "dve": 0.45, "pool": 0.6, "sp": 0.1}
        HOP = 0.25
        n = len(seg)
        idx = {id(o): i for i, o in enumerate(seg)}
        cost = [0.0] * n
        lat = [0.0] * n
        for i, o in enumerate(seg):
            c = o.cost if o.cost is not None else COST[o.eng]
            if o.is_dma:
                cost[i] = 0.08 * o.n_inst
                lat[i] = c if o.cost is not None else 2.5
            else:
                cost[i] = c
        deps = [[idx[id(d)] for d in o.deps if id(d) in idx] for o in seg]
        succ = [[] for _ in range(n)]
        for i, dl in enumerate(deps):
            for d in dl:
                succ[d].append(i)
        prio = [0.0] * n
        for i in range(n - 1, -1, -1):
            m = 0.0
            for j in succ[i]:
                if prio[j] > m:
                    m = prio[j]
            prio[i] = m + cost[i] + lat[i] + HOP
        ndep = [len(dl) for dl in deps]
        est = [0.0] * n
        fin = [0.0] * n
        free_at = {e: 0.0 for e in ENGS}
        pend = {e: [] for e in ENGS}
        avail = {e: [] for e in ENGS}
        order = {e: [] for e in ENGS}
        for i in range(n):
            if ndep[i] == 0:
                heapq.heappush(pend[seg[i].eng], (0.0, -prio[i], i))
        left = n
        while left:
            best_e, best_t = None, None
            for e in ENGS:
                if not pend[e] and not avail[e]:
                    continue
                while pend[e] and pend[e][0][0] <= free_at[e]:
                    t_, p_, i_ = heapq.heappop(pend[e])
                    heapq.heappush(avail[e], (p_, i_))
                t = free_at[e] if avail[e] else max(free_at[e], pend[e][0][0])
                if best_t is None or t < best_t:
                    best_e, best_t = e, t
            e = best_e
            if avail[e]:
                p_, i = heapq.heappop(avail[e])
            else:
                t_, p_, i = heapq.heappop(pend[e])
            start = max(free_at[e], est[i])
            free_at[e] = start + cost[i]
            fin[i] = start + cost[i] + lat[i]
            order[e].append(seg[i])
            left -= 1
            for j in succ[i]:
                if fin[i] + HOP > est[j]:
                    est[j] = fin[i] + HOP
                ndep[j] -= 1
                if ndep[j] == 0:
                    heapq.heappush(pend[seg[j].eng], (est[j], -prio[j], j))
        return order

    def emit(self, final_wait_ops=(), schedule=True):
        nc = self.nc
        segs, cur = [], []
        for o in self.all_ops:
            if o is None:
                if cur:
                    segs.append(cur)
                cur = []
            else:
                cur.append(o)
        if cur:
            segs.append(cur)
        seg_orders = []
        for seg in segs:
            if schedule:
                seg_orders.append(self._schedule(seg))
            else:
                od = {e: [] for e in ENGS}
                for o in seg:
                    od[o.eng].append(o)
                seg_orders.append(od)
        bar_deps = [set() for _ in segs]
        last_comp = {}
        for k, od in enumerate(seg_orders):
            if k > 0:
                bar_deps[k] = set(last_comp.values()) | {o for o in segs[k - 1] if o.is_dma}
            for e in ENGS:
                comp = [o for o in od[e] if not o.is_dma]
                if comp:
                    last_comp[e] = comp[-1]
        for o in (x for x in self.all_ops if x is not None):
            for d in o.deps:
                if d.eng == "pe" and o.eng == "pe" and not d.is_dma and not o.is_dma:
                    continue
                d.signal = True
        for bd in bar_deps:
            for d in bd:
                d.signal = True
        for o in final_wait_ops:
            o.signal = True
        with ExitStack() as es:
            esem = {e: es.enter_context(nc.semaphore("c_" + e)) for e in ENGS}
            gsem = {}
            gcount = {}
            for e in ENGS:
                for od in seg_orders:
                    for o in od[e]:
                        if o.is_dma:
                            if o.dsem not in gsem:
                                gsem[o.dsem] = es.enter_context(nc.semaphore("d_" + str(o.dsem)))
                                gcount[o.dsem] = 0
                            gcount[o.dsem] += 16 * o.n_inst
                            o.dval = gcount[o.dsem]
            for e in ENGS:
                c = 0
                for od in seg_orders:
                    for o in od[e]:
                        if not o.is_dma and o.signal:
                            c += 1
                            o.seq = c
            block = es.enter_context(nc.Block())
            engobj = {"pe": "tensor", "act": "scalar", "dve": "vector", "pool": "gpsimd", "sp": "sync"}

            def run(ename, eng):
                waited = {}

                def do_waits(deps, is_pe_compute):
                    need = {}
                    for d in deps:
                        if d.is_dma:
                            key, val, sem = ("g", d.dsem), d.dval, gsem[d.dsem]
                        else:
                            if d.eng == "pe" and ename == "pe" and is_pe_compute:
                                continue
                            if d.eng == ename and ename == "pe":
                                continue
                            key, val, sem = ("e", d.eng), d.seq, esem[d.eng]
                        if waited.get(key, 0) >= val:
                            continue
                        if key not in need or need[key][1] < val:
                            need[key] = (sem, val)
                    for key, (sem, val) in need.items():
                        eng.wait_ge(sem, val)
                        waited[key] = val

                for k, od in enumerate(seg_orders):
                    if bar_deps[k]:
                        do_waits(list(bar_deps[k]), False)
                    for o in od[ename]:
                        do_waits(o.deps, not o.is_dma)
                        r = o.fn(eng)
                        if o.is_dma:
                            insts = r if isinstance(r, (list, tuple)) else [r]
                            assert len(insts) == o.n_inst
                            for ins in insts:
                                ins.then_inc(gsem[o.dsem], 16)
                        elif o.signal:
                            ins = r[-1] if isinstance(r, (list, tuple)) else r
                            ins.then_inc(esem[ename], 1)
                if ename == "sp":
                    for o in final_wait_ops:
                        if o.is_dma:
                            eng.wait_ge(gsem[o.dsem], o.dval)
                        else:
                            eng.wait_ge(esem[o.eng], o.seq)

            for ename in ENGS:
                getattr(block, engobj[ename])(lambda eng, ename=ename: run(ename, eng))


def _consts():
    s = np.arange(128)[:, None]
    t = np.arange(128)[None, :]
    same = (s // 64) == (t // 64)
    c = {}
    c["ident"] = np.eye(128, dtype=np.float32)
    c["ones"] = np.ones((128, 128), np.float32)
    c["triF"] = (same & (s <= t)).astype(np.float32)
    c["triB"] = (same & (s >= t)).astype(np.float32)
    c["sufF"] = (same & (s > t)).astype(np.float32)
    c["sufB"] = (same & (s < t)).astype(np.float32)
    c["sel0"] = np.repeat((s < 64).astype(np.float32), 128, axis=1)
    c["sel1"] = np.repeat((s >= 64).astype(np.float32), 128, axis=1)
    c["tF"] = (s <= t).astype(np.float32)
    c["tB"] = (s >= t).astype(np.float32)
    c["sF"] = (s > t).astype(np.float32)
    c["sB"] = (s < t).astype(np.float32)
    names = ["ident", "ones", "triF", "triB", "sufF", "sufB", "sel0", "sel1", "tF", "tB", "sF", "sB"]
    arr = np.stack([c[n] for n in names], axis=1)
    selE = np.zeros((128, NE, 128), np.float32)
    for e in range(NE):
        selE[e, e, :] = 1.0
    rst = np.ones((128, NT), np.float32)
    rst[:, ::128] = 0.0
    return names, np.ascontiguousarray(arr), selE, rst


CN, CARR, SELE, RST = _consts()
CI = {n: i for i, n in enumerate(CN)}


def _blocks(lo, hi, bs=512):
    out = []
    t = lo
    while t < hi:
        n = min(bs, hi - t)
        if t < CTX:
            n = min(n, CTX - t)
        out.append((t, n, 1 if t < CTX else 0))
        t += n
    return out


def build_program(phases=("mods", "mix0", "moe0", "mix1", "moe1", "final"), dump=None):
    nc = bass.Bass("TRN2", target_bir_lowering=False)
    es = ExitStack()
    P = Prog(nc)

    def din(name, shape, dt=F32):
        return nc.dram_tensor(name, list(shape), dt, kind="ExternalInput").ap()

    d_xT = din("xT", [128, 8, NT])
    d_cT = din("cT", [128, 8, 2])
    d_adaw = din("adaw", [2, 12, 128, 8, 512])
    d_adab = din("adab", [128, 2, 48])
    d_gmix = din("gmix", [128, 2, 8])
    d_gffn = din("gffn", [128, 2, 8])
    d_gfin = din("gfin", [128, 8])
    d_mwin = din("mwin", [128, 8, 3088])
    d_mconv = din("mconv", [128, 8, 4])
    d_mgb = din("mgb", [128, 16])
    d_mhg = din("mhg", [128, D])
    d_mwout = din("mwout", [128, 8, D])
    d_hwin = din("hwin", [128, 8, 5120])
    d_hlb = din("hlb", [128, 2, 16])
    d_hhg = din("hhg", [128, D])
    d_hwout = din("hwout", [128, 8, D])
    d_rw = din("rw", [128, 8, NE])
    d_rb = din("rb", [128, NTILE, NE])
    d_wg = din("ewg", [2, NE, 128, 8, DEXP])
    d_wu = din("ewu", [2, NE, 128, 8, DEXP])
    d_wd = din("ewd", [2, NE, 128, 4, D])
    d_cf = din("cf", [128, 12, 128])
    d_rst = din("rst", [128, NT])
    d_out = nc.dram_tensor("outT", [128, 8, SEQ], F32, kind="ExternalOutput").ap()
    d_dump = None
    if dump is not None:
        d_dump = nc.dram_tensor("dump", [128, 8, NT], F32, kind="ExternalOutput").ap()

    with es:
        def sb(name, shape, dt=F32):
            return es.enter_context(nc.sbuf_tensor("s_" + name, list(shape), dt))

        sT = sb("sT", [128, 8, NT])
        hT = sb("hT", [128, 8, NT], BF16)
        cf = sb("cf", [128, 12, 128])
        cb = sb("cb", [128, 12, 128], BF16)
        maskLE = sb("maskLE", [128, 128], BF16)
        maskGE = sb("maskGE", [128, 128], BF16)
        modT = [sb(f"modT{l}", [128, 48, 2]) for l in range(2)]
        adab = sb("adab", [128, 2, 48])
        gmix = sb("gmix", [128, 2, 8])
        gffn = sb("gffn", [128, 2, 8])
        gfin = sb("gfin", [128, 8])
        cT = sb("cT", [128, 8, 2])
        scT = sb("scT", [128, 8, 2])
        A1 = [sb(f"A1_{l}", [128, 8, 2]) for l in range(2)]
        A2 = [sb(f"A2_{l}", [128, 8, 2]) for l in range(2)]
        pb = [es.enter_context(nc.psum_tensor(f"pb{i}", [128, 512], F32)) for i in range(8)]
        PB = [f"pb{i}" for i in range(8)]

        ARENA = ((nc.sbuf_bytes_remaining - 2048) // 64) * 64
        arena = sb("arena", [128, ARENA // 4], F32)

        class Carver:
            def __init__(self):
                self.off = 0

            def take(self, shape, dt=F32):
                esz = 4 if dt == F32 else 2
                n = int(np.prod(shape[1:]))
                nbytes = ((n * esz + 63) // 64) * 64
                assert self.off + nbytes <= ARENA, (self.off, nbytes, ARENA)
                a = arena[:, self.off // 4:(self.off + nbytes) // 4]
                self.off += nbytes
                if dt != F32:
                    a = a.bitcast(dt)
                a = a[:, 0:n]
                if len(shape) == 3:
                    a = a.rearrange("p (a b) -> p a b", a=shape[1])
                elif len(shape) == 4:
                    a = a.rearrange("p (a b c) -> p a b c", a=shape[1], b=shape[2])
                return a

        dbg_outs = []

        def dbg(name, ap, shape, key, dt=F32):
            if dump is None or dump is True or name not in dump:
                return
            dd = nc.dram_tensor("dbg_" + name, list(shape), dt, kind="ExternalOutput").ap()
            dbg_outs.append(P.dma("sp", "dbg_" + name, lambda e: e.dma_start(out=dd, in_=ap), reads=[key]))

        def mm(out, lhsT, rhs, start, stop):
            return lambda e: e.matmul(out, lhsT=lhsT, rhs=rhs, start=start, stop=stop)

        for c in range(8):
            P.dma("sp", f"sT{c}", lambda e, c=c: e.dma_start(out=sT[:, c, :], in_=d_xT[:, c, :]), writes=[f"sT{c}"])
        P.dma("sp", "cf", lambda e: e.dma_start(out=cf[:], in_=d_cf), writes=["cf"])
        for nm, t, dsrc in (("adab", adab, d_adab), ("gmix", gmix, d_gmix), ("gffn", gffn, d_gffn),
                            ("gfin", gfin, d_gfin), ("cT", cT, d_cT)):
            P.dma("sp", nm, lambda e, t=t, dsrc=dsrc: e.dma_start(out=t[:], in_=dsrc), writes=[nm])
        P.op("dve", lambda e: e.tensor_copy(out=cb[:], in_=cf[:]), reads=["cf"], writes=["cb"])
        P.op("dve", lambda e: e.tensor_copy(out=maskLE[:], in_=cf[:, CI["triF"], :]), reads=["cf"], writes=["masks"])
        P.op("dve", lambda e: e.tensor_copy(out=maskGE[:], in_=cf[:, CI["triB"], :]), reads=["cf"], writes=["masks"])
        ident_b = cb[:, CI["ident"], :]
        ident_f = cf[:, CI["ident"], :]
        ones_f = cf[:, CI["ones"], :]
        SKEY = [f"sT{c}" for c in range(8)]

        MODS_BASE = ((ARENA - (4 * 16384 + 2 * 2048)) // 64) * 64
        if "mods" in phases:
            cv = Carver()
            cv.off = MODS_BASE
            NB_ = 4
            adaw = [cv.take([128, 8, 512]) for _ in range(NB_)]
            modrow = [cv.take([128, 512]) for _ in range(2)]
            P.op("act", lambda e: e.activation(out=scT[:], in_=cT[:], func=AF.Silu), reads=["cT"], writes=["scT"])
            for l in range(2):
                for nb in range(12):
                    bi = nb % NB_
                    mi = nb % 2
                    P.dma("sp" if nb % 2 == 0 else "act", f"adaw{bi}", lambda e, l=l, nb=nb, bi=bi: e.dma_start(out=adaw[bi], in_=d_adaw[l, nb]), writes=[f"arena_adaw{bi}"])
                    for kc in range(8):
                        P.op("pe", mm(pb[mi][0:2, :], scT[:, kc, :], adaw[bi][:, kc, :], kc == 0, kc == 7), reads=[f"arena_adaw{bi}", "scT"], writes=[PB[mi]])
                    P.op("dve", lambda e, mi=mi: e.tensor_copy(out=modrow[mi][0:2, :], in_=pb[mi][0:2, :]), writes=[PB[mi], f"arena_modrow{mi}"])
                    for j in range(4):
                        idx = nb * 4 + j
                        P.op("pe", lambda e, idx=idx, j=j, mi=mi: e.transpose(out=pb[2][:, 2 * idx:2 * idx + 2], in_=modrow[mi][0:2, j * 128:(j + 1) * 128], identity=cf[0:2, CI["ident"], 0:2]),
                             reads=[f"arena_modrow{mi}", "cf"], writes=[PB[2]])
                P.op("dve", lambda e, l=l: e.tensor_tensor(out=modT[l][:], in0=pb[2][:, 0:96].rearrange("p (a b) -> p a b", b=2),
                                                          in1=adab[:, l, :].unsqueeze(2).to_broadcast([128, 48, 2]), op=ALU.add),
                     reads=["adab"], writes=[PB[2], f"modT{l}"])
                for col in range(2):
                    P.op("dve", lambda e, l=l, col=col: e.scalar_tensor_tensor(
                        out=A1[l][:, :, col], in0=modT[l][:, 8:16, col], scalar=1.0, in1=gmix[:, l, :], op0=ALU.add, op1=ALU.mult),
                        reads=[f"modT{l}", "gmix"], writes=[f"A1_{l}"])
                    P.op("dve", lambda e, l=l, col=col: e.scalar_tensor_tensor(
                        out=A2[l][:, :, col], in0=modT[l][:, 32:40, col], scalar=1.0, in1=gffn[:, l, :], op0=ALU.add, op1=ALU.mult),
                        reads=[f"modT{l}", "gffn"], writes=[f"A2_{l}"])

        def SH1(l, c, col): return modT[l][:, 0 + c, col:col + 1]
        def G1(l, c, col): return modT[l][:, 16 + c, col:col + 1]
        def SH2(l, c, col): return modT[l][:, 24 + c, col:col + 1]
        def G2(l, c, col): return modT[l][:, 40 + c, col:col + 1]

        def norm_mod(cv, A, SH, l, lo, hi, out_bf, out_key, out_f32=None, perm_cols=False, after_block=None, bs=512, skey=None):
            if skey is None:
                skey = lambda c, t0: SKEY[c]
            sq = [cv.take([128, bs]) for _ in range(2)]
            rstd = cv.take([128, bs])
            tmp = [cv.take([128, bs]) for _ in range(2)]
            for bix, (t0, n, col) in enumerate(_blocks(lo, hi, bs)):
                o32 = out_f32[bix % 2] if out_f32 is not None else None
                okey = out_key(t0) if callable(out_key) else out_key
                k32 = "nm_h32_0"
                for c in range(8):
                    P.op("act", lambda e, c=c, t0=t0, n=n: e.activation(out=sq[c % 2][:, 0:n], in_=sT[:, c, t0:t0 + n], func=AF.Square),
                         reads=[skey(c, t0)], writes=[f"nm_sq{c % 2}"])
                    P.op("pe", mm(pb[7][:, 0:n], ones_f, sq[c % 2][:, 0:n], c == 0, c == 7), reads=[f"nm_sq{c % 2}", "cf"], writes=[PB[7]])
                P.op("dve", lambda e, n=n: e.tensor_scalar(out=rstd[:, 0:n], in0=pb[7][:, 0:n], scalar1=1.0 / D, scalar2=EPS,
                                                          op0=ALU.mult, op1=ALU.add), reads=[PB[7]], writes=["nm_rstd"])
                P.op("act", lambda e, n=n: e.activation(out=rstd[:, 0:n], in_=rstd[:, 0:n], func=AF.Sqrt), reads=["nm_rstd"], writes=["nm_rstd"])
                P.op("dve", lambda e, n=n: e.reciprocal(out=rstd[:, 0:n], in_=rstd[:, 0:n]), reads=["nm_rstd"], writes=["nm_rstd"])
                for c in range(8):
                    tp = tmp[c % 2]
                    P.op("dve", lambda e, c=c, t0=t0, n=n, tp=tp: e.tensor_tensor(out=tp[:, 0:n], in0=sT[:, c, t0:t0 + n], in1=rstd[:, 0:n], op=ALU.mult),
                         reads=[skey(c, t0), "nm_rstd"], writes=[f"nm_tmp{c % 2}"])
                    src = tp[:, 0:n]
                    if out_f32 is not None:
                        dst, dkey = o32[:, c, 0:n], k32
                    else:
                        dst, dkey = out_bf[:, c, t0:t0 + n], okey
                        if perm_cols and t0 >= CTX:
                            r0, nr = (t0 - CTX) // 64, n // 64
                            dst = out_bf[:, c, CTX:NT].rearrange("p (col row) -> p row col", row=32)[:, r0:r0 + nr, :]
                            src = src.rearrange("p (r c) -> p r c", c=64)
                    P.op("dve", lambda e, c=c, col=col, dst=dst, src=src: e.tensor_scalar(
                        out=dst, in0=src, scalar1=A[l][:, c, col:col + 1], scalar2=SH(l, c, col), op0=ALU.mult, op1=ALU.add),
                        reads=[f"nm_tmp{c % 2}", f"A1_{l}", f"A2_{l}", f"modT{l}"], writes=[dkey])
                    if out_f32 is not None:
                        P.op("act", lambda e, c=c, t0=t0, n=n, o32=o32: e.copy(out=out_bf[:, c, t0:t0 + n], in_=o32[:, c, 0:n]), reads=[k32], writes=[okey])
                if after_block is not None:
                    after_block(t0, n)

        def moe(l, lo, hi):
            P.barrier()
            cv = Carver()
            h32s = [cv.take([128, 8, 256])] * 2
            rw = cv.take([128, 8, NE])
            rb = cv.take([128, NTILE, NE])
            cw_all = cv.take([128, NTILE, NE])
            NTN = NTILE * NE
            rt = [cv.take([128, NTILE, NE]) for _ in range(5)]
            r4 = [cv.take([128, NTILE * 4]) for _ in range(4)]
            r1 = [cv.take([128, NTILE]) for _ in range(2)]
            wgb = [cv.take([128, 8, DEXP], BF16) for _ in range(2)]
            wub = [cv.take([128, 8, DEXP], BF16) for _ in range(2)]
            wdb = [cv.take([128, 4, D], BF16) for _ in range(2)]
            aT = [cv.take([128, 4, 512], BF16) for _ in range(2)]
            sg = [cv.take([128, 512], BF16) for _ in range(2)]
            t1 = [cv.take([128, 512], BF16) for _ in range(2)]
            cwb = [cv.take([128, 512], BF16) for _ in range(2)]
            P.dma("sp", "rw", lambda e: e.dma_start(out=rw, in_=d_rw), writes=["moe_rw"])
            P.dma("sp", "rb", lambda e: e.dma_start(out=rb, in_=d_rb), writes=["moe_rb"])
            T0, T1 = lo // 128, hi // 128
            s_all = rt[0]
            blk_ctr = [0]

            def route(t0, n):
                hb = 0
                for sub in range(n // 128):
                    tix = (t0 + sub * 128) // 128
                    pl, kl = pb[7], PB[7]
                    for c in range(8):
                        P.op("pe", mm(pl[:, 256:256 + NE], h32s[hb][:, c, sub * 128:(sub + 1) * 128], rw[:, c, :], c == 0, c == 7),
                             reads=[f"nm_h32_{hb}", "moe_rw"], writes=[kl])
                    P.op("act", lambda e, tix=tix, pl=pl: e.activation(out=s_all[:, tix, :], in_=pl[:, 256:256 + NE], func=AF.Sigmoid), writes=[kl, f"rt_s{tix}"])

            sel_, sel2_, eq1, eq2 = rt[1:5]
            w_ = sel2_
            m1, m2, gs, geq = r4
            bm, ws = r1
            cw16 = cw_all

            def route_batch(Ta, Tb):
                TS = slice(Ta, Tb)
                nT = Tb - Ta
                K = lambda nm: f"{nm}{Ta}"
                v3 = lambda a: a[:, TS, :].rearrange("p t (g k) -> p (t g) k", k=4)
                g2 = lambda a: a[:, Ta * 4:Tb * 4]
                bc4 = lambda a: g2(a).unsqueeze(2).to_broadcast([128, nT * 4, 4])
                g3 = lambda a: g2(a).rearrange("p (t g) -> p t g", g=4)
                skeys = [f"rt_s{t_}" for t_ in range(Ta, Tb)]
                P.op("dve", lambda e: e.tensor_tensor(out=sel_[:, TS, :], in0=s_all[:, TS, :], in1=rb[:, TS, :], op=ALU.add), reads=skeys + ["moe_rb"], writes=[K("rt_sel")])
                P.op("dve", lambda e: e.tensor_reduce(out=g2(m1), in_=v3(sel_), axis=AX.X, op=ALU.max), reads=[K("rt_sel")], writes=[K("rt_m1")])
                P.op("dve", lambda e: e.tensor_tensor(out=v3(eq1), in0=v3(sel_), in1=bc4(m1), op=ALU.is_equal), reads=[K("rt_sel"), K("rt_m1")], writes=[K("rt_eq1")])
                P.op("dve", lambda e: e.scalar_tensor_tensor(out=sel2_[:, TS, :], in0=eq1[:, TS, :], scalar=-1e9, in1=sel_[:, TS, :], op0=ALU.mult, op1=ALU.add),
                     reads=[K("rt_eq1"), K("rt_sel")], writes=[K("rt_sel2")])
                P.op("dve", lambda e: e.tensor_reduce(out=g2(m2), in_=v3(sel2_), axis=AX.X, op=ALU.max), reads=[K("rt_sel2")], writes=[K("rt_m2")])
                P.op("dve", lambda e: e.tensor_tensor(out=v3(eq2), in0=v3(sel2_), in1=bc4(m2), op=ALU.is_equal), reads=[K("rt_sel2"), K("rt_m2")], writes=[K("rt_eq2")])
                P.op("dve", lambda e: e.tensor_tensor(out=g2(gs), in0=g2(m1), in1=g2(m2), op=ALU.add), reads=[K("rt_m1"), K("rt_m2")], writes=[K("rt_gs")])
                P.op("dve", lambda e: e.tensor_reduce(out=bm[:, TS], in_=g3(gs), axis=AX.X, op=ALU.max), reads=[K("rt_gs")], writes=[K("rt_bm")])
                P.op("dve", lambda e: e.tensor_tensor(out=g3(geq), in0=g3(gs), in1=bm[:, TS].unsqueeze(2).to_broadcast([128, nT, 4]), op=ALU.is_equal),
                     reads=[K("rt_gs"), K("rt_bm")], writes=[K("rt_geq")])
                P.op("dve", lambda e: e.tensor_tensor(out=eq1[:, TS, :], in0=eq1[:, TS, :], in1=eq2[:, TS, :], op=ALU.add), reads=[K("rt_eq2")], writes=[K("rt_eq1")])
                P.op("dve", lambda e: e.tensor_tensor(out=v3(eq1), in0=v3(eq1), in1=bc4(geq), op=ALU.mult), reads=[K("rt_geq")], writes=[K("rt_eq1")])
                P.op("dve", lambda e: e.tensor_tensor(out=w_[:, TS, :], in0=eq1[:, TS, :], in1=s_all[:, TS, :], op=ALU.mult),
                     reads=[K("rt_eq1"), K("rt_eq2"), K("rt_m2")] + skeys, writes=[K("rt_w"), K("rt_sel2")])
                P.op("dve", lambda e: e.tensor_reduce(out=ws[:, TS], in_=w_[:, TS, :], axis=AX.X, op=ALU.add), reads=[K("rt_w")], writes=[K("rt_ws")])
                P.op("dve", lambda e: e.reciprocal(out=ws[:, TS], in_=ws[:, TS]), writes=[K("rt_ws")])
                P.op("dve", lambda e: e.tensor_tensor(out=cw16[:, TS, :], in0=w_[:, TS, :], in1=ws[:, TS].unsqueeze(2).to_broadcast([128, nT, NE]), op=ALU.mult),
                     reads=[K("rt_w"), K("rt_ws")], writes=[f"moe_cw{t_}" for t_ in range(Ta, Tb)])

            def after_blk(t0, n):
                route(t0, n)
                t1_ = t0 + n
                for (bt0, bn, _c) in _blocks(lo, hi):
                    if bt0 + bn == t1_:
                        route_batch(bt0 // 128, (bt0 + bn) // 128)

            norm_mod(cv, A2, SH2, l, lo, hi, out_bf=hT, out_key=lambda t0: f"hT_{t0 // 256}", out_f32=h32s, after_block=after_blk, bs=256,
                     skey=lambda c, t0: f"sT{c}_{t0 // 256}")

            blocks = _blocks(lo, hi)
            items = [(e_, b_) for e_ in range(NE) for b_ in range(len(blocks))]

            def load_w(e_):
                bi = e_ % 2
                P.dma("pool", f"wg{bi}", lambda e: e.dma_start(out=wgb[bi], in_=d_wg[l, e_], max_dma_last_dim=4096), writes=[f"moe_wg{bi}"])
                P.dma("pool", f"wu{bi}", lambda e: e.dma_start(out=wub[bi], in_=d_wu[l, e_], max_dma_last_dim=4096), writes=[f"moe_wu{bi}"])
                P.dma("pool", f"wd{bi}", lambda e: e.dma_start(out=wdb[bi], in_=d_wd[l, e_], max_dma_last_dim=4096), writes=[f"moe_wd{bi}"])

            cwcol = [cv.take([128, 128], BF16) for _ in range(4)]
            cwc_ctr = [0]

            def stage1(i):
                e_, b_ = items[i]
                t0, n, col = blocks[b_]
                bi = e_ % 2
                ai = i % 2
                hkeys = [f"hT_{k_}" for k_ in range(t0 // 256, (t0 + n + 255) // 256)]
                for sub in range(n // 128):
                    tix = (t0 + sub * 128) // 128
                    ci_ = cwc_ctr[0] % 4
                    cwc_ctr[0] += 1
                    P.op("dve", lambda e, tix=tix, ci_=ci_: e.tensor_copy(out=cwcol[ci_], in_=cw16[:, tix, e_:e_ + 1].to_broadcast([128, 128])),
                         reads=[f"moe_cw{tix}"], writes=[f"moe_cwcol{ci_}"])
                    P.op("pe", mm(pb[6][:, sub * 128:(sub + 1) * 128], cwcol[ci_], ident_b, True, True),
                         reads=[f"moe_cwcol{ci_}", "cb"], writes=[PB[6]])
                P.op("act", lambda e: e.copy(out=cwb[ai][:, 0:n], in_=pb[6][:, 0:n]), reads=[PB[6]], writes=[f"moe_cwb{ai}"])
                for fc in range(4):
                    pg, pu = pb[(fc % 2) * 2], pb[(fc % 2) * 2 + 1]
                    kg, ku = PB[(fc % 2) * 2], PB[(fc % 2) * 2 + 1]
                    for c in range(8):
                        P.op("pe", mm(pg[:, 0:n], wgb[bi][:, c, fc * 128:(fc + 1) * 128], hT[:, c, t0:t0 + n], c == 0, c == 7),
                             reads=[f"moe_wg{bi}"] + hkeys, writes=[kg])
                    for c in range(8):
                        P.op("pe", mm(pu[:, 0:n], wub[bi][:, c, fc * 128:(fc + 1) * 128], hT[:, c, t0:t0 + n], c == 0, c == 7),
                             reads=[f"moe_wu{bi}"] + hkeys, writes=[ku])
                    si = fc % 2
                    P.op("act", lambda e, pg=pg, si=si: e.activation(out=sg[si][:, 0:n], in_=pg[:, 0:n], func=AF.Silu), reads=[kg], writes=[f"moe_sg{si}"])
                    P.op("dve", lambda e, pu=pu, si=si: e.tensor_tensor(out=t1[si][:, 0:n], in0=sg[si][:, 0:n], in1=pu[:, 0:n], op=ALU.mult),
                         reads=[f"moe_sg{si}", ku], writes=[f"moe_t1{si}"])
                    P.op("pool", lambda e, si=si, fc=fc: e.tensor_tensor(out=aT[ai][:, fc, 0:n], in0=t1[si][:, 0:n], in1=cwb[ai][:, 0:n], op=ALU.mult),
                         reads=[f"moe_t1{si}", f"moe_cwb{ai}"], writes=[f"moe_aT{ai}"])

            def stage2(i):
                e_, b_ = items[i]
                t0, n, col = blocks[b_]
                bi = e_ % 2
                ai = i % 2
                for dc in range(8):
                    pd, kd = pb[4 + dc % 2], PB[4 + dc % 2]
                    for fc in range(4):
                        P.op("pe", mm(pd[:, 0:n], wdb[bi][:, fc, dc * 128:(dc + 1) * 128], aT[ai][:, fc, 0:n], fc == 0, fc == 3),
                             reads=[f"moe_wd{bi}", f"moe_aT{ai}"], writes=[kd])
                    sk = [f"sT{dc}_{k_}" for k_ in range(t0 // 256, (t0 + n + 255) // 256)]
                    P.op("dve", lambda e, dc=dc, pd=pd: e.scalar_tensor_tensor(
                        out=sT[:, dc, t0:t0 + n], in0=pd[:, 0:n], scalar=G2(l, dc, col), in1=sT[:, dc, t0:t0 + n], op0=ALU.mult, op1=ALU.add),
                        reads=[kd, f"modT{l}"] + sk, writes=sk)

            load_w(0)
            for i in range(len(items)):
                e_, b_ = items[i]
                stage1(i)
                if i > 0:
                    stage2(i - 1)
                if b_ == 0 and e_ + 1 < NE:
                    load_w(e_ + 1)
            stage2(len(items) - 1)

        def mlstm(l):
            cv = Carver()
            wgt = cv.take([128, 8, 16], BF16)
            mgb = cv.take([128, 16])
            mhg = cv.take([128, 256])
            cvw = cv.take([128, 8, 4])
            G = cv.take([128, NTILE, 16])
            Lg = cv.take([128, NTILE, 8])
            EE = cv.take([128, NTILE, 24])
            EB = cv.take([128, NTILE, 8])
            arg = cv.take([128, 24])
            P.dma("pool", "m_wgt", lambda e: e.dma_start(out=wgt, in_=d_mwin[:, :, 3072:3088]), writes=["m_wgt"])
            P.dma("sp", "m_mgb", lambda e: e.dma_start(out=mgb, in_=d_mgb), writes=["m_mgb"])
            P.dma("sp", "m_cvw", lambda e: e.dma_start(out=cvw, in_=d_mconv), writes=["m_cvw"])

            norm_mod(cv, A1, SH1, l, 0, NT, out_bf=hT, out_key="hT", bs=256)

            for tl in range(NTILE):
                ts_ = slice(tl * 128, (tl + 1) * 128)
                for c in range(8):
                    P.op("pe", mm(pb[0][:, 0:16], hT[:, c, ts_], wgt[:, c, :], c == 0, c == 7), reads=["hT", "m_wgt"], writes=[PB[0]])
                P.op("dve", lambda e, tl=tl: e.tensor_tensor(out=G[:, tl, :], in0=pb[0][:, 0:16], in1=mgb, op=ALU.add), reads=[PB[0], "m_mgb"], writes=["m_G"])
                for k, src in enumerate((slice(4, 8), slice(12, 16))):
                    P.op("act", lambda e, tl=tl, k=k, src=src: e.activation(out=Lg[:, tl, 4 * k:4 * k + 4], in_=G[:, tl, src], func=AF.Exp, scale=-1.0),
                         reads=["m_G"], writes=["m_L"])
                P.op("dve", lambda e, tl=tl: e.tensor_scalar(out=Lg[:, tl, :], in0=Lg[:, tl, :], scalar1=1.0, scalar2=None, op0=ALU.add), reads=["m_L"], writes=["m_L"])
                P.op("act", lambda e, tl=tl: e.activation(out=Lg[:, tl, :], in_=Lg[:, tl, :], func=AF.Ln), reads=["m_L"], writes=["m_L"])
                q_ = pb[1]
                for j, (mat, cs) in enumerate((("tF", slice(0, 4)), ("tB", slice(4, 8)), ("sF", slice(0, 4)), ("sB", slice(4, 8)))):
                    P.op("pe", mm(q_[:, 4 * j:4 * j + 4], cf[:, CI[mat], :], Lg[:, tl, cs], True, True), reads=["cf", "m_L"], writes=[PB[1]])
                P.op("pe", mm(q_[:, 16:24], cf[:, CI["ones"], :], Lg[:, tl, :], True, True), reads=["cf", "m_L"], writes=[PB[1]])
                P.op("dve", lambda e, tl=tl: e.tensor_tensor(out=arg[:, 0:4], in0=G[:, tl, 0:4], in1=q_[:, 0:4], op=ALU.add), reads=["m_G"], writes=["m_arg", PB[1]])
                P.op("dve", lambda e, tl=tl: e.tensor_tensor(out=arg[:, 4:8], in0=G[:, tl, 0:4], in1=q_[:, 8:12], op=ALU.subtract), reads=["m_G"], writes=["m_arg", PB[1]])
                P.op("dve", lambda e, tl=tl: e.tensor_tensor(out=arg[:, 8:12], in0=G[:, tl, 8:12], in1=q_[:, 4:8], op=ALU.add), reads=["m_G"], writes=["m_arg", PB[1]])
                P.op("dve", lambda e, tl=tl: e.tensor_tensor(out=arg[:, 12:16], in0=G[:, tl, 8:12], in1=q_[:, 12:16], op=ALU.subtract), reads=["m_G"], writes=["m_arg", PB[1]])
                P.op("dve", lambda e, tl=tl: e.tensor_copy(out=arg[:, 16:24], in_=q_[:, 0:8]), writes=["m_arg", PB[1]])
                P.op("act", lambda e, tl=tl: e.activation(out=EE[:, tl, :], in_=arg, func=AF.Exp), reads=["m_arg"], writes=["m_EE"])
                P.op("act", lambda e, tl=tl: e.activation(out=EB[:, tl, :], in_=q_[:, 16:24], func=AF.Exp, scale=-1.0), writes=["m_EB", PB[1]])

            assert cv.off <= MODS_BASE, (cv.off, MODS_BASE)
            P.barrier()
            wqk = cv.take([128, 8, 2, 128], BF16)
            wv = cv.take([128, 8, 256], BF16)
            wo = cv.take([128, 8, 256], BF16)
            wout = cv.take([128, 2, D], BF16)
            qT = cv.take([128, NT], BF16)
            kT = cv.take([128, NT], BF16)
            vaug = cv.take([128, NTILE, 257], BF16)
            ktok = cv.take([128, NTILE, 128], BF16)
            hfirst = cv.take([128, NTILE, 256], BF16)
            ybuf = [cv.take([128, 512]) for _ in range(2)]
            cvs = cv
            STf = [[cvs.take([128, 128], BF16) for _ in range(2)] for _ in range(2)]
            vt = [[cvs.take([128, 257], BF16) for _ in range(2)] for _ in range(2)]
            vt2 = [[cvs.take([128, 257], BF16) for _ in range(2)] for _ in range(2)]
            X = [cvs.take([128, 257]) for _ in range(2)]
            Xb = [[cvs.take([128, 257], BF16) for _ in range(2)] for _ in range(2)]
            numS = [[cvs.take([128, 257]) for _ in range(2)] for _ in range(2)]
            hs = [cvs.take([128, 256]) for _ in range(2)]
            hn = hs
            hg = [cvs.take([128, 256], BF16) for _ in range(2)]
            sgo = [cvs.take([128, 256]) for _ in range(2)]
            tmpo = [cvs.take([128, 4, 128]) for _ in range(2)]
            sm = [[cvs.take([128, 1]) for _ in range(2)] for _ in range(2)]
            xTt = [cvs.take([128, 2, 128], BF16) for _ in range(2)]
            P.op("pool", lambda e: e.memset(vaug[:, :, 256:257], 1.0), writes=["m_vaug"])
            pb6b = pb[6].bitcast(BF16)

            for h in range(4):
                P.dma("sp", "m_mhg", lambda e, h=h: e.dma_start(out=mhg, in_=d_mhg[:, h * 256:(h + 1) * 256]), writes=["m_mhg"])
                P.dma("pool", "m_wqk", lambda e, h=h: [e.dma_start(out=wqk[:, :, 0, :], in_=d_mwin[:, :, h * 128:(h + 1) * 128]),
                                                       e.dma_start(out=wqk[:, :, 1, :], in_=d_mwin[:, :, 512 + h * 128:512 + (h + 1) * 128])],
                      writes=["m_wqk"], n=2)
                P.dma("pool", "m_wv", lambda e, h=h: e.dma_start(out=wv, in_=d_mwin[:, :, 1024 + h * 256:1024 + (h + 1) * 256]), writes=["m_wv"])
                P.dma("pool", "m_wo", lambda e, h=h: e.dma_start(out=wo, in_=d_mwin[:, :, 2048 + h * 256:2048 + (h + 1) * 256]), writes=["m_wo"])
                P.dma("pool", "m_wout", lambda e, h=h: e.dma_start(out=wout, in_=d_mwout[:, 2 * h:2 * h + 2, :], max_dma_last_dim=4096), writes=["m_wout"])
                qk_blocks = [(0, CTX, 0, CTX)] + [(CTX + 410 * j, min(CTX + 410 * (j + 1), NT), CTX, NT) for j in range(5)]
                cnt_ = [0]
                for which, dstT, dkey in ((0, qT, "m_qT"), (1, kT, "m_kT")):
                    ch = (0 if which == 0 else 4) + h
                    for (a_, b_, A_, B_) in qk_blocks:
                        n = b_ - a_
                        a2, b2 = max(a_ - 1, A_), min(b_ + 1, B_)
                        off = a_ - a2
                        N_ = b2 - a2
                        bi_ = cnt_[0] % 2
                        cnt_[0] += 1
                        pq, kq = pb[bi_], PB[bi_]
                        yb, ky = ybuf[bi_], f"m_y{bi_}"
                        for c in range(8):
                            P.op("pe", mm(pq[:, 0:N_], wqk[:, c, which, :], hT[:, c, a2:b2], c == 0, c == 7), reads=["m_wqk", "hT"], writes=[kq])
                        P.op("dve", lambda e, pq=pq, yb=yb, off=off, n=n, ch=ch: e.tensor_scalar(out=yb[:, 0:n], in0=pq[:, off:off + n], scalar1=cvw[:, ch, 1:2], scalar2=cvw[:, ch, 3:4],
                                                                                       op0=ALU.mult, op1=ALU.add), reads=["m_cvw"], writes=[kq, ky])
                        if off == 1:
                            o0, i0, m0 = 0, 0, n
                        else:
                            o0, i0, m0 = 1, 0, n - 1
                        P.op("dve", lambda e, pq=pq, yb=yb, o0=o0, i0=i0, m0=m0, ch=ch: e.scalar_tensor_tensor(
                            out=yb[:, o0:o0 + m0], in0=pq[:, i0:i0 + m0], scalar=cvw[:, ch, 0:1], in1=yb[:, o0:o0 + m0], op0=ALU.mult, op1=ALU.add),
                            reads=["m_cvw"], writes=[kq, ky])
                        m2 = n if b2 > b_ else n - 1
                        P.op("dve", lambda e, pq=pq, yb=yb, off=off, m2=m2, ch=ch: e.scalar_tensor_tensor(
                            out=yb[:, 0:m2], in0=pq[:, off + 1:off + 1 + m2], scalar=cvw[:, ch, 2:3], in1=yb[:, 0:m2], op0=ALU.mult, op1=ALU.add),
                            reads=["m_cvw"], writes=[kq, ky])
                        if which == 0:
                            P.op("act", lambda e, yb=yb, n=n: e.activation(out=yb[:, 0:n], in_=yb[:, 0:n], func=AF.Silu), writes=[ky])
                            P.op("act", lambda e, yb=yb, n=n, a_=a_, b_=b_: e.activation(out=qT[:, a_:b_], in_=yb[:, 0:n], func=AF.Copy, scale=float(128 ** -0.5)),
                                 reads=[ky], writes=[dkey])
                        else:
                            P.op("act", lambda e, yb=yb, n=n, a_=a_, b_=b_: e.activation(out=kT[:, a_:b_], in_=yb[:, 0:n], func=AF.Silu), reads=[ky], writes=[dkey])
                for tl in range(NTILE):
                    ts_ = slice(tl * 128, (tl + 1) * 128)
                    pv, kv_ = pb[2 + tl % 2], PB[2 + tl % 2]
                    for c in range(8):
                        P.op("pe", mm(pv[:, 0:256], hT[:, c, ts_], wv[:, c, :], c == 0, c == 7), reads=["hT", "m_wv"], writes=[kv_])
                    P.op("act", lambda e, tl=tl, pv=pv: e.copy(out=vaug[:, tl, 0:256], in_=pv[:, 0:256]), reads=[kv_], writes=["m_vaug"])
                    tb = pb[4 + tl % 2].bitcast(BF16)
                    P.op("pe", lambda e, ts_=ts_, tb=tb: e.transpose(out=tb[:, 0:128], in_=kT[:, ts_], identity=ident_b), reads=["m_kT", "cb"], writes=[PB[4 + tl % 2]])
                    P.op("act", lambda e, tl=tl, tb=tb: e.copy(out=ktok[:, tl, :], in_=tb[:, 0:128]), reads=[PB[4 + tl % 2]], writes=["m_ktok"])

                orders = [list(range(NTILE)), [1, 0] + list(range(NTILE - 1, 1, -1))]
                if DEBUG_NT is not None:
                    orders = [o_[:DEBUG_NT] for o_ in orders]
                visited = set()
                for d_ in range(2):
                    P.op("dve", lambda e, d_=d_: e.memset(X[d_], 0.0), writes=[f"m_X{d_}"])
                    P.op("dve", lambda e, d_=d_: e.memset(Xb[d_][0], 0.0), writes=[f"m_Xb{d_}_0"])
                ncomp = [0]

                def step(d_, it, tl):
                    ts_ = slice(tl * 128, (tl + 1) * 128)
                    pi = it % 2
                    eoff = 0 if d_ == 0 else 8
                    doff = 4 if d_ else 0
                    second = tl in visited
                    visited.add(tl)
                    sc_cols = slice(128 * d_, 128 * d_ + 128)
                    P.op("pe", mm(pb[6][:, sc_cols], kT[:, ts_], qT[:, ts_], True, True), reads=["m_kT", "m_qT"], writes=[PB[6]])
                    msk = cb[:, CI["tF"], :] if d_ == 0 else cb[:, CI["tB"], :]
                    P.op("dve", lambda e: e.tensor_tensor(out=STf[d_][pi], in0=pb[6][:, sc_cols], in1=msk, op=ALU.mult),
                         reads=["cb"], writes=[PB[6], f"m_STf{d_}{pi}"])
                    e1 = EE[:, tl, eoff + h:eoff + h + 1]
                    e2 = EE[:, tl, eoff + 4 + h:eoff + 5 + h]
                    P.op("act", lambda e: e.activation(out=vt[d_][pi], in_=vaug[:, tl, :], func=AF.Copy, scale=e1),
                         reads=["m_vaug", "m_EE"], writes=[f"m_vt{d_}{pi}"])
                    P.op("dve", lambda e: e.tensor_scalar(out=vt2[d_][pi], in0=vaug[:, tl, :], scalar1=e2, scalar2=None, op0=ALU.mult),
                         reads=["m_vaug", "m_EE"], writes=[f"m_vt2{d_}{pi}"])
                    if second:
                        for c in range(8):
                            P.op("pe", mm(pb[7][:, 0:256], hT[:, c, ts_], wo[:, c, :], c == 0, c == 7), reads=["hT", "m_wo"], writes=[PB[7]])
                        P.op("act", lambda e: e.activation(out=sgo[d_], in_=pb[7][:, 0:256], func=AF.Exp, scale=-1.0), writes=[PB[7], f"m_sgo{d_}"])
                        P.op("dve", lambda e: e.tensor_scalar(out=sgo[d_], in0=sgo[d_], scalar1=1.0, scalar2=None, op0=ALU.add), writes=[f"m_sgo{d_}"])
                        P.op("act", lambda e: e.activation(out=sgo[d_], in_=sgo[d_], func=AF.Ln), writes=[f"m_sgo{d_}"])
                        P.op("act", lambda e: e.activation(out=sgo[d_], in_=sgo[d_], func=AF.Exp, scale=-1.0), writes=[f"m_sgo{d_}"])
                    num, knum = pb[d_], PB[d_]
                    P.op("pe", mm(num[:, 0:257], STf[d_][pi], vt[d_][pi], True, False), reads=[f"m_STf{d_}{pi}", f"m_vt{d_}{pi}"], writes=[knum])
                    kv, kkv = pb[2 + d_], PB[2 + d_]
                    P.op("pe", mm(kv[:, 0:257], ktok[:, tl, :], vt2[d_][pi], True, True), reads=["m_ktok", f"m_vt2{d_}{pi}"], writes=[kkv])
                    r0, r1 = it % 2, (it + 1) % 2
                    P.op("pe", mm(num[:, 0:257], qT[:, ts_], Xb[d_][r0], False, True), reads=["m_qT", f"m_Xb{d_}_{r0}", knum], writes=[knum])
                    ebc = EB[:, tl, doff + h:doff + h + 1]
                    P.op("dve", lambda e: e.scalar_tensor_tensor(out=X[d_], in0=X[d_], scalar=ebc, in1=kv[:, 0:257], op0=ALU.mult, op1=ALU.add),
                         reads=["m_EB", f"m_X{d_}"], writes=[kkv, f"m_X{d_}"])
                    P.op("act", lambda e: e.copy(out=Xb[d_][r1], in_=X[d_]), reads=[f"m_X{d_}"], writes=[f"m_Xb{d_}_{r1}"])
                    ns, kns = numS[d_][pi], f"m_numS{d_}{pi}"
                    P.op("act", lambda e: e.copy(out=ns, in_=num[:, 0:257]), writes=[knum, kns])
                    thr = EE[:, tl, 16 + doff + h:16 + doff + h + 1]
                    s0, ks0 = sm[d_][0], f"m_sm{d_}0"
                    P.op("act", lambda e: e.activation(out=s0, in_=ns[:, 256:257], func=AF.Abs), reads=[kns], writes=[ks0])
                    P.op("dve", lambda e: e.tensor_tensor(out=s0, in0=s0, in1=thr, op=ALU.max), reads=[ks0, "m_EE"], writes=[ks0])
                    P.op("dve", lambda e: e.reciprocal(out=s0, in_=s0), reads=[ks0], writes=[ks0])
                    if not second:
                        P.op("dve", lambda e: e.tensor_scalar(out=hfirst[:, tl, :], in0=ns[:, 0:256], scalar1=s0[:, 0:1], scalar2=None, op0=ALU.mult),
                             reads=[kns, ks0], writes=[f"m_hf{tl}"])
                        return
                    s1, ks1 = sm[d_][1], f"m_sm{d_}1"
                    P.op("dve", lambda e: e.scalar_tensor_tensor(out=hs[d_], in0=ns[:, 0:256], scalar=s0[:, 0:1], in1=hfirst[:, tl, :], op0=ALU.mult, op1=ALU.add),
                         reads=[kns, ks0, f"m_hf{tl}"], writes=[f"m_hs{d_}"])
                    P.op("act", lambda e: e.activation(out=hg[d_], in_=hs[d_], func=AF.Square, accum_out=s1), reads=[f"m_hs{d_}"], writes=[f"m_hg{d_}", ks1])
                    P.op("dve", lambda e: e.tensor_scalar(out=s1, in0=s1, scalar1=1.0 / 256, scalar2=EPS, op0=ALU.mult, op1=ALU.add), reads=[ks1], writes=[ks1])
                    P.op("act", lambda e: e.activation(out=s1, in_=s1, func=AF.Ln), reads=[ks1], writes=[ks1])
                    P.op("act", lambda e: e.activation(out=s1, in_=s1, func=AF.Exp, scale=-0.5), reads=[ks1], writes=[ks1])
                    P.op("dve", lambda e: e.scalar_tensor_tensor(out=hs[d_], in0=hs[d_], scalar=s1[:, 0:1], in1=mhg, op0=ALU.mult, op1=ALU.mult),
                         reads=[f"m_hs{d_}", ks1, "m_mhg"], writes=[f"m_hs{d_}"])
                    P.op("dve", lambda e: e.tensor_tensor(out=hg[d_], in0=hs[d_], in1=sgo[d_], op=ALU.mult), reads=[f"m_hs{d_}", f"m_sgo{d_}", f"m_hg{d_}"], writes=[f"m_hg{d_}"])
                    for fc in range(2):
                        P.op("pe", lambda e, fc=fc: e.transpose(out=pb6b[:, 512 + fc * 128:512 + (fc + 1) * 128], in_=hg[d_][:, fc * 128:(fc + 1) * 128], identity=ident_b),
                             reads=[f"m_hg{d_}", "cb"], writes=[PB[6]])
                    xi = ncomp[0] % 2
                    ncomp[0] += 1
                    P.op("act", lambda e: e.copy(out=xTt[xi], in_=pb6b[:, 512:768].rearrange("p (a b) -> p a b", a=2)), writes=[PB[6], f"m_xT{xi}"])
                    col = 1 if tl < 2 else 0
                    for half in range(2):
                        po, ko = pb[4 + half], PB[4 + half]
                        for j in range(4):
                            dc = half * 4 + j
                            for fc in range(2):
                                P.op("pe", mm(po[:, j * 128:(j + 1) * 128], wout[:, fc, dc * 128:(dc + 1) * 128], xTt[xi][:, fc, :], fc == 0, fc == 1), reads=["m_wout", f"m_xT{xi}"], writes=[ko])
                        g1b = modT[l][:, 16 + half * 4:16 + half * 4 + 4, col:col + 1].to_broadcast([128, 4, 128])
                        P.op("dve", lambda e, po=po, g1b=g1b, half=half: e.tensor_tensor(out=tmpo[half], in0=po[:, :].rearrange("p (a b) -> p a b", a=4), in1=g1b, op=ALU.mult),
                             reads=[f"modT{l}"], writes=[ko, f"m_tmpo{half}"])
                        keys = SKEY[half * 4:half * 4 + 4]
                        P.op("dve", lambda e, half=half: e.tensor_tensor(out=sT[:, half * 4:half * 4 + 4, ts_], in0=sT[:, half * 4:half * 4 + 4, ts_], in1=tmpo[half], op=ALU.add),
                             reads=[f"m_tmpo{half}"] + keys, writes=keys)

                for it in range(len(orders[0])):
                    step(0, it, orders[0][it])
                    step(1, it, orders[1][it])

        def hgrn(l):
            P.barrier()
            cv = Carver()
            hlb = cv.take([128, 2, 16])
            lbT = cv.take([128, 16])
            omlb = cv.take([128, 16])
            hhg = cv.take([128, 128])
            rst = cv.take([128, 512])
            P.dma("sp", "h_hlb", lambda e: e.dma_start(out=hlb, in_=d_hlb), writes=["h_hlb"])
            P.dma("sp", "h_rst", lambda e: e.dma_start(out=rst, in_=d_rst[:, 0:512]), writes=["h_rst"])
            P.op("dve", lambda e: e.tensor_tensor(out=lbT, in0=hlb[:, 1, :], in1=hlb[:, 0, :], op=ALU.subtract), reads=["h_hlb"], writes=["h_lb"])
            P.op("act", lambda e: e.activation(out=omlb, in_=lbT, func=AF.Exp), reads=["h_lb"], writes=["h_omlb"])
            P.op("act", lambda e: e.activation(out=lbT, in_=lbT, func=AF.Exp, scale=-1.0), reads=["h_lb", "h_omlb"], writes=["h_lb"])
            for t_, k_ in ((omlb, "h_omlb"), (lbT, "h_lb")):
                P.op("dve", lambda e, t_=t_: e.tensor_scalar(out=t_, in0=t_, scalar1=1.0, scalar2=None, op0=ALU.add), writes=[k_])
                P.op("dve", lambda e, t_=t_: e.reciprocal(out=t_, in_=t_), writes=[k_])

            ptmp = [cv.take([128, SEQ]) for _ in range(2)]
            for c in range(8):
                pt = ptmp[c % 2]
                src = sT[:, c, CTX:NT].rearrange("p (row col) -> p col row", col=64)
                P.op("dve" if c % 2 == 0 else "pool", lambda e, pt=pt, src=src: e.tensor_copy(out=pt.rearrange("p (col row) -> p col row", row=32), in_=src),
                     reads=[SKEY[c]], writes=[f"h_ptmp{c % 2}"])
                P.op("act", lambda e, pt=pt, c=c: e.copy(out=sT[:, c, CTX:NT], in_=pt), reads=[f"h_ptmp{c % 2}"], writes=[SKEY[c]])
            P.barrier()
            cv.off -= 2 * SEQ * 4
            norm_base = cv.off
            norm_mod(cv, A1, SH1, l, 0, NT, out_bf=hT, out_key="hT", bs=256)
            norm_end = cv.off
            P.barrier()

            wq = cv.take([128, 8, 128], BF16)
            wz = cv.take([128, 8, 2, 128], BF16)
            wi = cv.take([128, 8, 128], BF16)
            wgg = cv.take([128, 8, 128], BF16)
            wout = cv.take([128, D], BF16)
            qs = cv.take([128, NT], BF16)
            cvn = Carver()
            cvn.off = norm_base
            fTs = [cv.take([128, 512]), cvn.take([128, 512])]
            bTs = [cv.take([128, 512]), cvn.take([128, 512])]
            t32s = [cv.take([128, 512]), cv.take([128, 512])]
            kks = [cv.take([128, 512], BF16), cvn.take([128, 512], BF16)]
            assert cvn.off <= norm_end
            totcs = [cv.take([128, 4]) for _ in range(2)]
            rcs = [cv.take([128, 4]) for _ in range(2)]
            t32 = t32s[0]
            qtl = [cv.take([128, NT], BF16) for _ in range(2)]
            ktl = [cv.take([128, NT], BF16) for _ in range(2)]
            kht = [cv.take([128, NT], BF16) for _ in range(2)]
            ebT = [cv.take([128, NTILE]) for _ in range(2)]
            erT = [cv.take([128, NTILE]) for _ in range(2)]
            itok = cv.take([128, NTILE, 128], BF16)
            sgt = cv.take([128, NTILE, 128], BF16)
            gtmp = cv.take([128, 128])
            ofirst = cv.take([128, NTILE, 128], BF16)
            AT = [[cv.take([128, 128], BF16) for _ in range(2)] for _ in range(2)]
            khtok = [[cv.take([128, 128], BF16) for _ in range(2)] for _ in range(2)]
            S = [cv.take([128, 128]) for _ in range(2)]
            Sb = [[cv.take([128, 128], BF16) for _ in range(2)] for _ in range(2)]
            os_ = [cv.take([128, 128]) for _ in range(2)]
            og = [cv.take([128, 128], BF16) for _ in range(2)]
            sm = [cv.take([128, 1]) for _ in range(2)]
            xTt = [cv.take([128, 128], BF16) for _ in range(2)]
            pb7b = pb[7].bitcast(BF16)
            g1row = cv.take([128, D], BF16)
            for dc in range(8):
                P.op("pe", mm(pb[dc % 2][:, 0:128], modT[l][:, 16 + dc, 0:1].to_broadcast([128, 128]), ident_f, True, True), reads=[f"modT{l}", "cf"], writes=[PB[dc % 2]])
                P.op("act", lambda e, dc=dc: e.copy(out=g1row[:, dc * 128:(dc + 1) * 128], in_=pb[dc % 2][:, 0:128]), writes=[PB[dc % 2], "h_g1row"])
            REF = 64
            pieces = [(0, 512), (512, 512), (1024, 512), (1536, 512), (2048, 256)]

            def v128(a):
                return a.rearrange("p (c k) -> p c k", k=128)

            for h in range(8):
                P.dma("sp", "h_hhg", lambda e, h=h: e.dma_start(out=hhg, in_=d_hhg[:, h * 128:(h + 1) * 128]), writes=["h_hhg"])
                P.dma("pool", "h_wq", lambda e, h=h: e.dma_start(out=wq, in_=d_hwin[:, :, h * 128:(h + 1) * 128]), writes=["h_wq"])
                P.dma("pool", "h_wz", lambda e, h=h: [e.dma_start(out=wz[:, :, 0, :], in_=d_hwin[:, :, 1024 + h * 128:1024 + (h + 1) * 128]),
                                                      e.dma_start(out=wz[:, :, 1, :], in_=d_hwin[:, :, 2048 + h * 128:2048 + (h + 1) * 128])], writes=["h_wz"], n=2)
                P.dma("pool", "h_wi", lambda e, h=h: e.dma_start(out=wi, in_=d_hwin[:, :, 3072 + h * 128:3072 + (h + 1) * 128]), writes=["h_wi"])
                P.dma("pool", "h_wg", lambda e, h=h: e.dma_start(out=wgg, in_=d_hwin[:, :, 4096 + h * 128:4096 + (h + 1) * 128]), writes=["h_wg"])
                P.dma("pool", "h_wout", lambda e, h=h: e.dma_start(out=wout, in_=d_hwout[:, h, :], max_dma_last_dim=4096), writes=["h_wout"])
                P.op("dve", lambda e: e.tensor_tensor(out=wout, in0=wout, in1=g1row, op=ALU.mult), reads=["h_g1row"], writes=["h_wout"])
                for bi_, (t0, n, col) in enumerate(_blocks(0, NT)):
                    pq, kq = pb[bi_ % 2], PB[bi_ % 2]
                    for c in range(8):
                        P.op("pe", mm(pq[:, 0:n], wq[:, c, :], hT[:, c, t0:t0 + n], c == 0, c == 7), reads=["h_wq", "hT"], writes=[kq])
                    P.op("act", lambda e, n=n, pq=pq: e.activation(out=t32[:, 0:n], in_=pq[:, 0:n], func=AF.Exp, scale=-1.0), writes=[kq, "h_t32_0"])
                    P.op("act", lambda e, n=n: e.activation(out=t32[:, 0:n], in_=t32[:, 0:n], func=AF.Ln, bias=1.0), writes=["h_t32_0"])
                    P.op("act", lambda e, n=n: e.activation(out=t32[:, 0:n], in_=t32[:, 0:n], func=AF.Exp, scale=-1.0), writes=["h_t32_0"])
                    P.op("dve", lambda e, t0=t0, n=n, pq=pq: e.tensor_tensor(out=qs[:, t0:t0 + n], in0=t32[:, 0:n], in1=pq[:, 0:n], op=ALU.mult),
                         reads=["h_t32_0"], writes=[kq, "h_qs"])
                for tl in range(NTILE):
                    ts_ = slice(tl * 128, (tl + 1) * 128)
                    pi_, ki_ = pb[2 + tl % 2], PB[2 + tl % 2]
                    for c in range(8):
                        P.op("pe", mm(pi_[:, 0:128], hT[:, c, ts_], wi[:, c, :], c == 0, c == 7), reads=["hT", "h_wi"], writes=[ki_])
                    if tl >= 2:
                        for c in range(8):
                            P.op("pe", mm(pi_[:, 128:256], hT[:, c, ts_], wgg[:, c, :], c == 0, c == 7), reads=["hT", "h_wg"], writes=[ki_])
                    P.op("act", lambda e, tl=tl, pi_=pi_: e.copy(out=itok[:, tl, :], in_=pi_[:, 0:128]), writes=[ki_, "h_itok"])
                    if tl >= 2:
                        P.op("act", lambda e, pi_=pi_: e.activation(out=gtmp, in_=pi_[:, 128:256], func=AF.Exp, scale=-1.0), writes=[ki_, "h_gtmp"])
                        P.op("act", lambda e: e.activation(out=gtmp, in_=gtmp, func=AF.Ln, bias=1.0), writes=["h_gtmp"])
                        P.op("act", lambda e: e.activation(out=gtmp, in_=gtmp, func=AF.Exp, scale=-1.0), writes=["h_gtmp"])
                        P.op("dve", lambda e, tl=tl, pi_=pi_: e.tensor_tensor(out=sgt[:, tl, :], in0=gtmp, in1=pi_[:, 128:256], op=ALU.mult), reads=["h_gtmp"], writes=[ki_, "h_sgt"])

                def precompute(d_, p0, pn, pk):
                    fT, bT, t32, kk, totc, rc = fTs[d_], bTs[d_], t32s[d_], kks[d_], totcs[d_], rcs[d_]
                    kx = f"_{d_}"
                    lbc = lbT[:, d_ * 8 + h:d_ * 8 + h + 1]
                    omc = omlb[:, d_ * 8 + h:d_ * 8 + h + 1]
                    nt = pn // 128
                    tsl = slice(p0 // 128, p0 // 128 + nt)
                    ps_ = slice(p0, p0 + pn)
                    pz, kz = pb[4 + d_], PB[4 + d_]
                    for c in range(8):
                        P.op("pe", mm(pz[:, 0:pn], wz[:, c, d_, :], hT[:, c, ps_], c == 0, c == 7), reads=["h_wz", "hT"], writes=[kz])
                    P.op("act", lambda e: e.activation(out=fT[:, 0:pn], in_=pz[:, 0:pn], func=AF.Exp, scale=-1.0), writes=[kz, "h_fT" + kx])
                    P.op("act", lambda e: e.activation(out=fT[:, 0:pn], in_=fT[:, 0:pn], func=AF.Ln, bias=1.0), writes=["h_fT" + kx])
                    P.op("act", lambda e: e.activation(out=fT[:, 0:pn], in_=fT[:, 0:pn], func=AF.Exp, scale=-1.0), writes=["h_fT" + kx])
                    P.op("act", lambda e: e.activation(out=fT[:, 0:pn], in_=fT[:, 0:pn], func=AF.Identity, scale=omc, bias=lbc),
                         reads=["h_lb", "h_omlb"], writes=["h_fT" + kx])
                    P.op("act", lambda e: e.activation(out=kk[:, 0:pn], in_=fT[:, 0:pn], func=AF.Identity, scale=-1.0, bias=1.0), reads=["h_fT" + kx], writes=["h_kk" + kx])
                    P.op("act", lambda e: e.activation(out=fT[:, 0:pn], in_=fT[:, 0:pn], func=AF.Ln), reads=["h_kk" + kx], writes=["h_fT" + kx])
                    P.op("dve", lambda e: e.tensor_tensor_scan(out=bT[:, 0:pn], data0=rst[:, 0:pn], data1=fT[:, 0:pn], initial=0.0, op0=ALU.mult, op1=ALU.add),
                         reads=["h_rst", "h_fT" + kx], writes=["h_bT" + kx])
                    P.op("dve", lambda e: e.tensor_copy(out=totc[:, 0:nt], in_=v128(bT[:, 0:pn])[:, :, 127]), reads=["h_bT" + kx], writes=["h_totc" + kx])
                    totb = totc[:, 0:nt].unsqueeze(2).to_broadcast([128, nt, 128])
                    P.op("act", lambda e: e.activation(out=ebT[d_][:, tsl], in_=totc[:, 0:nt], func=AF.Exp), reads=["h_totc" + kx], writes=[f"h_ebT{pk}"])
                    if d_ == 0:
                        P.op("dve", lambda e: e.tensor_tensor(out=v128(t32[:, 0:pn]), in0=totb, in1=v128(bT[:, 0:pn]), op=ALU.subtract),
                             reads=["h_bT" + kx, "h_totc" + kx], writes=["h_t32" + kx])
                    else:
                        P.op("dve", lambda e: e.tensor_tensor(out=t32[:, 0:pn], in0=bT[:, 0:pn], in1=fT[:, 0:pn], op=ALU.subtract), reads=["h_bT" + kx, "h_fT" + kx], writes=["h_t32" + kx])
                        P.op("dve", lambda e: e.tensor_tensor(out=v128(bT[:, 0:pn]), in0=totb, in1=v128(t32[:, 0:pn]), op=ALU.subtract),
                             reads=["h_totc" + kx, "h_t32" + kx], writes=["h_bT" + kx])
                    P.op("act", lambda e: e.activation(out=t32[:, 0:pn], in_=t32[:, 0:pn], func=AF.Exp), writes=["h_t32" + kx])
                    P.op("dve", lambda e: e.tensor_tensor(out=kht[d_][:, ps_], in0=t32[:, 0:pn], in1=kk[:, 0:pn], op=ALU.mult),
                         reads=["h_t32" + kx, "h_kk" + kx], writes=[f"h_kht{pk}"])
                    P.op("dve", lambda e: e.tensor_copy(out=rc[:, 0:nt], in_=v128(bT[:, 0:pn])[:, :, REF]), reads=["h_bT" + kx], writes=["h_rc" + kx])
                    rb = rc[:, 0:nt].unsqueeze(2).to_broadcast([128, nt, 128])
                    P.op("act", lambda e: e.activation(out=erT[d_][:, tsl], in_=rc[:, 0:nt], func=AF.Exp), reads=["h_rc" + kx], writes=[f"h_erT{pk}"])
                    P.op("dve", lambda e: e.tensor_tensor(out=v128(bT[:, 0:pn]), in0=v128(bT[:, 0:pn]), in1=rb, op=ALU.subtract), reads=["h_rc" + kx], writes=["h_bT" + kx])
                    P.op("act", lambda e: e.activation(out=t32[:, 0:pn], in_=bT[:, 0:pn], func=AF.Exp), reads=["h_bT" + kx, f"h_kht{pk}"], writes=["h_t32" + kx])
                    P.op("dve", lambda e: e.tensor_tensor(out=qtl[d_][:, ps_], in0=t32[:, 0:pn], in1=qs[:, ps_], op=ALU.mult),
                         reads=["h_t32" + kx, "h_qs"], writes=[f"h_qtl{pk}"])
                    P.op("act", lambda e: e.activation(out=t32[:, 0:pn], in_=bT[:, 0:pn], func=AF.Exp, scale=-1.0), reads=["h_bT" + kx, f"h_qtl{pk}"], writes=["h_t32" + kx])
                    P.op("dve", lambda e: e.tensor_tensor(out=ktl[d_][:, ps_], in0=t32[:, 0:pn], in1=kk[:, 0:pn], op=ALU.mult),
                         reads=["h_t32" + kx, "h_kk" + kx], writes=[f"h_ktl{pk}"])

                orders = [list(range(NTILE)), [1, 0] + list(range(NTILE - 1, 1, -1))]
                piecesD = [[(0, 512), (512, 512), (1024, 512), (1536, 512), (2048, 256)],
                           [(0, 256), (1792, 512), (1280, 512), (768, 512), (256, 512)]]
                pkey = {}
                for d_ in range(2):
                    for k_, (p0, pn) in enumerate(piecesD[d_]):
                        for tl in range(p0 // 128, (p0 + pn) // 128):
                            pkey[(d_, tl)] = f"{d_}_{k_}"
                visited = set()
                for d_ in range(2):
                    P.op("dve", lambda e, d_=d_: e.memset(S[d_], 0.0), writes=[f"h_S{d_}"])
                ncomp = [0]

                def step(d_, it, tl):
                    ts_ = slice(tl * 128, (tl + 1) * 128)
                    pi = it % 2
                    pk = pkey[(d_, tl)]
                    lat = tl >= 2
                    second = tl in visited
                    visited.add(tl)
                    o_, ko = pb[d_], PB[d_]
                    sc_cols = slice(128 * d_, 128 * d_ + 128)
                    if lat:
                        erc = erT[d_][:, tl:tl + 1]
                        P.op("act", lambda e: e.activation(out=Sb[d_][pi], in_=S[d_], func=AF.Copy, scale=erc), reads=[f"h_S{d_}", f"h_erT{pk}"], writes=[f"h_Sb{d_}_{pi}"])
                        P.op("pe", mm(pb[6][:, sc_cols], ktl[d_][:, ts_], qtl[d_][:, ts_], True, True), reads=[f"h_ktl{pk}", f"h_qtl{pk}"], writes=[PB[6]])
                        msk = cb[:, CI["tF"], :] if d_ == 0 else cb[:, CI["tB"], :]
                        P.op("dve", lambda e: e.tensor_tensor(out=AT[d_][pi], in0=pb[6][:, sc_cols], in1=msk, op=ALU.mult), reads=["cb"], writes=[PB[6], f"h_AT{d_}{pi}"])
                        P.op("pe", mm(o_[:, 0:128], AT[d_][pi], itok[:, tl, :], True, False), reads=[f"h_AT{d_}{pi}", "h_itok"], writes=[ko])
                    P.op("pe", lambda e: e.transpose(out=pb7b[:, sc_cols], in_=kht[d_][:, ts_], identity=ident_b), reads=[f"h_kht{pk}", "cb"], writes=[PB[7]])
                    P.op("act", lambda e: e.copy(out=khtok[d_][pi], in_=pb7b[:, sc_cols]), writes=[PB[7], f"h_khtok{d_}{pi}"])
                    kv, kkv = pb[2 + d_], PB[2 + d_]
                    P.op("pe", mm(kv[:, 0:128], khtok[d_][pi], itok[:, tl, :], True, True), reads=[f"h_khtok{d_}{pi}", "h_itok"], writes=[kkv])
                    if lat:
                        P.op("pe", mm(o_[:, 0:128], qtl[d_][:, ts_], Sb[d_][pi], False, True), reads=[f"h_qtl{pk}", f"h_Sb{d_}_{pi}", ko], writes=[ko])
                    ebc = ebT[d_][:, tl:tl + 1]
                    P.op("dve", lambda e: e.scalar_tensor_tensor(out=S[d_], in0=S[d_], scalar=ebc, in1=kv[:, 0:128], op0=ALU.mult, op1=ALU.add),
                         reads=[f"h_ebT{pk}", f"h_S{d_}"], writes=[kkv, f"h_S{d_}"])
                    if not lat:
                        return
                    if not second:
                        P.op("act", lambda e: e.copy(out=ofirst[:, tl, :], in_=o_[:, 0:128]), writes=[ko, f"h_of{tl}"])
                        return
                    s1, ks1 = sm[d_], f"h_sm{d_}"
                    P.op("dve", lambda e: e.tensor_tensor(out=os_[d_], in0=o_[:, 0:128], in1=ofirst[:, tl, :], op=ALU.add), reads=[f"h_of{tl}"], writes=[ko, f"h_os{d_}"])
                    P.op("act", lambda e: e.activation(out=og[d_], in_=os_[d_], func=AF.Square, accum_out=s1), reads=[f"h_os{d_}"], writes=[f"h_og{d_}", ks1])
                    P.op("dve", lambda e: e.tensor_scalar(out=s1, in0=s1, scalar1=1.0 / 128, scalar2=EPS, op0=ALU.mult, op1=ALU.add), writes=[ks1])
                    P.op("act", lambda e: e.activation(out=s1, in_=s1, func=AF.Ln), writes=[ks1])
                    P.op("act", lambda e: e.activation(out=s1, in_=s1, func=AF.Exp, scale=-0.5), writes=[ks1])
                    P.op("dve", lambda e: e.scalar_tensor_tensor(out=os_[d_], in0=os_[d_], scalar=s1[:, 0:1], in1=hhg, op0=ALU.mult, op1=ALU.mult),
                         reads=[ks1, "h_hhg"], writes=[f"h_os{d_}"])
                    P.op("dve", lambda e: e.tensor_tensor(out=og[d_], in0=os_[d_], in1=sgt[:, tl, :], op=ALU.mult), reads=[f"h_os{d_}", "h_sgt"], writes=[f"h_og{d_}"])
                    P.op("pe", lambda e: e.transpose(out=pb7b[:, 256:384], in_=og[d_], identity=ident_b), reads=[f"h_og{d_}", "cb"], writes=[PB[7]])
                    xi = ncomp[0] % 2
                    ncomp[0] += 1
                    P.op("act", lambda e: e.copy(out=xTt[xi], in_=pb7b[:, 256:384]), writes=[PB[7], f"h_xT{xi}"])
                    c0 = (tl * 128 - CTX) // 32
                    for half in range(2):
                        po, kpo = pb[4 + half], PB[4 + half]
                        for j in range(4):
                            dc = half * 4 + j
                            P.op("pe", mm(po[:, j * 128:(j + 1) * 128], wout[:, dc * 128:(dc + 1) * 128], xTt[xi], True, True), reads=["h_wout", f"h_xT{xi}"], writes=[kpo])
                        keys = SKEY[half * 4:half * 4 + 4]
                        dstv = sT[:, half * 4:half * 4 + 4, ts_]
                        srcv = po[:, :].rearrange("p (d t) -> p d t", d=4)
                        P.op("dve", lambda e, dstv=dstv, srcv=srcv: e.tensor_tensor(out=dstv, in0=dstv, in1=srcv, op=ALU.add),
                             reads=keys, writes=[kpo] + keys)

                done = [0, 0]
                for k_ in range(5):
                    P.interleave([lambda d_=d_: precompute(d_, piecesD[d_][k_][0], piecesD[d_][k_][1], f"{d_}_{k_}") for d_ in range(2)])
                    avail = [min(NTILE, 4 * (k_ + 1)) if k_ < 4 else NTILE, min(NTILE, 2 + 4 * k_)]
                    if DEBUG_NT is not None:
                        avail = [min(a_, DEBUG_NT) for a_ in avail]
                    while done[0] < avail[0] or done[1] < avail[1]:
                        for d_ in range(2):
                            if done[d_] < avail[d_]:
                                step(d_, done[d_], orders[d_][done[d_]])
                                done[d_] += 1

        def final():
            P.barrier()
            cv = Carver()
            ob = [cv.take([128, 512]) for _ in range(2)]
            outs = []
            cnt = [0]
            sq = [cv.take([128, 512]) for _ in range(2)]
            rstd = cv.take([128, 512])
            for (t0, n, col) in _blocks(CTX, NT):
                for c in range(8):
                    P.op("act", lambda e, c=c, t0=t0, n=n: e.activation(out=sq[c % 2][:, 0:n], in_=sT[:, c, t0:t0 + n], func=AF.Square), reads=[SKEY[c]], writes=[f"nm_sq{c % 2}"])
                    P.op("pe", mm(pb[7][:, 0:n], ones_f, sq[c % 2][:, 0:n], c == 0, c == 7), reads=[f"nm_sq{c % 2}", "cf"], writes=[PB[7]])
                P.op("dve", lambda e, n=n: e.tensor_scalar(out=rstd[:, 0:n], in0=pb[7][:, 0:n], scalar1=1.0 / D, scalar2=EPS, op0=ALU.mult, op1=ALU.add), reads=[PB[7]], writes=["nm_rstd"])
                P.op("act", lambda e, n=n: e.activation(out=rstd[:, 0:n], in_=rstd[:, 0:n], func=AF.Sqrt), reads=["nm_rstd"], writes=["nm_rstd"])
                P.op("dve", lambda e, n=n: e.reciprocal(out=rstd[:, 0:n], in_=rstd[:, 0:n]), reads=["nm_rstd"], writes=["nm_rstd"])
                for c in range(8):
                    i = cnt[0] % 2
                    cnt[0] += 1
                    P.op("dve", lambda e, c=c, t0=t0, n=n, i=i: e.scalar_tensor_tensor(out=ob[i][:, 0:n], in0=sT[:, c, t0:t0 + n], scalar=gfin[:, c:c + 1], in1=rstd[:, 0:n],
                                                                                 op0=ALU.mult, op1=ALU.mult), reads=[SKEY[c], "gfin", "nm_rstd"], writes=[f"fin_ob{i}"])
                    outs.append(P.dma("sp", f"fin_ob{i}", lambda e, c=c, t0=t0, n=n, i=i: e.dma_start(out=d_out[:, c, t0 - CTX:t0 - CTX + n], in_=ob[i][:, 0:n]),
                                      reads=[f"fin_ob{i}"]))
            return outs

        outs = []
        if "mix0" in phases:
            mlstm(0)
        if "moe0" in phases:
            moe(0, 0, NT)
        if "mix1" in phases:
            hgrn(1)
        if "moe1" in phases:
            moe(1, CTX, NT)
        if "final" in phases:
            outs = final()
        if d_dump is not None:
            P.barrier()
            for c in range(8):
                outs.append(P.dma("sp", f"dump{c}", lambda e, c=c: e.dma_start(out=d_dump[:, c, :], in_=sT[:, c, :]), reads=[SKEY[c]]))
        P.emit(final_wait_ops=outs + dbg_outs)
    return nc


def _fm(v, lead=()):
    v = np.asarray(v, np.float32)
    k = v.shape[-1] // 128
    r = v.reshape(v.shape[:-1] + (k, 128))
    return np.ascontiguousarray(np.moveaxis(r, -1, 0))


def _wl(w):
    w = np.asarray(w, np.float32)
    K, N = w.shape
    return np.ascontiguousarray(w.reshape(K // 128, 128, N).transpose(1, 0, 2))


def prep_shared(x, c, ctx, c_ctx, ada_w, ada_b, norm_mix_g, norm_ffn_g, final_g,
                m_w_in, m_conv_w, m_conv_b, m_gate_b, m_head_g, m_w_out,
                h_w_in, h_lower_bounds, h_head_g, h_w_out,
                router_w, router_bias, e_w_gate, e_w_up, e_w_down):
    sh = {}
    sh["adaw"] = np.ascontiguousarray(np.asarray(ada_w, np.float32).reshape(2, 8, 128, 12, 512).transpose(0, 3, 2, 1, 4))
    sh["adab"] = np.ascontiguousarray(_fm(ada_b))
    sh["gmix"] = _fm(norm_mix_g)
    sh["gffn"] = _fm(norm_ffn_g)
    sh["gfin"] = _fm(final_g)
    sh["mwin"] = _wl(m_w_in[0])
    cw = _fm(m_conv_w[0])
    cbias = _fm(m_conv_b[0])
    sh["mconv"] = np.ascontiguousarray(np.concatenate([cw.transpose(0, 2, 1), cbias[:, :, None]], axis=2))
    sh["mgb"] = np.ascontiguousarray(np.broadcast_to(np.asarray(m_gate_b[0], np.float32)[None, :], (128, 16)))
    sh["mhg"] = np.ascontiguousarray(np.broadcast_to(np.asarray(m_head_g[0], np.float32)[None, :], (128, D)))
    sh["mwout"] = _wl(m_w_out[0])
    sh["hwin"] = _wl(h_w_in[0])
    sh["hlb"] = np.ascontiguousarray(_fm(h_lower_bounds))
    sh["hhg"] = np.ascontiguousarray(np.broadcast_to(np.asarray(h_head_g[0], np.float32)[None, :], (128, D)))
    sh["hwout"] = _wl(h_w_out[0])
    sh["rw"] = _wl(router_w)
    sh["rb"] = np.ascontiguousarray(np.broadcast_to(np.asarray(router_bias, np.float32)[None, None, :], (128, NTILE, NE)))
    sh["ewg"] = np.ascontiguousarray(np.asarray(e_w_gate, np.float32).reshape(2, NE, 8, 128, DEXP).transpose(0, 1, 3, 2, 4))
    sh["ewu"] = np.ascontiguousarray(np.asarray(e_w_up, np.float32).reshape(2, NE, 8, 128, DEXP).transpose(0, 1, 3, 2, 4))
    sh["ewd"] = np.ascontiguousarray(np.asarray(e_w_down, np.float32).reshape(2, NE, 4, 128, D).transpose(0, 1, 3, 2, 4))
    sh["cf"] = CARR
    sh["rst"] = RST
    return sh


def prep_core(b, x, c, ctx, c_ctx, s_override=None):
    if s_override is not None:
        s = s_override
    else:
        s = np.concatenate([np.asarray(ctx[b], np.float32), np.asarray(x[b], np.float32)], axis=0)
    xT = np.ascontiguousarray(s.reshape(NT, 8, 128).transpose(2, 1, 0))
    cc = np.stack([np.asarray(c[b], np.float32), np.asarray(c_ctx, np.float32)], axis=-1)
    cT = np.ascontiguousarray(cc.reshape(8, 128, 2).transpose(1, 0, 2))
    return {"xT": xT, "cT": cT}


_NC_CACHE = {}


def kernel(**inputs):
    x = inputs["x"]
    B = x.shape[0]
    sh = prep_shared(**inputs)
    if "full" not in _NC_CACHE:
        _NC_CACHE["full"] = build_program()
    nc = _NC_CACHE["full"]
    in_maps = []
    for b in range(B):
        m = dict(sh)
        m.update(prep_core(b, inputs["x"], inputs["c"], inputs["ctx"], inputs["c_ctx"]))
        in_maps.append(m)
    res = run_bass_kernel_spmd(nc, in_maps, core_ids=list(range(B)))
    out = np.empty((B, SEQ, D), np.float32)
    for b in range(B):
        oT = np.asarray(res.results[b]["outT"])
        out[b] = oT.transpose(2, 1, 0).reshape(64, 32, D).transpose(1, 0, 2).reshape(SEQ, D)
    return out
```

```python
import numpy as np
import concourse.bass as bass
import concourse.mybir as mybir
from contextlib import ExitStack
from concourse.bass_utils import run_bass_kernel_spmd

F32 = mybir.dt.float32
BF16 = mybir.dt.bfloat16
AF = mybir.ActivationFunctionType
ALU = mybir.AluOpType
AX = mybir.AxisListType

D = 1024
SEQ = 2048
CTX = 256
NT = SEQ + CTX
NTILE = NT // 128
EPS = 1e-6
NE = 16
DEXP = 512
ENGS = ("pe", "act", "dve", "pool", "sp")
DEBUG_NT = None


class Op:
    __slots__ = ("eng", "fn", "deps", "signal", "seq", "is_dma", "dsem", "dval", "n_inst", "name", "cost")

    def __init__(self, eng, fn, is_dma, dsem, name):
        self.eng = eng
        self.fn = fn
        self.deps = set()
        self.signal = False
        self.seq = None
        self.is_dma = is_dma
        self.dsem = dsem
        self.dval = None
        self.n_inst = 1
        self.name = name
        self.cost = None


class Prog:
    def __init__(self, nc):
        self.nc = nc
        self.ops = {e: [] for e in ENGS}
        self.last_w = {}
        self.readers = {}
        self.all_ops = []
        self._bar_from = 0
        self._capture = None

    def _add(self, eng, fn, reads, writes, is_dma=False, dsem=None, name=None):
        o = Op(eng, fn, is_dma, dsem, name)
        for k in reads:
            w = self.last_w.get(k)
            if w is not None:
                o.deps.add(w)
        for k in writes:
            w = self.last_w.get(k)
            if w is not None:
                o.deps.add(w)
            for r in self.readers.get(k, ()):
                o.deps.add(r)
        for k in reads:
            self.readers.setdefault(k, []).append(o)
        for k in writes:
            self.last_w[k] = o
            self.readers[k] = []
        o.deps.discard(o)
        self.ops[eng].append(o)
        self.all_ops.append(o)
        return o

    def op(self, eng, fn, reads=(), writes=(), name=None):
        if self._capture is not None:
            self._capture.append(("op", eng, fn, tuple(reads), tuple(writes), None, 1))
            return None
        return self._add(eng, fn, reads, writes, name=name)

    def dma(self, eng, group, fn, reads=(), writes=(), n=1, name=None):
        if self._capture is not None:
            self._capture.append(("dma", eng, fn, tuple(reads), tuple(writes), group, n))
            return None
        o = self._add(eng, fn, reads, writes, is_dma=True, dsem=group, name=name)
        o.n_inst = n
        return o

    def interleave(self, builders):
        streams = []
        for b in builders:
            self._capture = []
            b()
            streams.append(self._capture)
            self._capture = None
        idx = [0] * len(streams)
        while any(idx[i] < len(st) for i, st in enumerate(streams)):
            for i, st in enumerate(streams):
                if idx[i] < len(st):
                    kind, eng, fn, reads, writes, group, n = st[idx[i]]
                    idx[i] += 1
                    if kind == "op":
                        self._add(eng, fn, reads, writes)
                    else:
                        o = self._add(eng, fn, reads, writes, is_dma=True, dsem=group)
                        o.n_inst = n

    def barrier(self):
        self.all_ops.append(None)

    def _schedule(self, seg):
        import heapq
        COST = {"pe": 0.16, "act": 0.42, "dve": 0.45, "pool": 0.6, "sp": 0.1}
        HOP = 0.25
        n = len(seg)
        idx = {id(o): i for i, o in enumerate(seg)}
        cost = [0.0] * n
        lat = [0.0] * n
        for i, o in enumerate(seg):
            c = o.cost if o.cost is not None else COST[o.eng]
            if o.is_dma:
                cost[i] = 0.08 * o.n_inst
                lat[i] = c if o.cost is not None else 2.5
            else:
                cost[i] = c
        deps = [[idx[id(d)] for d in o.deps if id(d) in idx] for o in seg]
        succ = [[] for _ in range(n)]
        for i, dl in enumerate(deps):
            for d in dl:
                succ[d].append(i)
        prio = [0.0] * n
        for i in range(n - 1, -1, -1):
            m = 0.0
            for j in succ[i]:
                if prio[j] > m:
                    m = prio[j]
            prio[i] = m + cost[i] + lat[i] + HOP
        ndep = [len(dl) for dl in deps]
        est = [0.0] * n
        fin = [0.0] * n
        free_at = {e: 0.0 for e in ENGS}
        pend = {e: [] for e in ENGS}
        avail = {e: [] for e in ENGS}
        order = {e: [] for e in ENGS}
        for i in range(n):
            if ndep[i] == 0:
                heapq.heappush(pend[seg[i].eng], (0.0, -prio[i], i))
        left = n
        while left:
            best_e, best_t = None, None
            for e in ENGS:
                if not pend[e] and not avail[e]:
                    continue
                while pend[e] and pend[e][0][0] <= free_at[e]:
                    t_, p_, i_ = heapq.heappop(pend[e])
                    heapq.heappush(avail[e], (p_, i_))
                t = free_at[e] if avail[e] else max(free_at[e], pend[e][0][0])
                if best_t is None or t < best_t:
                    best_e, best_t = e, t
            e = best_e
            if avail[e]:
                p_, i = heapq.heappop(avail[e])
            else:
                t_, p_, i = heapq.heappop(pend[e])
            start = max(free_at[e], est[i])
            free_at[e] = start + cost[i]
            fin[i] = start + cost[i] + lat[i]
            order[e].append(seg[i])
            left -= 1
            for j in succ[i]:
                if fin[i] + HOP > est[j]:
                    est[j] = fin[i] + HOP
                ndep[j] -= 1
                if ndep[j] == 0:
                    heapq.heappush(pend[seg[j].eng], (est[j], -prio[j], j))
        return order

    def emit(self, final_wait_ops=(), schedule=True):
        nc = self.nc
        segs, cur = [], []
        for o in self.all_ops:
            if o is None:
                if cur:
                    segs.append(cur)
                cur = []
            else:
                cur.append(o)
        if cur:
            segs.append(cur)
        seg_orders = []
        for seg in segs:
            if schedule:
                seg_orders.append(self._schedule(seg))
            else:
                od = {e: [] for e in ENGS}
                for o in seg:
                    od[o.eng].append(o)
                seg_orders.append(od)
        bar_deps = [set() for _ in segs]
        last_comp = {}
        for k, od in enumerate(seg_orders):
            if k > 0:
                bar_deps[k] = set(last_comp.values()) | {o for o in segs[k - 1] if o.is_dma}
            for e in ENGS:
                comp = [o for o in od[e] if not o.is_dma]
                if comp:
                    last_comp[e] = comp[-1]
        for o in (x for x in self.all_ops if x is not None):
            for d in o.deps:
                if d.eng == "pe" and o.eng == "pe" and not d.is_dma and not o.is_dma:
                    continue
                d.signal = True
        for bd in bar_deps:
            for d in bd:
                d.signal = True
        for o in final_wait_ops:
            o.signal = True
        with ExitStack() as es:
            esem = {e: es.enter_context(nc.semaphore("c_" + e)) for e in ENGS}
            gsem = {}
            gcount = {}
            for e in ENGS:
                for od in seg_orders:
                    for o in od[e]:
                        if o.is_dma:
                            if o.dsem not in gsem:
                                gsem[o.dsem] = es.enter_context(nc.semaphore("d_" + str(o.dsem)))
                                gcount[o.dsem] = 0
                            gcount[o.dsem] += 16 * o.n_inst
                            o.dval = gcount[o.dsem]
            for e in ENGS:
                c = 0
                for od in seg_orders:
                    for o in od[e]:
                        if not o.is_dma and o.signal:
                            c += 1
                            o.seq = c
            block = es.enter_context(nc.Block())
            engobj = {"pe": "tensor", "act": "scalar", "dve": "vector", "pool": "gpsimd", "sp": "sync"}

            def run(ename, eng):
                waited = {}

                def do_waits(deps, is_pe_compute):
                    need = {}
                    for d in deps:
                        if d.is_dma:
                            key, val, sem = ("g", d.dsem), d.dval, gsem[d.dsem]
                        else:
                            if d.eng == "pe" and ename == "pe" and is_pe_compute:
                                continue
                            if d.eng == ename and ename == "pe":
                                continue
                            key, val, sem = ("e", d.eng), d.seq, esem[d.eng]
                        if waited.get(key, 0) >= val:
                            continue
                        if key not in need or need[key][1] < val:
                            need[key] = (sem, val)
                    for key, (sem, val) in need.items():
                        eng.wait_ge(sem, val)
                        waited[key] = val

                for k, od in enumerate(seg_orders):
                    if bar_deps[k]:
                        do_waits(list(bar_deps[k]), False)
                    for o in od[ename]:
                        do_waits(o.deps, not o.is_dma)
                        r = o.fn(eng)
                        if o.is_dma:
                            insts = r if isinstance(r, (list, tuple)) else [r]
                            assert len(insts) == o.n_inst
                            for ins in insts:
                                ins.then_inc(gsem[o.dsem], 16)
                        elif o.signal:
                            ins = r[-1] if isinstance(r, (list, tuple)) else r
                            ins.then_inc(esem[ename], 1)
                if ename == "sp":
                    for o in final_wait_ops:
                        if o.is_dma:
                            eng.wait_ge(gsem[o.dsem], o.dval)
                        else:
                            eng.wait_ge(esem[o.eng], o.seq)

            for ename in ENGS:
                getattr(block, engobj[ename])(lambda eng, ename=ename: run(ename, eng))


def _consts():
    s = np.arange(128)[:, None]
    t = np.arange(128)[None, :]
    same = (s // 64) == (t // 64)
    c = {}
    c["ident"] = np.eye(128, dtype=np.float32)
    c["ones"] = np.ones((128, 128), np.float32)
    c["triF"] = (same & (s <= t)).astype(np.float32)
    c["triB"] = (same & (s >= t)).astype(np.float32)
    c["sufF"] = (same & (s > t)).astype(np.float32)
    c["sufB"] = (same & (s < t)).astype(np.float32)
    c["sel0"] = np.repeat((s < 64).astype(np.float32), 128, axis=1)
    c["sel1"] = np.repeat((s >= 64).astype(np.float32), 128, axis=1)
    c["tF"] = (s <= t).astype(np.float32)
    c["tB"] = (s >= t).astype(np.float32)
    c["sF"] = (s > t).astype(np.float32)
    c["sB"] = (s < t).astype(np.float32)
    names = ["ident", "ones", "triF", "triB", "sufF", "sufB", "sel0", "sel1", "tF", "tB", "sF", "sB"]
    arr = np.stack([c[n] for n in names], axis=1)
    selE = np.zeros((128, NE, 128), np.float32)
    for e in range(NE):
        selE[e, e, :] = 1.0
    rst = np.ones((128, NT), np.float32)
    rst[:, ::128] = 0.0
    return names, np.ascontiguousarray(arr), selE, rst


CN, CARR, SELE, RST = _consts()
CI = {n: i for i, n in enumerate(CN)}


def _blocks(lo, hi, bs=512):
    out = []
    t = lo
    while t < hi:
        n = min(bs, hi - t)
        if t < CTX:
            n = min(n, CTX - t)
        out.append((t, n, 1 if t < CTX else 0))
        t += n
    return out


def build_program(phases=("mods", "mix0", "moe0", "mix1", "moe1", "final"), dump=None):
    nc = bass.Bass("TRN2", target_bir_lowering=False)
    es = ExitStack()
    P = Prog(nc)

    def din(name, shape, dt=F32):
        return nc.dram_tensor(name, list(shape), dt, kind="ExternalInput").ap()

    d_xT = din("xT", [128, 8, NT])
    d_cT = din("cT", [128, 8, 2])
    d_adaw = din("adaw", [2, 12, 128, 8, 512])
    d_adab = din("adab", [128, 2, 48])
    d_gmix = din("gmix", [128, 2, 8])
    d_gffn = din("gffn", [128, 2, 8])
    d_gfin = din("gfin", [128, 8])
    d_mwin = din("mwin", [128, 8, 3088])
    d_mconv = din("mconv", [128, 8, 4])
    d_mgb = din("mgb", [128, 16])
    d_mhg = din("mhg", [128, D])
    d_mwout = din("mwout", [128, 8, D])
    d_hwin = din("hwin", [128, 8, 5120])
    d_hlb = din("hlb", [128, 2, 16])
    d_hhg = din("hhg", [128, D])
    d_hwout = din("hwout", [128, 8, D])
    d_rw = din("rw", [128, 8, NE])
    d_rb = din("rb", [128, NTILE, NE])
    d_wg = din("ewg", [2, NE, 128, 8, DEXP])
    d_wu = din("ewu", [2, NE, 128, 8, DEXP])
    d_wd = din("ewd", [2, NE, 128, 4, D])
    d_cf = din("cf", [128, 12, 128])
    d_rst = din("rst", [128, NT])
    d_out = nc.dram_tensor("outT", [128, 8, SEQ], F32, kind="ExternalOutput").ap()
    d_dump = None
    if dump is not None:
        d_dump = nc.dram_tensor("dump", [128, 8, NT], F32, kind="ExternalOutput").ap()

    with es:
        def sb(name, shape, dt=F32):
            return es.enter_context(nc.sbuf_tensor("s_" + name, list(shape), dt))

        sT = sb("sT", [128, 8, NT])
        hT = sb("hT", [128, 8, NT], BF16)
        cf = sb("cf", [128, 12, 128])
        cb = sb("cb", [128, 12, 128], BF16)
        maskLE = sb("maskLE", [128, 128], BF16)
        maskGE = sb("maskGE", [128, 128], BF16)
        modT = [sb(f"modT{l}", [128, 48, 2]) for l in range(2)]
        adab = sb("adab", [128, 2, 48])
        gmix = sb("gmix", [128, 2, 8])
        gffn = sb("gffn", [128, 2, 8])
        gfin = sb("gfin", [128, 8])
        cT = sb("cT", [128, 8, 2])
        scT = sb("scT", [128, 8, 2])
        A1 = [sb(f"A1_{l}", [128, 8, 2]) for l in range(2)]
        A2 = [sb(f"A2_{l}", [128, 8, 2]) for l in range(2)]
        pb = [es.enter_context(nc.psum_tensor(f"pb{i}", [128, 512], F32)) for i in range(8)]
        PB = [f"pb{i}" for i in range(8)]

        ARENA = ((nc.sbuf_bytes_remaining - 2048) // 64) * 64
        arena = sb("arena", [128, ARENA // 4], F32)

        class Carver:
            def __init__(self):
                self.off = 0

            def take(self, shape, dt=F32):
                esz = 4 if dt == F32 else 2
                n = int(np.prod(shape[1:]))
                nbytes = ((n * esz + 63) // 64) * 64
                assert self.off + nbytes <= ARENA, (self.off, nbytes, ARENA)
                a = arena[:, self.off // 4:(self.off + nbytes) // 4]
                self.off += nbytes
                if dt != F32:
                    a = a.bitcast(dt)
                a = a[:, 0:n]
                if len(shape) == 3:
                    a = a.rearrange("p (a b) -> p a b", a=shape[1])
                elif len(shape) == 4:
                    a = a.rearrange("p (a b c) -> p a b c", a=shape[1], b=shape[2])
                return a

        dbg_outs = []

        def dbg(name, ap, shape, key, dt=F32):
            if dump is None or dump is True or name not in dump:
                return
            dd = nc.dram_tensor("dbg_" + name, list(shape), dt, kind="ExternalOutput").ap()
            dbg_outs.append(P.dma("sp", "dbg_" + name, lambda e: e.dma_start(out=dd, in_=ap), reads=[key]))

        def mm(out, lhsT, rhs, start, stop):
            return lambda e: e.matmul(out, lhsT=lhsT, rhs=rhs, start=start, stop=stop)

        for c in range(8):
            P.dma("sp", f"sT{c}", lambda e, c=c: e.dma_start(out=sT[:, c, :], in_=d_xT[:, c, :]), writes=[f"sT{c}"])
        P.dma("sp", "cf", lambda e: e.dma_start(out=cf[:], in_=d_cf), writes=["cf"])
        for nm, t, dsrc in (("adab", adab, d_adab), ("gmix", gmix, d_gmix), ("gffn", gffn, d_gffn),
                            ("gfin", gfin, d_gfin), ("cT", cT, d_cT)):
            P.dma("sp", nm, lambda e, t=t, dsrc=dsrc: e.dma_start(out=t[:], in_=dsrc), writes=[nm])
        P.op("dve", lambda e: e.tensor_copy(out=cb[:], in_=cf[:]), reads=["cf"], writes=["cb"])
        P.op("dve", lambda e: e.tensor_copy(out=maskLE[:], in_=cf[:, CI["triF"], :]), reads=["cf"], writes=["masks"])
        P.op("dve", lambda e: e.tensor_copy(out=maskGE[:], in_=cf[:, CI["triB"], :]), reads=["cf"], writes=["masks"])
        ident_b = cb[:, CI["ident"], :]
        ident_f = cf[:, CI["ident"], :]
        ones_f = cf[:, CI["ones"], :]
        SKEY = [f"sT{c}" for c in range(8)]

        MODS_BASE = ((ARENA - (4 * 16384 + 2 * 2048)) // 64) * 64
        if "mods" in phases:
            cv = Carver()
            NB_ = 4
            adaw = [cv.take([128, 8, 512]) for _ in range(NB_)]
            modrow = [cv.take([128, 512]) for _ in range(2)]
            P.op("act", lambda e: e.activation(out=scT[:], in_=cT[:], func=AF.Silu), reads=["cT"], writes=["scT"])
            for l in range(2):
                for nb in range(12):
                    bi = nb % NB_
                    mi = nb % 2
                    P.dma("sp" if nb % 2 == 0 else "act", f"adaw{bi}", lambda e, l=l, nb=nb, bi=bi: e.dma_start(out=adaw[bi], in_=d_adaw[l, nb]), writes=[f"arena_adaw{bi}"])
                    for kc in range(8):
                        P.op("pe", mm(pb[mi][0:2, :], scT[:, kc, :], adaw[bi][:, kc, :], kc == 0, kc == 7), reads=[f"arena_adaw{bi}", "scT"], writes=[PB[mi]])
                    P.op("dve", lambda e, mi=mi: e.tensor_copy(out=modrow[mi][0:2, :], in_=pb[mi][0:2, :]), writes=[PB[mi], f"arena_modrow{mi}"])
                    for j in range(4):
                        idx = nb * 4 + j
                        P.op("pe", lambda e, idx=idx, j=j, mi=mi: e.transpose(out=pb[2][:, 2 * idx:2 * idx + 2], in_=modrow[mi][0:2, j * 128:(j + 1) * 128], identity=cf[0:2, CI["ident"], 0:2]),
                             reads=[f"arena_modrow{mi}", "cf"], writes=[PB[2]])
                P.op("dve", lambda e, l=l: e.tensor_tensor(out=modT[l][:], in0=pb[2][:, 0:96].rearrange("p (a b) -> p a b", b=2),
                                                          in1=adab[:, l, :].unsqueeze(2).to_broadcast([128, 48, 2]), op=ALU.add),
                     reads=["adab"], writes=[PB[2], f"modT{l}"])
                for col in range(2):
                    P.op("dve", lambda e, l=l, col=col: e.scalar_tensor_tensor(
                        out=A1[l][:, :, col], in0=modT[l][:, 8:16, col], scalar=1.0, in1=gmix[:, l, :], op0=ALU.add, op1=ALU.mult),
                        reads=[f"modT{l}", "gmix"], writes=[f"A1_{l}"])
                    P.op("dve", lambda e, l=l, col=col: e.scalar_tensor_tensor(
                        out=A2[l][:, :, col], in0=modT[l][:, 32:40, col], scalar=1.0, in1=gffn[:, l, :], op0=ALU.add, op1=ALU.mult),
                        reads=[f"modT{l}", "gffn"], writes=[f"A2_{l}"])

        def SH1(l, c, col): return modT[l][:, 0 + c, col:col + 1]
        def G1(l, c, col): return modT[l][:, 16 + c, col:col + 1]
        def SH2(l, c, col): return modT[l][:, 24 + c, col:col + 1]
        def G2(l, c, col): return modT[l][:, 40 + c, col:col + 1]

        def norm_mod(cv, A, SH, l, lo, hi, out_bf, out_key, out_f32=None, perm_cols=False, after_block=None, bs=512, skey=None):
            if skey is None:
                skey = lambda c, t0: SKEY[c]
            sq = [cv.take([128, bs]) for _ in range(2)]
            rstd = cv.take([128, bs])
            tmp = [cv.take([128, bs]) for _ in range(2)]
            for bix, (t0, n, col) in enumerate(_blocks(lo, hi, bs)):
                o32 = out_f32[bix % 2] if out_f32 is not None else None
                okey = out_key(t0) if callable(out_key) else out_key
                k32 = "nm_h32_0"
                for c in range(8):
                    P.op("act", lambda e, c=c, t0=t0, n=n: e.activation(out=sq[c % 2][:, 0:n], in_=sT[:, c, t0:t0 + n], func=AF.Square),
                         reads=[skey(c, t0)], writes=[f"nm_sq{c % 2}"])
                    P.op("pe", mm(pb[7][:, 0:n], ones_f, sq[c % 2][:, 0:n], c == 0, c == 7), reads=[f"nm_sq{c % 2}", "cf"], writes=[PB[7]])
                P.op("dve", lambda e, n=n: e.tensor_scalar(out=rstd[:, 0:n], in0=pb[7][:, 0:n], scalar1=1.0 / D, scalar2=EPS,
                                                          op0=ALU.mult, op1=ALU.add), reads=[PB[7]], writes=["nm_rstd"])
                P.op("act", lambda e, n=n: e.activation(out=rstd[:, 0:n], in_=rstd[:, 0:n], func=AF.Sqrt), reads=["nm_rstd"], writes=["nm_rstd"])
                P.op("dve", lambda e, n=n: e.reciprocal(out=rstd[:, 0:n], in_=rstd[:, 0:n]), reads=["nm_rstd"], writes=["nm_rstd"])
                for c in range(8):
                    tp = tmp[c % 2]
                    P.op("dve", lambda e, c=c, t0=t0, n=n, tp=tp: e.tensor_tensor(out=tp[:, 0:n], in0=sT[:, c, t0:t0 + n], in1=rstd[:, 0:n], op=ALU.mult),
                         reads=[skey(c, t0), "nm_rstd"], writes=[f"nm_tmp{c % 2}"])
                    src = tp[:, 0:n]
                    if out_f32 is not None:
                        dst, dkey = o32[:, c, 0:n], k32
                    else:
                        dst, dkey = out_bf[:, c, t0:t0 + n], okey
                        if perm_cols and t0 >= CTX:
                            r0, nr = (t0 - CTX) // 64, n // 64
                            dst = out_bf[:, c, CTX:NT].rearrange("p (col row) -> p row col", row=32)[:, r0:r0 + nr, :]
                            src = src.rearrange("p (r c) -> p r c", c=64)
                    P.op("dve", lambda e, c=c, col=col, dst=dst, src=src: e.tensor_scalar(
                        out=dst, in0=src, scalar1=A[l][:, c, col:col + 1], scalar2=SH(l, c, col), op0=ALU.mult, op1=ALU.add),
                        reads=[f"nm_tmp{c % 2}", f"A1_{l}", f"A2_{l}", f"modT{l}"], writes=[dkey])
                    if out_f32 is not None:
                        P.op("act", lambda e, c=c, t0=t0, n=n, o32=o32: e.copy(out=out_bf[:, c, t0:t0 + n], in_=o32[:, c, 0:n]), reads=[k32], writes=[okey])
                if after_block is not None:
                    after_block(t0, n)

        def moe(l, lo, hi):
            P.barrier()
            cv = Carver()
            h32s = [cv.take([128, 8, 256])] * 2
            rw = cv.take([128, 8, NE])
            rb = cv.take([128, NTILE, NE])
            cw_all = cv.take([128, NTILE, NE])
            NTN = NTILE * NE
            rt = [cv.take([128, NTILE, NE]) for _ in range(5)]
            r4 = [cv.take([128, NTILE * 4]) for _ in range(4)]
            r1 = [cv.take([128, NTILE]) for _ in range(2)]
            wgb = [cv.take([128, 8, DEXP], BF16) for _ in range(2)]
            wub = [cv.take([128, 8, DEXP], BF16) for _ in range(2)]
            wdb = [cv.take([128, 4, D], BF16) for _ in range(2)]
            aT = [cv.take([128, 4, 512], BF16) for _ in range(2)]
            sg = [cv.take([128, 512], BF16) for _ in range(2)]
            t1 = [cv.take([128, 512], BF16) for _ in range(2)]
            cwb = [cv.take([128, 512], BF16) for _ in range(2)]
            P.dma("sp", "rw", lambda e: e.dma_start(out=rw, in_=d_rw), writes=["moe_rw"])
            P.dma("sp", "rb", lambda e: e.dma_start(out=rb, in_=d_rb), writes=["moe_rb"])
            T0, T1 = lo // 128, hi // 128
            s_all = rt[0]
            blk_ctr = [0]

            def route(t0, n):
                hb = 0
                for sub in range(n // 128):
                    tix = (t0 + sub * 128) // 128
                    pl, kl = pb[7], PB[7]
                    for c in range(8):
                        P.op("pe", mm(pl[:, 256:256 + NE], h32s[hb][:, c, sub * 128:(sub + 1) * 128], rw[:, c, :], c == 0, c == 7),
                             reads=[f"nm_h32_{hb}", "moe_rw"], writes=[kl])
                    P.op("act", lambda e, tix=tix, pl=pl: e.activation(out=s_all[:, tix, :], in_=pl[:, 256:256 + NE], func=AF.Sigmoid), writes=[kl, f"rt_s{tix}"])

            sel_, sel2_, eq1, eq2 = rt[1:5]
            w_ = sel2_
            m1, m2, gs, geq = r4
            bm, ws = r1
            cw16 = cw_all

            def route_batch(Ta, Tb):
                TS = slice(Ta, Tb)
                nT = Tb - Ta
                K = lambda nm: f"{nm}{Ta}"
                v3 = lambda a: a[:, TS, :].rearrange("p t (g k) -> p (t g) k", k=4)
                g2 = lambda a: a[:, Ta * 4:Tb * 4]
                bc4 = lambda a: g2(a).unsqueeze(2).to_broadcast([128, nT * 4, 4])
                g3 = lambda a: g2(a).rearrange("p (t g) -> p t g", g=4)
                skeys = [f"rt_s{t_}" for t_ in range(Ta, Tb)]
                P.op("dve", lambda e: e.tensor_tensor(out=sel_[:, TS, :], in0=s_all[:, TS, :], in1=rb[:, TS, :], op=ALU.add), reads=skeys + ["moe_rb"], writes=[K("rt_sel")])
                P.op("dve", lambda e: e.tensor_reduce(out=g2(m1), in_=v3(sel_), axis=AX.X, op=ALU.max), reads=[K("rt_sel")], writes=[K("rt_m1")])
                P.op("dve", lambda e: e.tensor_tensor(out=v3(eq1), in0=v3(sel_), in1=bc4(m1), op=ALU.is_equal), reads=[K("rt_sel"), K("rt_m1")], writes=[K("rt_eq1")])
                P.op("dve", lambda e: e.scalar_tensor_tensor(out=sel2_[:, TS, :], in0=eq1[:, TS, :], scalar=-1e9, in1=sel_[:, TS, :], op0=ALU.mult, op1=ALU.add),
                     reads=[K("rt_eq1"), K("rt_sel")], writes=[K("rt_sel2")])
                P.op("dve", lambda e: e.tensor_reduce(out=g2(m2), in_=v3(sel2_), axis=AX.X, op=ALU.max), reads=[K("rt_sel2")], writes=[K("rt_m2")])
                P.op("dve", lambda e: e.tensor_tensor(out=v3(eq2), in0=v3(sel2_), in1=bc4(m2), op=ALU.is_equal), reads=[K("rt_sel2"), K("rt_m2")], writes=[K("rt_eq2")])
                P.op("dve", lambda e: e.tensor_tensor(out=g2(gs), in0=g2(m1), in1=g2(m2), op=ALU.add), reads=[K("rt_m1"), K("rt_m2")], writes=[K("rt_gs")])
                P.op("dve", lambda e: e.tensor_reduce(out=bm[:, TS], in_=g3(gs), axis=AX.X, op=ALU.max), reads=[K("rt_gs")], writes=[K("rt_bm")])
                P.op("dve", lambda e: e.tensor_tensor(out=g3(geq), in0=g3(gs), in1=bm[:, TS].unsqueeze(2).to_broadcast([128, nT, 4]), op=ALU.is_equal),
                     reads=[K("rt_gs"), K("rt_bm")], writes=[K("rt_geq")])
                P.op("dve", lambda e: e.tensor_tensor(out=eq1[:, TS, :], in0=eq1[:, TS, :], in1=eq2[:, TS, :], op=ALU.add), reads=[K("rt_eq2")], writes=[K("rt_eq1")])
                P.op("dve", lambda e: e.tensor_tensor(out=v3(eq1), in0=v3(eq1), in1=bc4(geq), op=ALU.mult), reads=[K("rt_geq")], writes=[K("rt_eq1")])
                P.op("dve", lambda e: e.tensor_tensor(out=w_[:, TS, :], in0=eq1[:, TS, :], in1=s_all[:, TS, :], op=ALU.mult),
                     reads=[K("rt_eq1"), K("rt_eq2"), K("rt_m2")] + skeys, writes=[K("rt_w"), K("rt_sel2")])
                P.op("dve", lambda e: e.tensor_reduce(out=ws[:, TS], in_=w_[:, TS, :], axis=AX.X, op=ALU.add), reads=[K("rt_w")], writes=[K("rt_ws")])
                P.op("dve", lambda e: e.reciprocal(out=ws[:, TS], in_=ws[:, TS]), writes=[K("rt_ws")])
                P.op("dve", lambda e: e.tensor_tensor(out=cw16[:, TS, :], in0=w_[:, TS, :], in1=ws[:, TS].unsqueeze(2).to_broadcast([128, nT, NE]), op=ALU.mult),
                     reads=[K("rt_w"), K("rt_ws")], writes=[f"moe_cw{t_}" for t_ in range(Ta, Tb)])

            def after_blk(t0, n):
                route(t0, n)
                t1_ = t0 + n
                for (bt0, bn, _c) in _blocks(lo, hi):
                    if bt0 + bn == t1_:
                        route_batch(bt0 // 128, (bt0 + bn) // 128)

            norm_mod(cv, A2, SH2, l, lo, hi, out_bf=hT, out_key=lambda t0: f"hT_{t0 // 256}", out_f32=h32s, after_block=after_blk, bs=256,
                     skey=lambda c, t0: f"sT{c}_{t0 // 256}")

            blocks = _blocks(lo, hi)
            items = [(e_, b_) for e_ in range(NE) for b_ in range(len(blocks))]

            def load_w(e_):
                bi = e_ % 2
                P.dma("pool", f"wg{bi}", lambda e: e.dma_start(out=wgb[bi], in_=d_wg[l, e_], max_dma_last_dim=4096), writes=[f"moe_wg{bi}"])
                P.dma("pool", f"wu{bi}", lambda e: e.dma_start(out=wub[bi], in_=d_wu[l, e_], max_dma_last_dim=4096), writes=[f"moe_wu{bi}"])
                P.dma("pool", f"wd{bi}", lambda e: e.dma_start(out=wdb[bi], in_=d_wd[l, e_], max_dma_last_dim=4096), writes=[f"moe_wd{bi}"])

            cwcol = [cv.take([128, 128], BF16) for _ in range(4)]
            cwc_ctr = [0]

            def stage1(i):
                e_, b_ = items[i]
                t0, n, col = blocks[b_]
                bi = e_ % 2
                ai = i % 2
                hkeys = [f"hT_{k_}" for k_ in range(t0 // 256, (t0 + n + 255) // 256)]
                for sub in range(n // 128):
                    tix = (t0 + sub * 128) // 128
                    ci_ = cwc_ctr[0] % 4
                    cwc_ctr[0] += 1
                    P.op("dve", lambda e, tix=tix, ci_=ci_: e.tensor_copy(out=cwcol[ci_], in_=cw16[:, tix, e_:e_ + 1].to_broadcast([128, 128])),
                         reads=[f"moe_cw{tix}"], writes=[f"moe_cwcol{ci_}"])
                    P.op("pe", mm(pb[6][:, sub * 128:(sub + 1) * 128], cwcol[ci_], ident_b, True, True),
                         reads=[f"moe_cwcol{ci_}", "cb"], writes=[PB[6]])
                P.op("act", lambda e: e.copy(out=cwb[ai][:, 0:n], in_=pb[6][:, 0:n]), reads=[PB[6]], writes=[f"moe_cwb{ai}"])
                for fc in range(4):
                    pg, pu = pb[(fc % 2) * 2], pb[(fc % 2) * 2 + 1]
                    kg, ku = PB[(fc % 2) * 2], PB[(fc % 2) * 2 + 1]
                    for c in range(8):
                        P.op("pe", mm(pg[:, 0:n], wgb[bi][:, c, fc * 128:(fc + 1) * 128], hT[:, c, t0:t0 + n], c == 0, c == 7),
                             reads=[f"moe_wg{bi}"] + hkeys, writes=[kg])
                    for c in range(8):
                        P.op("pe", mm(pu[:, 0:n], wub[bi][:, c, fc * 128:(fc + 1) * 128], hT[:, c, t0:t0 + n], c == 0, c == 7),
                             reads=[f"moe_wu{bi}"] + hkeys, writes=[ku])
                    si = fc % 2
                    P.op("act", lambda e, pg=pg, si=si: e.activation(out=sg[si][:, 0:n], in_=pg[:, 0:n], func=AF.Silu), reads=[kg], writes=[f"moe_sg{si}"])
                    P.op("dve", lambda e, pu=pu, si=si: e.tensor_tensor(out=t1[si][:, 0:n], in0=sg[si][:, 0:n], in1=pu[:, 0:n], op=ALU.mult),
                         reads=[f"moe_sg{si}", ku], writes=[f"moe_t1{si}"])
                    P.op("pool", lambda e, si=si, fc=fc: e.tensor_tensor(out=aT[ai][:, fc, 0:n], in0=t1[si][:, 0:n], in1=cwb[ai][:, 0:n], op=ALU.mult),
                         reads=[f"moe_t1{si}", f"moe_cwb{ai}"], writes=[f"moe_aT{ai}"])

            def stage2(i):
                e_, b_ = items[i]
                t0, n, col = blocks[b_]
                bi = e_ % 2
                ai = i % 2
                for dc in range(8):
                    pd, kd = pb[4 + dc % 2], PB[4 + dc % 2]
                    for fc in range(4):
                        P.op("pe", mm(pd[:, 0:n], wdb[bi][:, fc, dc * 128:(dc + 1) * 128], aT[ai][:, fc, 0:n], fc == 0, fc == 3),
                             reads=[f"moe_wd{bi}", f"moe_aT{ai}"], writes=[kd])
                    sk = [f"sT{dc}_{k_}" for k_ in range(t0 // 256, (t0 + n + 255) // 256)]
                    P.op("dve", lambda e, dc=dc, pd=pd: e.scalar_tensor_tensor(
                        out=sT[:, dc, t0:t0 + n], in0=pd[:, 0:n], scalar=G2(l, dc, col), in1=sT[:, dc, t0:t0 + n], op0=ALU.mult, op1=ALU.add),
                        reads=[kd, f"modT{l}"] + sk, writes=sk)

            load_w(0)
            for i in range(len(items)):
                e_, b_ = items[i]
                stage1(i)
                if i > 0:
                    stage2(i - 1)
                if b_ == 0 and e_ + 1 < NE:
                    load_w(e_ + 1)
            stage2(len(items) - 1)

        def mlstm(l):
            P.barrier()
            cv = Carver()
            wgt = cv.take([128, 8, 16], BF16)
            mgb = cv.take([128, 16])
            mhg = cv.take([128, 256])
            cvw = cv.take([128, 8, 4])
            G = cv.take([128, NTILE, 16])
            Lg = cv.take([128, NTILE, 8])
            EE = cv.take([128, NTILE, 24])
            EB = cv.take([128, NTILE, 8])
            arg = cv.take([128, 24])
            P.dma("pool", "m_wgt", lambda e: e.dma_start(out=wgt, in_=d_mwin[:, :, 3072:3088]), writes=["m_wgt"])
            P.dma("sp", "m_mgb", lambda e: e.dma_start(out=mgb, in_=d_mgb), writes=["m_mgb"])
            P.dma("sp", "m_cvw", lambda e: e.dma_start(out=cvw, in_=d_mconv), writes=["m_cvw"])

            norm_mod(cv, A1, SH1, l, 0, NT, out_bf=hT, out_key="hT", bs=256)

            for tl in range(NTILE):
                ts_ = slice(tl * 128, (tl + 1) * 128)
                for c in range(8):
                    P.op("pe", mm(pb[0][:, 0:16], hT[:, c, ts_], wgt[:, c, :], c == 0, c == 7), reads=["hT", "m_wgt"], writes=[PB[0]])
                P.op("dve", lambda e, tl=tl: e.tensor_tensor(out=G[:, tl, :], in0=pb[0][:, 0:16], in1=mgb, op=ALU.add), reads=[PB[0], "m_mgb"], writes=["m_G"])
                for k, src in enumerate((slice(4, 8), slice(12, 16))):
                    P.op("act", lambda e, tl=tl, k=k, src=src: e.activation(out=Lg[:, tl, 4 * k:4 * k + 4], in_=G[:, tl, src], func=AF.Exp, scale=-1.0),
                         reads=["m_G"], writes=["m_L"])
                P.op("dve", lambda e, tl=tl: e.tensor_scalar(out=Lg[:, tl, :], in0=Lg[:, tl, :], scalar1=1.0, scalar2=None, op0=ALU.add), reads=["m_L"], writes=["m_L"])
                P.op("act", lambda e, tl=tl: e.activation(out=Lg[:, tl, :], in_=Lg[:, tl, :], func=AF.Ln), reads=["m_L"], writes=["m_L"])
                q_ = pb[1]
                for j, (mat, cs) in enumerate((("tF", slice(0, 4)), ("tB", slice(4, 8)), ("sF", slice(0, 4)), ("sB", slice(4, 8)))):
                    P.op("pe", mm(q_[:, 4 * j:4 * j + 4], cf[:, CI[mat], :], Lg[:, tl, cs], True, True), reads=["cf", "m_L"], writes=[PB[1]])
                P.op("pe", mm(q_[:, 16:24], cf[:, CI["ones"], :], Lg[:, tl, :], True, True), reads=["cf", "m_L"], writes=[PB[1]])
                P.op("dve", lambda e, tl=tl: e.tensor_tensor(out=arg[:, 0:4], in0=G[:, tl, 0:4], in1=q_[:, 0:4], op=ALU.add), reads=["m_G"], writes=["m_arg", PB[1]])
                P.op("dve", lambda e, tl=tl: e.tensor_tensor(out=arg[:, 4:8], in0=G[:, tl, 0:4], in1=q_[:, 8:12], op=ALU.subtract), reads=["m_G"], writes=["m_arg", PB[1]])
                P.op("dve", lambda e, tl=tl: e.tensor_tensor(out=arg[:, 8:12], in0=G[:, tl, 8:12], in1=q_[:, 4:8], op=ALU.add), reads=["m_G"], writes=["m_arg", PB[1]])
                P.op("dve", lambda e, tl=tl: e.tensor_tensor(out=arg[:, 12:16], in0=G[:, tl, 8:12], in1=q_[:, 12:16], op=ALU.subtract), reads=["m_G"], writes=["m_arg", PB[1]])
                P.op("dve", lambda e, tl=tl: e.tensor_copy(out=arg[:, 16:24], in_=q_[:, 0:8]), writes=["m_arg", PB[1]])
                P.op("act", lambda e, tl=tl: e.activation(out=EE[:, tl, :], in_=arg, func=AF.Exp), reads=["m_arg"], writes=["m_EE"])
                P.op("act", lambda e, tl=tl: e.activation(out=EB[:, tl, :], in_=q_[:, 16:24], func=AF.Exp, scale=-1.0), writes=["m_EB", PB[1]])

            wqk = cv.take([128, 8, 2, 128], BF16)
            wv = cv.take([128, 8, 256], BF16)
            wo = cv.take([128, 8, 256], BF16)
            wout = cv.take([128, 2, D], BF16)
            qT = cv.take([128, NT], BF16)
            kT = cv.take([128, NT], BF16)
            vaug = cv.take([128, NTILE, 257], BF16)
            ktok = cv.take([128, NTILE, 128], BF16)
            hfirst = cv.take([128, NTILE, 256], BF16)
            ybuf = [cv.take([128, 512]) for _ in range(2)]
            cvs = cv
            STf = [[cvs.take([128, 128], BF16) for _ in range(2)] for _ in range(2)]
            vt = [[cvs.take([128, 257], BF16) for _ in range(2)] for _ in range(2)]
            vt2 = [[cvs.take([128, 257], BF16) for _ in range(2)] for _ in range(2)]
            X = [cvs.take([128, 257]) for _ in range(2)]
            Xb = [[cvs.take([128, 257], BF16) for _ in range(2)] for _ in range(2)]
            numS = [[cvs.take([128, 257]) for _ in range(2)] for _ in range(2)]
            hs = [cvs.take([128, 256]) for _ in range(2)]
            hn = hs
            hg = [cvs.take([128, 256], BF16) for _ in range(2)]
            sgo = [cvs.take([128, 256]) for _ in range(2)]
            tmpo = [cvs.take([128, 4, 128]) for _ in range(2)]
            sm = [[cvs.take([128, 1]) for _ in range(2)] for _ in range(2)]
            xTt = [cvs.take([128, 2, 128], BF16) for _ in range(2)]
            P.op("pool", lambda e: e.memset(vaug[:, :, 256:257], 1.0), writes=["m_vaug"])
            pb6b = pb[6].bitcast(BF16)

            for h in range(4):
                P.dma("sp", "m_mhg", lambda e, h=h: e.dma_start(out=mhg, in_=d_mhg[:, h * 256:(h + 1) * 256]), writes=["m_mhg"])
                P.dma("pool", "m_wqk", lambda e, h=h: [e.dma_start(out=wqk[:, :, 0, :], in_=d_mwin[:, :, h * 128:(h + 1) * 128]),
                                                       e.dma_start(out=wqk[:, :, 1, :], in_=d_mwin[:, :, 512 + h * 128:512 + (h + 1) * 128])],
                      writes=["m_wqk"], n=2)
                P.dma("pool", "m_wv", lambda e, h=h: e.dma_start(out=wv, in_=d_mwin[:, :, 1024 + h * 256:1024 + (h + 1) * 256]), writes=["m_wv"])
                P.dma("pool", "m_wo", lambda e, h=h: e.dma_start(out=wo, in_=d_mwin[:, :, 2048 + h * 256:2048 + (h + 1) * 256]), writes=["m_wo"])
                P.dma("pool", "m_wout", lambda e, h=h: e.dma_start(out=wout, in_=d_mwout[:, 2 * h:2 * h + 2, :], max_dma_last_dim=4096), writes=["m_wout"])
                qk_blocks = [(0, CTX, 0, CTX)] + [(CTX + 410 * j, min(CTX + 410 * (j + 1), NT), CTX, NT) for j in range(5)]
                cnt_ = [0]
                for which, dstT, dkey in ((0, qT, "m_qT"), (1, kT, "m_kT")):
                    ch = (0 if which == 0 else 4) + h
                    for (a_, b_, A_, B_) in qk_blocks:
                        n = b_ - a_
                        a2, b2 = max(a_ - 1, A_), min(b_ + 1, B_)
                        off = a_ - a2
                        N_ = b2 - a2
                        bi_ = cnt_[0] % 2
                        cnt_[0] += 1
                        pq, kq = pb[bi_], PB[bi_]
                        yb, ky = ybuf[bi_], f"m_y{bi_}"
                        for c in range(8):
                            P.op("pe", mm(pq[:, 0:N_], wqk[:, c, which, :], hT[:, c, a2:b2], c == 0, c == 7), reads=["m_wqk", "hT"], writes=[kq])
                        P.op("dve", lambda e, pq=pq, yb=yb, off=off, n=n, ch=ch: e.tensor_scalar(out=yb[:, 0:n], in0=pq[:, off:off + n], scalar1=cvw[:, ch, 1:2], scalar2=cvw[:, ch, 3:4],
                                                                                       op0=ALU.mult, op1=ALU.add), reads=["m_cvw"], writes=[kq, ky])
                        if off == 1:
                            o0, i0, m0 = 0, 0, n
                        else:
                            o0, i0, m0 = 1, 0, n - 1
                        P.op("dve", lambda e, pq=pq, yb=yb, o0=o0, i0=i0, m0=m0, ch=ch: e.scalar_tensor_tensor(
                            out=yb[:, o0:o0 + m0], in0=pq[:, i0:i0 + m0], scalar=cvw[:, ch, 0:1], in1=yb[:, o0:o0 + m0], op0=ALU.mult, op1=ALU.add),
                            reads=["m_cvw"], writes=[kq, ky])
                        m2 = n if b2 > b_ else n - 1
                        P.op("dve", lambda e, pq=pq, yb=yb, off=off, m2=m2, ch=ch: e.scalar_tensor_tensor(
                            out=yb[:, 0:m2], in0=pq[:, off + 1:off + 1 + m2], scalar=cvw[:, ch, 2:3], in1=yb[:, 0:m2], op0=ALU.mult, op1=ALU.add),
                            reads=["m_cvw"], writes=[kq, ky])
                        if which == 0:
                            P.op("act", lambda e, yb=yb, n=n: e.activation(out=yb[:, 0:n], in_=yb[:, 0:n], func=AF.Silu), writes=[ky])
                            P.op("act", lambda e, yb=yb, n=n, a_=a_, b_=b_: e.activation(out=qT[:, a_:b_], in_=yb[:, 0:n], func=AF.Copy, scale=float(128 ** -0.5)),
                                 reads=[ky], writes=[dkey])
                        else:
                            P.op("act", lambda e, yb=yb, n=n, a_=a_, b_=b_: e.activation(out=kT[:, a_:b_], in_=yb[:, 0:n], func=AF.Silu), reads=[ky], writes=[dkey])
                for tl in range(NTILE):
                    ts_ = slice(tl * 128, (tl + 1) * 128)
                    pv, kv_ = pb[2 + tl % 2], PB[2 + tl % 2]
                    for c in range(8):
                        P.op("pe", mm(pv[:, 0:256], hT[:, c, ts_], wv[:, c, :], c == 0, c == 7), reads=["hT", "m_wv"], writes=[kv_])
                    P.op("act", lambda e, tl=tl, pv=pv: e.copy(out=vaug[:, tl, 0:256], in_=pv[:, 0:256]), reads=[kv_], writes=["m_vaug"])
                    tb = pb[4 + tl % 2].bitcast(BF16)
                    P.op("pe", lambda e, ts_=ts_, tb=tb: e.transpose(out=tb[:, 0:128], in_=kT[:, ts_], identity=ident_b), reads=["m_kT", "cb"], writes=[PB[4 + tl % 2]])
                    P.op("act", lambda e, tl=tl, tb=tb: e.copy(out=ktok[:, tl, :], in_=tb[:, 0:128]), reads=[PB[4 + tl % 2]], writes=["m_ktok"])

                orders = [list(range(NTILE)), [1, 0] + list(range(NTILE - 1, 1, -1))]
                if DEBUG_NT is not None:
                    orders = [o_[:DEBUG_NT] for o_ in orders]
                visited = set()
                for d_ in range(2):
                    P.op("dve", lambda e, d_=d_: e.memset(X[d_], 0.0), writes=[f"m_X{d_}"])
                    P.op("dve", lambda e, d_=d_: e.memset(Xb[d_][0], 0.0), writes=[f"m_Xb{d_}_0"])
                ncomp = [0]

                def step(d_, it, tl):
                    ts_ = slice(tl * 128, (tl + 1) * 128)
                    pi = it % 2
                    eoff = 0 if d_ == 0 else 8
                    doff = 4 if d_ else 0
                    second = tl in visited
                    visited.add(tl)
                    sc_cols = slice(128 * d_, 128 * d_ + 128)
                    P.op("pe", mm(pb[6][:, sc_cols], kT[:, ts_], qT[:, ts_], True, True), reads=["m_kT", "m_qT"], writes=[PB[6]])
                    msk = cb[:, CI["tF"], :] if d_ == 0 else cb[:, CI["tB"], :]
                    P.op("dve", lambda e: e.tensor_tensor(out=STf[d_][pi], in0=pb[6][:, sc_cols], in1=msk, op=ALU.mult),
                         reads=["cb"], writes=[PB[6], f"m_STf{d_}{pi}"])
                    e1 = EE[:, tl, eoff + h:eoff + h + 1]
                    e2 = EE[:, tl, eoff + 4 + h:eoff + 5 + h]
                    P.op("act", lambda e: e.activation(out=vt[d_][pi], in_=vaug[:, tl, :], func=AF.Copy, scale=e1),
                         reads=["m_vaug", "m_EE"], writes=[f"m_vt{d_}{pi}"])
                    P.op("dve", lambda e: e.tensor_scalar(out=vt2[d_][pi], in0=vaug[:, tl, :], scalar1=e2, scalar2=None, op0=ALU.mult),
                         reads=["m_vaug", "m_EE"], writes=[f"m_vt2{d_}{pi}"])
                    if second:
                        for c in range(8):
                            P.op("pe", mm(pb[7][:, 0:256], hT[:, c, ts_], wo[:, c, :], c == 0, c == 7), reads=["hT", "m_wo"], writes=[PB[7]])
                        P.op("act", lambda e: e.activation(out=sgo[d_], in_=pb[7][:, 0:256], func=AF.Exp, scale=-1.0), writes=[PB[7], f"m_sgo{d_}"])
                        P.op("dve", lambda e: e.tensor_scalar(out=sgo[d_], in0=sgo[d_], scalar1=1.0, scalar2=None, op0=ALU.add), writes=[f"m_sgo{d_}"])
                        P.op("act", lambda e: e.activation(out=sgo[d_], in_=sgo[d_], func=AF.Ln), writes=[f"m_sgo{d_}"])
                        P.op("act", lambda e: e.activation(out=sgo[d_], in_=sgo[d_], func=AF.Exp, scale=-1.0), writes=[f"m_sgo{d_}"])
                    num, knum = pb[d_], PB[d_]
                    P.op("pe", mm(num[:, 0:257], STf[d_][pi], vt[d_][pi], True, False), reads=[f"m_STf{d_}{pi}", f"m_vt{d_}{pi}"], writes=[knum])
                    kv, kkv = pb[2 + d_], PB[2 + d_]
                    P.op("pe", mm(kv[:, 0:257], ktok[:, tl, :], vt2[d_][pi], True, True), reads=["m_ktok", f"m_vt2{d_}{pi}"], writes=[kkv])
                    r0, r1 = it % 2, (it + 1) % 2
                    P.op("pe", mm(num[:, 0:257], qT[:, ts_], Xb[d_][r0], False, True), reads=["m_qT", f"m_Xb{d_}_{r0}", knum], writes=[knum])
                    ebc = EB[:, tl, doff + h:doff + h + 1]
                    P.op("dve", lambda e: e.scalar_tensor_tensor(out=X[d_], in0=X[d_], scalar=ebc, in1=kv[:, 0:257], op0=ALU.mult, op1=ALU.add),
                         reads=["m_EB", f"m_X{d_}"], writes=[kkv, f"m_X{d_}"])
                    P.op("act", lambda e: e.copy(out=Xb[d_][r1], in_=X[d_]), reads=[f"m_X{d_}"], writes=[f"m_Xb{d_}_{r1}"])
                    ns, kns = numS[d_][pi], f"m_numS{d_}{pi}"
                    P.op("act", lambda e: e.copy(out=ns, in_=num[:, 0:257]), writes=[knum, kns])
                    thr = EE[:, tl, 16 + doff + h:16 + doff + h + 1]
                    s0, ks0 = sm[d_][0], f"m_sm{d_}0"
                    P.op("act", lambda e: e.activation(out=s0, in_=ns[:, 256:257], func=AF.Abs), reads=[kns], writes=[ks0])
                    P.op("dve", lambda e: e.tensor_tensor(out=s0, in0=s0, in1=thr, op=ALU.max), reads=[ks0, "m_EE"], writes=[ks0])
                    P.op("dve", lambda e: e.reciprocal(out=s0, in_=s0), reads=[ks0], writes=[ks0])
                    if not second:
                        P.op("dve", lambda e: e.tensor_scalar(out=hfirst[:, tl, :], in0=ns[:, 0:256], scalar1=s0[:, 0:1], scalar2=None, op0=ALU.mult),
                             reads=[kns, ks0], writes=[f"m_hf{tl}"])
                        return
                    s1, ks1 = sm[d_][1], f"m_sm{d_}1"
                    P.op("dve", lambda e: e.scalar_tensor_tensor(out=hs[d_], in0=ns[:, 0:256], scalar=s0[:, 0:1], in1=hfirst[:, tl, :], op0=ALU.mult, op1=ALU.add),
                         reads=[kns, ks0, f"m_hf{tl}"], writes=[f"m_hs{d_}"])
                    P.op("act", lambda e: e.activation(out=hg[d_], in_=hs[d_], func=AF.Square, accum_out=s1), reads=[f"m_hs{d_}"], writes=[f"m_hg{d_}", ks1])
                    P.op("dve", lambda e: e.tensor_scalar(out=s1, in0=s1, scalar1=1.0 / 256, scalar2=EPS, op0=ALU.mult, op1=ALU.add), reads=[ks1], writes=[ks1])
                    P.op("act", lambda e: e.activation(out=s1, in_=s1, func=AF.Ln), reads=[ks1], writes=[ks1])
                    P.op("act", lambda e: e.activation(out=s1, in_=s1, func=AF.Exp, scale=-0.5), reads=[ks1], writes=[ks1])
                    P.op("dve", lambda e: e.scalar_tensor_tensor(out=hs[d_], in0=hs[d_], scalar=s1[:, 0:1], in1=mhg, op0=ALU.mult, op1=ALU.mult),
                         reads=[f"m_hs{d_}", ks1, "m_mhg"], writes=[f"m_hs{d_}"])
                    P.op("dve", lambda e: e.tensor_tensor(out=hg[d_], in0=hs[d_], in1=sgo[d_], op=ALU.mult), reads=[f"m_hs{d_}", f"m_sgo{d_}", f"m_hg{d_}"], writes=[f"m_hg{d_}"])
                    for fc in range(2):
                        P.op("pe", lambda e, fc=fc: e.transpose(out=pb6b[:, 512 + fc * 128:512 + (fc + 1) * 128], in_=hg[d_][:, fc * 128:(fc + 1) * 128], identity=ident_b),
                             reads=[f"m_hg{d_}", "cb"], writes=[PB[6]])
                    xi = ncomp[0] % 2
                    ncomp[0] += 1
                    P.op("act", lambda e: e.copy(out=xTt[xi], in_=pb6b[:, 512:768].rearrange("p (a b) -> p a b", a=2)), writes=[PB[6], f"m_xT{xi}"])
                    col = 1 if tl < 2 else 0
                    for half in range(2):
                        po, ko = pb[4 + half], PB[4 + half]
                        for j in range(4):
                            dc = half * 4 + j
                            for fc in range(2):
                                P.op("pe", mm(po[:, j * 128:(j + 1) * 128], wout[:, fc, dc * 128:(dc + 1) * 128], xTt[xi][:, fc, :], fc == 0, fc == 1), reads=["m_wout", f"m_xT{xi}"], writes=[ko])
                        g1b = modT[l][:, 16 + half * 4:16 + half * 4 + 4, col:col + 1].to_broadcast([128, 4, 128])
                        P.op("dve", lambda e, po=po, g1b=g1b, half=half: e.tensor_tensor(out=tmpo[half], in0=po[:, :].rearrange("p (a b) -> p a b", a=4), in1=g1b, op=ALU.mult),
                             reads=[f"modT{l}"], writes=[ko, f"m_tmpo{half}"])
                        keys = SKEY[half * 4:half * 4 + 4]
                        P.op("dve", lambda e, half=half: e.tensor_tensor(out=sT[:, half * 4:half * 4 + 4, ts_], in0=sT[:, half * 4:half * 4 + 4, ts_], in1=tmpo[half], op=ALU.add),
                             reads=[f"m_tmpo{half}"] + keys, writes=keys)

                for it in range(len(orders[0])):
                    step(0, it, orders[0][it])
                    step(1, it, orders[1][it])

        def hgrn(l):
            P.barrier()
            cv = Carver()
            hlb = cv.take([128, 2, 16])
            lbT = cv.take([128, 16])
            omlb = cv.take([128, 16])
            hhg = cv.take([128, 128])
            rst = cv.take([128, 512])
            P.dma("sp", "h_hlb", lambda e: e.dma_start(out=hlb, in_=d_hlb), writes=["h_hlb"])
            P.dma("sp", "h_rst", lambda e: e.dma_start(out=rst, in_=d_rst[:, 0:512]), writes=["h_rst"])
            P.op("dve", lambda e: e.tensor_tensor(out=lbT, in0=hlb[:, 1, :], in1=hlb[:, 0, :], op=ALU.subtract), reads=["h_hlb"], writes=["h_lb"])
            P.op("act", lambda e: e.activation(out=omlb, in_=lbT, func=AF.Exp), reads=["h_lb"], writes=["h_omlb"])
            P.op("act", lambda e: e.activation(out=lbT, in_=lbT, func=AF.Exp, scale=-1.0), reads=["h_lb", "h_omlb"], writes=["h_lb"])
            for t_, k_ in ((omlb, "h_omlb"), (lbT, "h_lb")):
                P.op("dve", lambda e, t_=t_: e.tensor_scalar(out=t_, in0=t_, scalar1=1.0, scalar2=None, op0=ALU.add), writes=[k_])
                P.op("dve", lambda e, t_=t_: e.reciprocal(out=t_, in_=t_), writes=[k_])

            ptmp = [cv.take([128, SEQ]) for _ in range(2)]
            for c in range(8):
                pt = ptmp[c % 2]
                src = sT[:, c, CTX:NT].rearrange("p (row col) -> p col row", col=64)
                P.op("dve" if c % 2 == 0 else "pool", lambda e, pt=pt, src=src: e.tensor_copy(out=pt.rearrange("p (col row) -> p col row", row=32), in_=src),
                     reads=[SKEY[c]], writes=[f"h_ptmp{c % 2}"])
                P.op("act", lambda e, pt=pt, c=c: e.copy(out=sT[:, c, CTX:NT], in_=pt), reads=[f"h_ptmp{c % 2}"], writes=[SKEY[c]])
            P.barrier()
            cv.off -= 2 * SEQ * 4
            norm_base = cv.off
            norm_mod(cv, A1, SH1, l, 0, NT, out_bf=hT, out_key="hT", bs=256)
            norm_end = cv.off
            P.barrier()

            wq = cv.take([128, 8, 128], BF16)
            wz = cv.take([128, 8, 2, 128], BF16)
            wi = cv.take([128, 8, 128], BF16)
            wgg = cv.take([128, 8, 128], BF16)
            wout = cv.take([128, D], BF16)
            qs = cv.take([128, NT], BF16)
            cvn = Carver()
            cvn.off = norm_base
            fTs = [cv.take([128, 512]), cvn.take([128, 512])]
            bTs = [cv.take([128, 512]), cvn.take([128, 512])]
            t32s = [cv.take([128, 512]), cv.take([128, 512])]
            kks = [cv.take([128, 512], BF16), cvn.take([128, 512], BF16)]
            assert cvn.off <= norm_end
            totcs = [cv.take([128, 4]) for _ in range(2)]
            rcs = [cv.take([128, 4]) for _ in range(2)]
            t32 = t32s[0]
            qtl = [cv.take([128, NT], BF16) for _ in range(2)]
            ktl = [cv.take([128, NT], BF16) for _ in range(2)]
            kht = [cv.take([128, NT], BF16) for _ in range(2)]
            ebT = [cv.take([128, NTILE]) for _ in range(2)]
            erT = [cv.take([128, NTILE]) for _ in range(2)]
            itok = cv.take([128, NTILE, 128], BF16)
            sgt = cv.take([128, NTILE, 128], BF16)
            gtmp = cv.take([128, 128])
            ofirst = cv.take([128, NTILE, 128], BF16)
            AT = [[cv.take([128, 128], BF16) for _ in range(2)] for _ in range(2)]
            khtok = [[cv.take([128, 128], BF16) for _ in range(2)] for _ in range(2)]
            S = [cv.take([128, 128]) for _ in range(2)]
            Sb = [[cv.take([128, 128], BF16) for _ in range(2)] for _ in range(2)]
            os_ = [cv.take([128, 128]) for _ in range(2)]
            og = [cv.take([128, 128], BF16) for _ in range(2)]
            sm = [cv.take([128, 1]) for _ in range(2)]
            xTt = [cv.take([128, 128], BF16) for _ in range(2)]
            pb7b = pb[7].bitcast(BF16)
            g1row = cv.take([128, D], BF16)
            for dc in range(8):
                P.op("pe", mm(pb[dc % 2][:, 0:128], modT[l][:, 16 + dc, 0:1].to_broadcast([128, 128]), ident_f, True, True), reads=[f"modT{l}", "cf"], writes=[PB[dc % 2]])
                P.op("act", lambda e, dc=dc: e.copy(out=g1row[:, dc * 128:(dc + 1) * 128], in_=pb[dc % 2][:, 0:128]), writes=[PB[dc % 2], "h_g1row"])
            REF = 64
            pieces = [(0, 512), (512, 512), (1024, 512), (1536, 512), (2048, 256)]

            def v128(a):
                return a.rearrange("p (c k) -> p c k", k=128)

            for h in range(8):
                P.dma("sp", "h_hhg", lambda e, h=h: e.dma_start(out=hhg, in_=d_hhg[:, h * 128:(h + 1) * 128]), writes=["h_hhg"])
                P.dma("pool", "h_wq", lambda e, h=h: e.dma_start(out=wq, in_=d_hwin[:, :, h * 128:(h + 1) * 128]), writes=["h_wq"])
                P.dma("pool", "h_wz", lambda e, h=h: [e.dma_start(out=wz[:, :, 0, :], in_=d_hwin[:, :, 1024 + h * 128:1024 + (h + 1) * 128]),
                                                      e.dma_start(out=wz[:, :, 1, :], in_=d_hwin[:, :, 2048 + h * 128:2048 + (h + 1) * 128])], writes=["h_wz"], n=2)
                P.dma("pool", "h_wi", lambda e, h=h: e.dma_start(out=wi, in_=d_hwin[:, :, 3072 + h * 128:3072 + (h + 1) * 128]), writes=["h_wi"])
                P.dma("pool", "h_wg", lambda e, h=h: e.dma_start(out=wgg, in_=d_hwin[:, :, 4096 + h * 128:4096 + (h + 1) * 128]), writes=["h_wg"])
                P.dma("pool", "h_wout", lambda e, h=h: e.dma_start(out=wout, in_=d_hwout[:, h, :], max_dma_last_dim=4096), writes=["h_wout"])
                P.op("dve", lambda e: e.tensor_tensor(out=wout, in0=wout, in1=g1row, op=ALU.mult), reads=["h_g1row"], writes=["h_wout"])
                for bi_, (t0, n, col) in enumerate(_blocks(0, NT)):
                    pq, kq = pb[bi_ % 2], PB[bi_ % 2]
                    for c in range(8):
                        P.op("pe", mm(pq[:, 0:n], wq[:, c, :], hT[:, c, t0:t0 + n], c == 0, c == 7), reads=["h_wq", "hT"], writes=[kq])
                    P.op("act", lambda e, n=n, pq=pq: e.activation(out=t32[:, 0:n], in_=pq[:, 0:n], func=AF.Exp, scale=-1.0), writes=[kq, "h_t32_0"])
                    P.op("act", lambda e, n=n: e.activation(out=t32[:, 0:n], in_=t32[:, 0:n], func=AF.Ln, bias=1.0), writes=["h_t32_0"])
                    P.op("act", lambda e, n=n: e.activation(out=t32[:, 0:n], in_=t32[:, 0:n], func=AF.Exp, scale=-1.0), writes=["h_t32_0"])
                    P.op("dve", lambda e, t0=t0, n=n, pq=pq: e.tensor_tensor(out=qs[:, t0:t0 + n], in0=t32[:, 0:n], in1=pq[:, 0:n], op=ALU.mult),
                         reads=["h_t32_0"], writes=[kq, "h_qs"])
                for tl in range(NTILE):
                    ts_ = slice(tl * 128, (tl + 1) * 128)
                    pi_, ki_ = pb[2 + tl % 2], PB[2 + tl % 2]
                    for c in range(8):
                        P.op("pe", mm(pi_[:, 0:128], hT[:, c, ts_], wi[:, c, :], c == 0, c == 7), reads=["hT", "h_wi"], writes=[ki_])
                    if tl >= 2:
                        for c in range(8):
                            P.op("pe", mm(pi_[:, 128:256], hT[:, c, ts_], wgg[:, c, :], c == 0, c == 7), reads=["hT", "h_wg"], writes=[ki_])
                    P.op("act", lambda e, tl=tl, pi_=pi_: e.copy(out=itok[:, tl, :], in_=pi_[:, 0:128]), writes=[ki_, "h_itok"])
                    if tl >= 2:
                        P.op("act", lambda e, pi_=pi_: e.activation(out=gtmp, in_=pi_[:, 128:256], func=AF.Exp, scale=-1.0), writes=[ki_, "h_gtmp"])
                        P.op("act", lambda e: e.activation(out=gtmp, in_=gtmp, func=AF.Ln, bias=1.0), writes=["h_gtmp"])
                        P.op("act", lambda e: e.activation(out=gtmp, in_=gtmp, func=AF.Exp, scale=-1.0), writes=["h_gtmp"])
                        P.op("dve", lambda e, tl=tl, pi_=pi_: e.tensor_tensor(out=sgt[:, tl, :], in0=gtmp, in1=pi_[:, 128:256], op=ALU.mult), reads=["h_gtmp"], writes=[ki_, "h_sgt"])

                def precompute(d_, p0, pn, pk):
                    fT, bT, t32, kk, totc, rc = fTs[d_], bTs[d_], t32s[d_], kks[d_], totcs[d_], rcs[d_]
                    kx = f"_{d_}"
                    lbc = lbT[:, d_ * 8 + h:d_ * 8 + h + 1]
                    omc = omlb[:, d_ * 8 + h:d_ * 8 + h + 1]
                    nt = pn // 128
                    tsl = slice(p0 // 128, p0 // 128 + nt)
                    ps_ = slice(p0, p0 + pn)
                    pz, kz = pb[4 + d_], PB[4 + d_]
                    for c in range(8):
                        P.op("pe", mm(pz[:, 0:pn], wz[:, c, d_, :], hT[:, c, ps_], c == 0, c == 7), reads=["h_wz", "hT"], writes=[kz])
                    P.op("act", lambda e: e.activation(out=fT[:, 0:pn], in_=pz[:, 0:pn], func=AF.Exp, scale=-1.0), writes=[kz, "h_fT" + kx])
                    P.op("act", lambda e: e.activation(out=fT[:, 0:pn], in_=fT[:, 0:pn], func=AF.Ln, bias=1.0), writes=["h_fT" + kx])
                    P.op("act", lambda e: e.activation(out=fT[:, 0:pn], in_=fT[:, 0:pn], func=AF.Exp, scale=-1.0), writes=["h_fT" + kx])
                    P.op("act", lambda e: e.activation(out=fT[:, 0:pn], in_=fT[:, 0:pn], func=AF.Identity, scale=omc, bias=lbc),
                         reads=["h_lb", "h_omlb"], writes=["h_fT" + kx])
                    P.op("act", lambda e: e.activation(out=kk[:, 0:pn], in_=fT[:, 0:pn], func=AF.Identity, scale=-1.0, bias=1.0), reads=["h_fT" + kx], writes=["h_kk" + kx])
                    P.op("act", lambda e: e.activation(out=fT[:, 0:pn], in_=fT[:, 0:pn], func=AF.Ln), reads=["h_kk" + kx], writes=["h_fT" + kx])
                    P.op("dve", lambda e: e.tensor_tensor_scan(out=bT[:, 0:pn], data0=rst[:, 0:pn], data1=fT[:, 0:pn], initial=0.0, op0=ALU.mult, op1=ALU.add),
                         reads=["h_rst", "h_fT" + kx], writes=["h_bT" + kx])
                    P.op("dve", lambda e: e.tensor_copy(out=totc[:, 0:nt], in_=v128(bT[:, 0:pn])[:, :, 127]), reads=["h_bT" + kx], writes=["h_totc" + kx])
                    totb = totc[:, 0:nt].unsqueeze(2).to_broadcast([128, nt, 128])
                    P.op("act", lambda e: e.activation(out=ebT[d_][:, tsl], in_=totc[:, 0:nt], func=AF.Exp), reads=["h_totc" + kx], writes=[f"h_ebT{pk}"])
                    if d_ == 0:
                        P.op("dve", lambda e: e.tensor_tensor(out=v128(t32[:, 0:pn]), in0=totb, in1=v128(bT[:, 0:pn]), op=ALU.subtract),
                             reads=["h_bT" + kx, "h_totc" + kx], writes=["h_t32" + kx])
                    else:
                        P.op("dve", lambda e: e.tensor_tensor(out=t32[:, 0:pn], in0=bT[:, 0:pn], in1=fT[:, 0:pn], op=ALU.subtract), reads=["h_bT" + kx, "h_fT" + kx], writes=["h_t32" + kx])
                        P.op("dve", lambda e: e.tensor_tensor(out=v128(bT[:, 0:pn]), in0=totb, in1=v128(t32[:, 0:pn]), op=ALU.subtract),
                             reads=["h_totc" + kx, "h_t32" + kx], writes=["h_bT" + kx])
                    P.op("act", lambda e: e.activation(out=t32[:, 0:pn], in_=t32[:, 0:pn], func=AF.Exp), writes=["h_t32" + kx])
                    P.op("dve", lambda e: e.tensor_tensor(out=kht[d_][:, ps_], in0=t32[:, 0:pn], in1=kk[:, 0:pn], op=ALU.mult),
                         reads=["h_t32" + kx, "h_kk" + kx], writes=[f"h_kht{pk}"])
                    P.op("dve", lambda e: e.tensor_copy(out=rc[:, 0:nt], in_=v128(bT[:, 0:pn])[:, :, REF]), reads=["h_bT" + kx], writes=["h_rc" + kx])
                    rb = rc[:, 0:nt].unsqueeze(2).to_broadcast([128, nt, 128])
                    P.op("act", lambda e: e.activation(out=erT[d_][:, tsl], in_=rc[:, 0:nt], func=AF.Exp), reads=["h_rc" + kx], writes=[f"h_erT{pk}"])
                    P.op("dve", lambda e: e.tensor_tensor(out=v128(bT[:, 0:pn]), in0=v128(bT[:, 0:pn]), in1=rb, op=ALU.subtract), reads=["h_rc" + kx], writes=["h_bT" + kx])
                    P.op("act", lambda e: e.activation(out=t32[:, 0:pn], in_=bT[:, 0:pn], func=AF.Exp), reads=["h_bT" + kx, f"h_kht{pk}"], writes=["h_t32" + kx])
                    P.op("dve", lambda e: e.tensor_tensor(out=qtl[d_][:, ps_], in0=t32[:, 0:pn], in1=qs[:, ps_], op=ALU.mult),
                         reads=["h_t32" + kx, "h_qs"], writes=[f"h_qtl{pk}"])
                    P.op("act", lambda e: e.activation(out=t32[:, 0:pn], in_=bT[:, 0:pn], func=AF.Exp, scale=-1.0), reads=["h_bT" + kx, f"h_qtl{pk}"], writes=["h_t32" + kx])
                    P.op("dve", lambda e: e.tensor_tensor(out=ktl[d_][:, ps_], in0=t32[:, 0:pn], in1=kk[:, 0:pn], op=ALU.mult),
                         reads=["h_t32" + kx, "h_kk" + kx], writes=[f"h_ktl{pk}"])

                orders = [list(range(NTILE)), [1, 0] + list(range(NTILE - 1, 1, -1))]
                piecesD = [[(0, 512), (512, 512), (1024, 512), (1536, 512), (2048, 256)],
                           [(0, 256), (1792, 512), (1280, 512), (768, 512), (256, 512)]]
                pkey = {}
                for d_ in range(2):
                    for k_, (p0, pn) in enumerate(piecesD[d_]):
                        for tl in range(p0 // 128, (p0 + pn) // 128):
                            pkey[(d_, tl)] = f"{d_}_{k_}"
                visited = set()
                for d_ in range(2):
                    P.op("dve", lambda e, d_=d_: e.memset(S[d_], 0.0), writes=[f"h_S{d_}"])
                ncomp = [0]

                def step(d_, it, tl):
                    ts_ = slice(tl * 128, (tl + 1) * 128)
                    pi = it % 2
                    pk = pkey[(d_, tl)]
                    lat = tl >= 2
                    second = tl in visited
                    visited.add(tl)
                    o_, ko = pb[d_], PB[d_]
                    sc_cols = slice(128 * d_, 128 * d_ + 128)
                    if lat:
                        erc = erT[d_][:, tl:tl + 1]
                        P.op("act", lambda e: e.activation(out=Sb[d_][pi], in_=S[d_], func=AF.Copy, scale=erc), reads=[f"h_S{d_}", f"h_erT{pk}"], writes=[f"h_Sb{d_}_{pi}"])
                        P.op("pe", mm(pb[6][:, sc_cols], ktl[d_][:, ts_], qtl[d_][:, ts_], True, True), reads=[f"h_ktl{pk}", f"h_qtl{pk}"], writes=[PB[6]])
                        msk = cb[:, CI["tF"], :] if d_ == 0 else cb[:, CI["tB"], :]
                        P.op("dve", lambda e: e.tensor_tensor(out=AT[d_][pi], in0=pb[6][:, sc_cols], in1=msk, op=ALU.mult), reads=["cb"], writes=[PB[6], f"h_AT{d_}{pi}"])
                        P.op("pe", mm(o_[:, 0:128], AT[d_][pi], itok[:, tl, :], True, False), reads=[f"h_AT{d_}{pi}", "h_itok"], writes=[ko])
                    P.op("pe", lambda e: e.transpose(out=pb7b[:, sc_cols], in_=kht[d_][:, ts_], identity=ident_b), reads=[f"h_kht{pk}", "cb"], writes=[PB[7]])
                    P.op("act", lambda e: e.copy(out=khtok[d_][pi], in_=pb7b[:, sc_cols]), writes=[PB[7], f"h_khtok{d_}{pi}"])
                    kv, kkv = pb[2 + d_], PB[2 + d_]
                    P.op("pe", mm(kv[:, 0:128], khtok[d_][pi], itok[:, tl, :], True, True), reads=[f"h_khtok{d_}{pi}", "h_itok"], writes=[kkv])
                    if lat:
                        P.op("pe", mm(o_[:, 0:128], qtl[d_][:, ts_], Sb[d_][pi], False, True), reads=[f"h_qtl{pk}", f"h_Sb{d_}_{pi}", ko], writes=[ko])
                    ebc = ebT[d_][:, tl:tl + 1]
                    P.op("dve", lambda e: e.scalar_tensor_tensor(out=S[d_], in0=S[d_], scalar=ebc, in1=kv[:, 0:128], op0=ALU.mult, op1=ALU.add),
                         reads=[f"h_ebT{pk}", f"h_S{d_}"], writes=[kkv, f"h_S{d_}"])
                    if not lat:
                        return
                    if not second:
                        P.op("act", lambda e: e.copy(out=ofirst[:, tl, :], in_=o_[:, 0:128]), writes=[ko, f"h_of{tl}"])
                        return
                    s1, ks1 = sm[d_], f"h_sm{d_}"
                    P.op("dve", lambda e: e.tensor_tensor(out=os_[d_], in0=o_[:, 0:128], in1=ofirst[:, tl, :], op=ALU.add), reads=[f"h_of{tl}"], writes=[ko, f"h_os{d_}"])
                    P.op("act", lambda e: e.activation(out=og[d_], in_=os_[d_], func=AF.Square, accum_out=s1), reads=[f"h_os{d_}"], writes=[f"h_og{d_}", ks1])
                    P.op("dve", lambda e: e.tensor_scalar(out=s1, in0=s1, scalar1=1.0 / 128, scalar2=EPS, op0=ALU.mult, op1=ALU.add), writes=[ks1])
                    P.op("act", lambda e: e.activation(out=s1, in_=s1, func=AF.Ln), writes=[ks1])
                    P.op("act", lambda e: e.activation(out=s1, in_=s1, func=AF.Exp, scale=-0.5), writes=[ks1])
                    P.op("dve", lambda e: e.scalar_tensor_tensor(out=os_[d_], in0=os_[d_], scalar=s1[:, 0:1], in1=hhg, op0=ALU.mult, op1=ALU.mult),
                         reads=[ks1, "h_hhg"], writes=[f"h_os{d_}"])
                    P.op("dve", lambda e: e.tensor_tensor(out=og[d_], in0=os_[d_], in1=sgt[:, tl, :], op=ALU.mult), reads=[f"h_os{d_}", "h_sgt"], writes=[f"h_og{d_}"])
                    P.op("pe", lambda e: e.transpose(out=pb7b[:, 256:384], in_=og[d_], identity=ident_b), reads=[f"h_og{d_}", "cb"], writes=[PB[7]])
                    xi = ncomp[0] % 2
                    ncomp[0] += 1
                    P.op("act", lambda e: e.copy(out=xTt[xi], in_=pb7b[:, 256:384]), writes=[PB[7], f"h_xT{xi}"])
                    c0 = (tl * 128 - CTX) // 32
                    for half in range(2):
                        po, kpo = pb[4 + half], PB[4 + half]
                        for j in range(4):
                            dc = half * 4 + j
                            P.op("pe", mm(po[:, j * 128:(j + 1) * 128], wout[:, dc * 128:(dc + 1) * 128], xTt[xi], True, True), reads=["h_wout", f"h_xT{xi}"], writes=[kpo])
                        keys = SKEY[half * 4:half * 4 + 4]
                        dstv = sT[:, half * 4:half * 4 + 4, ts_]
                        srcv = po[:, :].rearrange("p (d t) -> p d t", d=4)
                        P.op("dve", lambda e, dstv=dstv, srcv=srcv: e.tensor_tensor(out=dstv, in0=dstv, in1=srcv, op=ALU.add),
                             reads=keys, writes=[kpo] + keys)

                done = [0, 0]
                for k_ in range(5):
                    P.interleave([lambda d_=d_: precompute(d_, piecesD[d_][k_][0], piecesD[d_][k_][1], f"{d_}_{k_}") for d_ in range(2)])
                    avail = [min(NTILE, 4 * (k_ + 1)) if k_ < 4 else NTILE, min(NTILE, 2 + 4 * k_)]
                    if DEBUG_NT is not None:
                        avail = [min(a_, DEBUG_NT) for a_ in avail]
                    while done[0] < avail[0] or done[1] < avail[1]:
                        for d_ in range(2):
                            if done[d_] < avail[d_]:
                                step(d_, done[d_], orders[d_][done[d_]])
                                done[d_] += 1

        def final():
            P.barrier()
            cv = Carver()
            ob = [cv.take([128, 512]) for _ in range(2)]
            outs = []
            cnt = [0]
            sq = [cv.take([128, 512]) for _ in range(2)]
            rstd = cv.take([128, 512])
            for (t0, n, col) in _blocks(CTX, NT):
                for c in range(8):
                    P.op("act", lambda e, c=c, t0=t0, n=n: e.activation(out=sq[c % 2][:, 0:n], in_=sT[:, c, t0:t0 + n], func=AF.Square), reads=[SKEY[c]], writes=[f"nm_sq{c % 2}"])
                    P.op("pe", mm(pb[7][:, 0:n], ones_f, sq[c % 2][:, 0:n], c == 0, c == 7), reads=[f"nm_sq{c % 2}", "cf"], writes=[PB[7]])
                P.op("dve", lambda e, n=n: e.tensor_scalar(out=rstd[:, 0:n], in0=pb[7][:, 0:n], scalar1=1.0 / D, scalar2=EPS, op0=ALU.mult, op1=ALU.add), reads=[PB[7]], writes=["nm_rstd"])
                P.op("act", lambda e, n=n: e.activation(out=rstd[:, 0:n], in_=rstd[:, 0:n], func=AF.Sqrt), reads=["nm_rstd"], writes=["nm_rstd"])
                P.op("dve", lambda e, n=n: e.reciprocal(out=rstd[:, 0:n], in_=rstd[:, 0:n]), reads=["nm_rstd"], writes=["nm_rstd"])
                for c in range(8):
                    i = cnt[0] % 2
                    cnt[0] += 1
                    P.op("dve", lambda e, c=c, t0=t0, n=n, i=i: e.scalar_tensor_tensor(out=ob[i][:, 0:n], in0=sT[:, c, t0:t0 + n], scalar=gfin[:, c:c + 1], in1=rstd[:, 0:n],
                                                                                 op0=ALU.mult, op1=ALU.mult), reads=[SKEY[c], "gfin", "nm_rstd"], writes=[f"fin_ob{i}"])
                    outs.append(P.dma("sp", f"fin_ob{i}", lambda e, c=c, t0=t0, n=n, i=i: e.dma_start(out=d_out[:, c, t0 - CTX:t0 - CTX + n], in_=ob[i][:, 0:n]),
                                      reads=[f"fin_ob{i}"]))
            return outs

        outs = []
        if "mix0" in phases:
            mlstm(0)
        if "moe0" in phases:
            moe(0, 0, NT)
        if "mix1" in phases:
            hgrn(1)
        if "moe1" in phases:
            moe(1, CTX, NT)
        if "final" in phases:
            outs = final()
        if d_dump is not None:
            P.barrier()
            for c in range(8):
                outs.append(P.dma("sp", f"dump{c}", lambda e, c=c: e.dma_start(out=d_dump[:, c, :], in_=sT[:, c, :]), reads=[SKEY[c]]))
        P.emit(final_wait_ops=outs + dbg_outs)
    return nc


def _fm(v, lead=()):
    v = np.asarray(v, np.float32)
    k = v.shape[-1] // 128
    r = v.reshape(v.shape[:-1] + (k, 128))
    return np.ascontiguousarray(np.moveaxis(r, -1, 0))


def _wl(w):
    w = np.asarray(w, np.float32)
    K, N = w.shape
    return np.ascontiguousarray(w.reshape(K // 128, 128, N).transpose(1, 0, 2))


def prep_shared(x, c, ctx, c_ctx, ada_w, ada_b, norm_mix_g, norm_ffn_g, final_g,
                m_w_in, m_conv_w, m_conv_b, m_gate_b, m_head_g, m_w_out,
                h_w_in, h_lower_bounds, h_head_g, h_w_out,
                router_w, router_bias, e_w_gate, e_w_up, e_w_down):
    sh = {}
    sh["adaw"] = np.ascontiguousarray(np.asarray(ada_w, np.float32).reshape(2, 8, 128, 12, 512).transpose(0, 3, 2, 1, 4))
    sh["adab"] = np.ascontiguousarray(_fm(ada_b))
    sh["gmix"] = _fm(norm_mix_g)
    sh["gffn"] = _fm(norm_ffn_g)
    sh["gfin"] = _fm(final_g)
    sh["mwin"] = _wl(m_w_in[0])
    cw = _fm(m_conv_w[0])
    cbias = _fm(m_conv_b[0])
    sh["mconv"] = np.ascontiguousarray(np.concatenate([cw.transpose(0, 2, 1), cbias[:, :, None]], axis=2))
    sh["mgb"] = np.ascontiguousarray(np.broadcast_to(np.asarray(m_gate_b[0], np.float32)[None, :], (128, 16)))
    sh["mhg"] = np.ascontiguousarray(np.broadcast_to(np.asarray(m_head_g[0], np.float32)[None, :], (128, D)))
    sh["mwout"] = _wl(m_w_out[0])
    sh["hwin"] = _wl(h_w_in[0])
    sh["hlb"] = np.ascontiguousarray(_fm(h_lower_bounds))
    sh["hhg"] = np.ascontiguousarray(np.broadcast_to(np.asarray(h_head_g[0], np.float32)[None, :], (128, D)))
    sh["hwout"] = _wl(h_w_out[0])
    sh["rw"] = _wl(router_w)
    sh["rb"] = np.ascontiguousarray(np.broadcast_to(np.asarray(router_bias, np.float32)[None, None, :], (128, NTILE, NE)))
    sh["ewg"] = np.ascontiguousarray(np.asarray(e_w_gate, np.float32).reshape(2, NE, 8, 128, DEXP).transpose(0, 1, 3, 2, 4))
    sh["ewu"] = np.ascontiguousarray(np.asarray(e_w_up, np.float32).reshape(2, NE, 8, 128, DEXP).transpose(0, 1, 3, 2, 4))
    sh["ewd"] = np.ascontiguousarray(np.asarray(e_w_down, np.float32).reshape(2, NE, 4, 128, D).transpose(0, 1, 3, 2, 4))
    sh["cf"] = CARR
    sh["rst"] = RST
    return sh


def prep_core(b, x, c, ctx, c_ctx, s_override=None):
    if s_override is not None:
        s = s_override
    else:
        s = np.concatenate([np.asarray(ctx[b], np.float32), np.asarray(x[b], np.float32)], axis=0)
    xT = np.ascontiguousarray(s.reshape(NT, 8, 128).transpose(2, 1, 0))
    cc = np.stack([np.asarray(c[b], np.float32), np.asarray(c_ctx, np.float32)], axis=-1)
    cT = np.ascontiguousarray(cc.reshape(8, 128, 2).transpose(1, 0, 2))
    return {"xT": xT, "cT": cT}


_NC_CACHE = {}


def kernel(**inputs):
    x = inputs["x"]
    B = x.shape[0]
    sh = prep_shared(**inputs)
    if "full" not in _NC_CACHE:
        _NC_CACHE["full"] = build_program()
    nc = _NC_CACHE["full"]
    in_maps = []
    for b in range(B):
        m = dict(sh)
        m.update(prep_core(b, inputs["x"], inputs["c"], inputs["ctx"], inputs["c_ctx"]))
        in_maps.append(m)
    res = run_bass_kernel_spmd(nc, in_maps, core_ids=list(range(B)))
    out = np.empty((B, SEQ, D), np.float32)
    for b in range(B):
        oT = np.asarray(res.results[b]["outT"])
        out[b] = oT.transpose(2, 1, 0).reshape(64, 32, D).transpose(1, 0, 2).reshape(SEQ, D)
    return out
```

```python
import numpy as np
import concourse.bass as bass
import concourse.mybir as mybir
from contextlib import ExitStack
from concourse.bass_utils import run_bass_kernel_spmd

F32 = mybir.dt.float32
BF16 = mybir.dt.bfloat16
AF = mybir.ActivationFunctionType
ALU = mybir.AluOpType
AX = mybir.AxisListType

D = 1024
SEQ = 2048
CTX = 256
NT = SEQ + CTX
NTILE = NT // 128
EPS = 1e-6
NE = 16
DEXP = 512
ENGS = ("pe", "act", "dve", "pool", "sp")
DEBUG_NT = None


class Op:
    __slots__ = ("eng", "fn", "deps", "signal", "seq", "is_dma", "dsem", "dval", "n_inst", "name", "cost")

    def __init__(self, eng, fn, is_dma, dsem, name):
        self.eng = eng
        self.fn = fn
        self.deps = set()
        self.signal = False
        self.seq = None
        self.is_dma = is_dma
        self.dsem = dsem
        self.dval = None
        self.n_inst = 1
        self.name = name
        self.cost = None


class Prog:
    def __init__(self, nc):
        self.nc = nc
        self.ops = {e: [] for e in ENGS}
        self.last_w = {}
        self.readers = {}
        self.all_ops = []
        self._bar_from = 0
        self._capture = None

    def _add(self, eng, fn, reads, writes, is_dma=False, dsem=None, name=None):
        o = Op(eng, fn, is_dma, dsem, name)
        for k in reads:
            w = self.last_w.get(k)
            if w is not None:
                o.deps.add(w)
        for k in writes:
            w = self.last_w.get(k)
            if w is not None:
                o.deps.add(w)
            for r in self.readers.get(k, ()):
                o.deps.add(r)
        for k in reads:
            self.readers.setdefault(k, []).append(o)
        for k in writes:
            self.last_w[k] = o
            self.readers[k] = []
        o.deps.discard(o)
        self.ops[eng].append(o)
        self.all_ops.append(o)
        return o

    def op(self, eng, fn, reads=(), writes=(), name=None):
        if self._capture is not None:
            self._capture.append(("op", eng, fn, tuple(reads), tuple(writes), None, 1))
            return None
        return self._add(eng, fn, reads, writes, name=name)

    def dma(self, eng, group, fn, reads=(), writes=(), n=1, name=None):
        if self._capture is not None:
            self._capture.append(("dma", eng, fn, tuple(reads), tuple(writes), group, n))
            return None
        o = self._add(eng, fn, reads, writes, is_dma=True, dsem=group, name=name)
        o.n_inst = n
        return o

    def interleave(self, builders):
        streams = []
        for b in builders:
            self._capture = []
            b()
            streams.append(self._capture)
            self._capture = None
        idx = [0] * len(streams)
        while any(idx[i] < len(st) for i, st in enumerate(streams)):
            for i, st in enumerate(streams):
                if idx[i] < len(st):
                    kind, eng, fn, reads, writes, group, n = st[idx[i]]
                    idx[i] += 1
                    if kind == "op":
                        self._add(eng, fn, reads, writes)
                    else:
                        o = self._add(eng, fn, reads, writes, is_dma=True, dsem=group)
                        o.n_inst = n

    def barrier(self):
        self.all_ops.append(None)

    def _schedule(self, seg):
        import heapq
        COST = {"pe": 0.16, "act": 0.42, "dve": 0.45, "pool": 0.6, "sp": 0.1}
        HOP = 0.25
        n = len(seg)
        idx = {id(o): i for i, o in enumerate(seg)}
        cost = [0.0] * n
        lat = [0.0] * n
        for i, o in enumerate(seg):
            c = o.cost if o.cost is not None else COST[o.eng]
            if o.is_dma:
                cost[i] = 0.08 * o.n_inst
                lat[i] = c if o.cost is not None else 2.5
            else:
                cost[i] = c
        deps = [[idx[id(d)] for d in o.deps if id(d) in idx] for o in seg]
        succ = [[] for _ in range(n)]
        for i, dl in enumerate(deps):
            for d in dl:
                succ[d].append(i)
        prio = [0.0] * n
        for i in range(n - 1, -1, -1):
            m = 0.0
            for j in succ[i]:
                if prio[j] > m:
                    m = prio[j]
            prio[i] = m + cost[i] + lat[i] + HOP
        ndep = [len(dl) for dl in deps]
        est = [0.0] * n
        fin = [0.0] * n
        free_at = {e: 0.0 for e in ENGS}
        pend = {e: [] for e in ENGS}
        avail = {e: [] for e in ENGS}
        order = {e: [] for e in ENGS}
        for i in range(n):
            if ndep[i] == 0:
                heapq.heappush(pend[seg[i].eng], (0.0, -prio[i], i))
        left = n
        while left:
            best_e, best_t = None, None
            for e in ENGS:
                if not pend[e] and not avail[e]:
                    continue
                while pend[e] and pend[e][0][0] <= free_at[e]:
                    t_, p_, i_ = heapq.heappop(pend[e])
                    heapq.heappush(avail[e], (p_, i_))
                t = free_at[e] if avail[e] else max(free_at[e], pend[e][0][0])
                if best_t is None or t < best_t:
                    best_e, best_t = e, t
            e = best_e
            if avail[e]:
                p_, i = heapq.heappop(avail[e])
            else:
                t_, p_, i = heapq.heappop(pend[e])
            start = max(free_at[e], est[i])
            free_at[e] = start + cost[i]
            fin[i] = start + cost[i] + lat[i]
            order[e].append(seg[i])
            left -= 1
            for j in succ[i]:
                if fin[i] + HOP > est[j]:
                    est[j] = fin[i] + HOP
                ndep[j] -= 1
                if ndep[j] == 0:
                    heapq.heappush(pend[seg[j].eng], (est[j], -prio[j], j))
        return order

    def emit(self, final_wait_ops=(), schedule=True):
        nc = self.nc
        segs, cur = [], []
        for o in self.all_ops:
            if o is None:
                if cur:
                    segs.append(cur)
                cur = []
            else:
                cur.append(o)
        if cur:
            segs.append(cur)
        seg_orders = []
        for seg in segs:
            if schedule:
                seg_orders.append(self._schedule(seg))
            else:
                od = {e: [] for e in ENGS}
                for o in seg:
                    od[o.eng].append(o)
                seg_orders.append(od)
        bar_deps = [set() for _ in segs]
        last_comp = {}
        for k, od in enumerate(seg_orders):
            if k > 0:
                bar_deps[k] = set(last_comp.values()) | {o for o in segs[k - 1] if o.is_dma}
            for e in ENGS:
                comp = [o for o in od[e] if not o.is_dma]
                if comp:
                    last_comp[e] = comp[-1]
        for o in (x for x in self.all_ops if x is not None):
            for d in o.deps:
                if d.eng == "pe" and o.eng == "pe" and not d.is_dma and not o.is_dma:
                    continue
                d.signal = True
        for bd in bar_deps:
            for d in bd:
                d.signal = True
        for o in final_wait_ops:
            o.signal = True
        with ExitStack() as es:
            esem = {e: es.enter_context(nc.semaphore("c_" + e)) for e in ENGS}
            gsem = {}
            gcount = {}
            for e in ENGS:
                for od in seg_orders:
                    for o in od[e]:
                        if o.is_dma:
                            if o.dsem not in gsem:
                                gsem[o.dsem] = es.enter_context(nc.semaphore("d_" + str(o.dsem)))
                                gcount[o.dsem] = 0
                            gcount[o.dsem] += 16 * o.n_inst
                            o.dval = gcount[o.dsem]
            for e in ENGS:
                c = 0
                for od in seg_orders:
                    for o in od[e]:
                        if not o.is_dma and o.signal:
                            c += 1
                            o.seq = c
            block = es.enter_context(nc.Block())
            engobj = {"pe": "tensor", "act": "scalar", "dve": "vector", "pool": "gpsimd", "sp": "sync"}

            def run(ename, eng):
                waited = {}

                def do_waits(deps, is_pe_compute):
                    need = {}
                    for d in deps:
                        if d.is_dma:
                            key, val, sem = ("g", d.dsem), d.dval, gsem[d.dsem]
                        else:
                            if d.eng == "pe" and ename == "pe" and is_pe_compute:
                                continue
                            if d.eng == ename and ename == "pe":
                                continue
                            key, val, sem = ("e", d.eng), d.seq, esem[d.eng]
                        if waited.get(key, 0) >= val:
                            continue
                        if key not in need or need[key][1] < val:
                            need[key] = (sem, val)
                    for key, (sem, val) in need.items():
                        eng.wait_ge(sem, val)
                        waited[key] = val

                for k, od in enumerate(seg_orders):
                    if bar_deps[k]:
                        do_waits(list(bar_deps[k]), False)
                    for o in od[ename]:
                        do_waits(o.deps, not o.is_dma)
                        r = o.fn(eng)
                        if o.is_dma:
                            insts = r if isinstance(r, (list, tuple)) else [r]
                            assert len(insts) == o.n_inst
                            for ins in insts:
                                ins.then_inc(gsem[o.dsem], 16)
                        elif o.signal:
                            ins = r[-1] if isinstance(r, (list, tuple)) else r
                            ins.then_inc(esem[ename], 1)
                if ename == "sp":
                    for o in final_wait_ops:
                        if o.is_dma:
                            eng.wait_ge(gsem[o.dsem], o.dval)
                        else:
                            eng.wait_ge(esem[o.eng], o.seq)

            for ename in ENGS:
                getattr(block, engobj[ename])(lambda eng, ename=ename: run(ename, eng))


def _consts():
    s = np.arange(128)[:, None]
    t = np.arange(128)[None, :]
    same = (s // 64) == (t // 64)
    c = {}
    c["ident"] = np.eye(128, dtype=np.float32)
    c["ones"] = np.ones((128, 128), np.float32)
    c["triF"] = (same & (s <= t)).astype(np.float32)
    c["triB"] = (same & (s >= t)).astype(np.float32)
    c["sufF"] = (same & (s > t)).astype(np.float32)
    c["sufB"] = (same & (s < t)).astype(np.float32)
    c["sel0"] = np.repeat((s < 64).astype(np.float32), 128, axis=1)
    c["sel1"] = np.repeat((s >= 64).astype(np.float32), 128, axis=1)
    c["tF"] = (s <= t).astype(np.float32)
    c["tB"] = (s >= t).astype(np.float32)
    c["sF"] = (s > t).astype(np.float32)
    c["sB"] = (s < t).astype(np.float32)
    names = ["ident", "ones", "triF", "triB", "sufF", "sufB", "sel0", "sel1", "tF", "tB", "sF", "sB"]
    arr = np.stack([c[n] for n in names], axis=1)
    selE = np.zeros((128, NE, 128), np.float32)
    for e in range(NE):
        selE[e, e, :] = 1.0
    rst = np.ones((128, NT), np.float32)
    rst[:, ::128] = 0.0
    return names, np.ascontiguousarray(arr), selE, rst


CN, CARR, SELE, RST = _consts()
CI = {n: i for i, n in enumerate(CN)}


def _blocks(lo, hi, bs=512):
    out = []
    t = lo
    while t < hi:
        n = min(bs, hi - t)
        if t < CTX:
            n = min(n, CTX - t)
        out.append((t, n, 1 if t < CTX else 0))
        t += n
    return out


def build_program(phases=("mods", "mix0", "moe0", "mix1", "moe1", "final"), dump=None):
    nc = bass.Bass("TRN2", target_bir_lowering=False)
    es = ExitStack()
    P = Prog(nc)

    def din(name, shape, dt=F32):
        return nc.dram_tensor(name, list(shape), dt, kind="ExternalInput").ap()

    d_xT = din("xT", [128, 8, NT])
    d_cT = din("cT", [128, 8, 2])
    d_adaw = din("adaw", [2, 12, 128, 8, 512])
    d_adab = din("adab", [128, 2, 48])
    d_gmix = din("gmix", [128, 2, 8])
    d_gffn = din("gffn", [128, 2, 8])
    d_gfin = din("gfin", [128, 8])
    d_mwin = din("mwin", [128, 8, 3088])
    d_mconv = din("mconv", [128, 8, 4])
    d_mgb = din("mgb", [128, 16])
    d_mhg = din("mhg", [128, D])
    d_mwout = din("mwout", [128, 8, D])
    d_hwin = din("hwin", [128, 8, 5120])
    d_hlb = din("hlb", [128, 2, 16])
    d_hhg = din("hhg", [128, D])
    d_hwout = din("hwout", [128, 8, D])
    d_rw = din("rw", [128, 8, NE])
    d_rb = din("rb", [128, NTILE, NE])
    d_wg = din("ewg", [2, NE, 128, 8, DEXP])
    d_wu = din("ewu", [2, NE, 128, 8, DEXP])
    d_wd = din("ewd", [2, NE, 128, 4, D])
    d_cf = din("cf", [128, 12, 128])
    d_rst = din("rst", [128, NT])
    d_out = nc.dram_tensor("outT", [128, 8, SEQ], F32, kind="ExternalOutput").ap()
    d_dump = None
    if dump is not None:
        d_dump = nc.dram_tensor("dump", [128, 8, NT], F32, kind="ExternalOutput").ap()

    with es:
        def sb(name, shape, dt=F32):
            return es.enter_context(nc.sbuf_tensor("s_" + name, list(shape), dt))

        sT = sb("sT", [128, 8, NT])
        hT = sb("hT", [128, 8, NT], BF16)
        cf = sb("cf", [128, 12, 128])
        cb = sb("cb", [128, 12, 128], BF16)
        maskLE = sb("maskLE", [128, 128], BF16)
        maskGE = sb("maskGE", [128, 128], BF16)
        modT = [sb(f"modT{l}", [128, 48, 2]) for l in range(2)]
        adab = sb("adab", [128, 2, 48])
        gmix = sb("gmix", [128, 2, 8])
        gffn = sb("gffn", [128, 2, 8])
        gfin = sb("gfin", [128, 8])
        cT = sb("cT", [128, 8, 2])
        scT = sb("scT", [128, 8, 2])
        A1 = [sb(f"A1_{l}", [128, 8, 2]) for l in range(2)]
        A2 = [sb(f"A2_{l}", [128, 8, 2]) for l in range(2)]
        pb = [es.enter_context(nc.psum_tensor(f"pb{i}", [128, 512], F32)) for i in range(8)]
        PB = [f"pb{i}" for i in range(8)]

        ARENA = ((nc.sbuf_bytes_remaining - 2048) // 64) * 64
        arena = sb("arena", [128, ARENA // 4], F32)

        class Carver:
            def __init__(self):
                self.off = 0

            def take(self, shape, dt=F32):
                esz = 4 if dt == F32 else 2
                n = int(np.prod(shape[1:]))
                nbytes = ((n * esz + 63) // 64) * 64
                assert self.off + nbytes <= ARENA, (self.off, nbytes, ARENA)
                a = arena[:, self.off // 4:(self.off + nbytes) // 4]
                self.off += nbytes
                if dt != F32:
                    a = a.bitcast(dt)
                a = a[:, 0:n]
                if len(shape) == 3:
                    a = a.rearrange("p (a b) -> p a b", a=shape[1])
                elif len(shape) == 4:
                    a = a.rearrange("p (a b c) -> p a b c", a=shape[1], b=shape[2])
                return a

        dbg_outs = []

        def dbg(name, ap, shape, key, dt=F32):
            if dump is None or dump is True or name not in dump:
                return
            dd = nc.dram_tensor("dbg_" + name, list(shape), dt, kind="ExternalOutput").ap()
            dbg_outs.append(P.dma("sp", "dbg_" + name, lambda e: e.dma_start(out=dd, in_=ap), reads=[key]))

        def mm(out, lhsT, rhs, start, stop):
            return lambda e: e.matmul(out, lhsT=lhsT, rhs=rhs, start=start, stop=stop)

        for c in range(8):
            P.dma("sp", f"sT{c}", lambda e, c=c: e.dma_start(out=sT[:, c, :], in_=d_xT[:, c, :]), writes=[f"sT{c}"])
        P.dma("sp", "cf", lambda e: e.dma_start(out=cf[:], in_=d_cf), writes=["cf"])
        for nm, t, dsrc in (("adab", adab, d_adab), ("gmix", gmix, d_gmix), ("gffn", gffn, d_gffn),
                            ("gfin", gfin, d_gfin), ("cT", cT, d_cT)):
            P.dma("sp", nm, lambda e, t=t, dsrc=dsrc: e.dma_start(out=t[:], in_=dsrc), writes=[nm])
        P.op("dve", lambda e: e.tensor_copy(out=cb[:], in_=cf[:]), reads=["cf"], writes=["cb"])
        P.op("dve", lambda e: e.tensor_copy(out=maskLE[:], in_=cf[:, CI["triF"], :]), reads=["cf"], writes=["masks"])
        P.op("dve", lambda e: e.tensor_copy(out=maskGE[:], in_=cf[:, CI["triB"], :]), reads=["cf"], writes=["masks"])
        ident_b = cb[:, CI["ident"], :]
        ident_f = cf[:, CI["ident"], :]
        ones_f = cf[:, CI["ones"], :]
        SKEY = [f"sT{c}" for c in range(8)]

        MODS_BASE = ((ARENA - (4 * 16384 + 2 * 2048)) // 64) * 64
        if "mods" in phases:
            cv = Carver()
            cv.off = MODS_BASE
            NB_ = 4
            adaw = [cv.take([128, 8, 512]) for _ in range(NB_)]
            modrow = [cv.take([128, 512]) for _ in range(2)]
            P.op("act", lambda e: e.activation(out=scT[:], in_=cT[:], func=AF.Silu), reads=["cT"], writes=["scT"])
            for l in range(2):
                for nb in range(12):
                    bi = nb % NB_
                    mi = nb % 2
                    P.dma("sp" if nb % 2 == 0 else "act", f"adaw{bi}", lambda e, l=l, nb=nb, bi=bi: e.dma_start(out=adaw[bi], in_=d_adaw[l, nb]), writes=[f"arena_adaw{bi}"])
                    for kc in range(8):
                        P.op("pe", mm(pb[mi][0:2, :], scT[:, kc, :], adaw[bi][:, kc, :], kc == 0, kc == 7), reads=[f"arena_adaw{bi}", "scT"], writes=[PB[mi]])
                    P.op("dve", lambda e, mi=mi: e.tensor_copy(out=modrow[mi][0:2, :], in_=pb[mi][0:2, :]), writes=[PB[mi], f"arena_modrow{mi}"])
                    for j in range(4):
                        idx = nb * 4 + j
                        P.op("pe", lambda e, idx=idx, j=j, mi=mi: e.transpose(out=pb[2][:, 2 * idx:2 * idx + 2], in_=modrow[mi][0:2, j * 128:(j + 1) * 128], identity=cf[0:2, CI["ident"], 0:2]),
                             reads=[f"arena_modrow{mi}", "cf"], writes=[PB[2]])
                P.op("dve", lambda e, l=l: e.tensor_tensor(out=modT[l][:], in0=pb[2][:, 0:96].rearrange("p (a b) -> p a b", b=2),
                                                          in1=adab[:, l, :].unsqueeze(2).to_broadcast([128, 48, 2]), op=ALU.add),
                     reads=["adab"], writes=[PB[2], f"modT{l}"])
                for col in range(2):
                    P.op("dve", lambda e, l=l, col=col: e.scalar_tensor_tensor(
                        out=A1[l][:, :, col], in0=modT[l][:, 8:16, col], scalar=1.0, in1=gmix[:, l, :], op0=ALU.add, op1=ALU.mult),
                        reads=[f"modT{l}", "gmix"], writes=[f"A1_{l}"])
                    P.op("dve", lambda e, l=l, col=col: e.scalar_tensor_tensor(
                        out=A2[l][:, :, col], in0=modT[l][:, 32:40, col], scalar=1.0, in1=gffn[:, l, :], op0=ALU.add, op1=ALU.mult),
                        reads=[f"modT{l}", "gffn"], writes=[f"A2_{l}"])

        def SH1(l, c, col): return modT[l][:, 0 + c, col:col + 1]
        def G1(l, c, col): return modT[l][:, 16 + c, col:col + 1]
        def SH2(l, c, col): return modT[l][:, 24 + c, col:col + 1]
        def G2(l, c, col): return modT[l][:, 40 + c, col:col + 1]

        def norm_mod(cv, A, SH, l, lo, hi, out_bf, out_key, out_f32=None, perm_cols=False, after_block=None, bs=512, skey=None):
            if skey is None:
                skey = lambda c, t0: SKEY[c]
            sq = [cv.take([128, bs]) for _ in range(2)]
            rstd = cv.take([128, bs])
            tmp = [cv.take([128, bs]) for _ in range(2)]
            for bix, (t0, n, col) in enumerate(_blocks(lo, hi, bs)):
                o32 = out_f32[bix % 2] if out_f32 is not None else None
                okey = out_key(t0) if callable(out_key) else out_key
                k32 = "nm_h32_0"
                for c in range(8):
                    P.op("act", lambda e, c=c, t0=t0, n=n: e.activation(out=sq[c % 2][:, 0:n], in_=sT[:, c, t0:t0 + n], func=AF.Square),
                         reads=[skey(c, t0)], writes=[f"nm_sq{c % 2}"])
                    P.op("pe", mm(pb[7][:, 0:n], ones_f, sq[c % 2][:, 0:n], c == 0, c == 7), reads=[f"nm_sq{c % 2}", "cf"], writes=[PB[7]])
                P.op("dve", lambda e, n=n: e.tensor_scalar(out=rstd[:, 0:n], in0=pb[7][:, 0:n], scalar1=1.0 / D, scalar2=EPS,
                                                          op0=ALU.mult, op1=ALU.add), reads=[PB[7]], writes=["nm_rstd"])
                P.op("act", lambda e, n=n: e.activation(out=rstd[:, 0:n], in_=rstd[:, 0:n], func=AF.Sqrt), reads=["nm_rstd"], writes=["nm_rstd"])
                P.op("dve", lambda e, n=n: e.reciprocal(out=rstd[:, 0:n], in_=rstd[:, 0:n]), reads=["nm_rstd"], writes=["nm_rstd"])
                for c in range(8):
                    tp = tmp[c % 2]
                    P.op("dve", lambda e, c=c, t0=t0, n=n, tp=tp: e.tensor_tensor(out=tp[:, 0:n], in0=sT[:, c, t0:t0 + n], in1=rstd[:, 0:n], op=ALU.mult),
                         reads=[skey(c, t0), "nm_rstd"], writes=[f"nm_tmp{c % 2}"])
                    src = tp[:, 0:n]
                    if out_f32 is not None:
                        dst, dkey = o32[:, c, 0:n], k32
                    else:
                        dst, dkey = out_bf[:, c, t0:t0 + n], okey
                        if perm_cols and t0 >= CTX:
                            r0, nr = (t0 - CTX) // 64, n // 64
                            dst = out_bf[:, c, CTX:NT].rearrange("p (col row) -> p row col", row=32)[:, r0:r0 + nr, :]
                            src = src.rearrange("p (r c) -> p r c", c=64)
                    P.op("dve", lambda e, c=c, col=col, dst=dst, src=src: e.tensor_scalar(
                        out=dst, in0=src, scalar1=A[l][:, c, col:col + 1], scalar2=SH(l, c, col), op0=ALU.mult, op1=ALU.add),
                        reads=[f"nm_tmp{c % 2}", f"A1_{l}", f"A2_{l}", f"modT{l}"], writes=[dkey])
                    if out_f32 is not None:
                        P.op("act", lambda e, c=c, t0=t0, n=n, o32=o32: e.copy(out=out_bf[:, c, t0:t0 + n], in_=o32[:, c, 0:n]), reads=[k32], writes=[okey])
                if after_block is not None:
                    after_block(t0, n)

        def moe(l, lo, hi):
            P.barrier()
            cv = Carver()
            h32s = [cv.take([128, 8, 256])] * 2
            rw = cv.take([128, 8, NE])
            rb = cv.take([128, NTILE, NE])
            cw_all = cv.take([128, NTILE, NE])
            NTN = NTILE * NE
            rt = [cv.take([128, NTILE, NE]) for _ in range(5)]
            r4 = [cv.take([128, NTILE * 4]) for _ in range(4)]
            r1 = [cv.take([128, NTILE]) for _ in range(2)]
            wgb = [cv.take([128, 8, DEXP], BF16) for _ in range(2)]
            wub = [cv.take([128, 8, DEXP], BF16) for _ in range(2)]
            wdb = [cv.take([128, 4, D], BF16) for _ in range(2)]
            aT = [cv.take([128, 4, 512], BF16) for _ in range(2)]
            sg = [cv.take([128, 512], BF16) for _ in range(2)]
            t1 = [cv.take([128, 512], BF16) for _ in range(2)]
            cwb = [cv.take([128, 512], BF16) for _ in range(2)]
            P.dma("sp", "rw", lambda e: e.dma_start(out=rw, in_=d_rw), writes=["moe_rw"])
            P.dma("sp", "rb", lambda e: e.dma_start(out=rb, in_=d_rb), writes=["moe_rb"])
            T0, T1 = lo // 128, hi // 128
            s_all = rt[0]
            blk_ctr = [0]

            def route(t0, n):
                hb = 0
                for sub in range(n // 128):
                    tix = (t0 + sub * 128) // 128
                    pl, kl = pb[7], PB[7]
                    for c in range(8):
                        P.op("pe", mm(pl[:, 256:256 + NE], h32s[hb][:, c, sub * 128:(sub + 1) * 128], rw[:, c, :], c == 0, c == 7),
                             reads=[f"nm_h32_{hb}", "moe_rw"], writes=[kl])
                    P.op("act", lambda e, tix=tix, pl=pl: e.activation(out=s_all[:, tix, :], in_=pl[:, 256:256 + NE], func=AF.Sigmoid), writes=[kl, f"rt_s{tix}"])

            sel_, sel2_, eq1, eq2 = rt[1:5]
            w_ = sel2_
            m1, m2, gs, geq = r4
            bm, ws = r1
            cw16 = cw_all

            def route_batch(Ta, Tb):
                TS = slice(Ta, Tb)
                nT = Tb - Ta
                K = lambda nm: f"{nm}{Ta}"
                v3 = lambda a: a[:, TS, :].rearrange("p t (g k) -> p (t g) k", k=4)
                g2 = lambda a: a[:, Ta * 4:Tb * 4]
                bc4 = lambda a: g2(a).unsqueeze(2).to_broadcast([128, nT * 4, 4])
                g3 = lambda a: g2(a).rearrange("p (t g) -> p t g", g=4)
                skeys = [f"rt_s{t_}" for t_ in range(Ta, Tb)]
                P.op("dve", lambda e: e.tensor_tensor(out=sel_[:, TS, :], in0=s_all[:, TS, :], in1=rb[:, TS, :], op=ALU.add), reads=skeys + ["moe_rb"], writes=[K("rt_sel")])
                P.op("dve", lambda e: e.tensor_reduce(out=g2(m1), in_=v3(sel_), axis=AX.X, op=ALU.max), reads=[K("rt_sel")], writes=[K("rt_m1")])
                P.op("dve", lambda e: e.tensor_tensor(out=v3(eq1), in0=v3(sel_), in1=bc4(m1), op=ALU.is_equal), reads=[K("rt_sel"), K("rt_m1")], writes=[K("rt_eq1")])
                P.op("dve", lambda e: e.scalar_tensor_tensor(out=sel2_[:, TS, :], in0=eq1[:, TS, :], scalar=-1e9, in1=sel_[:, TS, :], op0=ALU.mult, op1=ALU.add),
                     reads=[K("rt_eq1"), K("rt_sel")], writes=[K("rt_sel2")])
                P.op("dve", lambda e: e.tensor_reduce(out=g2(m2), in_=v3(sel2_), axis=AX.X, op=ALU.max), reads=[K("rt_sel2")], writes=[K("rt_m2")])
                P.op("dve", lambda e: e.tensor_tensor(out=v3(eq2), in0=v3(sel2_), in1=bc4(m2), op=ALU.is_equal), reads=[K("rt_sel2"), K("rt_m2")], writes=[K("rt_eq2")])
                P.op("dve", lambda e: e.tensor_tensor(out=g2(gs), in0=g2(m1), in1=g2(m2), op=ALU.add), reads=[K("rt_m1"), K("rt_m2")], writes=[K("rt_gs")])
                P.op("dve", lambda e: e.tensor_reduce(out=bm[:, TS], in_=g3(gs), axis=AX.X, op=ALU.max), reads=[K("rt_gs")], writes=[K("rt_bm")])
                P.op("dve", lambda e: e.tensor_tensor(out=g3(geq), in0=g3(gs), in1=bm[:, TS].unsqueeze(2).to_broadcast([128, nT, 4]), op=ALU.is_equal),
                     reads=[K("rt_gs"), K("rt_bm")], writes=[K("rt_geq")])
                P.op("dve", lambda e: e.tensor_tensor(out=eq1[:, TS, :], in0=eq1[:, TS, :], in1=eq2[:, TS, :], op=ALU.add), reads=[K("rt_eq2")], writes=[K("rt_eq1")])
                P.op("dve", lambda e: e.tensor_tensor(out=v3(eq1), in0=v3(eq1), in1=bc4(geq), op=ALU.mult), reads=[K("rt_geq")], writes=[K("rt_eq1")])
                P.op("dve", lambda e: e.tensor_tensor(out=w_[:, TS, :], in0=eq1[:, TS, :], in1=s_all[:, TS, :], op=ALU.mult),
                     reads=[K("rt_eq1"), K("rt_eq2"), K("rt_m2")] + skeys, writes=[K("rt_w"), K("rt_sel2")])
                P.op("dve", lambda e: e.tensor_reduce(out=ws[:, TS], in_=w_[:, TS, :], axis=AX.X, op=ALU.add), reads=[K("rt_w")], writes=[K("rt_ws")])
                P.op("dve", lambda e: e.reciprocal(out=ws[:, TS], in_=ws[:, TS]), writes=[K("rt_ws")])
                P.op("dve", lambda e: e.tensor_tensor(out=cw16[:, TS, :], in0=w_[:, TS, :], in1=ws[:, TS].unsqueeze(2).to_broadcast([128, nT, NE]), op=ALU.mult),
                     reads=[K("rt_w"), K("rt_ws")], writes=[f"moe_cw{t_}" for t_ in range(Ta, Tb)])

            def after_blk(t0, n):
                route(t0, n)
                t1_ = t0 + n
                for (bt0, bn, _c) in _blocks(lo, hi):
                    if bt0 + bn == t1_:
                        route_batch(bt0 // 128, (bt0 + bn) // 128)

            norm_mod(cv, A2, SH2, l, lo, hi, out_bf=hT, out_key=lambda t0: f"hT_{t0 // 256}", out_f32=h32s, after_block=after_blk, bs=256,
                     skey=lambda c, t0: f"sT{c}_{t0 // 256}")

            blocks = _blocks(lo, hi)
            items = [(e_, b_) for e_ in range(NE) for b_ in range(len(blocks))]

            def load_w(e_):
                bi = e_ % 2
                P.dma("pool", f"wg{bi}", lambda e: e.dma_start(out=wgb[bi], in_=d_wg[l, e_], max_dma_last_dim=4096), writes=[f"moe_wg{bi}"])
                P.dma("pool", f"wu{bi}", lambda e: e.dma_start(out=wub[bi], in_=d_wu[l, e_], max_dma_last_dim=4096), writes=[f"moe_wu{bi}"])
                P.dma("pool", f"wd{bi}", lambda e: e.dma_start(out=wdb[bi], in_=d_wd[l, e_], max_dma_last_dim=4096), writes=[f"moe_wd{bi}"])

            cwcol = [cv.take([128, 128], BF16) for _ in range(4)]
            cwc_ctr = [0]

            def stage1(i):
                e_, b_ = items[i]
                t0, n, col = blocks[b_]
                bi = e_ % 2
                ai = i % 2
                hkeys = [f"hT_{k_}" for k_ in range(t0 // 256, (t0 + n + 255) // 256)]
                for sub in range(n // 128):
                    tix = (t0 + sub * 128) // 128
                    ci_ = cwc_ctr[0] % 4
                    cwc_ctr[0] += 1
                    P.op("dve", lambda e, tix=tix, ci_=ci_: e.tensor_copy(out=cwcol[ci_], in_=cw16[:, tix, e_:e_ + 1].to_broadcast([128, 128])),
                         reads=[f"moe_cw{tix}"], writes=[f"moe_cwcol{ci_}"])
                    P.op("pe", mm(pb[6][:, sub * 128:(sub + 1) * 128], cwcol[ci_], ident_b, True, True),
                         reads=[f"moe_cwcol{ci_}", "cb"], writes=[PB[6]])
                P.op("act", lambda e: e.copy(out=cwb[ai][:, 0:n], in_=pb[6][:, 0:n]), reads=[PB[6]], writes=[f"moe_cwb{ai}"])
                for fc in range(4):
                    pg, pu = pb[(fc % 2) * 2], pb[(fc % 2) * 2 + 1]
                    kg, ku = PB[(fc % 2) * 2], PB[(fc % 2) * 2 + 1]
                    for c in range(8):
                        P.op("pe", mm(pg[:, 0:n], wgb[bi][:, c, fc * 128:(fc + 1) * 128], hT[:, c, t0:t0 + n], c == 0, c == 7),
                             reads=[f"moe_wg{bi}"] + hkeys, writes=[kg])
                    for c in range(8):
                        P.op("pe", mm(pu[:, 0:n], wub[bi][:, c, fc * 128:(fc + 1) * 128], hT[:, c, t0:t0 + n], c == 0, c == 7),
                             reads=[f"moe_wu{bi}"] + hkeys, writes=[ku])
                    si = fc % 2
                    P.op("act", lambda e, pg=pg, si=si: e.activation(out=sg[si][:, 0:n], in_=pg[:, 0:n], func=AF.Silu), reads=[kg], writes=[f"moe_sg{si}"])
                    P.op("dve", lambda e, pu=pu, si=si: e.tensor_tensor(out=t1[si][:, 0:n], in0=sg[si][:, 0:n], in1=pu[:, 0:n], op=ALU.mult),
                         reads=[f"moe_sg{si}", ku], writes=[f"moe_t1{si}"])
                    P.op("pool", lambda e, si=si, fc=fc: e.tensor_tensor(out=aT[ai][:, fc, 0:n], in0=t1[si][:, 0:n], in1=cwb[ai][:, 0:n], op=ALU.mult),
                         reads=[f"moe_t1{si}", f"moe_cwb{ai}"], writes=[f"moe_aT{ai}"])

            def stage2(i):
                e_, b_ = items[i]
                t0, n, col = blocks[b_]
                bi = e_ % 2
                ai = i % 2
                for dc in range(8):
                    pd, kd = pb[4 + dc % 2], PB[4 + dc % 2]
                    for fc in range(4):
                        P.op("pe", mm(pd[:, 0:n], wdb[bi][:, fc, dc * 128:(dc + 1) * 128], aT[ai][:, fc, 0:n], fc == 0, fc == 3),
                             reads=[f"moe_wd{bi}", f"moe_aT{ai}"], writes=[kd])
                    sk = [f"sT{dc}_{k_}" for k_ in range(t0 // 256, (t0 + n + 255) // 256)]
                    P.op("dve", lambda e, dc=dc, pd=pd: e.scalar_tensor_tensor(
                        out=sT[:, dc, t0:t0 + n], in0=pd[:, 0:n], scalar=G2(l, dc, col), in1=sT[:, dc, t0:t0 + n], op0=ALU.mult, op1=ALU.add),
                        reads=[kd, f"modT{l}"] + sk, writes=sk)

            load_w(0)
            for i in range(len(items)):
                e_, b_ = items[i]
                stage1(i)
                if i > 0:
                    stage2(i - 1)
                if b_ == 0 and e_ + 1 < NE:
                    load_w(e_ + 1)
            stage2(len(items) - 1)

        def mlstm(l):
            cv = Carver()
            wgt = cv.take([128, 8, 16], BF16)
            mgb = cv.take([128, 16])
            mhg = cv.take([128, 256])
            cvw = cv.take([128, 8, 4])
            G = cv.take([128, NTILE, 16])
            Lg = cv.take([128, NTILE, 8])
            EE = cv.take([128, NTILE, 24])
            EB = cv.take([128, NTILE, 8])
            arg = cv.take([128, 24])
            P.dma("pool", "m_wgt", lambda e: e.dma_start(out=wgt, in_=d_mwin[:, :, 3072:3088]), writes=["m_wgt"])
            P.dma("sp", "m_mgb", lambda e: e.dma_start(out=mgb, in_=d_mgb), writes=["m_mgb"])
            P.dma("sp", "m_cvw", lambda e: e.dma_start(out=cvw, in_=d_mconv), writes=["m_cvw"])

            norm_mod(cv, A1, SH1, l, 0, NT, out_bf=hT, out_key="hT", bs=256)

            for tl in range(NTILE):
                ts_ = slice(tl * 128, (tl + 1) * 128)
                for c in range(8):
                    P.op("pe", mm(pb[0][:, 0:16], hT[:, c, ts_], wgt[:, c, :], c == 0, c == 7), reads=["hT", "m_wgt"], writes=[PB[0]])
                P.op("dve", lambda e, tl=tl: e.tensor_tensor(out=G[:, tl, :], in0=pb[0][:, 0:16], in1=mgb, op=ALU.add), reads=[PB[0], "m_mgb"], writes=["m_G"])
                for k, src in enumerate((slice(4, 8), slice(12, 16))):
                    P.op("act", lambda e, tl=tl, k=k, src=src: e.activation(out=Lg[:, tl, 4 * k:4 * k + 4], in_=G[:, tl, src], func=AF.Exp, scale=-1.0),
                         reads=["m_G"], writes=["m_L"])
                P.op("dve", lambda e, tl=tl: e.tensor_scalar(out=Lg[:, tl, :], in0=Lg[:, tl, :], scalar1=1.0, scalar2=None, op0=ALU.add), reads=["m_L"], writes=["m_L"])
                P.op("act", lambda e, tl=tl: e.activation(out=Lg[:, tl, :], in_=Lg[:, tl, :], func=AF.Ln), reads=["m_L"], writes=["m_L"])
                q_ = pb[1]
                for j, (mat, cs) in enumerate((("tF", slice(0, 4)), ("tB", slice(4, 8)), ("sF", slice(0, 4)), ("sB", slice(4, 8)))):
                    P.op("pe", mm(q_[:, 4 * j:4 * j + 4], cf[:, CI[mat], :], Lg[:, tl, cs], True, True), reads=["cf", "m_L"], writes=[PB[1]])
                P.op("pe", mm(q_[:, 16:24], cf[:, CI["ones"], :], Lg[:, tl, :], True, True), reads=["cf", "m_L"], writes=[PB[1]])
                P.op("dve", lambda e, tl=tl: e.tensor_tensor(out=arg[:, 0:4], in0=G[:, tl, 0:4], in1=q_[:, 0:4], op=ALU.add), reads=["m_G"], writes=["m_arg", PB[1]])
                P.op("dve", lambda e, tl=tl: e.tensor_tensor(out=arg[:, 4:8], in0=G[:, tl, 0:4], in1=q_[:, 8:12], op=ALU.subtract), reads=["m_G"], writes=["m_arg", PB[1]])
                P.op("dve", lambda e, tl=tl: e.tensor_tensor(out=arg[:, 8:12], in0=G[:, tl, 8:12], in1=q_[:, 4:8], op=ALU.add), reads=["m_G"], writes=["m_arg", PB[1]])
                P.op("dve", lambda e, tl=tl: e.tensor_tensor(out=arg[:, 12:16], in0=G[:, tl, 8:12], in1=q_[:, 12:16], op=ALU.subtract), reads=["m_G"], writes=["m_arg", PB[1]])
                P.op("dve", lambda e, tl=tl: e.tensor_copy(out=arg[:, 16:24], in_=q_[:, 0:8]), writes=["m_arg", PB[1]])
                P.op("act", lambda e, tl=tl: e.activation(out=EE[:, tl, :], in_=arg, func=AF.Exp), reads=["m_arg"], writes=["m_EE"])
                P.op("act", lambda e, tl=tl: e.activation(out=EB[:, tl, :], in_=q_[:, 16:24], func=AF.Exp, scale=-1.0), writes=["m_EB", PB[1]])

            assert cv.off <= MODS_BASE, (cv.off, MODS_BASE)
            P.barrier()
            wqk = cv.take([128, 8, 2, 128], BF16)
            wv = cv.take([128, 8, 256], BF16)
            wo = cv.take([128, 8, 256], BF16)
            wout = cv.take([128, 2, D], BF16)
            qT = cv.take([128, NT], BF16)
            kT = cv.take([128, NT], BF16)
            vaug = cv.take([128, NTILE, 257], BF16)
            ktok = cv.take([128, NTILE, 128], BF16)
            hfirst = cv.take([128, NTILE, 256], BF16)
            ybuf = [cv.take([128, 512]) for _ in range(2)]
            cvs = cv
            STf = [[cvs.take([128, 128], BF16) for _ in range(2)] for _ in range(2)]
            vt = [[cvs.take([128, 257], BF16) for _ in range(2)] for _ in range(2)]
            vt2 = [[cvs.take([128, 257], BF16) for _ in range(2)] for _ in range(2)]
            X = [cvs.take([128, 257]) for _ in range(2)]
            Xb = [[cvs.take([128, 257], BF16) for _ in range(2)] for _ in range(2)]
            numS = [[cvs.take([128, 257]) for _ in range(2)] for _ in range(2)]
            hs = [cvs.take([128, 256]) for _ in range(2)]
            hn = hs
            hg = [cvs.take([128, 256], BF16) for _ in range(2)]
            sgo = [cvs.take([128, 256]) for _ in range(2)]
            tmpo = [cvs.take([128, 4, 128]) for _ in range(2)]
            sm = [[cvs.take([128, 1]) for _ in range(2)] for _ in range(2)]
            xTt = [cvs.take([128, 2, 128], BF16) for _ in range(2)]
            P.op("pool", lambda e: e.memset(vaug[:, :, 256:257], 1.0), writes=["m_vaug"])
            pb6b = pb[6].bitcast(BF16)

            for h in range(4):
                P.dma("sp", "m_mhg", lambda e, h=h: e.dma_start(out=mhg, in_=d_mhg[:, h * 256:(h + 1) * 256]), writes=["m_mhg"])
                P.dma("pool", "m_wqk", lambda e, h=h: [e.dma_start(out=wqk[:, :, 0, :], in_=d_mwin[:, :, h * 128:(h + 1) * 128]),
                                                       e.dma_start(out=wqk[:, :, 1, :], in_=d_mwin[:, :, 512 + h * 128:512 + (h + 1) * 128])],
                      writes=["m_wqk"], n=2)
                P.dma("pool", "m_wv", lambda e, h=h: e.dma_start(out=wv, in_=d_mwin[:, :, 1024 + h * 256:1024 + (h + 1) * 256]), writes=["m_wv"])
                P.dma("pool", "m_wo", lambda e, h=h: e.dma_start(out=wo, in_=d_mwin[:, :, 2048 + h * 256:2048 + (h + 1) * 256]), writes=["m_wo"])
                P.dma("pool", "m_wout", lambda e, h=h: e.dma_start(out=wout, in_=d_mwout[:, 2 * h:2 * h + 2, :], max_dma_last_dim=4096), writes=["m_wout"])
                qk_blocks = [(0, CTX, 0, CTX)] + [(CTX + 410 * j, min(CTX + 410 * (j + 1), NT), CTX, NT) for j in range(5)]
                cnt_ = [0]
                for which, dstT, dkey in ((0, qT, "m_qT"), (1, kT, "m_kT")):
                    ch = (0 if which == 0 else 4) + h
                    for (a_, b_, A_, B_) in qk_blocks:
                        n = b_ - a_
                        a2, b2 = max(a_ - 1, A_), min(b_ + 1, B_)
                        off = a_ - a2
                        N_ = b2 - a2
                        bi_ = cnt_[0] % 2
                        cnt_[0] += 1
                        pq, kq = pb[bi_], PB[bi_]
                        yb, ky = ybuf[bi_], f"m_y{bi_}"
                        for c in range(8):
                            P.op("pe", mm(pq[:, 0:N_], wqk[:, c, which, :], hT[:, c, a2:b2], c == 0, c == 7), reads=["m_wqk", "hT"], writes=[kq])
                        P.op("dve", lambda e, pq=pq, yb=yb, off=off, n=n, ch=ch: e.tensor_scalar(out=yb[:, 0:n], in0=pq[:, off:off + n], scalar1=cvw[:, ch, 1:2], scalar2=cvw[:, ch, 3:4],
                                                                                       op0=ALU.mult, op1=ALU.add), reads=["m_cvw"], writes=[kq, ky])
                        if off == 1:
                            o0, i0, m0 = 0, 0, n
                        else:
                            o0, i0, m0 = 1, 0, n - 1
                        P.op("dve", lambda e, pq=pq, yb=yb, o0=o0, i0=i0, m0=m0, ch=ch: e.scalar_tensor_tensor(
                            out=yb[:, o0:o0 + m0], in0=pq[:, i0:i0 + m0], scalar=cvw[:, ch, 0:1], in1=yb[:, o0:o0 + m0], op0=ALU.mult, op1=ALU.add),
                            reads=["m_cvw"], writes=[kq, ky])
                        m2 = n if b2 > b_ else n - 1
                        P.op("dve", lambda e, pq=pq, yb=yb, off=off, m2=m2, ch=ch: e.scalar_tensor_tensor(
                            out=yb[:, 0:m2], in0=pq[:, off + 1:off + 1 + m2], scalar=cvw[:, ch, 2:3], in1=yb[:, 0:m2], op0=ALU.mult, op1=ALU.add),
                            reads=["m_cvw"], writes=[kq, ky])
                        if which == 0:
                            P.op("act", lambda e, yb=yb, n=n: e.activation(out=yb[:, 0:n], in_=yb[:, 0:n], func=AF.Silu), writes=[ky])
                            P.op("act", lambda e, yb=yb, n=n, a_=a_, b_=b_: e.activation(out=qT[:, a_:b_], in_=yb[:, 0:n], func=AF.Copy, scale=float(128 ** -0.5)),
                                 reads=[ky], writes=[dkey])
                        else:
                            P.op("act", lambda e, yb=yb, n=n, a_=a_, b_=b_: e.activation(out=kT[:, a_:b_], in_=yb[:, 0:n], func=AF.Silu), reads=[ky], writes=[dkey])
                for tl in range(NTILE):
                    ts_ = slice(tl * 128, (tl + 1) * 128)
                    pv, kv_ = pb[2 + tl % 2], PB[2 + tl % 2]
                    for c in range(8):
                        P.op("pe", mm(pv[:, 0:256], hT[:, c, ts_], wv[:, c, :], c == 0, c == 7), reads=["hT", "m_wv"], writes=[kv_])
                    P.op("act", lambda e, tl=tl, pv=pv: e.copy(out=vaug[:, tl, 0:256], in_=pv[:, 0:256]), reads=[kv_], writes=["m_vaug"])
                    tb = pb[4 + tl % 2].bitcast(BF16)
                    P.op("pe", lambda e, ts_=ts_, tb=tb: e.transpose(out=tb[:, 0:128], in_=kT[:, ts_], identity=ident_b), reads=["m_kT", "cb"], writes=[PB[4 + tl % 2]])
                    P.op("act", lambda e, tl=tl, tb=tb: e.copy(out=ktok[:, tl, :], in_=tb[:, 0:128]), reads=[PB[4 + tl % 2]], writes=["m_ktok"])

                orders = [list(range(NTILE)), [1, 0] + list(range(NTILE - 1, 1, -1))]
                if DEBUG_NT is not None:
                    orders = [o_[:DEBUG_NT] for o_ in orders]
                visited = set()
                for d_ in range(2):
                    P.op("dve", lambda e, d_=d_: e.memset(X[d_], 0.0), writes=[f"m_X{d_}"])
                    P.op("dve", lambda e, d_=d_: e.memset(Xb[d_][0], 0.0), writes=[f"m_Xb{d_}_0"])
                ncomp = [0]

                def step(d_, it, tl):
                    ts_ = slice(tl * 128, (tl + 1) * 128)
                    pi = it % 2
                    eoff = 0 if d_ == 0 else 8
                    doff = 4 if d_ else 0
                    second = tl in visited
                    visited.add(tl)
                    sc_cols = slice(128 * d_, 128 * d_ + 128)
                    P.op("pe", mm(pb[6][:, sc_cols], kT[:, ts_], qT[:, ts_], True, True), reads=["m_kT", "m_qT"], writes=[PB[6]])
                    msk = cb[:, CI["tF"], :] if d_ == 0 else cb[:, CI["tB"], :]
                    P.op("dve", lambda e: e.tensor_tensor(out=STf[d_][pi], in0=pb[6][:, sc_cols], in1=msk, op=ALU.mult),
                         reads=["cb"], writes=[PB[6], f"m_STf{d_}{pi}"])
                    e1 = EE[:, tl, eoff + h:eoff + h + 1]
                    e2 = EE[:, tl, eoff + 4 + h:eoff + 5 + h]
                    P.op("act", lambda e: e.activation(out=vt[d_][pi], in_=vaug[:, tl, :], func=AF.Copy, scale=e1),
                         reads=["m_vaug", "m_EE"], writes=[f"m_vt{d_}{pi}"])
                    P.op("dve", lambda e: e.tensor_scalar(out=vt2[d_][pi], in0=vaug[:, tl, :], scalar1=e2, scalar2=None, op0=ALU.mult),
                         reads=["m_vaug", "m_EE"], writes=[f"m_vt2{d_}{pi}"])
                    if second:
                        for c in range(8):
                            P.op("pe", mm(pb[7][:, 0:256], hT[:, c, ts_], wo[:, c, :], c == 0, c == 7), reads=["hT", "m_wo"], writes=[PB[7]])
                        P.op("act", lambda e: e.activation(out=sgo[d_], in_=pb[7][:, 0:256], func=AF.Exp, scale=-1.0), writes=[PB[7], f"m_sgo{d_}"])
                        P.op("dve", lambda e: e.tensor_scalar(out=sgo[d_], in0=sgo[d_], scalar1=1.0, scalar2=None, op0=ALU.add), writes=[f"m_sgo{d_}"])
                        P.op("act", lambda e: e.activation(out=sgo[d_], in_=sgo[d_], func=AF.Ln), writes=[f"m_sgo{d_}"])
                        P.op("act", lambda e: e.activation(out=sgo[d_], in_=sgo[d_], func=AF.Exp, scale=-1.0), writes=[f"m_sgo{d_}"])
                    num, knum = pb[d_], PB[d_]
                    P.op("pe", mm(num[:, 0:257], STf[d_][pi], vt[d_][pi], True, False), reads=[f"m_STf{d_}{pi}", f"m_vt{d_}{pi}"], writes=[knum])
                    kv, kkv = pb[2 + d_], PB[2 + d_]
                    P.op("pe", mm(kv[:, 0:257], ktok[:, tl, :], vt2[d_][pi], True, True), reads=["m_ktok", f"m_vt2{d_}{pi}"], writes=[kkv])
                    r0, r1 = it % 2, (it + 1) % 2
                    P.op("pe", mm(num[:, 0:257], qT[:, ts_], Xb[d_][r0], False, True), reads=["m_qT", f"m_Xb{d_}_{r0}", knum], writes=[knum])
                    ebc = EB[:, tl, doff + h:doff + h + 1]
                    P.op("dve", lambda e: e.scalar_tensor_tensor(out=X[d_], in0=X[d_], scalar=ebc, in1=kv[:, 0:257], op0=ALU.mult, op1=ALU.add),
                         reads=["m_EB", f"m_X{d_}"], writes=[kkv, f"m_X{d_}"])
                    P.op("act", lambda e: e.copy(out=Xb[d_][r1], in_=X[d_]), reads=[f"m_X{d_}"], writes=[f"m_Xb{d_}_{r1}"])
                    ns, kns = numS[d_][pi], f"m_numS{d_}{pi}"
                    P.op("act", lambda e: e.copy(out=ns, in_=num[:, 0:257]), writes=[knum, kns])
                    thr = EE[:, tl, 16 + doff + h:16 + doff + h + 1]
                    s0, ks0 = sm[d_][0], f"m_sm{d_}0"
                    P.op("act", lambda e: e.activation(out=s0, in_=ns[:, 256:257], func=AF.Abs), reads=[kns], writes=[ks0])
                    P.op("dve", lambda e: e.tensor_tensor(out=s0, in0=s0, in1=thr, op=ALU.max), reads=[ks0, "m_EE"], writes=[ks0])
                    P.op("dve", lambda e: e.reciprocal(out=s0, in_=s0), reads=[ks0], writes=[ks0])
                    if not second:
                        P.op("dve", lambda e: e.tensor_scalar(out=hfirst[:, tl, :], in0=ns[:, 0:256], scalar1=s0[:, 0:1], scalar2=None, op0=ALU.mult),
                             reads=[kns, ks0], writes=[f"m_hf{tl}"])
                        return
                    s1, ks1 = sm[d_][1], f"m_sm{d_}1"
                    P.op("dve", lambda e: e.scalar_tensor_tensor(out=hs[d_], in0=ns[:, 0:256], scalar=s0[:, 0:1], in1=hfirst[:, tl, :], op0=ALU.mult, op1=ALU.add),
                         reads=[kns, ks0, f"m_hf{tl}"], writes=[f"m_hs{d_}"])
                    P.op("act", lambda e: e.activation(out=hg[d_], in_=hs[d_], func=AF.Square, accum_out=s1), reads=[f"m_hs{d_}"], writes=[f"m_hg{d_}", ks1])
                    P.op("dve", lambda e: e.tensor_scalar(out=s1, in0=s1, scalar1=1.0 / 256, scalar2=EPS, op0=ALU.mult, op1=ALU.add), reads=[ks1], writes=[ks1])
                    P.op("act", lambda e: e.activation(out=s1, in_=s1, func=AF.Ln), reads=[ks1], writes=[ks1])
                    P.op("act", lambda e: e.activation(out=s1, in_=s1, func=AF.Exp, scale=-0.5), reads=[ks1], writes=[ks1])
                    P.op("dve", lambda e: e.scalar_tensor_tensor(out=hs[d_], in0=hs[d_], scalar=s1[:, 0:1], in1=mhg, op0=ALU.mult, op1=ALU.mult),
                         reads=[f"m_hs{d_}", ks1, "m_mhg"], writes=[f"m_hs{d_}"])
                    P.op("dve", lambda e: e.tensor_tensor(out=hg[d_], in0=hs[d_], in1=sgo[d_], op=ALU.mult), reads=[f"m_hs{d_}", f"m_sgo{d_}", f"m_hg{d_}"], writes=[f"m_hg{d_}"])
                    for fc in range(2):
                        P.op("pe", lambda e, fc=fc: e.transpose(out=pb6b[:, 512 + fc * 128:512 + (fc + 1) * 128], in_=hg[d_][:, fc * 128:(fc + 1) * 128], identity=ident_b),
                             reads=[f"m_hg{d_}", "cb"], writes=[PB[6]])
                    xi = ncomp[0] % 2
                    ncomp[0] += 1
                    P.op("act", lambda e: e.copy(out=xTt[xi], in_=pb6b[:, 512:768].rearrange("p (a b) -> p a b", a=2)), writes=[PB[6], f"m_xT{xi}"])
                    col = 1 if tl < 2 else 0
                    for half in range(2):
                        po, ko = pb[4 + half], PB[4 + half]
                        for j in range(4):
                            dc = half * 4 + j
                            for fc in range(2):
                                P.op("pe", mm(po[:, j * 128:(j + 1) * 128], wout[:, fc, dc * 128:(dc + 1) * 128], xTt[xi][:, fc, :], fc == 0, fc == 1), reads=["m_wout", f"m_xT{xi}"], writes=[ko])
                        g1b = modT[l][:, 16 + half * 4:16 + half * 4 + 4, col:col + 1].to_broadcast([128, 4, 128])
                        P.op("dve", lambda e, po=po, g1b=g1b, half=half: e.tensor_tensor(out=tmpo[half], in0=po[:, :].rearrange("p (a b) -> p a b", a=4), in1=g1b, op=ALU.mult),
                             reads=[f"modT{l}"], writes=[ko, f"m_tmpo{half}"])
                        keys = SKEY[half * 4:half * 4 + 4]
                        P.op("dve", lambda e, half=half: e.tensor_tensor(out=sT[:, half * 4:half * 4 + 4, ts_], in0=sT[:, half * 4:half * 4 + 4, ts_], in1=tmpo[half], op=ALU.add),
                             reads=[f"m_tmpo{half}"] + keys, writes=keys)

                for it in range(len(orders[0])):
                    step(0, it, orders[0][it])
                    step(1, it, orders[1][it])

        def hgrn(l):
            P.barrier()
            cv = Carver()
            hlb = cv.take([128, 2, 16])
            lbT = cv.take([128, 16])
            omlb = cv.take([128, 16])
            hhg = cv.take([128, 128])
            rst = cv.take([128, 512])
            P.dma("sp", "h_hlb", lambda e: e.dma_start(out=hlb, in_=d_hlb), writes=["h_hlb"])
            P.dma("sp", "h_rst", lambda e: e.dma_start(out=rst, in_=d_rst[:, 0:512]), writes=["h_rst"])
            P.op("dve", lambda e: e.tensor_tensor(out=lbT, in0=hlb[:, 1, :], in1=hlb[:, 0, :], op=ALU.subtract), reads=["h_hlb"], writes=["h_lb"])
            P.op("act", lambda e: e.activation(out=omlb, in_=lbT, func=AF.Exp), reads=["h_lb"], writes=["h_omlb"])
            P.op("act", lambda e: e.activation(out=lbT, in_=lbT, func=AF.Exp, scale=-1.0), reads=["h_lb", "h_omlb"], writes=["h_lb"])
            for t_, k_ in ((omlb, "h_omlb"), (lbT, "h_lb")):
                P.op("dve", lambda e, t_=t_: e.tensor_scalar(out=t_, in0=t_, scalar1=1.0, scalar2=None, op0=ALU.add), writes=[k_])
                P.op("dve", lambda e, t_=t_: e.reciprocal(out=t_, in_=t_), writes=[k_])

            ptmp = [cv.take([128, SEQ]) for _ in range(2)]
            for c in range(8):
                pt = ptmp[c % 2]
                src = sT[:, c, CTX:NT].rearrange("p (row col) -> p col row", col=64)
                P.op("dve" if c % 2 == 0 else "pool", lambda e, pt=pt, src=src: e.tensor_copy(out=pt.rearrange("p (col row) -> p col row", row=32), in_=src),
                     reads=[SKEY[c]], writes=[f"h_ptmp{c % 2}"])
                P.op("act", lambda e, pt=pt, c=c: e.copy(out=sT[:, c, CTX:NT], in_=pt), reads=[f"h_ptmp{c % 2}"], writes=[SKEY[c]])
            P.barrier()
            cv.off -= 2 * SEQ * 4
            norm_base = cv.off
            norm_mod(cv, A1, SH1, l, 0, NT, out_bf=hT, out_key="hT", bs=256)
            norm_end = cv.off
            P.barrier()

            wq = cv.take([128, 8, 128], BF16)
            wz = cv.take([128, 8, 2, 128], BF16)
            wi = cv.take([128, 8, 128], BF16)
            wgg = cv.take([128, 8, 128], BF16)
            wout = cv.take([128, D], BF16)
            qs = cv.take([128, NT], BF16)
            cvn = Carver()
            cvn.off = norm_base
            fTs = [cv.take([128, 512]), cvn.take([128, 512])]
            bTs = [cv.take([128, 512]), cvn.take([128, 512])]
            t32s = [cv.take([128, 512]), cv.take([128, 512])]
            kks = [cv.take([128, 512], BF16), cvn.take([128, 512], BF16)]
            assert cvn.off <= norm_end
            totcs = [cv.take([128, 4]) for _ in range(2)]
            rcs = [cv.take([128, 4]) for _ in range(2)]
            t32 = t32s[0]
            qtl = [cv.take([128, NT], BF16) for _ in range(2)]
            ktl = [cv.take([128, NT], BF16) for _ in range(2)]
            kht = [cv.take([128, NT], BF16) for _ in range(2)]
            ebT = [cv.take([128, NTILE]) for _ in range(2)]
            erT = [cv.take([128, NTILE]) for _ in range(2)]
            itok = cv.take([128, NTILE, 128], BF16)
            sgt = cv.take([128, NTILE, 128], BF16)
            gtmp = cv.take([128, 128])
            ofirst = cv.take([128, NTILE, 128], BF16)
            AT = [[cv.take([128, 128], BF16) for _ in range(2)] for _ in range(2)]
            khtok = [[cv.take([128, 128], BF16) for _ in range(2)] for _ in range(2)]
            S = [cv.take([128, 128]) for _ in range(2)]
            Sb = [[cv.take([128, 128], BF16) for _ in range(2)] for _ in range(2)]
            os_ = [cv.take([128, 128]) for _ in range(2)]
            og = [cv.take([128, 128], BF16) for _ in range(2)]
            sm = [cv.take([128, 1]) for _ in range(2)]
            xTt = [cv.take([128, 128], BF16) for _ in range(2)]
            pb7b = pb[7].bitcast(BF16)
            g1row = cv.take([128, D], BF16)
            for dc in range(8):
                P.op("pe", mm(pb[dc % 2][:, 0:128], modT[l][:, 16 + dc, 0:1].to_broadcast([128, 128]), ident_f, True, True), reads=[f"modT{l}", "cf"], writes=[PB[dc % 2]])
                P.op("act", lambda e, dc=dc: e.copy(out=g1row[:, dc * 128:(dc + 1) * 128], in_=pb[dc % 2][:, 0:128]), writes=[PB[dc % 2], "h_g1row"])
            REF = 64
            pieces = [(0, 512), (512, 512), (1024, 512), (1536, 512), (2048, 256)]

            def v128(a):
                return a.rearrange("p (c k) -> p c k", k=128)

            for h in range(8):
                P.dma("sp", "h_hhg", lambda e, h=h: e.dma_start(out=hhg, in_=d_hhg[:, h * 128:(h + 1) * 128]), writes=["h_hhg"])
                P.dma("pool", "h_wq", lambda e, h=h: e.dma_start(out=wq, in_=d_hwin[:, :, h * 128:(h + 1) * 128]), writes=["h_wq"])
                P.dma("pool", "h_wz", lambda e, h=h: [e.dma_start(out=wz[:, :, 0, :], in_=d_hwin[:, :, 1024 + h * 128:1024 + (h + 1) * 128]),
                                                      e.dma_start(out=wz[:, :, 1, :], in_=d_hwin[:, :, 2048 + h * 128:2048 + (h + 1) * 128])], writes=["h_wz"], n=2)
                P.dma("pool", "h_wi", lambda e, h=h: e.dma_start(out=wi, in_=d_hwin[:, :, 3072 + h * 128:3072 + (h + 1) * 128]), writes=["h_wi"])
                P.dma("pool", "h_wg", lambda e, h=h: e.dma_start(out=wgg, in_=d_hwin[:, :, 4096 + h * 128:4096 + (h + 1) * 128]), writes=["h_wg"])
                P.dma("pool", "h_wout", lambda e, h=h: e.dma_start(out=wout, in_=d_hwout[:, h, :], max_dma_last_dim=4096), writes=["h_wout"])
                P.op("dve", lambda e: e.tensor_tensor(out=wout, in0=wout, in1=g1row, op=ALU.mult), reads=["h_g1row"], writes=["h_wout"])
                for bi_, (t0, n, col) in enumerate(_blocks(0, NT)):
                    pq, kq = pb[bi_ % 2], PB[bi_ % 2]
                    for c in range(8):
                        P.op("pe", mm(pq[:, 0:n], wq[:, c, :], hT[:, c, t0:t0 + n], c == 0, c == 7), reads=["h_wq", "hT"], writes=[kq])
                    P.op("act", lambda e, n=n, pq=pq: e.activation(out=t32[:, 0:n], in_=pq[:, 0:n], func=AF.Exp, scale=-1.0), writes=[kq, "h_t32_0"])
                    P.op("act", lambda e, n=n: e.activation(out=t32[:, 0:n], in_=t32[:, 0:n], func=AF.Ln, bias=1.0), writes=["h_t32_0"])
                    P.op("act", lambda e, n=n: e.activation(out=t32[:, 0:n], in_=t32[:, 0:n], func=AF.Exp, scale=-1.0), writes=["h_t32_0"])
                    P.op("dve", lambda e, t0=t0, n=n, pq=pq: e.tensor_tensor(out=qs[:, t0:t0 + n], in0=t32[:, 0:n], in1=pq[:, 0:n], op=ALU.mult),
                         reads=["h_t32_0"], writes=[kq, "h_qs"])
                for tl in range(NTILE):
                    ts_ = slice(tl * 128, (tl + 1) * 128)
                    pi_, ki_ = pb[2 + tl % 2], PB[2 + tl % 2]
                    for c in range(8):
                        P.op("pe", mm(pi_[:, 0:128], hT[:, c, ts_], wi[:, c, :], c == 0, c == 7), reads=["hT", "h_wi"], writes=[ki_])
                    if tl >= 2:
                        for c in range(8):
                            P.op("pe", mm(pi_[:, 128:256], hT[:, c, ts_], wgg[:, c, :], c == 0, c == 7), reads=["hT", "h_wg"], writes=[ki_])
                    P.op("act", lambda e, tl=tl, pi_=pi_: e.copy(out=itok[:, tl, :], in_=pi_[:, 0:128]), writes=[ki_, "h_itok"])
                    if tl >= 2:
                        P.op("act", lambda e, pi_=pi_: e.activation(out=gtmp, in_=pi_[:, 128:256], func=AF.Exp, scale=-1.0), writes=[ki_, "h_gtmp"])
                        P.op("act", lambda e: e.activation(out=gtmp, in_=gtmp, func=AF.Ln, bias=1.0), writes=["h_gtmp"])
                        P.op("act", lambda e: e.activation(out=gtmp, in_=gtmp, func=AF.Exp, scale=-1.0), writes=["h_gtmp"])
                        P.op("dve", lambda e, tl=tl, pi_=pi_: e.tensor_tensor(out=sgt[:, tl, :], in0=gtmp, in1=pi_[:, 128:256], op=ALU.mult), reads=["h_gtmp"], writes=[ki_, "h_sgt"])

                def precompute(d_, p0, pn, pk):
                    fT, bT, t32, kk, totc, rc = fTs[d_], bTs[d_], t32s[d_], kks[d_], totcs[d_], rcs[d_]
                    kx = f"_{d_}"
                    lbc = lbT[:, d_ * 8 + h:d_ * 8 + h + 1]
                    omc = omlb[:, d_ * 8 + h:d_ * 8 + h + 1]
                    nt = pn // 128
                    tsl = slice(p0 // 128, p0 // 128 + nt)
                    ps_ = slice(p0, p0 + pn)
                    pz, kz = pb[4 + d_], PB[4 + d_]
                    for c in range(8):
                        P.op("pe", mm(pz[:, 0:pn], wz[:, c, d_, :], hT[:, c, ps_], c == 0, c == 7), reads=["h_wz", "hT"], writes=[kz])
                    P.op("act", lambda e: e.activation(out=fT[:, 0:pn], in_=pz[:, 0:pn], func=AF.Exp, scale=-1.0), writes=[kz, "h_fT" + kx])
                    P.op("act", lambda e: e.activation(out=fT[:, 0:pn], in_=fT[:, 0:pn], func=AF.Ln, bias=1.0), writes=["h_fT" + kx])
                    P.op("act", lambda e: e.activation(out=fT[:, 0:pn], in_=fT[:, 0:pn], func=AF.Exp, scale=-1.0), writes=["h_fT" + kx])
                    P.op("dve", lambda e: e.tensor_scalar(out=fT[:, 0:pn], in0=fT[:, 0:pn], scalar1=omc, scalar2=lbc, op0=ALU.mult, op1=ALU.add),
                         reads=["h_lb", "h_omlb"], writes=["h_fT" + kx])
                    P.op("dve", lambda e: e.tensor_scalar(out=kk[:, 0:pn], in0=fT[:, 0:pn], scalar1=-1.0, scalar2=1.0, op0=ALU.mult, op1=ALU.add), reads=["h_fT" + kx], writes=["h_kk" + kx])
                    P.op("act", lambda e: e.activation(out=fT[:, 0:pn], in_=fT[:, 0:pn], func=AF.Ln), reads=["h_kk" + kx], writes=["h_fT" + kx])
                    P.op("dve", lambda e: e.tensor_tensor_scan(out=bT[:, 0:pn], data0=rst[:, 0:pn], data1=fT[:, 0:pn], initial=0.0, op0=ALU.mult, op1=ALU.add),
                         reads=["h_rst", "h_fT" + kx], writes=["h_bT" + kx])
                    P.op("dve", lambda e: e.tensor_copy(out=totc[:, 0:nt], in_=v128(bT[:, 0:pn])[:, :, 127]), reads=["h_bT" + kx], writes=["h_totc" + kx])
                    totb = totc[:, 0:nt].unsqueeze(2).to_broadcast([128, nt, 128])
                    P.op("act", lambda e: e.activation(out=ebT[d_][:, tsl], in_=totc[:, 0:nt], func=AF.Exp), reads=["h_totc" + kx], writes=[f"h_ebT{pk}"])
                    if d_ == 0:
                        P.op("dve", lambda e: e.tensor_tensor(out=v128(t32[:, 0:pn]), in0=totb, in1=v128(bT[:, 0:pn]), op=ALU.subtract),
                             reads=["h_bT" + kx, "h_totc" + kx], writes=["h_t32" + kx])
                    else:
                        P.op("dve", lambda e: e.tensor_tensor(out=t32[:, 0:pn], in0=bT[:, 0:pn], in1=fT[:, 0:pn], op=ALU.subtract), reads=["h_bT" + kx, "h_fT" + kx], writes=["h_t32" + kx])
                        P.op("dve", lambda e: e.tensor_tensor(out=v128(bT[:, 0:pn]), in0=totb, in1=v128(t32[:, 0:pn]), op=ALU.subtract),
                             reads=["h_totc" + kx, "h_t32" + kx], writes=["h_bT" + kx])
                    P.op("act", lambda e: e.activation(out=t32[:, 0:pn], in_=t32[:, 0:pn], func=AF.Exp), writes=["h_t32" + kx])
                    P.op("dve", lambda e: e.tensor_tensor(out=kht[d_][:, ps_], in0=t32[:, 0:pn], in1=kk[:, 0:pn], op=ALU.mult),
                         reads=["h_t32" + kx, "h_kk" + kx], writes=[f"h_kht{pk}"])
                    P.op("dve", lambda e: e.tensor_copy(out=rc[:, 0:nt], in_=v128(bT[:, 0:pn])[:, :, REF]), reads=["h_bT" + kx], writes=["h_rc" + kx])
                    rb = rc[:, 0:nt].unsqueeze(2).to_broadcast([128, nt, 128])
                    P.op("act", lambda e: e.activation(out=erT[d_][:, tsl], in_=rc[:, 0:nt], func=AF.Exp), reads=["h_rc" + kx], writes=[f"h_erT{pk}"])
                    P.op("dve", lambda e: e.tensor_tensor(out=v128(bT[:, 0:pn]), in0=v128(bT[:, 0:pn]), in1=rb, op=ALU.subtract), reads=["h_rc" + kx], writes=["h_bT" + kx])
                    P.op("act", lambda e: e.activation(out=t32[:, 0:pn], in_=bT[:, 0:pn], func=AF.Exp), reads=["h_bT" + kx, f"h_kht{pk}"], writes=["h_t32" + kx])
                    P.op("dve", lambda e: e.tensor_tensor(out=qtl[d_][:, ps_], in0=t32[:, 0:pn], in1=qs[:, ps_], op=ALU.mult),
                         reads=["h_t32" + kx, "h_qs"], writes=[f"h_qtl{pk}"])
                    P.op("act", lambda e: e.activation(out=t32[:, 0:pn], in_=bT[:, 0:pn], func=AF.Exp, scale=-1.0), reads=["h_bT" + kx, f"h_qtl{pk}"], writes=["h_t32" + kx])
                    P.op("dve", lambda e: e.tensor_tensor(out=ktl[d_][:, ps_], in0=t32[:, 0:pn], in1=kk[:, 0:pn], op=ALU.mult),
                         reads=["h_t32" + kx, "h_kk" + kx], writes=[f"h_ktl{pk}"])

                orders = [list(range(NTILE)), [1, 0] + list(range(NTILE - 1, 1, -1))]
                piecesD = [[(0, 512), (512, 512), (1024, 512), (1536, 512), (2048, 256)],
                           [(0, 256), (1792, 512), (1280, 512), (768, 512), (256, 512)]]
                pkey = {}
                for d_ in range(2):
                    for k_, (p0, pn) in enumerate(piecesD[d_]):
                        for tl in range(p0 // 128, (p0 + pn) // 128):
                            pkey[(d_, tl)] = f"{d_}_{k_}"
                visited = set()
                for d_ in range(2):
                    P.op("dve", lambda e, d_=d_: e.memset(S[d_], 0.0), writes=[f"h_S{d_}"])
                ncomp = [0]

                def step(d_, it, tl):
                    ts_ = slice(tl * 128, (tl + 1) * 128)
                    pi = it % 2
                    pk = pkey[(d_, tl)]
                    lat = tl >= 2
                    second = tl in visited
                    visited.add(tl)
                    o_, ko = pb[d_], PB[d_]
                    sc_cols = slice(128 * d_, 128 * d_ + 128)
                    if lat:
                        erc = erT[d_][:, tl:tl + 1]
                        P.op("act", lambda e: e.activation(out=Sb[d_][pi], in_=S[d_], func=AF.Copy, scale=erc), reads=[f"h_S{d_}", f"h_erT{pk}"], writes=[f"h_Sb{d_}_{pi}"])
                        P.op("pe", mm(pb[6][:, sc_cols], ktl[d_][:, ts_], qtl[d_][:, ts_], True, True), reads=[f"h_ktl{pk}", f"h_qtl{pk}"], writes=[PB[6]])
                        msk = cb[:, CI["tF"], :] if d_ == 0 else cb[:, CI["tB"], :]
                        P.op("dve", lambda e: e.tensor_tensor(out=AT[d_][pi], in0=pb[6][:, sc_cols], in1=msk, op=ALU.mult), reads=["cb"], writes=[PB[6], f"h_AT{d_}{pi}"])
                        P.op("pe", mm(o_[:, 0:128], AT[d_][pi], itok[:, tl, :], True, False), reads=[f"h_AT{d_}{pi}", "h_itok"], writes=[ko])
                    P.op("pe", lambda e: e.transpose(out=pb7b[:, sc_cols], in_=kht[d_][:, ts_], identity=ident_b), reads=[f"h_kht{pk}", "cb"], writes=[PB[7]])
                    P.op("act", lambda e: e.copy(out=khtok[d_][pi], in_=pb7b[:, sc_cols]), writes=[PB[7], f"h_khtok{d_}{pi}"])
                    kv, kkv = pb[2 + d_], PB[2 + d_]
                    P.op("pe", mm(kv[:, 0:128], khtok[d_][pi], itok[:, tl, :], True, True), reads=[f"h_khtok{d_}{pi}", "h_itok"], writes=[kkv])
                    if lat:
                        P.op("pe", mm(o_[:, 0:128], qtl[d_][:, ts_], Sb[d_][pi], False, True), reads=[f"h_qtl{pk}", f"h_Sb{d_}_{pi}", ko], writes=[ko])
                    ebc = ebT[d_][:, tl:tl + 1]
                    P.op("dve", lambda e: e.scalar_tensor_tensor(out=S[d_], in0=S[d_], scalar=ebc, in1=kv[:, 0:128], op0=ALU.mult, op1=ALU.add),
                         reads=[f"h_ebT{pk}", f"h_S{d_}"], writes=[kkv, f"h_S{d_}"])
                    if not lat:
                        return
                    if not second:
                        P.op("act", lambda e: e.copy(out=ofirst[:, tl, :], in_=o_[:, 0:128]), writes=[ko, f"h_of{tl}"])
                        return
                    s1, ks1 = sm[d_], f"h_sm{d_}"
                    P.op("dve", lambda e: e.tensor_tensor(out=os_[d_], in0=o_[:, 0:128], in1=ofirst[:, tl, :], op=ALU.add), reads=[f"h_of{tl}"], writes=[ko, f"h_os{d_}"])
                    P.op("act", lambda e: e.activation(out=og[d_], in_=os_[d_], func=AF.Square, accum_out=s1), reads=[f"h_os{d_}"], writes=[f"h_og{d_}", ks1])
                    P.op("dve", lambda e: e.tensor_scalar(out=s1, in0=s1, scalar1=1.0 / 128, scalar2=EPS, op0=ALU.mult, op1=ALU.add), writes=[ks1])
                    P.op("act", lambda e: e.activation(out=s1, in_=s1, func=AF.Ln), writes=[ks1])
                    P.op("act", lambda e: e.activation(out=s1, in_=s1, func=AF.Exp, scale=-0.5), writes=[ks1])
                    P.op("dve", lambda e: e.scalar_tensor_tensor(out=os_[d_], in0=os_[d_], scalar=s1[:, 0:1], in1=hhg, op0=ALU.mult, op1=ALU.mult),
                         reads=[ks1, "h_hhg"], writes=[f"h_os{d_}"])
                    P.op("dve", lambda e: e.tensor_tensor(out=og[d_], in0=os_[d_], in1=sgt[:, tl, :], op=ALU.mult), reads=[f"h_os{d_}", "h_sgt"], writes=[f"h_og{d_}"])
                    P.op("pe", lambda e: e.transpose(out=pb7b[:, 256:384], in_=og[d_], identity=ident_b), reads=[f"h_og{d_}", "cb"], writes=[PB[7]])
                    xi = ncomp[0] % 2
                    ncomp[0] += 1
                    P.op("act", lambda e: e.copy(out=xTt[xi], in_=pb7b[:, 256:384]), writes=[PB[7], f"h_xT{xi}"])
                    c0 = (tl * 128 - CTX) // 32
                    for half in range(2):
                        po, kpo = pb[4 + half], PB[4 + half]
                        for j in range(4):
                            dc = half * 4 + j
                            P.op("pe", mm(po[:, j * 128:(j + 1) * 128], wout[:, dc * 128:(dc + 1) * 128], xTt[xi], True, True), reads=["h_wout", f"h_xT{xi}"], writes=[kpo])
                        keys = SKEY[half * 4:half * 4 + 4]
                        dstv = sT[:, half * 4:half * 4 + 4, ts_]
                        srcv = po[:, :].rearrange("p (d t) -> p d t", d=4)
                        P.op("dve", lambda e, dstv=dstv, srcv=srcv: e.tensor_tensor(out=dstv, in0=dstv, in1=srcv, op=ALU.add),
                             reads=keys, writes=[kpo] + keys)

                done = [0, 0]
                for k_ in range(5):
                    P.interleave([lambda d_=d_: precompute(d_, piecesD[d_][k_][0], piecesD[d_][k_][1], f"{d_}_{k_}") for d_ in range(2)])
                    avail = [min(NTILE, 4 * (k_ + 1)) if k_ < 4 else NTILE, min(NTILE, 2 + 4 * k_)]
                    if DEBUG_NT is not None:
                        avail = [min(a_, DEBUG_NT) for a_ in avail]
                    while done[0] < avail[0] or done[1] < avail[1]:
                        for d_ in range(2):
                            if done[d_] < avail[d_]:
                                step(d_, done[d_], orders[d_][done[d_]])
                                done[d_] += 1

        def final():
            P.barrier()
            cv = Carver()
            ob = [cv.take([128, 512]) for _ in range(2)]
            outs = []
            cnt = [0]
            sq = [cv.take([128, 512]) for _ in range(2)]
            rstd = cv.take([128, 512])
            for (t0, n, col) in _blocks(CTX, NT):
                for c in range(8):
                    P.op("act", lambda e, c=c, t0=t0, n=n: e.activation(out=sq[c % 2][:, 0:n], in_=sT[:, c, t0:t0 + n], func=AF.Square), reads=[SKEY[c]], writes=[f"nm_sq{c % 2}"])
                    P.op("pe", mm(pb[7][:, 0:n], ones_f, sq[c % 2][:, 0:n], c == 0, c == 7), reads=[f"nm_sq{c % 2}", "cf"], writes=[PB[7]])
                P.op("dve", lambda e, n=n: e.tensor_scalar(out=rstd[:, 0:n], in0=pb[7][:, 0:n], scalar1=1.0 / D, scalar2=EPS, op0=ALU.mult, op1=ALU.add), reads=[PB[7]], writes=["nm_rstd"])
                P.op("act", lambda e, n=n: e.activation(out=rstd[:, 0:n], in_=rstd[:, 0:n], func=AF.Sqrt), reads=["nm_rstd"], writes=["nm_rstd"])
                P.op("dve", lambda e, n=n: e.reciprocal(out=rstd[:, 0:n], in_=rstd[:, 0:n]), reads=["nm_rstd"], writes=["nm_rstd"])
                for c in range(8):
                    i = cnt[0] % 2
                    cnt[0] += 1
                    P.op("dve", lambda e, c=c, t0=t0, n=n, i=i: e.scalar_tensor_tensor(out=ob[i][:, 0:n], in0=sT[:, c, t0:t0 + n], scalar=gfin[:, c:c + 1], in1=rstd[:, 0:n],
                                                                                 op0=ALU.mult, op1=ALU.mult), reads=[SKEY[c], "gfin", "nm_rstd"], writes=[f"fin_ob{i}"])
                    outs.append(P.dma("sp", f"fin_ob{i}", lambda e, c=c, t0=t0, n=n, i=i: e.dma_start(out=d_out[:, c, t0 - CTX:t0 - CTX + n], in_=ob[i][:, 0:n]),
                                      reads=[f"fin_ob{i}"]))
            return outs

        outs = []
        if "mix0" in phases:
            mlstm(0)
        if "moe0" in phases:
            moe(0, 0, NT)
        if "mix1" in phases:
            hgrn(1)
        if "moe1" in phases:
            moe(1, CTX, NT)
        if "final" in phases:
            outs = final()
        if d_dump is not None:
            P.barrier()
            for c in range(8):
                outs.append(P.dma("sp", f"dump{c}", lambda e, c=c: e.dma_start(out=d_dump[:, c, :], in_=sT[:, c, :]), reads=[SKEY[c]]))
        P.emit(final_wait_ops=outs + dbg_outs)
    return nc


def _fm(v, lead=()):
    v = np.asarray(v, np.float32)
    k = v.shape[-1] // 128
    r = v.reshape(v.shape[:-1] + (k, 128))
    return np.ascontiguousarray(np.moveaxis(r, -1, 0))


def _wl(w):
    w = np.asarray(w, np.float32)
    K, N = w.shape
    return np.ascontiguousarray(w.reshape(K // 128, 128, N).transpose(1, 0, 2))


def prep_shared(x, c, ctx, c_ctx, ada_w, ada_b, norm_mix_g, norm_ffn_g, final_g,
                m_w_in, m_conv_w, m_conv_b, m_gate_b, m_head_g, m_w_out,
                h_w_in, h_lower_bounds, h_head_g, h_w_out,
                router_w, router_bias, e_w_gate, e_w_up, e_w_down):
    sh = {}
    sh["adaw"] = np.ascontiguousarray(np.asarray(ada_w, np.float32).reshape(2, 8, 128, 12, 512).transpose(0, 3, 2, 1, 4))
    sh["adab"] = np.ascontiguousarray(_fm(ada_b))
    sh["gmix"] = _fm(norm_mix_g)
    sh["gffn"] = _fm(norm_ffn_g)
    sh["gfin"] = _fm(final_g)
    sh["mwin"] = _wl(m_w_in[0])
    cw = _fm(m_conv_w[0])
    cbias = _fm(m_conv_b[0])
    sh["mconv"] = np.ascontiguousarray(np.concatenate([cw.transpose(0, 2, 1), cbias[:, :, None]], axis=2))
    sh["mgb"] = np.ascontiguousarray(np.broadcast_to(np.asarray(m_gate_b[0], np.float32)[None, :], (128, 16)))
    sh["mhg"] = np.ascontiguousarray(np.broadcast_to(np.asarray(m_head_g[0], np.float32)[None, :], (128, D)))
    sh["mwout"] = _wl(m_w_out[0])
    sh["hwin"] = _wl(h_w_in[0])
    sh["hlb"] = np.ascontiguousarray(_fm(h_lower_bounds))
    sh["hhg"] = np.ascontiguousarray(np.broadcast_to(np.asarray(h_head_g[0], np.float32)[None, :], (128, D)))
    sh["hwout"] = _wl(h_w_out[0])
    sh["rw"] = _wl(router_w)
    sh["rb"] = np.ascontiguousarray(np.broadcast_to(np.asarray(router_bias, np.float32)[None, None, :], (128, NTILE, NE)))
    sh["ewg"] = np.ascontiguousarray(np.asarray(e_w_gate, np.float32).reshape(2, NE, 8, 128, DEXP).transpose(0, 1, 3, 2, 4))
    sh["ewu"] = np.ascontiguousarray(np.asarray(e_w_up, np.float32).reshape(2, NE, 8, 128, DEXP).transpose(0, 1, 3, 2, 4))
    sh["ewd"] = np.ascontiguousarray(np.asarray(e_w_down, np.float32).reshape(2, NE, 4, 128, D).transpose(0, 1, 3, 2, 4))
    sh["cf"] = CARR
    sh["rst"] = RST
    return sh


def prep_core(b, x, c, ctx, c_ctx, s_override=None):
    if s_override is not None:
        s = s_override
    else:
        s = np.concatenate([np.asarray(ctx[b], np.float32), np.asarray(x[b], np.float32)], axis=0)
    xT = np.ascontiguousarray(s.reshape(NT, 8, 128).transpose(2, 1, 0))
    cc = np.stack([np.asarray(c[b], np.float32), np.asarray(c_ctx, np.float32)], axis=-1)
    cT = np.ascontiguousarray(cc.reshape(8, 128, 2).transpose(1, 0, 2))
    return {"xT": xT, "cT": cT}


_NC_CACHE = {}


def kernel(**inputs):
    x = inputs["x"]
    B = x.shape[0]
    sh = prep_shared(**inputs)
    if "full" not in _NC_CACHE:
        _NC_CACHE["full"] = build_program()
    nc = _NC_CACHE["full"]
    in_maps = []
    for b in range(B):
        m = dict(sh)
        m.update(prep_core(b, inputs["x"], inputs["c"], inputs["ctx"], inputs["c_ctx"]))
        in_maps.append(m)
    res = run_bass_kernel_spmd(nc, in_maps, core_ids=list(range(B)))
    out = np.empty((B, SEQ, D), np.float32)
    for b in range(B):
        oT = np.asarray(res.results[b]["outT"])
        out[b] = oT.transpose(2, 1, 0).reshape(64, 32, D).transpose(1, 0, 2).reshape(SEQ, D)
    return out
```

```python
import numpy as np
import concourse.bass as bass
import concourse.mybir as mybir
from contextlib import ExitStack
from concourse.bass_utils import run_bass_kernel_spmd

F32 = mybir.dt.float32
BF16 = mybir.dt.bfloat16
AF = mybir.ActivationFunctionType
ALU = mybir.AluOpType
AX = mybir.AxisListType

D = 1024
SEQ = 2048
CTX = 256
NT = SEQ + CTX
NTILE = NT // 128
EPS = 1e-6
NE = 16
DEXP = 512
ENGS = ("pe", "act", "dve", "pool", "sp")
DEBUG_NT = None


class Op:
    __slots__ = ("eng", "fn", "deps", "signal", "seq", "is_dma", "dsem", "dval", "n_inst", "name", "cost")

    def __init__(self, eng, fn, is_dma, dsem, name):
        self.eng = eng
        self.fn = fn
        self.deps = set()
        self.signal = False
        self.seq = None
        self.is_dma = is_dma
        self.dsem = dsem
        self.dval = None
        self.n_inst = 1
        self.name = name
        self.cost = None


class Prog:
    def __init__(self, nc):
        self.nc = nc
        self.ops = {e: [] for e in ENGS}
        self.last_w = {}
        self.readers = {}
        self.all_ops = []
        self._bar_from = 0
        self._capture = None

    def _add(self, eng, fn, reads, writes, is_dma=False, dsem=None, name=None):
        o = Op(eng, fn, is_dma, dsem, name)
        for k in reads:
            w = self.last_w.get(k)
            if w is not None:
                o.deps.add(w)
        for k in writes:
            w = self.last_w.get(k)
            if w is not None:
                o.deps.add(w)
            for r in self.readers.get(k, ()):
                o.deps.add(r)
        for k in reads:
            self.readers.setdefault(k, []).append(o)
        for k in writes:
            self.last_w[k] = o
            self.readers[k] = []
        o.deps.discard(o)
        self.ops[eng].append(o)
        self.all_ops.append(o)
        return o

    def op(self, eng, fn, reads=(), writes=(), name=None):
        if self._capture is not None:
            self._capture.append(("op", eng, fn, tuple(reads), tuple(writes), None, 1))
            return None
        return self._add(eng, fn, reads, writes, name=name)

    def dma(self, eng, group, fn, reads=(), writes=(), n=1, name=None):
        if self._capture is not None:
            self._capture.append(("dma", eng, fn, tuple(reads), tuple(writes), group, n))
            return None
        o = self._add(eng, fn, reads, writes, is_dma=True, dsem=group, name=name)
        o.n_inst = n
        return o

    def interleave(self, builders):
        streams = []
        for b in builders:
            self._capture = []
            b()
            streams.append(self._capture)
            self._capture = None
        idx = [0] * len(streams)
        while any(idx[i] < len(st) for i, st in enumerate(streams)):
            for i, st in enumerate(streams):
                if idx[i] < len(st):
                    kind, eng, fn, reads, writes, group, n = st[idx[i]]
                    idx[i] += 1
                    if kind == "op":
                        self._add(eng, fn, reads, writes)
                    else:
                        o = self._add(eng, fn, reads, writes, is_dma=True, dsem=group)
                        o.n_inst = n

    def barrier(self):
        self.all_ops.append(None)

    def _schedule(self, seg):
        import heapq
        COST = {"pe": 0.2, "act": 0.5, "dve": 0.55, "pool": 0.9, "sp": 0.1}
        HOP = 0.15
        n = len(seg)
        idx = {id(o): i for i, o in enumerate(seg)}
        cost = [0.0] * n
        lat = [0.0] * n
        for i, o in enumerate(seg):
            c = o.cost if o.cost is not None else COST[o.eng]
            if o.is_dma:
                cost[i] = 0.08 * o.n_inst
                lat[i] = c if o.cost is not None else 2.5
            else:
                cost[i] = c
        deps = [[idx[id(d)] for d in o.deps if id(d) in idx] for o in seg]
        succ = [[] for _ in range(n)]
        for i, dl in enumerate(deps):
            for d in dl:
                succ[d].append(i)
        prio = [0.0] * n
        for i in range(n - 1, -1, -1):
            m = 0.0
            for j in succ[i]:
                if prio[j] > m:
                    m = prio[j]
            prio[i] = m + cost[i] + lat[i] + HOP
        ndep = [len(dl) for dl in deps]
        est = [0.0] * n
        fin = [0.0] * n
        free_at = {e: 0.0 for e in ENGS}
        pend = {e: [] for e in ENGS}
        avail = {e: [] for e in ENGS}
        order = {e: [] for e in ENGS}
        for i in range(n):
            if ndep[i] == 0:
                heapq.heappush(pend[seg[i].eng], (0.0, -prio[i], i))
        left = n
        while left:
            best_e, best_t = None, None
            for e in ENGS:
                if not pend[e] and not avail[e]:
                    continue
                while pend[e] and pend[e][0][0] <= free_at[e]:
                    t_, p_, i_ = heapq.heappop(pend[e])
                    heapq.heappush(avail[e], (p_, i_))
                t = free_at[e] if avail[e] else max(free_at[e], pend[e][0][0])
                if best_t is None or t < best_t:
                    best_e, best_t = e, t
            e = best_e
            if avail[e]:
                p_, i = heapq.heappop(avail[e])
            else:
                t_, p_, i = heapq.heappop(pend[e])
            start = max(free_at[e], est[i])
            free_at[e] = start + cost[i]
            fin[i] = start + cost[i] + lat[i]
            order[e].append(seg[i])
            left -= 1
            for j in succ[i]:
                if fin[i] + HOP > est[j]:
                    est[j] = fin[i] + HOP
                ndep[j] -= 1
                if ndep[j] == 0:
                    heapq.heappush(pend[seg[j].eng], (est[j], -prio[j], j))
        return order

    def emit(self, final_wait_ops=(), schedule=True):
        nc = self.nc
        segs, cur = [], []
        for o in self.all_ops:
            if o is None:
                if cur:
                    segs.append(cur)
                cur = []
            else:
                cur.append(o)
        if cur:
            segs.append(cur)
        seg_orders = []
        for seg in segs:
            if schedule:
                seg_orders.append(self._schedule(seg))
            else:
                od = {e: [] for e in ENGS}
                for o in seg:
                    od[o.eng].append(o)
                seg_orders.append(od)
        bar_deps = [set() for _ in segs]
        last_comp = {}
        for k, od in enumerate(seg_orders):
            if k > 0:
                bar_deps[k] = set(last_comp.values()) | {o for o in segs[k - 1] if o.is_dma}
            for e in ENGS:
                comp = [o for o in od[e] if not o.is_dma]
                if comp:
                    last_comp[e] = comp[-1]
        for o in (x for x in self.all_ops if x is not None):
            for d in o.deps:
                if d.eng == "pe" and o.eng == "pe" and not d.is_dma and not o.is_dma:
                    continue
                d.signal = True
        for bd in bar_deps:
            for d in bd:
                d.signal = True
        for o in final_wait_ops:
            o.signal = True
        with ExitStack() as es:
            esem = {e: es.enter_context(nc.semaphore("c_" + e)) for e in ENGS}
            gsem = {}
            gcount = {}
            for e in ENGS:
                for od in seg_orders:
                    for o in od[e]:
                        if o.is_dma:
                            if o.dsem not in gsem:
                                gsem[o.dsem] = es.enter_context(nc.semaphore("d_" + str(o.dsem)))
                                gcount[o.dsem] = 0
                            gcount[o.dsem] += 16 * o.n_inst
                            o.dval = gcount[o.dsem]
            for e in ENGS:
                c = 0
                for od in seg_orders:
                    for o in od[e]:
                        if not o.is_dma and o.signal:
                            c += 1
                            o.seq = c
            block = es.enter_context(nc.Block())
            engobj = {"pe": "tensor", "act": "scalar", "dve": "vector", "pool": "gpsimd", "sp": "sync"}

            def run(ename, eng):
                waited = {}

                def do_waits(deps, is_pe_compute):
                    need = {}
                    for d in deps:
                        if d.is_dma:
                            key, val, sem = ("g", d.dsem), d.dval, gsem[d.dsem]
                        else:
                            if d.eng == "pe" and ename == "pe" and is_pe_compute:
                                continue
                            if d.eng == ename and ename == "pe":
                                continue
                            key, val, sem = ("e", d.eng), d.seq, esem[d.eng]
                        if waited.get(key, 0) >= val:
                            continue
                        if key not in need or need[key][1] < val:
                            need[key] = (sem, val)
                    for key, (sem, val) in need.items():
                        eng.wait_ge(sem, val)
                        waited[key] = val

                for k, od in enumerate(seg_orders):
                    if bar_deps[k]:
                        do_waits(list(bar_deps[k]), False)
                    for o in od[ename]:
                        do_waits(o.deps, not o.is_dma)
                        r = o.fn(eng)
                        if o.is_dma:
                            insts = r if isinstance(r, (list, tuple)) else [r]
                            assert len(insts) == o.n_inst
                            for ins in insts:
                                ins.then_inc(gsem[o.dsem], 16)
                        elif o.signal:
                            ins = r[-1] if isinstance(r, (list, tuple)) else r
                            ins.then_inc(esem[ename], 1)
                if ename == "sp":
                    for o in final_wait_ops:
                        if o.is_dma:
                            eng.wait_ge(gsem[o.dsem], o.dval)
                        else:
                            eng.wait_ge(esem[o.eng], o.seq)

            for ename in ENGS:
                getattr(block, engobj[ename])(lambda eng, ename=ename: run(ename, eng))


def _consts():
    s = np.arange(128)[:, None]
    t = np.arange(128)[None, :]
    same = (s // 64) == (t // 64)
    c = {}
    c["ident"] = np.eye(128, dtype=np.float32)
    c["ones"] = np.ones((128, 128), np.float32)
    c["triF"] = (same & (s <= t)).astype(np.float32)
    c["triB"] = (same & (s >= t)).astype(np.float32)
    c["sufF"] = (same & (s > t)).astype(np.float32)
    c["sufB"] = (same & (s < t)).astype(np.float32)
    c["sel0"] = np.repeat((s < 64).astype(np.float32), 128, axis=1)
    c["sel1"] = np.repeat((s >= 64).astype(np.float32), 128, axis=1)
    c["tF"] = (s <= t).astype(np.float32)
    c["tB"] = (s >= t).astype(np.float32)
    c["sF"] = (s > t).astype(np.float32)
    c["sB"] = (s < t).astype(np.float32)
    names = ["ident", "ones", "triF", "triB", "sufF", "sufB", "sel0", "sel1", "tF", "tB", "sF", "sB"]
    arr = np.stack([c[n] for n in names], axis=1)
    selE = np.zeros((128, NE, 128), np.float32)
    for e in range(NE):
        selE[e, e, :] = 1.0
    rst = np.ones((128, NT), np.float32)
    rst[:, ::128] = 0.0
    return names, np.ascontiguousarray(arr), selE, rst


CN, CARR, SELE, RST = _consts()
CI = {n: i for i, n in enumerate(CN)}


def _blocks(lo, hi, bs=512):
    out = []
    t = lo
    while t < hi:
        n = min(bs, hi - t)
        if t < CTX:
            n = min(n, CTX - t)
        out.append((t, n, 1 if t < CTX else 0))
        t += n
    return out


def build_program(phases=("mods", "mix0", "moe0", "mix1", "moe1", "final"), dump=None):
    nc = bass.Bass("TRN2", target_bir_lowering=False)
    es = ExitStack()
    P = Prog(nc)

    def din(name, shape, dt=F32):
        return nc.dram_tensor(name, list(shape), dt, kind="ExternalInput").ap()

    d_xT = din("xT", [128, 8, NT])
    d_cT = din("cT", [128, 8, 2])
    d_adaw = din("adaw", [2, 12, 128, 8, 512])
    d_adab = din("adab", [128, 2, 48])
    d_gmix = din("gmix", [128, 2, 8])
    d_gffn = din("gffn", [128, 2, 8])
    d_gfin = din("gfin", [128, 8])
    d_mwin = din("mwin", [128, 8, 3088])
    d_mconv = din("mconv", [128, 8, 4])
    d_mgb = din("mgb", [128, 16])
    d_mhg = din("mhg", [128, D])
    d_mwout = din("mwout", [128, 8, D])
    d_hwin = din("hwin", [128, 8, 5120])
    d_hlb = din("hlb", [128, 2, 16])
    d_hhg = din("hhg", [128, D])
    d_hwout = din("hwout", [128, 8, D])
    d_rw = din("rw", [128, 8, NE])
    d_rb = din("rb", [128, NTILE, NE])
    d_wg = din("ewg", [2, NE, 128, 8, DEXP])
    d_wu = din("ewu", [2, NE, 128, 8, DEXP])
    d_wd = din("ewd", [2, NE, 128, 4, D])
    d_cf = din("cf", [128, 12, 128])
    d_rst = din("rst", [128, NT])
    d_out = nc.dram_tensor("outT", [128, 8, SEQ], F32, kind="ExternalOutput").ap()
    d_dump = None
    if dump is not None:
        d_dump = nc.dram_tensor("dump", [128, 8, NT], F32, kind="ExternalOutput").ap()

    with es:
        def sb(name, shape, dt=F32):
            return es.enter_context(nc.sbuf_tensor("s_" + name, list(shape), dt))

        sT = sb("sT", [128, 8, NT])
        hT = sb("hT", [128, 8, NT], BF16)
        cf = sb("cf", [128, 12, 128])
        cb = sb("cb", [128, 12, 128], BF16)
        maskLE = sb("maskLE", [128, 128], BF16)
        maskGE = sb("maskGE", [128, 128], BF16)
        modT = [sb(f"modT{l}", [128, 48, 2]) for l in range(2)]
        adab = sb("adab", [128, 2, 48])
        gmix = sb("gmix", [128, 2, 8])
        gffn = sb("gffn", [128, 2, 8])
        gfin = sb("gfin", [128, 8])
        cT = sb("cT", [128, 8, 2])
        scT = sb("scT", [128, 8, 2])
        A1 = [sb(f"A1_{l}", [128, 8, 2]) for l in range(2)]
        A2 = [sb(f"A2_{l}", [128, 8, 2]) for l in range(2)]
        pb = [es.enter_context(nc.psum_tensor(f"pb{i}", [128, 512], F32)) for i in range(8)]
        PB = [f"pb{i}" for i in range(8)]

        ARENA = ((nc.sbuf_bytes_remaining - 2048) // 64) * 64
        arena = sb("arena", [128, ARENA // 4], F32)

        class Carver:
            def __init__(self):
                self.off = 0

            def take(self, shape, dt=F32):
                esz = 4 if dt == F32 else 2
                n = int(np.prod(shape[1:]))
                nbytes = ((n * esz + 63) // 64) * 64
                assert self.off + nbytes <= ARENA, (self.off, nbytes, ARENA)
                a = arena[:, self.off // 4:(self.off + nbytes) // 4]
                self.off += nbytes
                if dt != F32:
                    a = a.bitcast(dt)
                a = a[:, 0:n]
                if len(shape) == 3:
                    a = a.rearrange("p (a b) -> p a b", a=shape[1])
                elif len(shape) == 4:
                    a = a.rearrange("p (a b c) -> p a b c", a=shape[1], b=shape[2])
                return a

        dbg_outs = []

        def dbg(name, ap, shape, key, dt=F32):
            if dump is None or dump is True or name not in dump:
                return
            dd = nc.dram_tensor("dbg_" + name, list(shape), dt, kind="ExternalOutput").ap()
            dbg_outs.append(P.dma("sp", "dbg_" + name, lambda e: e.dma_start(out=dd, in_=ap), reads=[key]))

        def mm(out, lhsT, rhs, start, stop):
            return lambda e: e.matmul(out, lhsT=lhsT, rhs=rhs, start=start, stop=stop)

        for c in range(8):
            P.dma("sp", f"sT{c}", lambda e, c=c: e.dma_start(out=sT[:, c, :], in_=d_xT[:, c, :]), writes=[f"sT{c}"])
        P.dma("sp", "cf", lambda e: e.dma_start(out=cf[:], in_=d_cf), writes=["cf"])
        for nm, t, dsrc in (("adab", adab, d_adab), ("gmix", gmix, d_gmix), ("gffn", gffn, d_gffn),
                            ("gfin", gfin, d_gfin), ("cT", cT, d_cT)):
            P.dma("sp", nm, lambda e, t=t, dsrc=dsrc: e.dma_start(out=t[:], in_=dsrc), writes=[nm])
        P.op("dve", lambda e: e.tensor_copy(out=cb[:], in_=cf[:]), reads=["cf"], writes=["cb"])
        P.op("dve", lambda e: e.tensor_copy(out=maskLE[:], in_=cf[:, CI["triF"], :]), reads=["cf"], writes=["masks"])
        P.op("dve", lambda e: e.tensor_copy(out=maskGE[:], in_=cf[:, CI["triB"], :]), reads=["cf"], writes=["masks"])
        ident_b = cb[:, CI["ident"], :]
        ident_f = cf[:, CI["ident"], :]
        ones_f = cf[:, CI["ones"], :]
        SKEY = [f"sT{c}" for c in range(8)]

        MODS_BASE = ((ARENA - (4 * 16384 + 2 * 2048)) // 64) * 64
        if "mods" in phases:
            cv = Carver()
            cv.off = MODS_BASE
            NB_ = 4
            adaw = [cv.take([128, 8, 512]) for _ in range(NB_)]
            modrow = [cv.take([128, 512]) for _ in range(2)]
            P.op("act", lambda e: e.activation(out=scT[:], in_=cT[:], func=AF.Silu), reads=["cT"], writes=["scT"])
            for l in range(2):
                for nb in range(12):
                    bi = nb % NB_
                    mi = nb % 2
                    P.dma("sp" if nb % 2 == 0 else "act", f"adaw{bi}", lambda e, l=l, nb=nb, bi=bi: e.dma_start(out=adaw[bi], in_=d_adaw[l, nb]), writes=[f"arena_adaw{bi}"])
                    for kc in range(8):
                        P.op("pe", mm(pb[mi][0:2, :], scT[:, kc, :], adaw[bi][:, kc, :], kc == 0, kc == 7), reads=[f"arena_adaw{bi}", "scT"], writes=[PB[mi]])
                    P.op("dve", lambda e, mi=mi: e.tensor_copy(out=modrow[mi][0:2, :], in_=pb[mi][0:2, :]), writes=[PB[mi], f"arena_modrow{mi}"])
                    for j in range(4):
                        idx = nb * 4 + j
                        P.op("pe", lambda e, idx=idx, j=j, mi=mi: e.transpose(out=pb[2][:, 2 * idx:2 * idx + 2], in_=modrow[mi][0:2, j * 128:(j + 1) * 128], identity=cf[0:2, CI["ident"], 0:2]),
                             reads=[f"arena_modrow{mi}", "cf"], writes=[PB[2]])
                P.op("dve", lambda e, l=l: e.tensor_tensor(out=modT[l][:], in0=pb[2][:, 0:96].rearrange("p (a b) -> p a b", b=2),
                                                          in1=adab[:, l, :].unsqueeze(2).to_broadcast([128, 48, 2]), op=ALU.add),
                     reads=["adab"], writes=[PB[2], f"modT{l}"])
                for col in range(2):
                    P.op("dve", lambda e, l=l, col=col: e.scalar_tensor_tensor(
                        out=A1[l][:, :, col], in0=modT[l][:, 8:16, col], scalar=1.0, in1=gmix[:, l, :], op0=ALU.add, op1=ALU.mult),
                        reads=[f"modT{l}", "gmix"], writes=[f"A1_{l}"])
                    P.op("dve", lambda e, l=l, col=col: e.scalar_tensor_tensor(
                        out=A2[l][:, :, col], in0=modT[l][:, 32:40, col], scalar=1.0, in1=gffn[:, l, :], op0=ALU.add, op1=ALU.mult),
                        reads=[f"modT{l}", "gffn"], writes=[f"A2_{l}"])

        def SH1(l, c, col): return modT[l][:, 0 + c, col:col + 1]
        def G1(l, c, col): return modT[l][:, 16 + c, col:col + 1]
        def SH2(l, c, col): return modT[l][:, 24 + c, col:col + 1]
        def G2(l, c, col): return modT[l][:, 40 + c, col:col + 1]

        def norm_mod(cv, A, SH, l, lo, hi, out_bf, out_key, out_f32=None, perm_cols=False, after_block=None, bs=512, skey=None):
            if skey is None:
                skey = lambda c, t0: SKEY[c]
            sq = [cv.take([128, bs]) for _ in range(2)]
            rstd = cv.take([128, bs])
            tmp = [cv.take([128, bs]) for _ in range(2)]
            for bix, (t0, n, col) in enumerate(_blocks(lo, hi, bs)):
                o32 = out_f32[bix % 2] if out_f32 is not None else None
                okey = out_key(t0) if callable(out_key) else out_key
                k32 = "nm_h32_0"
                for c in range(8):
                    P.op("act", lambda e, c=c, t0=t0, n=n: e.activation(out=sq[c % 2][:, 0:n], in_=sT[:, c, t0:t0 + n], func=AF.Square),
                         reads=[skey(c, t0)], writes=[f"nm_sq{c % 2}"])
                    P.op("pe", mm(pb[7][:, 0:n], ones_f, sq[c % 2][:, 0:n], c == 0, c == 7), reads=[f"nm_sq{c % 2}", "cf"], writes=[PB[7]])
                P.op("dve", lambda e, n=n: e.tensor_scalar(out=rstd[:, 0:n], in0=pb[7][:, 0:n], scalar1=1.0 / D, scalar2=EPS,
                                                          op0=ALU.mult, op1=ALU.add), reads=[PB[7]], writes=["nm_rstd"])
                P.op("act", lambda e, n=n: e.activation(out=rstd[:, 0:n], in_=rstd[:, 0:n], func=AF.Sqrt), reads=["nm_rstd"], writes=["nm_rstd"])
                P.op("dve", lambda e, n=n: e.reciprocal(out=rstd[:, 0:n], in_=rstd[:, 0:n]), reads=["nm_rstd"], writes=["nm_rstd"])
                for c in range(8):
                    tp = tmp[c % 2]
                    P.op("dve", lambda e, c=c, t0=t0, n=n, tp=tp: e.tensor_tensor(out=tp[:, 0:n], in0=sT[:, c, t0:t0 + n], in1=rstd[:, 0:n], op=ALU.mult),
                         reads=[skey(c, t0), "nm_rstd"], writes=[f"nm_tmp{c % 2}"])
                    src = tp[:, 0:n]
                    if out_f32 is not None:
                        dst, dkey = o32[:, c, 0:n], k32
                    else:
                        dst, dkey = out_bf[:, c, t0:t0 + n], okey
                        if perm_cols and t0 >= CTX:
                            r0, nr = (t0 - CTX) // 64, n // 64
                            dst = out_bf[:, c, CTX:NT].rearrange("p (col row) -> p row col", row=32)[:, r0:r0 + nr, :]
                            src = src.rearrange("p (r c) -> p r c", c=64)
                    P.op("dve", lambda e, c=c, col=col, dst=dst, src=src: e.tensor_scalar(
                        out=dst, in0=src, scalar1=A[l][:, c, col:col + 1], scalar2=SH(l, c, col), op0=ALU.mult, op1=ALU.add),
                        reads=[f"nm_tmp{c % 2}", f"A1_{l}", f"A2_{l}", f"modT{l}"], writes=[dkey])
                    if out_f32 is not None:
                        P.op("act", lambda e, c=c, t0=t0, n=n, o32=o32: e.copy(out=out_bf[:, c, t0:t0 + n], in_=o32[:, c, 0:n]), reads=[k32], writes=[okey])
                if after_block is not None:
                    after_block(t0, n)

        def moe(l, lo, hi):
            P.barrier()
            cv = Carver()
            h32s = [cv.take([128, 8, 256])] * 2
            rw = cv.take([128, 8, NE])
            rb = cv.take([128, NTILE, NE])
            cw_all = cv.take([128, NTILE, NE])
            NTN = NTILE * NE
            rt = [cv.take([128, NTILE, NE]) for _ in range(5)]
            r4 = [cv.take([128, NTILE * 4]) for _ in range(4)]
            r1 = [cv.take([128, NTILE]) for _ in range(2)]
            wgb = [cv.take([128, 8, DEXP], BF16) for _ in range(2)]
            wub = [cv.take([128, 8, DEXP], BF16) for _ in range(2)]
            wdb = [cv.take([128, 4, D], BF16) for _ in range(2)]
            aT = [cv.take([128, 4, 512], BF16) for _ in range(2)]
            sg = [cv.take([128, 512], BF16) for _ in range(2)]
            t1 = [cv.take([128, 512], BF16) for _ in range(2)]
            cwb = [cv.take([128, 512], BF16) for _ in range(2)]
            P.dma("sp", "rw", lambda e: e.dma_start(out=rw, in_=d_rw), writes=["moe_rw"])
            P.dma("sp", "rb", lambda e: e.dma_start(out=rb, in_=d_rb), writes=["moe_rb"])
            T0, T1 = lo // 128, hi // 128
            s_all = rt[0]
            blk_ctr = [0]

            def route(t0, n):
                hb = 0
                for sub in range(n // 128):
                    tix = (t0 + sub * 128) // 128
                    pl, kl = pb[7], PB[7]
                    for c in range(8):
                        P.op("pe", mm(pl[:, 256:256 + NE], h32s[hb][:, c, sub * 128:(sub + 1) * 128], rw[:, c, :], c == 0, c == 7),
                             reads=[f"nm_h32_{hb}", "moe_rw"], writes=[kl])
                    P.op("act", lambda e, tix=tix, pl=pl: e.activation(out=s_all[:, tix, :], in_=pl[:, 256:256 + NE], func=AF.Sigmoid), writes=[kl, f"rt_s{tix}"])

            sel_, sel2_, eq1, eq2 = rt[1:5]
            w_ = sel2_
            m1, m2, gs, geq = r4
            bm, ws = r1
            cw16 = cw_all

            def route_batch(Ta, Tb):
                TS = slice(Ta, Tb)
                nT = Tb - Ta
                K = lambda nm: f"{nm}{Ta}"
                v3 = lambda a: a[:, TS, :].rearrange("p t (g k) -> p (t g) k", k=4)
                g2 = lambda a: a[:, Ta * 4:Tb * 4]
                bc4 = lambda a: g2(a).unsqueeze(2).to_broadcast([128, nT * 4, 4])
                g3 = lambda a: g2(a).rearrange("p (t g) -> p t g", g=4)
                skeys = [f"rt_s{t_}" for t_ in range(Ta, Tb)]
                P.op("dve", lambda e: e.tensor_tensor(out=sel_[:, TS, :], in0=s_all[:, TS, :], in1=rb[:, TS, :], op=ALU.add), reads=skeys + ["moe_rb"], writes=[K("rt_sel")])
                P.op("dve", lambda e: e.tensor_reduce(out=g2(m1), in_=v3(sel_), axis=AX.X, op=ALU.max), reads=[K("rt_sel")], writes=[K("rt_m1")])
                P.op("dve", lambda e: e.tensor_tensor(out=v3(eq1), in0=v3(sel_), in1=bc4(m1), op=ALU.is_equal), reads=[K("rt_sel"), K("rt_m1")], writes=[K("rt_eq1")])
                P.op("dve", lambda e: e.scalar_tensor_tensor(out=sel2_[:, TS, :], in0=eq1[:, TS, :], scalar=-1e9, in1=sel_[:, TS, :], op0=ALU.mult, op1=ALU.add),
                     reads=[K("rt_eq1"), K("rt_sel")], writes=[K("rt_sel2")])
                P.op("dve", lambda e: e.tensor_reduce(out=g2(m2), in_=v3(sel2_), axis=AX.X, op=ALU.max), reads=[K("rt_sel2")], writes=[K("rt_m2")])
                P.op("dve", lambda e: e.tensor_tensor(out=v3(eq2), in0=v3(sel2_), in1=bc4(m2), op=ALU.is_equal), reads=[K("rt_sel2"), K("rt_m2")], writes=[K("rt_eq2")])
                P.op("dve", lambda e: e.tensor_tensor(out=g2(gs), in0=g2(m1), in1=g2(m2), op=ALU.add), reads=[K("rt_m1"), K("rt_m2")], writes=[K("rt_gs")])
                P.op("dve", lambda e: e.tensor_reduce(out=bm[:, TS], in_=g3(gs), axis=AX.X, op=ALU.max), reads=[K("rt_gs")], writes=[K("rt_bm")])
                P.op("dve", lambda e: e.tensor_tensor(out=g3(geq), in0=g3(gs), in1=bm[:, TS].unsqueeze(2).to_broadcast([128, nT, 4]), op=ALU.is_equal),
                     reads=[K("rt_gs"), K("rt_bm")], writes=[K("rt_geq")])
                P.op("dve", lambda e: e.tensor_tensor(out=eq1[:, TS, :], in0=eq1[:, TS, :], in1=eq2[:, TS, :], op=ALU.add), reads=[K("rt_eq2")], writes=[K("rt_eq1")])
                P.op("dve", lambda e: e.tensor_tensor(out=v3(eq1), in0=v3(eq1), in1=bc4(geq), op=ALU.mult), reads=[K("rt_geq")], writes=[K("rt_eq1")])
                P.op("dve", lambda e: e.tensor_tensor(out=w_[:, TS, :], in0=eq1[:, TS, :], in1=s_all[:, TS, :], op=ALU.mult),
                     reads=[K("rt_eq1"), K("rt_eq2"), K("rt_m2")] + skeys, writes=[K("rt_w"), K("rt_sel2")])
                P.op("dve", lambda e: e.tensor_reduce(out=ws[:, TS], in_=w_[:, TS, :], axis=AX.X, op=ALU.add), reads=[K("rt_w")], writes=[K("rt_ws")])
                P.op("dve", lambda e: e.reciprocal(out=ws[:, TS], in_=ws[:, TS]), writes=[K("rt_ws")])
                P.op("dve", lambda e: e.tensor_tensor(out=cw16[:, TS, :], in0=w_[:, TS, :], in1=ws[:, TS].unsqueeze(2).to_broadcast([128, nT, NE]), op=ALU.mult),
                     reads=[K("rt_w"), K("rt_ws")], writes=[f"moe_cw{t_}" for t_ in range(Ta, Tb)])

            def after_blk(t0, n):
                route(t0, n)
                t1_ = t0 + n
                for (bt0, bn, _c) in _blocks(lo, hi):
                    if bt0 + bn == t1_:
                        route_batch(bt0 // 128, (bt0 + bn) // 128)

            norm_mod(cv, A2, SH2, l, lo, hi, out_bf=hT, out_key=lambda t0: f"hT_{t0 // 256}", out_f32=h32s, after_block=after_blk, bs=256,
                     skey=lambda c, t0: f"sT{c}_{t0 // 256}")

            blocks = _blocks(lo, hi)
            items = [(e_, b_) for e_ in range(NE) for b_ in range(len(blocks))]

            def load_w(e_):
                bi = e_ % 2
                P.dma("pool", f"wg{bi}", lambda e: e.dma_start(out=wgb[bi], in_=d_wg[l, e_], max_dma_last_dim=4096), writes=[f"moe_wg{bi}"])
                P.dma("pool", f"wu{bi}", lambda e: e.dma_start(out=wub[bi], in_=d_wu[l, e_], max_dma_last_dim=4096), writes=[f"moe_wu{bi}"])
                P.dma("pool", f"wd{bi}", lambda e: e.dma_start(out=wdb[bi], in_=d_wd[l, e_], max_dma_last_dim=4096), writes=[f"moe_wd{bi}"])

            cwcol = [cv.take([128, 128], BF16) for _ in range(4)]
            cwc_ctr = [0]

            def stage1(i):
                e_, b_ = items[i]
                t0, n, col = blocks[b_]
                bi = e_ % 2
                ai = i % 2
                hkeys = [f"hT_{k_}" for k_ in range(t0 // 256, (t0 + n + 255) // 256)]
                for sub in range(n // 128):
                    tix = (t0 + sub * 128) // 128
                    ci_ = cwc_ctr[0] % 4
                    cwc_ctr[0] += 1
                    P.op("dve", lambda e, tix=tix, ci_=ci_: e.tensor_copy(out=cwcol[ci_], in_=cw16[:, tix, e_:e_ + 1].to_broadcast([128, 128])),
                         reads=[f"moe_cw{tix}"], writes=[f"moe_cwcol{ci_}"])
                    P.op("pe", mm(pb[6][:, sub * 128:(sub + 1) * 128], cwcol[ci_], ident_b, True, True),
                         reads=[f"moe_cwcol{ci_}", "cb"], writes=[PB[6]])
                P.op("act", lambda e: e.copy(out=cwb[ai][:, 0:n], in_=pb[6][:, 0:n]), reads=[PB[6]], writes=[f"moe_cwb{ai}"])
                for fc in range(4):
                    pg, pu = pb[(fc % 2) * 2], pb[(fc % 2) * 2 + 1]
                    kg, ku = PB[(fc % 2) * 2], PB[(fc % 2) * 2 + 1]
                    for c in range(8):
                        P.op("pe", mm(pg[:, 0:n], wgb[bi][:, c, fc * 128:(fc + 1) * 128], hT[:, c, t0:t0 + n], c == 0, c == 7),
                             reads=[f"moe_wg{bi}"] + hkeys, writes=[kg])
                    for c in range(8):
                        P.op("pe", mm(pu[:, 0:n], wub[bi][:, c, fc * 128:(fc + 1) * 128], hT[:, c, t0:t0 + n], c == 0, c == 7),
                             reads=[f"moe_wu{bi}"] + hkeys, writes=[ku])
                    si = fc % 2
                    P.op("act", lambda e, pg=pg, si=si: e.activation(out=sg[si][:, 0:n], in_=pg[:, 0:n], func=AF.Silu), reads=[kg], writes=[f"moe_sg{si}"])
                    P.op("dve", lambda e, pu=pu, si=si: e.tensor_tensor(out=t1[si][:, 0:n], in0=sg[si][:, 0:n], in1=pu[:, 0:n], op=ALU.mult),
                         reads=[f"moe_sg{si}", ku], writes=[f"moe_t1{si}"])
                    P.op("pool", lambda e, si=si, fc=fc: e.tensor_tensor(out=aT[ai][:, fc, 0:n], in0=t1[si][:, 0:n], in1=cwb[ai][:, 0:n], op=ALU.mult),
                         reads=[f"moe_t1{si}", f"moe_cwb{ai}"], writes=[f"moe_aT{ai}"])

            def stage2(i):
                e_, b_ = items[i]
                t0, n, col = blocks[b_]
                bi = e_ % 2
                ai = i % 2
                for dc in range(8):
                    pd, kd = pb[4 + dc % 2], PB[4 + dc % 2]
                    for fc in range(4):
                        P.op("pe", mm(pd[:, 0:n], wdb[bi][:, fc, dc * 128:(dc + 1) * 128], aT[ai][:, fc, 0:n], fc == 0, fc == 3),
                             reads=[f"moe_wd{bi}", f"moe_aT{ai}"], writes=[kd])
                    sk = [f"sT{dc}_{k_}" for k_ in range(t0 // 256, (t0 + n + 255) // 256)]
                    P.op("dve", lambda e, dc=dc, pd=pd: e.scalar_tensor_tensor(
                        out=sT[:, dc, t0:t0 + n], in0=pd[:, 0:n], scalar=G2(l, dc, col), in1=sT[:, dc, t0:t0 + n], op0=ALU.mult, op1=ALU.add),
                        reads=[kd, f"modT{l}"] + sk, writes=sk)

            load_w(0)
            for i in range(len(items)):
                e_, b_ = items[i]
                stage1(i)
                if i > 0:
                    stage2(i - 1)
                if b_ == 0 and e_ + 1 < NE:
                    load_w(e_ + 1)
            stage2(len(items) - 1)

        def mlstm(l):
            cv = Carver()
            wgt = cv.take([128, 8, 16], BF16)
            mgb = cv.take([128, 16])
            mhg = cv.take([128, 256])
            cvw = cv.take([128, 8, 4])
            G = cv.take([128, NTILE, 16])
            Lg = cv.take([128, NTILE, 8])
            EE = cv.take([128, NTILE, 24])
            EB = cv.take([128, NTILE, 8])
            arg = cv.take([128, 24])
            P.dma("pool", "m_wgt", lambda e: e.dma_start(out=wgt, in_=d_mwin[:, :, 3072:3088]), writes=["m_wgt"])
            P.dma("sp", "m_mgb", lambda e: e.dma_start(out=mgb, in_=d_mgb), writes=["m_mgb"])
            P.dma("sp", "m_cvw", lambda e: e.dma_start(out=cvw, in_=d_mconv), writes=["m_cvw"])

            norm_mod(cv, A1, SH1, l, 0, NT, out_bf=hT, out_key="hT", bs=256)

            for tl in range(NTILE):
                ts_ = slice(tl * 128, (tl + 1) * 128)
                for c in range(8):
                    P.op("pe", mm(pb[0][:, 0:16], hT[:, c, ts_], wgt[:, c, :], c == 0, c == 7), reads=["hT", "m_wgt"], writes=[PB[0]])
                P.op("dve", lambda e, tl=tl: e.tensor_tensor(out=G[:, tl, :], in0=pb[0][:, 0:16], in1=mgb, op=ALU.add), reads=[PB[0], "m_mgb"], writes=["m_G"])
                for k, src in enumerate((slice(4, 8), slice(12, 16))):
                    P.op("act", lambda e, tl=tl, k=k, src=src: e.activation(out=Lg[:, tl, 4 * k:4 * k + 4], in_=G[:, tl, src], func=AF.Exp, scale=-1.0),
                         reads=["m_G"], writes=["m_L"])
                P.op("dve", lambda e, tl=tl: e.tensor_scalar(out=Lg[:, tl, :], in0=Lg[:, tl, :], scalar1=1.0, scalar2=None, op0=ALU.add), reads=["m_L"], writes=["m_L"])
                P.op("act", lambda e, tl=tl: e.activation(out=Lg[:, tl, :], in_=Lg[:, tl, :], func=AF.Ln), reads=["m_L"], writes=["m_L"])
                q_ = pb[1]
                for j, (mat, cs) in enumerate((("tF", slice(0, 4)), ("tB", slice(4, 8)), ("sF", slice(0, 4)), ("sB", slice(4, 8)))):
                    P.op("pe", mm(q_[:, 4 * j:4 * j + 4], cf[:, CI[mat], :], Lg[:, tl, cs], True, True), reads=["cf", "m_L"], writes=[PB[1]])
                P.op("pe", mm(q_[:, 16:24], cf[:, CI["ones"], :], Lg[:, tl, :], True, True), reads=["cf", "m_L"], writes=[PB[1]])
                P.op("dve", lambda e, tl=tl: e.tensor_tensor(out=arg[:, 0:4], in0=G[:, tl, 0:4], in1=q_[:, 0:4], op=ALU.add), reads=["m_G"], writes=["m_arg", PB[1]])
                P.op("dve", lambda e, tl=tl: e.tensor_tensor(out=arg[:, 4:8], in0=G[:, tl, 0:4], in1=q_[:, 8:12], op=ALU.subtract), reads=["m_G"], writes=["m_arg", PB[1]])
                P.op("dve", lambda e, tl=tl: e.tensor_tensor(out=arg[:, 8:12], in0=G[:, tl, 8:12], in1=q_[:, 4:8], op=ALU.add), reads=["m_G"], writes=["m_arg", PB[1]])
                P.op("dve", lambda e, tl=tl: e.tensor_tensor(out=arg[:, 12:16], in0=G[:, tl, 8:12], in1=q_[:, 12:16], op=ALU.subtract), reads=["m_G"], writes=["m_arg", PB[1]])
                P.op("dve", lambda e, tl=tl: e.tensor_copy(out=arg[:, 16:24], in_=q_[:, 0:8]), writes=["m_arg", PB[1]])
                P.op("act", lambda e, tl=tl: e.activation(out=EE[:, tl, :], in_=arg, func=AF.Exp), reads=["m_arg"], writes=["m_EE"])
                P.op("act", lambda e, tl=tl: e.activation(out=EB[:, tl, :], in_=q_[:, 16:24], func=AF.Exp, scale=-1.0), writes=["m_EB", PB[1]])

            assert cv.off <= MODS_BASE, (cv.off, MODS_BASE)
            P.barrier()
            wqk = cv.take([128, 8, 2, 128], BF16)
            wv = cv.take([128, 8, 256], BF16)
            wo = cv.take([128, 8, 256], BF16)
            wout = cv.take([128, 2, D], BF16)
            qT = cv.take([128, NT], BF16)
            kT = cv.take([128, NT], BF16)
            vaug = cv.take([128, NTILE, 257], BF16)
            ktok = cv.take([128, NTILE, 128], BF16)
            hfirst = cv.take([128, NTILE, 256], BF16)
            ybuf = [cv.take([128, 512]) for _ in range(2)]
            cvs = cv
            STf = [[cvs.take([128, 128], BF16) for _ in range(2)] for _ in range(2)]
            vt = [[cvs.take([128, 257], BF16) for _ in range(2)] for _ in range(2)]
            vt2 = [[cvs.take([128, 257], BF16) for _ in range(2)] for _ in range(2)]
            X = [cvs.take([128, 257]) for _ in range(2)]
            Xb = [[cvs.take([128, 257], BF16) for _ in range(2)] for _ in range(2)]
            numS = [[cvs.take([128, 257]) for _ in range(2)] for _ in range(2)]
            hs = [cvs.take([128, 256]) for _ in range(2)]
            hn = hs
            hg = [cvs.take([128, 256], BF16) for _ in range(2)]
            sgo = [cvs.take([128, 256]) for _ in range(2)]
            tmpo = [cvs.take([128, 4, 128]) for _ in range(2)]
            sm = [[cvs.take([128, 1]) for _ in range(2)] for _ in range(2)]
            xTt = [cvs.take([128, 2, 128], BF16) for _ in range(2)]
            P.op("pool", lambda e: e.memset(vaug[:, :, 256:257], 1.0), writes=["m_vaug"])
            pb6b = pb[6].bitcast(BF16)

            for h in range(4):
                P.dma("sp", "m_mhg", lambda e, h=h: e.dma_start(out=mhg, in_=d_mhg[:, h * 256:(h + 1) * 256]), writes=["m_mhg"])
                P.dma("pool", "m_wqk", lambda e, h=h: [e.dma_start(out=wqk[:, :, 0, :], in_=d_mwin[:, :, h * 128:(h + 1) * 128]),
                                                       e.dma_start(out=wqk[:, :, 1, :], in_=d_mwin[:, :, 512 + h * 128:512 + (h + 1) * 128])],
                      writes=["m_wqk"], n=2)
                P.dma("pool", "m_wv", lambda e, h=h: e.dma_start(out=wv, in_=d_mwin[:, :, 1024 + h * 256:1024 + (h + 1) * 256]), writes=["m_wv"])
                P.dma("pool", "m_wo", lambda e, h=h: e.dma_start(out=wo, in_=d_mwin[:, :, 2048 + h * 256:2048 + (h + 1) * 256]), writes=["m_wo"])
                P.dma("pool", "m_wout", lambda e, h=h: e.dma_start(out=wout, in_=d_mwout[:, 2 * h:2 * h + 2, :], max_dma_last_dim=4096), writes=["m_wout"])
                qk_blocks = [(0, CTX, 0, CTX)] + [(CTX + 410 * j, min(CTX + 410 * (j + 1), NT), CTX, NT) for j in range(5)]
                cnt_ = [0]
                for which, dstT, dkey in ((0, qT, "m_qT"), (1, kT, "m_kT")):
                    ch = (0 if which == 0 else 4) + h
                    for (a_, b_, A_, B_) in qk_blocks:
                        n = b_ - a_
                        a2, b2 = max(a_ - 1, A_), min(b_ + 1, B_)
                        off = a_ - a2
                        N_ = b2 - a2
                        bi_ = cnt_[0] % 2
                        cnt_[0] += 1
                        pq, kq = pb[bi_], PB[bi_]
                        yb, ky = ybuf[bi_], f"m_y{bi_}"
                        for c in range(8):
                            P.op("pe", mm(pq[:, 0:N_], wqk[:, c, which, :], hT[:, c, a2:b2], c == 0, c == 7), reads=["m_wqk", "hT"], writes=[kq])
                        P.op("dve", lambda e, pq=pq, yb=yb, off=off, n=n, ch=ch: e.tensor_scalar(out=yb[:, 0:n], in0=pq[:, off:off + n], scalar1=cvw[:, ch, 1:2], scalar2=cvw[:, ch, 3:4],
                                                                                       op0=ALU.mult, op1=ALU.add), reads=["m_cvw"], writes=[kq, ky])
                        if off == 1:
                            o0, i0, m0 = 0, 0, n
                        else:
                            o0, i0, m0 = 1, 0, n - 1
                        P.op("dve", lambda e, pq=pq, yb=yb, o0=o0, i0=i0, m0=m0, ch=ch: e.scalar_tensor_tensor(
                            out=yb[:, o0:o0 + m0], in0=pq[:, i0:i0 + m0], scalar=cvw[:, ch, 0:1], in1=yb[:, o0:o0 + m0], op0=ALU.mult, op1=ALU.add),
                            reads=["m_cvw"], writes=[kq, ky])
                        m2 = n if b2 > b_ else n - 1
                        P.op("dve", lambda e, pq=pq, yb=yb, off=off, m2=m2, ch=ch: e.scalar_tensor_tensor(
                            out=yb[:, 0:m2], in0=pq[:, off + 1:off + 1 + m2], scalar=cvw[:, ch, 2:3], in1=yb[:, 0:m2], op0=ALU.mult, op1=ALU.add),
                            reads=["m_cvw"], writes=[kq, ky])
                        if which == 0:
                            P.op("act", lambda e, yb=yb, n=n: e.activation(out=yb[:, 0:n], in_=yb[:, 0:n], func=AF.Silu), writes=[ky])
                            P.op("act", lambda e, yb=yb, n=n, a_=a_, b_=b_: e.activation(out=qT[:, a_:b_], in_=yb[:, 0:n], func=AF.Copy, scale=float(128 ** -0.5)),
                                 reads=[ky], writes=[dkey])
                        else:
                            P.op("act", lambda e, yb=yb, n=n, a_=a_, b_=b_: e.activation(out=kT[:, a_:b_], in_=yb[:, 0:n], func=AF.Silu), reads=[ky], writes=[dkey])
                for tl in range(NTILE):
                    ts_ = slice(tl * 128, (tl + 1) * 128)
                    pv, kv_ = pb[2 + tl % 2], PB[2 + tl % 2]
                    for c in range(8):
                        P.op("pe", mm(pv[:, 0:256], hT[:, c, ts_], wv[:, c, :], c == 0, c == 7), reads=["hT", "m_wv"], writes=[kv_])
                    P.op("act", lambda e, tl=tl, pv=pv: e.copy(out=vaug[:, tl, 0:256], in_=pv[:, 0:256]), reads=[kv_], writes=["m_vaug"])
                    tb = pb[4 + tl % 2].bitcast(BF16)
                    P.op("pe", lambda e, ts_=ts_, tb=tb: e.transpose(out=tb[:, 0:128], in_=kT[:, ts_], identity=ident_b), reads=["m_kT", "cb"], writes=[PB[4 + tl % 2]])
                    P.op("act", lambda e, tl=tl, tb=tb: e.copy(out=ktok[:, tl, :], in_=tb[:, 0:128]), reads=[PB[4 + tl % 2]], writes=["m_ktok"])

                orders = [list(range(NTILE)), [1, 0] + list(range(NTILE - 1, 1, -1))]
                if DEBUG_NT is not None:
                    orders = [o_[:DEBUG_NT] for o_ in orders]
                visited = set()
                for d_ in range(2):
                    P.op("dve", lambda e, d_=d_: e.memset(X[d_], 0.0), writes=[f"m_X{d_}"])
                    P.op("dve", lambda e, d_=d_: e.memset(Xb[d_][0], 0.0), writes=[f"m_Xb{d_}_0"])
                ncomp = [0]

                def step(d_, it, tl):
                    ts_ = slice(tl * 128, (tl + 1) * 128)
                    pi = it % 2
                    eoff = 0 if d_ == 0 else 8
                    doff = 4 if d_ else 0
                    second = tl in visited
                    visited.add(tl)
                    sc_cols = slice(128 * d_, 128 * d_ + 128)
                    P.op("pe", mm(pb[6][:, sc_cols], kT[:, ts_], qT[:, ts_], True, True), reads=["m_kT", "m_qT"], writes=[PB[6]])
                    msk = cb[:, CI["tF"], :] if d_ == 0 else cb[:, CI["tB"], :]
                    P.op("dve", lambda e: e.tensor_tensor(out=STf[d_][pi], in0=pb[6][:, sc_cols], in1=msk, op=ALU.mult),
                         reads=["cb"], writes=[PB[6], f"m_STf{d_}{pi}"])
                    e1 = EE[:, tl, eoff + h:eoff + h + 1]
                    e2 = EE[:, tl, eoff + 4 + h:eoff + 5 + h]
                    P.op("act", lambda e: e.activation(out=vt[d_][pi], in_=vaug[:, tl, :], func=AF.Copy, scale=e1),
                         reads=["m_vaug", "m_EE"], writes=[f"m_vt{d_}{pi}"])
                    P.op("dve", lambda e: e.tensor_scalar(out=vt2[d_][pi], in0=vaug[:, tl, :], scalar1=e2, scalar2=None, op0=ALU.mult),
                         reads=["m_vaug", "m_EE"], writes=[f"m_vt2{d_}{pi}"])
                    if second:
                        for c in range(8):
                            P.op("pe", mm(pb[7][:, 0:256], hT[:, c, ts_], wo[:, c, :], c == 0, c == 7), reads=["hT", "m_wo"], writes=[PB[7]])
                        P.op("act", lambda e: e.activation(out=sgo[d_], in_=pb[7][:, 0:256], func=AF.Exp, scale=-1.0), writes=[PB[7], f"m_sgo{d_}"])
                        P.op("dve", lambda e: e.tensor_scalar(out=sgo[d_], in0=sgo[d_], scalar1=1.0, scalar2=None, op0=ALU.add), writes=[f"m_sgo{d_}"])
                        P.op("act", lambda e: e.activation(out=sgo[d_], in_=sgo[d_], func=AF.Ln), writes=[f"m_sgo{d_}"])
                        P.op("act", lambda e: e.activation(out=sgo[d_], in_=sgo[d_], func=AF.Exp, scale=-1.0), writes=[f"m_sgo{d_}"])
                    num, knum = pb[d_], PB[d_]
                    P.op("pe", mm(num[:, 0:257], STf[d_][pi], vt[d_][pi], True, False), reads=[f"m_STf{d_}{pi}", f"m_vt{d_}{pi}"], writes=[knum])
                    kv, kkv = pb[2 + d_], PB[2 + d_]
                    P.op("pe", mm(kv[:, 0:257], ktok[:, tl, :], vt2[d_][pi], True, True), reads=["m_ktok", f"m_vt2{d_}{pi}"], writes=[kkv])
                    r0, r1 = it % 2, (it + 1) % 2
                    P.op("pe", mm(num[:, 0:257], qT[:, ts_], Xb[d_][r0], False, True), reads=["m_qT", f"m_Xb{d_}_{r0}", knum], writes=[knum])
                    ebc = EB[:, tl, doff + h:doff + h + 1]
                    P.op("dve", lambda e: e.scalar_tensor_tensor(out=X[d_], in0=X[d_], scalar=ebc, in1=kv[:, 0:257], op0=ALU.mult, op1=ALU.add),
                         reads=["m_EB", f"m_X{d_}"], writes=[kkv, f"m_X{d_}"])
                    P.op("act", lambda e: e.copy(out=Xb[d_][r1], in_=X[d_]), reads=[f"m_X{d_}"], writes=[f"m_Xb{d_}_{r1}"])
                    ns, kns = numS[d_][pi], f"m_numS{d_}{pi}"
                    P.op("act", lambda e: e.copy(out=ns, in_=num[:, 0:257]), writes=[knum, kns])
                    thr = EE[:, tl, 16 + doff + h:16 + doff + h + 1]
                    s0, ks0 = sm[d_][0], f"m_sm{d_}0"
                    P.op("act", lambda e: e.activation(out=s0, in_=ns[:, 256:257], func=AF.Abs), reads=[kns], writes=[ks0])
                    P.op("dve", lambda e: e.tensor_tensor(out=s0, in0=s0, in1=thr, op=ALU.max), reads=[ks0, "m_EE"], writes=[ks0])
                    P.op("dve", lambda e: e.reciprocal(out=s0, in_=s0), reads=[ks0], writes=[ks0])
                    if not second:
                        P.op("dve", lambda e: e.tensor_scalar(out=hfirst[:, tl, :], in0=ns[:, 0:256], scalar1=s0[:, 0:1], scalar2=None, op0=ALU.mult),
                             reads=[kns, ks0], writes=[f"m_hf{tl}"])
                        return
                    s1, ks1 = sm[d_][1], f"m_sm{d_}1"
                    P.op("dve", lambda e: e.scalar_tensor_tensor(out=hs[d_], in0=ns[:, 0:256], scalar=s0[:, 0:1], in1=hfirst[:, tl, :], op0=ALU.mult, op1=ALU.add),
                         reads=[kns, ks0, f"m_hf{tl}"], writes=[f"m_hs{d_}"])
                    P.op("act", lambda e: e.activation(out=hg[d_], in_=hs[d_], func=AF.Square, accum_out=s1), reads=[f"m_hs{d_}"], writes=[f"m_hg{d_}", ks1])
                    P.op("dve", lambda e: e.tensor_scalar(out=s1, in0=s1, scalar1=1.0 / 256, scalar2=EPS, op0=ALU.mult, op1=ALU.add), reads=[ks1], writes=[ks1])
                    P.op("act", lambda e: e.activation(out=s1, in_=s1, func=AF.Ln), reads=[ks1], writes=[ks1])
                    P.op("act", lambda e: e.activation(out=s1, in_=s1, func=AF.Exp, scale=-0.5), reads=[ks1], writes=[ks1])
                    P.op("dve", lambda e: e.scalar_tensor_tensor(out=hs[d_], in0=hs[d_], scalar=s1[:, 0:1], in1=mhg, op0=ALU.mult, op1=ALU.mult),
                         reads=[f"m_hs{d_}", ks1, "m_mhg"], writes=[f"m_hs{d_}"])
                    P.op("dve", lambda e: e.tensor_tensor(out=hg[d_], in0=hs[d_], in1=sgo[d_], op=ALU.mult), reads=[f"m_hs{d_}", f"m_sgo{d_}", f"m_hg{d_}"], writes=[f"m_hg{d_}"])
                    for fc in range(2):
                        P.op("pe", lambda e, fc=fc: e.transpose(out=pb6b[:, 512 + fc * 128:512 + (fc + 1) * 128], in_=hg[d_][:, fc * 128:(fc + 1) * 128], identity=ident_b),
                             reads=[f"m_hg{d_}", "cb"], writes=[PB[6]])
                    xi = ncomp[0] % 2
                    ncomp[0] += 1
                    P.op("act", lambda e: e.copy(out=xTt[xi], in_=pb6b[:, 512:768].rearrange("p (a b) -> p a b", a=2)), writes=[PB[6], f"m_xT{xi}"])
                    col = 1 if tl < 2 else 0
                    for half in range(2):
                        po, ko = pb[4 + half], PB[4 + half]
                        for j in range(4):
                            dc = half * 4 + j
                            for fc in range(2):
                                P.op("pe", mm(po[:, j * 128:(j + 1) * 128], wout[:, fc, dc * 128:(dc + 1) * 128], xTt[xi][:, fc, :], fc == 0, fc == 1), reads=["m_wout", f"m_xT{xi}"], writes=[ko])
                        g1b = modT[l][:, 16 + half * 4:16 + half * 4 + 4, col:col + 1].to_broadcast([128, 4, 128])
                        P.op("dve", lambda e, po=po, g1b=g1b, half=half: e.tensor_tensor(out=tmpo[half], in0=po[:, :].rearrange("p (a b) -> p a b", a=4), in1=g1b, op=ALU.mult),
                             reads=[f"modT{l}"], writes=[ko, f"m_tmpo{half}"])
                        keys = SKEY[half * 4:half * 4 + 4]
                        P.op("dve", lambda e, half=half: e.tensor_tensor(out=sT[:, half * 4:half * 4 + 4, ts_], in0=sT[:, half * 4:half * 4 + 4, ts_], in1=tmpo[half], op=ALU.add),
                             reads=[f"m_tmpo{half}"] + keys, writes=keys)

                for it in range(len(orders[0])):
                    step(0, it, orders[0][it])
                    step(1, it, orders[1][it])

        def hgrn(l):
            P.barrier()
            cv = Carver()
            hlb = cv.take([128, 2, 16])
            lbT = cv.take([128, 16])
            omlb = cv.take([128, 16])
            hhg = cv.take([128, 128])
            rst = cv.take([128, 512])
            P.dma("sp", "h_hlb", lambda e: e.dma_start(out=hlb, in_=d_hlb), writes=["h_hlb"])
            P.dma("sp", "h_rst", lambda e: e.dma_start(out=rst, in_=d_rst[:, 0:512]), writes=["h_rst"])
            P.op("dve", lambda e: e.tensor_tensor(out=lbT, in0=hlb[:, 1, :], in1=hlb[:, 0, :], op=ALU.subtract), reads=["h_hlb"], writes=["h_lb"])
            P.op("act", lambda e: e.activation(out=omlb, in_=lbT, func=AF.Exp), reads=["h_lb"], writes=["h_omlb"])
            P.op("act", lambda e: e.activation(out=lbT, in_=lbT, func=AF.Exp, scale=-1.0), reads=["h_lb", "h_omlb"], writes=["h_lb"])
            for t_, k_ in ((omlb, "h_omlb"), (lbT, "h_lb")):
                P.op("dve", lambda e, t_=t_: e.tensor_scalar(out=t_, in0=t_, scalar1=1.0, scalar2=None, op0=ALU.add), writes=[k_])
                P.op("dve", lambda e, t_=t_: e.reciprocal(out=t_, in_=t_), writes=[k_])

            ptmp = [cv.take([128, SEQ]) for _ in range(2)]
            for c in range(8):
                pt = ptmp[c % 2]
                src = sT[:, c, CTX:NT].rearrange("p (row col) -> p col row", col=64)
                P.op("dve" if c % 2 == 0 else "pool", lambda e, pt=pt, src=src: e.tensor_copy(out=pt.rearrange("p (col row) -> p col row", row=32), in_=src),
                     reads=[SKEY[c]], writes=[f"h_ptmp{c % 2}"])
                P.op("act", lambda e, pt=pt, c=c: e.copy(out=sT[:, c, CTX:NT], in_=pt), reads=[f"h_ptmp{c % 2}"], writes=[SKEY[c]])
            P.barrier()
            cv.off -= 2 * SEQ * 4
            norm_base = cv.off
            norm_mod(cv, A1, SH1, l, 0, NT, out_bf=hT, out_key="hT", bs=256)
            norm_end = cv.off
            P.barrier()

            wq = cv.take([128, 8, 128], BF16)
            wz = cv.take([128, 8, 2, 128], BF16)
            wi = cv.take([128, 8, 128], BF16)
            wgg = cv.take([128, 8, 128], BF16)
            wout = cv.take([128, D], BF16)
            qs = cv.take([128, NT], BF16)
            cvn = Carver()
            cvn.off = norm_base
            fTs = [cv.take([128, 512]), cvn.take([128, 512])]
            bTs = [cv.take([128, 512]), cvn.take([128, 512])]
            t32s = [cv.take([128, 512]), cv.take([128, 512])]
            kks = [cv.take([128, 512], BF16), cvn.take([128, 512], BF16)]
            assert cvn.off <= norm_end
            totcs = [cv.take([128, 4]) for _ in range(2)]
            rcs = [cv.take([128, 4]) for _ in range(2)]
            t32 = t32s[0]
            qtl = [cv.take([128, NT], BF16) for _ in range(2)]
            ktl = [cv.take([128, NT], BF16) for _ in range(2)]
            kht = [cv.take([128, NT], BF16) for _ in range(2)]
            ebT = [cv.take([128, NTILE]) for _ in range(2)]
            erT = [cv.take([128, NTILE]) for _ in range(2)]
            itok = cv.take([128, NTILE, 128], BF16)
            sgt = cv.take([128, NTILE, 128], BF16)
            gtmp = cv.take([128, 128])
            ofirst = cv.take([128, NTILE, 128], BF16)
            AT = [[cv.take([128, 128], BF16) for _ in range(2)] for _ in range(2)]
            khtok = [[cv.take([128, 128], BF16) for _ in range(2)] for _ in range(2)]
            S = [cv.take([128, 128]) for _ in range(2)]
            Sb = [[cv.take([128, 128], BF16) for _ in range(2)] for _ in range(2)]
            os_ = [cv.take([128, 128]) for _ in range(2)]
            og = [cv.take([128, 128], BF16) for _ in range(2)]
            sm = [cv.take([128, 1]) for _ in range(2)]
            xTt = [cv.take([128, 128], BF16) for _ in range(2)]
            pb7b = pb[7].bitcast(BF16)
            g1row = cv.take([128, D], BF16)
            for dc in range(8):
                P.op("pe", mm(pb[dc % 2][:, 0:128], modT[l][:, 16 + dc, 0:1].to_broadcast([128, 128]), ident_f, True, True), reads=[f"modT{l}", "cf"], writes=[PB[dc % 2]])
                P.op("act", lambda e, dc=dc: e.copy(out=g1row[:, dc * 128:(dc + 1) * 128], in_=pb[dc % 2][:, 0:128]), writes=[PB[dc % 2], "h_g1row"])
            REF = 64
            pieces = [(0, 512), (512, 512), (1024, 512), (1536, 512), (2048, 256)]

            def v128(a):
                return a.rearrange("p (c k) -> p c k", k=128)

            for h in range(8):
                P.dma("sp", "h_hhg", lambda e, h=h: e.dma_start(out=hhg, in_=d_hhg[:, h * 128:(h + 1) * 128]), writes=["h_hhg"])
                P.dma("pool", "h_wq", lambda e, h=h: e.dma_start(out=wq, in_=d_hwin[:, :, h * 128:(h + 1) * 128]), writes=["h_wq"])
                P.dma("pool", "h_wz", lambda e, h=h: [e.dma_start(out=wz[:, :, 0, :], in_=d_hwin[:, :, 1024 + h * 128:1024 + (h + 1) * 128]),
                                                      e.dma_start(out=wz[:, :, 1, :], in_=d_hwin[:, :, 2048 + h * 128:2048 + (h + 1) * 128])], writes=["h_wz"], n=2)
                P.dma("pool", "h_wi", lambda e, h=h: e.dma_start(out=wi, in_=d_hwin[:, :, 3072 + h * 128:3072 + (h + 1) * 128]), writes=["h_wi"])
                P.dma("pool", "h_wg", lambda e, h=h: e.dma_start(out=wgg, in_=d_hwin[:, :, 4096 + h * 128:4096 + (h + 1) * 128]), writes=["h_wg"])
                P.dma("pool", "h_wout", lambda e, h=h: e.dma_start(out=wout, in_=d_hwout[:, h, :], max_dma_last_dim=4096), writes=["h_wout"])
                P.op("dve", lambda e: e.tensor_tensor(out=wout, in0=wout, in1=g1row, op=ALU.mult), reads=["h_g1row"], writes=["h_wout"])
                for bi_, (t0, n, col) in enumerate(_blocks(0, NT)):
                    pq, kq = pb[bi_ % 2], PB[bi_ % 2]
                    for c in range(8):
                        P.op("pe", mm(pq[:, 0:n], wq[:, c, :], hT[:, c, t0:t0 + n], c == 0, c == 7), reads=["h_wq", "hT"], writes=[kq])
                    P.op("act", lambda e, n=n, pq=pq: e.activation(out=t32[:, 0:n], in_=pq[:, 0:n], func=AF.Exp, scale=-1.0), writes=[kq, "h_t32_0"])
                    P.op("act", lambda e, n=n: e.activation(out=t32[:, 0:n], in_=t32[:, 0:n], func=AF.Ln, bias=1.0), writes=["h_t32_0"])
                    P.op("act", lambda e, n=n: e.activation(out=t32[:, 0:n], in_=t32[:, 0:n], func=AF.Exp, scale=-1.0), writes=["h_t32_0"])
                    P.op("dve", lambda e, t0=t0, n=n, pq=pq: e.tensor_tensor(out=qs[:, t0:t0 + n], in0=t32[:, 0:n], in1=pq[:, 0:n], op=ALU.mult),
                         reads=["h_t32_0"], writes=[kq, "h_qs"])
                for tl in range(NTILE):
                    ts_ = slice(tl * 128, (tl + 1) * 128)
                    pi_, ki_ = pb[2 + tl % 2], PB[2 + tl % 2]
                    for c in range(8):
                        P.op("pe", mm(pi_[:, 0:128], hT[:, c, ts_], wi[:, c, :], c == 0, c == 7), reads=["hT", "h_wi"], writes=[ki_])
                    if tl >= 2:
                        for c in range(8):
                            P.op("pe", mm(pi_[:, 128:256], hT[:, c, ts_], wgg[:, c, :], c == 0, c == 7), reads=["hT", "h_wg"], writes=[ki_])
                    P.op("act", lambda e, tl=tl, pi_=pi_: e.copy(out=itok[:, tl, :], in_=pi_[:, 0:128]), writes=[ki_, "h_itok"])
                    if tl >= 2:
                        P.op("act", lambda e, pi_=pi_: e.activation(out=gtmp, in_=pi_[:, 128:256], func=AF.Exp, scale=-1.0), writes=[ki_, "h_gtmp"])
                        P.op("act", lambda e: e.activation(out=gtmp, in_=gtmp, func=AF.Ln, bias=1.0), writes=["h_gtmp"])
                        P.op("act", lambda e: e.activation(out=gtmp, in_=gtmp, func=AF.Exp, scale=-1.0), writes=["h_gtmp"])
                        P.op("dve", lambda e, tl=tl, pi_=pi_: e.tensor_tensor(out=sgt[:, tl, :], in0=gtmp, in1=pi_[:, 128:256], op=ALU.mult), reads=["h_gtmp"], writes=[ki_, "h_sgt"])

                def precompute(d_, p0, pn, pk):
                    fT, bT, t32, kk, totc, rc = fTs[d_], bTs[d_], t32s[d_], kks[d_], totcs[d_], rcs[d_]
                    kx = f"_{d_}"
                    lbc = lbT[:, d_ * 8 + h:d_ * 8 + h + 1]
                    omc = omlb[:, d_ * 8 + h:d_ * 8 + h + 1]
                    nt = pn // 128
                    tsl = slice(p0 // 128, p0 // 128 + nt)
                    ps_ = slice(p0, p0 + pn)
                    pz, kz = pb[4 + d_], PB[4 + d_]
                    for c in range(8):
                        P.op("pe", mm(pz[:, 0:pn], wz[:, c, d_, :], hT[:, c, ps_], c == 0, c == 7), reads=["h_wz", "hT"], writes=[kz])
                    P.op("act", lambda e: e.activation(out=fT[:, 0:pn], in_=pz[:, 0:pn], func=AF.Exp, scale=-1.0), writes=[kz, "h_fT" + kx])
                    P.op("act", lambda e: e.activation(out=fT[:, 0:pn], in_=fT[:, 0:pn], func=AF.Ln, bias=1.0), writes=["h_fT" + kx])
                    P.op("act", lambda e: e.activation(out=fT[:, 0:pn], in_=fT[:, 0:pn], func=AF.Exp, scale=-1.0), writes=["h_fT" + kx])
                    P.op("dve", lambda e: e.tensor_scalar(out=fT[:, 0:pn], in0=fT[:, 0:pn], scalar1=omc, scalar2=lbc, op0=ALU.mult, op1=ALU.add),
                         reads=["h_lb", "h_omlb"], writes=["h_fT" + kx])
                    P.op("dve", lambda e: e.tensor_scalar(out=kk[:, 0:pn], in0=fT[:, 0:pn], scalar1=-1.0, scalar2=1.0, op0=ALU.mult, op1=ALU.add), reads=["h_fT" + kx], writes=["h_kk" + kx])
                    P.op("act", lambda e: e.activation(out=fT[:, 0:pn], in_=fT[:, 0:pn], func=AF.Ln), reads=["h_kk" + kx], writes=["h_fT" + kx])
                    P.op("dve", lambda e: e.tensor_tensor_scan(out=bT[:, 0:pn], data0=rst[:, 0:pn], data1=fT[:, 0:pn], initial=0.0, op0=ALU.mult, op1=ALU.add),
                         reads=["h_rst", "h_fT" + kx], writes=["h_bT" + kx])
                    P.op("dve", lambda e: e.tensor_copy(out=totc[:, 0:nt], in_=v128(bT[:, 0:pn])[:, :, 127]), reads=["h_bT" + kx], writes=["h_totc" + kx])
                    totb = totc[:, 0:nt].unsqueeze(2).to_broadcast([128, nt, 128])
                    P.op("act", lambda e: e.activation(out=ebT[d_][:, tsl], in_=totc[:, 0:nt], func=AF.Exp), reads=["h_totc" + kx], writes=[f"h_ebT{pk}"])
                    if d_ == 0:
                        P.op("dve", lambda e: e.tensor_tensor(out=v128(t32[:, 0:pn]), in0=totb, in1=v128(bT[:, 0:pn]), op=ALU.subtract),
                             reads=["h_bT" + kx, "h_totc" + kx], writes=["h_t32" + kx])
                    else:
                        P.op("dve", lambda e: e.tensor_tensor(out=t32[:, 0:pn], in0=bT[:, 0:pn], in1=fT[:, 0:pn], op=ALU.subtract), reads=["h_bT" + kx, "h_fT" + kx], writes=["h_t32" + kx])
                        P.op("dve", lambda e: e.tensor_tensor(out=v128(bT[:, 0:pn]), in0=totb, in1=v128(t32[:, 0:pn]), op=ALU.subtract),
                             reads=["h_totc" + kx, "h_t32" + kx], writes=["h_bT" + kx])
                    P.op("act", lambda e: e.activation(out=t32[:, 0:pn], in_=t32[:, 0:pn], func=AF.Exp), writes=["h_t32" + kx])
                    P.op("dve", lambda e: e.tensor_tensor(out=kht[d_][:, ps_], in0=t32[:, 0:pn], in1=kk[:, 0:pn], op=ALU.mult),
                         reads=["h_t32" + kx, "h_kk" + kx], writes=[f"h_kht{pk}"])
                    P.op("dve", lambda e: e.tensor_copy(out=rc[:, 0:nt], in_=v128(bT[:, 0:pn])[:, :, REF]), reads=["h_bT" + kx], writes=["h_rc" + kx])
                    rb = rc[:, 0:nt].unsqueeze(2).to_broadcast([128, nt, 128])
                    P.op("act", lambda e: e.activation(out=erT[d_][:, tsl], in_=rc[:, 0:nt], func=AF.Exp), reads=["h_rc" + kx], writes=[f"h_erT{pk}"])
                    P.op("dve", lambda e: e.tensor_tensor(out=v128(bT[:, 0:pn]), in0=v128(bT[:, 0:pn]), in1=rb, op=ALU.subtract), reads=["h_rc" + kx], writes=["h_bT" + kx])
                    P.op("act", lambda e: e.activation(out=t32[:, 0:pn], in_=bT[:, 0:pn], func=AF.Exp), reads=["h_bT" + kx, f"h_kht{pk}"], writes=["h_t32" + kx])
                    P.op("dve", lambda e: e.tensor_tensor(out=qtl[d_][:, ps_], in0=t32[:, 0:pn], in1=qs[:, ps_], op=ALU.mult),
                         reads=["h_t32" + kx, "h_qs"], writes=[f"h_qtl{pk}"])
                    P.op("act", lambda e: e.activation(out=t32[:, 0:pn], in_=bT[:, 0:pn], func=AF.Exp, scale=-1.0), reads=["h_bT" + kx, f"h_qtl{pk}"], writes=["h_t32" + kx])
                    P.op("dve", lambda e: e.tensor_tensor(out=ktl[d_][:, ps_], in0=t32[:, 0:pn], in1=kk[:, 0:pn], op=ALU.mult),
                         reads=["h_t32" + kx, "h_kk" + kx], writes=[f"h_ktl{pk}"])

                orders = [list(range(NTILE)), [1, 0] + list(range(NTILE - 1, 1, -1))]
                piecesD = [[(0, 512), (512, 512), (1024, 512), (1536, 512), (2048, 256)],
                           [(0, 256), (1792, 512), (1280, 512), (768, 512), (256, 512)]]
                pkey = {}
                for d_ in range(2):
                    for k_, (p0, pn) in enumerate(piecesD[d_]):
                        for tl in range(p0 // 128, (p0 + pn) // 128):
                            pkey[(d_, tl)] = f"{d_}_{k_}"
                visited = set()
                for d_ in range(2):
                    P.op("dve", lambda e, d_=d_: e.memset(S[d_], 0.0), writes=[f"h_S{d_}"])
                ncomp = [0]

                def step(d_, it, tl):
                    ts_ = slice(tl * 128, (tl + 1) * 128)
                    pi = it % 2
                    pk = pkey[(d_, tl)]
                    lat = tl >= 2
                    second = tl in visited
                    visited.add(tl)
                    o_, ko = pb[d_], PB[d_]
                    sc_cols = slice(128 * d_, 128 * d_ + 128)
                    if lat:
                        erc = erT[d_][:, tl:tl + 1]
                        P.op("act", lambda e: e.activation(out=Sb[d_][pi], in_=S[d_], func=AF.Copy, scale=erc), reads=[f"h_S{d_}", f"h_erT{pk}"], writes=[f"h_Sb{d_}_{pi}"])
                        P.op("pe", mm(pb[6][:, sc_cols], ktl[d_][:, ts_], qtl[d_][:, ts_], True, True), reads=[f"h_ktl{pk}", f"h_qtl{pk}"], writes=[PB[6]])
                        msk = cb[:, CI["tF"], :] if d_ == 0 else cb[:, CI["tB"], :]
                        P.op("dve", lambda e: e.tensor_tensor(out=AT[d_][pi], in0=pb[6][:, sc_cols], in1=msk, op=ALU.mult), reads=["cb"], writes=[PB[6], f"h_AT{d_}{pi}"])
                        P.op("pe", mm(o_[:, 0:128], AT[d_][pi], itok[:, tl, :], True, False), reads=[f"h_AT{d_}{pi}", "h_itok"], writes=[ko])
                    P.op("pe", lambda e: e.transpose(out=pb7b[:, sc_cols], in_=kht[d_][:, ts_], identity=ident_b), reads=[f"h_kht{pk}", "cb"], writes=[PB[7]])
                    P.op("act", lambda e: e.copy(out=khtok[d_][pi], in_=pb7b[:, sc_cols]), writes=[PB[7], f"h_khtok{d_}{pi}"])
                    kv, kkv = pb[2 + d_], PB[2 + d_]
                    P.op("pe", mm(kv[:, 0:128], khtok[d_][pi], itok[:, tl, :], True, True), reads=[f"h_khtok{d_}{pi}", "h_itok"], writes=[kkv])
                    if lat:
                        P.op("pe", mm(o_[:, 0:128], qtl[d_][:, ts_], Sb[d_][pi], False, True), reads=[f"h_qtl{pk}", f"h_Sb{d_}_{pi}", ko], writes=[ko])
                    ebc = ebT[d_][:, tl:tl + 1]
                    P.op("dve", lambda e: e.scalar_tensor_tensor(out=S[d_], in0=S[d_], scalar=ebc, in1=kv[:, 0:128], op0=ALU.mult, op1=ALU.add),
                         reads=[f"h_ebT{pk}", f"h_S{d_}"], writes=[kkv, f"h_S{d_}"])
                    if not lat:
                        return
                    if not second:
                        P.op("act", lambda e: e.copy(out=ofirst[:, tl, :], in_=o_[:, 0:128]), writes=[ko, f"h_of{tl}"])
                        return
                    s1, ks1 = sm[d_], f"h_sm{d_}"
                    P.op("dve", lambda e: e.tensor_tensor(out=os_[d_], in0=o_[:, 0:128], in1=ofirst[:, tl, :], op=ALU.add), reads=[f"h_of{tl}"], writes=[ko, f"h_os{d_}"])
                    P.op("act", lambda e: e.activation(out=og[d_], in_=os_[d_], func=AF.Square, accum_out=s1), reads=[f"h_os{d_}"], writes=[f"h_og{d_}", ks1])
                    P.op("dve", lambda e: e.tensor_scalar(out=s1, in0=s1, scalar1=1.0 / 128, scalar2=EPS, op0=ALU.mult, op1=ALU.add), writes=[ks1])
                    P.op("act", lambda e: e.activation(out=s1, in_=s1, func=AF.Ln), writes=[ks1])
                    P.op("act", lambda e: e.activation(out=s1, in_=s1, func=AF.Exp, scale=-0.5), writes=[ks1])
                    P.op("dve", lambda e: e.scalar_tensor_tensor(out=os_[d_], in0=os_[d_], scalar=s1[:, 0:1], in1=hhg, op0=ALU.mult, op1=ALU.mult),
                         reads=[ks1, "h_hhg"], writes=[f"h_os{d_}"])
                    P.op("dve", lambda e: e.tensor_tensor(out=og[d_], in0=os_[d_], in1=sgt[:, tl, :], op=ALU.mult), reads=[f"h_os{d_}", "h_sgt"], writes=[f"h_og{d_}"])
                    P.op("pe", lambda e: e.transpose(out=pb7b[:, 256:384], in_=og[d_], identity=ident_b), reads=[f"h_og{d_}", "cb"], writes=[PB[7]])
                    xi = ncomp[0] % 2
                    ncomp[0] += 1
                    P.op("act", lambda e: e.copy(out=xTt[xi], in_=pb7b[:, 256:384]), writes=[PB[7], f"h_xT{xi}"])
                    c0 = (tl * 128 - CTX) // 32
                    for half in range(2):
                        po, kpo = pb[4 + half], PB[4 + half]
                        for j in range(4):
                            dc = half * 4 + j
                            P.op("pe", mm(po[:, j * 128:(j + 1) * 128], wout[:, dc * 128:(dc + 1) * 128], xTt[xi], True, True), reads=["h_wout", f"h_xT{xi}"], writes=[kpo])
                        keys = SKEY[half * 4:half * 4 + 4]
                        dstv = sT[:, half * 4:half * 4 + 4, ts_]
                        srcv = po[:, :].rearrange("p (d t) -> p d t", d=4)
                        P.op("dve", lambda e, dstv=dstv, srcv=srcv: e.tensor_tensor(out=dstv, in0=dstv, in1=srcv, op=ALU.add),
                             reads=keys, writes=[kpo] + keys)

                done = [0, 0]
                for k_ in range(5):
                    P.interleave([lambda d_=d_: precompute(d_, piecesD[d_][k_][0], piecesD[d_][k_][1], f"{d_}_{k_}") for d_ in range(2)])
                    avail = [min(NTILE, 4 * (k_ + 1)) if k_ < 4 else NTILE, min(NTILE, 2 + 4 * k_)]
                    if DEBUG_NT is not None:
                        avail = [min(a_, DEBUG_NT) for a_ in avail]
                    while done[0] < avail[0] or done[1] < avail[1]:
                        for d_ in range(2):
                            if done[d_] < avail[d_]:
                                step(d_, done[d_], orders[d_][done[d_]])
                                done[d_] += 1

        def final():
            P.barrier()
            cv = Carver()
            ob = [cv.take([128, 512]) for _ in range(2)]
            outs = []
            cnt = [0]
            sq = [cv.take([128, 512]) for _ in range(2)]
            rstd = cv.take([128, 512])
            for (t0, n, col) in _blocks(CTX, NT):
                for c in range(8):
                    P.op("act", lambda e, c=c, t0=t0, n=n: e.activation(out=sq[c % 2][:, 0:n], in_=sT[:, c, t0:t0 + n], func=AF.Square), reads=[SKEY[c]], writes=[f"nm_sq{c % 2}"])
                    P.op("pe", mm(pb[7][:, 0:n], ones_f, sq[c % 2][:, 0:n], c == 0, c == 7), reads=[f"nm_sq{c % 2}", "cf"], writes=[PB[7]])
                P.op("dve", lambda e, n=n: e.tensor_scalar(out=rstd[:, 0:n], in0=pb[7][:, 0:n], scalar1=1.0 / D, scalar2=EPS, op0=ALU.mult, op1=ALU.add), reads=[PB[7]], writes=["nm_rstd"])
                P.op("act", lambda e, n=n: e.activation(out=rstd[:, 0:n], in_=rstd[:, 0:n], func=AF.Sqrt), reads=["nm_rstd"], writes=["nm_rstd"])
                P.op("dve", lambda e, n=n: e.reciprocal(out=rstd[:, 0:n], in_=rstd[:, 0:n]), reads=["nm_rstd"], writes=["nm_rstd"])
                for c in range(8):
                    i = cnt[0] % 2
                    cnt[0] += 1
                    P.op("dve", lambda e, c=c, t0=t0, n=n, i=i: e.scalar_tensor_tensor(out=ob[i][:, 0:n], in0=sT[:, c, t0:t0 + n], scalar=gfin[:, c:c + 1], in1=rstd[:, 0:n],
                                                                                 op0=ALU.mult, op1=ALU.mult), reads=[SKEY[c], "gfin", "nm_rstd"], writes=[f"fin_ob{i}"])
                    outs.append(P.dma("sp", f"fin_ob{i}", lambda e, c=c, t0=t0, n=n, i=i: e.dma_start(out=d_out[:, c, t0 - CTX:t0 - CTX + n], in_=ob[i][:, 0:n]),
                                      reads=[f"fin_ob{i}"]))
            return outs

        outs = []
        if "mix0" in phases:
            mlstm(0)
        if "moe0" in phases:
            moe(0, 0, NT)
        if "mix1" in phases:
            hgrn(1)
        if "moe1" in phases:
            moe(1, CTX, NT)
        if "final" in phases:
            outs = final()
        if d_dump is not None:
            P.barrier()
            for c in range(8):
                outs.append(P.dma("sp", f"dump{c}", lambda e, c=c: e.dma_start(out=d_dump[:, c, :], in_=sT[:, c, :]), reads=[SKEY[c]]))
        P.emit(final_wait_ops=outs + dbg_outs)
    return nc


def _fm(v, lead=()):
    v = np.asarray(v, np.float32)
    k = v.shape[-1] // 128
    r = v.reshape(v.shape[:-1] + (k, 128))
    return np.ascontiguousarray(np.moveaxis(r, -1, 0))


def _wl(w):
    w = np.asarray(w, np.float32)
    K, N = w.shape
    return np.ascontiguousarray(w.reshape(K // 128, 128, N).transpose(1, 0, 2))


def prep_shared(x, c, ctx, c_ctx, ada_w, ada_b, norm_mix_g, norm_ffn_g, final_g,
                m_w_in, m_conv_w, m_conv_b, m_gate_b, m_head_g, m_w_out,
                h_w_in, h_lower_bounds, h_head_g, h_w_out,
                router_w, router_bias, e_w_gate, e_w_up, e_w_down):
    sh = {}
    sh["adaw"] = np.ascontiguousarray(np.asarray(ada_w, np.float32).reshape(2, 8, 128, 12, 512).transpose(0, 3, 2, 1, 4))
    sh["adab"] = np.ascontiguousarray(_fm(ada_b))
    sh["gmix"] = _fm(norm_mix_g)
    sh["gffn"] = _fm(norm_ffn_g)
    sh["gfin"] = _fm(final_g)
    sh["mwin"] = _wl(m_w_in[0])
    cw = _fm(m_conv_w[0])
    cbias = _fm(m_conv_b[0])
    sh["mconv"] = np.ascontiguousarray(np.concatenate([cw.transpose(0, 2, 1), cbias[:, :, None]], axis=2))
    sh["mgb"] = np.ascontiguousarray(np.broadcast_to(np.asarray(m_gate_b[0], np.float32)[None, :], (128, 16)))
    sh["mhg"] = np.ascontiguousarray(np.broadcast_to(np.asarray(m_head_g[0], np.float32)[None, :], (128, D)))
    sh["mwout"] = _wl(m_w_out[0])
    sh["hwin"] = _wl(h_w_in[0])
    sh["hlb"] = np.ascontiguousarray(_fm(h_lower_bounds))
    sh["hhg"] = np.ascontiguousarray(np.broadcast_to(np.asarray(h_head_g[0], np.float32)[None, :], (128, D)))
    sh["hwout"] = _wl(h_w_out[0])
    sh["rw"] = _wl(router_w)
    sh["rb"] = np.ascontiguousarray(np.broadcast_to(np.asarray(router_bias, np.float32)[None, None, :], (128, NTILE, NE)))
    sh["ewg"] = np.ascontiguousarray(np.asarray(e_w_gate, np.float32).reshape(2, NE, 8, 128, DEXP).transpose(0, 1, 3, 2, 4))
    sh["ewu"] = np.ascontiguousarray(np.asarray(e_w_up, np.float32).reshape(2, NE, 8, 128, DEXP).transpose(0, 1, 3, 2, 4))
    sh["ewd"] = np.ascontiguousarray(np.asarray(e_w_down, np.float32).reshape(2, NE, 4, 128, D).transpose(0, 1, 3, 2, 4))
    sh["cf"] = CARR
    sh["rst"] = RST
    return sh


def prep_core(b, x, c, ctx, c_ctx, s_override=None):
    if s_override is not None:
        s = s_override
    else:
        s = np.concatenate([np.asarray(ctx[b], np.float32), np.asarray(x[b], np.float32)], axis=0)
    xT = np.ascontiguousarray(s.reshape(NT, 8, 128).transpose(2, 1, 0))
    cc = np.stack([np.asarray(c[b], np.float32), np.asarray(c_ctx, np.float32)], axis=-1)
    cT = np.ascontiguousarray(cc.reshape(8, 128, 2).transpose(1, 0, 2))
    return {"xT": xT, "cT": cT}


_NC_CACHE = {}


def kernel(**inputs):
    x = inputs["x"]
    B = x.shape[0]
    sh = prep_shared(**inputs)
    if "full" not in _NC_CACHE:
        _NC_CACHE["full"] = build_program()
    nc = _NC_CACHE["full"]
    in_maps = []
    for b in range(B):
        m = dict(sh)
        m.update(prep_core(b, inputs["x"], inputs["c"], inputs["ctx"], inputs["c_ctx"]))
        in_maps.append(m)
    res = run_bass_kernel_spmd(nc, in_maps, core_ids=list(range(B)))
    out = np.empty((B, SEQ, D), np.float32)
    for b in range(B):
        oT = np.asarray(res.results[b]["outT"])
        out[b] = oT.transpose(2, 1, 0).reshape(64, 32, D).transpose(1, 0, 2).reshape(SEQ, D)
    return out
```

```python
import numpy as np
import concourse.bass as bass
import concourse.mybir as mybir
from contextlib import ExitStack
from concourse.bass_utils import run_bass_kernel_spmd

F32 = mybir.dt.float32
BF16 = mybir.dt.bfloat16
AF = mybir.ActivationFunctionType
ALU = mybir.AluOpType
AX = mybir.AxisListType

D = 1024
SEQ = 2048
CTX = 256
NT = SEQ + CTX
NTILE = NT // 128
EPS = 1e-6
NE = 16
DEXP = 512
ENGS = ("pe", "act", "dve", "pool", "sp")
DEBUG_NT = None


class Op:
    __slots__ = ("eng", "fn", "deps", "signal", "seq", "is_dma", "dsem", "dval", "n_inst", "name", "cost")

    def __init__(self, eng, fn, is_dma, dsem, name):
        self.eng = eng
        self.fn = fn
        self.deps = set()
        self.signal = False
        self.seq = None
        self.is_dma = is_dma
        self.dsem = dsem
        self.dval = None
        self.n_inst = 1
        self.name = name
        self.cost = None


class Prog:
    def __init__(self, nc):
        self.nc = nc
        self.ops = {e: [] for e in ENGS}
        self.last_w = {}
        self.readers = {}
        self.all_ops = []
        self._bar_from = 0
        self._capture = None

    def _add(self, eng, fn, reads, writes, is_dma=False, dsem=None, name=None):
        o = Op(eng, fn, is_dma, dsem, name)
        for k in reads:
            w = self.last_w.get(k)
            if w is not None:
                o.deps.add(w)
        for k in writes:
            w = self.last_w.get(k)
            if w is not None:
                o.deps.add(w)
            for r in self.readers.get(k, ()):
                o.deps.add(r)
        for k in reads:
            self.readers.setdefault(k, []).append(o)
        for k in writes:
            self.last_w[k] = o
            self.readers[k] = []
        o.deps.discard(o)
        self.ops[eng].append(o)
        self.all_ops.append(o)
        return o

    def op(self, eng, fn, reads=(), writes=(), name=None):
        if self._capture is not None:
            self._capture.append(("op", eng, fn, tuple(reads), tuple(writes), None, 1))
            return None
        return self._add(eng, fn, reads, writes, name=name)

    def dma(self, eng, group, fn, reads=(), writes=(), n=1, name=None):
        if self._capture is not None:
            self._capture.append(("dma", eng, fn, tuple(reads), tuple(writes), group, n))
            return None
        o = self._add(eng, fn, reads, writes, is_dma=True, dsem=group, name=name)
        o.n_inst = n
        return o

    def interleave(self, builders):
        streams = []
        for b in builders:
            self._capture = []
            b()
            streams.append(self._capture)
            self._capture = None
        idx = [0] * len(streams)
        while any(idx[i] < len(st) for i, st in enumerate(streams)):
            for i, st in enumerate(streams):
                if idx[i] < len(st):
                    kind, eng, fn, reads, writes, group, n = st[idx[i]]
                    idx[i] += 1
                    if kind == "op":
                        self._add(eng, fn, reads, writes)
                    else:
                        o = self._add(eng, fn, reads, writes, is_dma=True, dsem=group)
                        o.n_inst = n

    def barrier(self):
        self.all_ops.append(None)

    def _schedule(self, seg):
        import heapq
        COST = {"pe": 0.22, "act": 0.55, "dve": 0.65, "pool": 0.9, "sp": 0.1}
        HOP = 0.15
        n = len(seg)
        idx = {id(o): i for i, o in enumerate(seg)}
        cost = [0.0] * n
        lat = [0.0] * n
        for i, o in enumerate(seg):
            c = o.cost if o.cost is not None else COST[o.eng]
            if o.is_dma:
                cost[i] = 0.08 * o.n_inst
                lat[i] = c if o.cost is not None else 2.5
            else:
                cost[i] = c
        deps = [[idx[id(d)] for d in o.deps if id(d) in idx] for o in seg]
        succ = [[] for _ in range(n)]
        for i, dl in enumerate(deps):
            for d in dl:
                succ[d].append(i)
        prio = [0.0] * n
        for i in range(n - 1, -1, -1):
            m = 0.0
            for j in succ[i]:
                if prio[j] > m:
                    m = prio[j]
            prio[i] = m + cost[i] + lat[i] + HOP
        ndep = [len(dl) for dl in deps]
        est = [0.0] * n
        fin = [0.0] * n
        free_at = {e: 0.0 for e in ENGS}
        pend = {e: [] for e in ENGS}
        avail = {e: [] for e in ENGS}
        order = {e: [] for e in ENGS}
        for i in range(n):
            if ndep[i] == 0:
                heapq.heappush(pend[seg[i].eng], (0.0, -prio[i], i))
        left = n
        while left:
            best_e, best_t = None, None
            for e in ENGS:
                if not pend[e] and not avail[e]:
                    continue
                while pend[e] and pend[e][0][0] <= free_at[e]:
                    t_, p_, i_ = heapq.heappop(pend[e])
                    heapq.heappush(avail[e], (p_, i_))
                t = free_at[e] if avail[e] else max(free_at[e], pend[e][0][0])
                if best_t is None or t < best_t:
                    best_e, best_t = e, t
            e = best_e
            if avail[e]:
                p_, i = heapq.heappop(avail[e])
            else:
                t_, p_, i = heapq.heappop(pend[e])
            start = max(free_at[e], est[i])
            free_at[e] = start + cost[i]
            fin[i] = start + cost[i] + lat[i]
            order[e].append(seg[i])
            left -= 1
            for j in succ[i]:
                if fin[i] + HOP > est[j]:
                    est[j] = fin[i] + HOP
                ndep[j] -= 1
                if ndep[j] == 0:
                    heapq.heappush(pend[seg[j].eng], (est[j], -prio[j], j))
        return order

    def emit(self, final_wait_ops=(), schedule=True):
        nc = self.nc
        segs, cur = [], []
        for o in self.all_ops:
            if o is None:
                if cur:
                    segs.append(cur)
                cur = []
            else:
                cur.append(o)
        if cur:
            segs.append(cur)
        seg_orders = []
        for seg in segs:
            if schedule:
                seg_orders.append(self._schedule(seg))
            else:
                od = {e: [] for e in ENGS}
                for o in seg:
                    od[o.eng].append(o)
                seg_orders.append(od)
        bar_deps = [set() for _ in segs]
        last_comp = {}
        for k, od in enumerate(seg_orders):
            if k > 0:
                bar_deps[k] = set(last_comp.values()) | {o for o in segs[k - 1] if o.is_dma}
            for e in ENGS:
                comp = [o for o in od[e] if not o.is_dma]
                if comp:
                    last_comp[e] = comp[-1]
        for o in (x for x in self.all_ops if x is not None):
            for d in o.deps:
                if d.eng == "pe" and o.eng == "pe" and not d.is_dma and not o.is_dma:
                    continue
                d.signal = True
        for bd in bar_deps:
            for d in bd:
                d.signal = True
        for o in final_wait_ops:
            o.signal = True
        with ExitStack() as es:
            esem = {e: es.enter_context(nc.semaphore("c_" + e)) for e in ENGS}
            gsem = {}
            gcount = {}
            for e in ENGS:
                for od in seg_orders:
                    for o in od[e]:
                        if o.is_dma:
                            if o.dsem not in gsem:
                                gsem[o.dsem] = es.enter_context(nc.semaphore("d_" + str(o.dsem)))
                                gcount[o.dsem] = 0
                            gcount[o.dsem] += 16 * o.n_inst
                            o.dval = gcount[o.dsem]
            for e in ENGS:
                c = 0
                for od in seg_orders:
                    for o in od[e]:
                        if not o.is_dma and o.signal:
                            c += 1
                            o.seq = c
            block = es.enter_context(nc.Block())
            engobj = {"pe": "tensor", "act": "scalar", "dve": "vector", "pool": "gpsimd", "sp": "sync"}

            def run(ename, eng):
                waited = {}

                def do_waits(deps, is_pe_compute):
                    need = {}
                    for d in deps:
                        if d.is_dma:
                            key, val, sem = ("g", d.dsem), d.dval, gsem[d.dsem]
                        else:
                            if d.eng == "pe" and ename == "pe" and is_pe_compute:
                                continue
                            if d.eng == ename and ename == "pe":
                                continue
                            key, val, sem = ("e", d.eng), d.seq, esem[d.eng]
                        if waited.get(key, 0) >= val:
                            continue
                        if key not in need or need[key][1] < val:
                            need[key] = (sem, val)
                    for key, (sem, val) in need.items():
                        eng.wait_ge(sem, val)
                        waited[key] = val

                for k, od in enumerate(seg_orders):
                    if bar_deps[k]:
                        do_waits(list(bar_deps[k]), False)
                    for o in od[ename]:
                        do_waits(o.deps, not o.is_dma)
                        r = o.fn(eng)
                        if o.is_dma:
                            insts = r if isinstance(r, (list, tuple)) else [r]
                            assert len(insts) == o.n_inst
                            for ins in insts:
                                ins.then_inc(gsem[o.dsem], 16)
                        elif o.signal:
                            ins = r[-1] if isinstance(r, (list, tuple)) else r
                            ins.then_inc(esem[ename], 1)
                if ename == "sp":
                    for o in final_wait_ops:
                        if o.is_dma:
                            eng.wait_ge(gsem[o.dsem], o.dval)
                        else:
                            eng.wait_ge(esem[o.eng], o.seq)

            for ename in ENGS:
                getattr(block, engobj[ename])(lambda eng, ename=ename: run(ename, eng))


def _consts():
    s = np.arange(128)[:, None]
    t = np.arange(128)[None, :]
    same = (s // 64) == (t // 64)
    c = {}
    c["ident"] = np.eye(128, dtype=np.float32)
    c["ones"] = np.ones((128, 128), np.float32)
    c["triF"] = (same & (s <= t)).astype(np.float32)
    c["triB"] = (same & (s >= t)).astype(np.float32)
    c["sufF"] = (same & (s > t)).astype(np.float32)
    c["sufB"] = (same & (s < t)).astype(np.float32)
    c["sel0"] = np.repeat((s < 64).astype(np.float32), 128, axis=1)
    c["sel1"] = np.repeat((s >= 64).astype(np.float32), 128, axis=1)
    c["tF"] = (s <= t).astype(np.float32)
    c["tB"] = (s >= t).astype(np.float32)
    c["sF"] = (s > t).astype(np.float32)
    c["sB"] = (s < t).astype(np.float32)
    names = ["ident", "ones", "triF", "triB", "sufF", "sufB", "sel0", "sel1", "tF", "tB", "sF", "sB"]
    arr = np.stack([c[n] for n in names], axis=1)
    selE = np.zeros((128, NE, 128), np.float32)
    for e in range(NE):
        selE[e, e, :] = 1.0
    rst = np.ones((128, NT), np.float32)
    rst[:, ::128] = 0.0
    return names, np.ascontiguousarray(arr), selE, rst


CN, CARR, SELE, RST = _consts()
CI = {n: i for i, n in enumerate(CN)}


def _blocks(lo, hi, bs=512):
    out = []
    t = lo
    while t < hi:
        n = min(bs, hi - t)
        if t < CTX:
            n = min(n, CTX - t)
        out.append((t, n, 1 if t < CTX else 0))
        t += n
    return out


def build_program(phases=("mods", "mix0", "moe0", "mix1", "moe1", "final"), dump=None):
    nc = bass.Bass("TRN2", target_bir_lowering=False)
    es = ExitStack()
    P = Prog(nc)

    def din(name, shape, dt=F32):
        return nc.dram_tensor(name, list(shape), dt, kind="ExternalInput").ap()

    d_xT = din("xT", [128, 8, NT])
    d_cT = din("cT", [128, 8, 2])
    d_adaw = din("adaw", [2, 12, 128, 8, 512])
    d_adab = din("adab", [128, 2, 48])
    d_gmix = din("gmix", [128, 2, 8])
    d_gffn = din("gffn", [128, 2, 8])
    d_gfin = din("gfin", [128, 8])
    d_mwin = din("mwin", [128, 8, 3088])
    d_mconv = din("mconv", [128, 8, 4])
    d_mgb = din("mgb", [128, 16])
    d_mhg = din("mhg", [128, D])
    d_mwout = din("mwout", [128, 8, D])
    d_hwin = din("hwin", [128, 8, 5120])
    d_hlb = din("hlb", [128, 2, 16])
    d_hhg = din("hhg", [128, D])
    d_hwout = din("hwout", [128, 8, D])
    d_rw = din("rw", [128, 8, NE])
    d_rb = din("rb", [128, NTILE, NE])
    d_wg = din("ewg", [2, NE, 128, 8, DEXP])
    d_wu = din("ewu", [2, NE, 128, 8, DEXP])
    d_wd = din("ewd", [2, NE, 128, 4, D])
    d_cf = din("cf", [128, 12, 128])
    d_rst = din("rst", [128, NT])
    d_out = nc.dram_tensor("outT", [128, 8, SEQ], F32, kind="ExternalOutput").ap()
    d_dump = None
    if dump is not None:
        d_dump = nc.dram_tensor("dump", [128, 8, NT], F32, kind="ExternalOutput").ap()

    with es:
        def sb(name, shape, dt=F32):
            return es.enter_context(nc.sbuf_tensor("s_" + name, list(shape), dt))

        sT = sb("sT", [128, 8, NT])
        hT = sb("hT", [128, 8, NT], BF16)
        cf = sb("cf", [128, 12, 128])
        cb = sb("cb", [128, 12, 128], BF16)
        maskLE = sb("maskLE", [128, 128], BF16)
        maskGE = sb("maskGE", [128, 128], BF16)
        modT = [sb(f"modT{l}", [128, 48, 2]) for l in range(2)]
        adab = sb("adab", [128, 2, 48])
        gmix = sb("gmix", [128, 2, 8])
        gffn = sb("gffn", [128, 2, 8])
        gfin = sb("gfin", [128, 8])
        cT = sb("cT", [128, 8, 2])
        scT = sb("scT", [128, 8, 2])
        A1 = [sb(f"A1_{l}", [128, 8, 2]) for l in range(2)]
        A2 = [sb(f"A2_{l}", [128, 8, 2]) for l in range(2)]
        pb = [es.enter_context(nc.psum_tensor(f"pb{i}", [128, 512], F32)) for i in range(8)]
        PB = [f"pb{i}" for i in range(8)]

        ARENA = ((nc.sbuf_bytes_remaining - 2048) // 64) * 64
        arena = sb("arena", [128, ARENA // 4], F32)

        class Carver:
            def __init__(self):
                self.off = 0

            def take(self, shape, dt=F32):
                esz = 4 if dt == F32 else 2
                n = int(np.prod(shape[1:]))
                nbytes = ((n * esz + 63) // 64) * 64
                assert self.off + nbytes <= ARENA, (self.off, nbytes, ARENA)
                a = arena[:, self.off // 4:(self.off + nbytes) // 4]
                self.off += nbytes
                if dt != F32:
                    a = a.bitcast(dt)
                a = a[:, 0:n]
                if len(shape) == 3:
                    a = a.rearrange("p (a b) -> p a b", a=shape[1])
                elif len(shape) == 4:
                    a = a.rearrange("p (a b c) -> p a b c", a=shape[1], b=shape[2])
                return a

        dbg_outs = []

        def dbg(name, ap, shape, key, dt=F32):
            if dump is None or dump is True or name not in dump:
                return
            dd = nc.dram_tensor("dbg_" + name, list(shape), dt, kind="ExternalOutput").ap()
            dbg_outs.append(P.dma("sp", "dbg_" + name, lambda e: e.dma_start(out=dd, in_=ap), reads=[key]))

        def mm(out, lhsT, rhs, start, stop):
            return lambda e: e.matmul(out, lhsT=lhsT, rhs=rhs, start=start, stop=stop)

        for c in range(8):
            P.dma("sp", f"sT{c}", lambda e, c=c: e.dma_start(out=sT[:, c, :], in_=d_xT[:, c, :]), writes=[f"sT{c}"])
        P.dma("sp", "cf", lambda e: e.dma_start(out=cf[:], in_=d_cf), writes=["cf"])
        for nm, t, dsrc in (("adab", adab, d_adab), ("gmix", gmix, d_gmix), ("gffn", gffn, d_gffn),
                            ("gfin", gfin, d_gfin), ("cT", cT, d_cT)):
            P.dma("sp", nm, lambda e, t=t, dsrc=dsrc: e.dma_start(out=t[:], in_=dsrc), writes=[nm])
        P.op("dve", lambda e: e.tensor_copy(out=cb[:], in_=cf[:]), reads=["cf"], writes=["cb"])
        P.op("dve", lambda e: e.tensor_copy(out=maskLE[:], in_=cf[:, CI["triF"], :]), reads=["cf"], writes=["masks"])
        P.op("dve", lambda e: e.tensor_copy(out=maskGE[:], in_=cf[:, CI["triB"], :]), reads=["cf"], writes=["masks"])
        ident_b = cb[:, CI["ident"], :]
        ident_f = cf[:, CI["ident"], :]
        ones_f = cf[:, CI["ones"], :]
        SKEY = [f"sT{c}" for c in range(8)]

        MODS_BASE = ((ARENA - (4 * 16384 + 2 * 2048)) // 64) * 64
        if "mods" in phases:
            cv = Carver()
            cv.off = MODS_BASE
            NB_ = 4
            adaw = [cv.take([128, 8, 512]) for _ in range(NB_)]
            modrow = [cv.take([128, 512]) for _ in range(2)]
            P.op("act", lambda e: e.activation(out=scT[:], in_=cT[:], func=AF.Silu), reads=["cT"], writes=["scT"])
            for l in range(2):
                for nb in range(12):
                    bi = nb % NB_
                    mi = nb % 2
                    P.dma("sp" if nb % 2 == 0 else "act", f"adaw{bi}", lambda e, l=l, nb=nb, bi=bi: e.dma_start(out=adaw[bi], in_=d_adaw[l, nb]), writes=[f"arena_adaw{bi}"])
                    for kc in range(8):
                        P.op("pe", mm(pb[mi][0:2, :], scT[:, kc, :], adaw[bi][:, kc, :], kc == 0, kc == 7), reads=[f"arena_adaw{bi}", "scT"], writes=[PB[mi]])
                    P.op("dve", lambda e, mi=mi: e.tensor_copy(out=modrow[mi][0:2, :], in_=pb[mi][0:2, :]), writes=[PB[mi], f"arena_modrow{mi}"])
                    for j in range(4):
                        idx = nb * 4 + j
                        P.op("pe", lambda e, idx=idx, j=j, mi=mi: e.transpose(out=pb[2][:, 2 * idx:2 * idx + 2], in_=modrow[mi][0:2, j * 128:(j + 1) * 128], identity=cf[0:2, CI["ident"], 0:2]),
                             reads=[f"arena_modrow{mi}", "cf"], writes=[PB[2]])
                P.op("dve", lambda e, l=l: e.tensor_tensor(out=modT[l][:], in0=pb[2][:, 0:96].rearrange("p (a b) -> p a b", b=2),
                                                          in1=adab[:, l, :].unsqueeze(2).to_broadcast([128, 48, 2]), op=ALU.add),
                     reads=["adab"], writes=[PB[2], f"modT{l}"])
                for col in range(2):
                    P.op("dve", lambda e, l=l, col=col: e.scalar_tensor_tensor(
                        out=A1[l][:, :, col], in0=modT[l][:, 8:16, col], scalar=1.0, in1=gmix[:, l, :], op0=ALU.add, op1=ALU.mult),
                        reads=[f"modT{l}", "gmix"], writes=[f"A1_{l}"])
                    P.op("dve", lambda e, l=l, col=col: e.scalar_tensor_tensor(
                        out=A2[l][:, :, col], in0=modT[l][:, 32:40, col], scalar=1.0, in1=gffn[:, l, :], op0=ALU.add, op1=ALU.mult),
                        reads=[f"modT{l}", "gffn"], writes=[f"A2_{l}"])

        def SH1(l, c, col): return modT[l][:, 0 + c, col:col + 1]
        def G1(l, c, col): return modT[l][:, 16 + c, col:col + 1]
        def SH2(l, c, col): return modT[l][:, 24 + c, col:col + 1]
        def G2(l, c, col): return modT[l][:, 40 + c, col:col + 1]

        def norm_mod(cv, A, SH, l, lo, hi, out_bf, out_key, out_f32=None, perm_cols=False, after_block=None, bs=512, skey=None):
            if skey is None:
                skey = lambda c, t0: SKEY[c]
            sq = [cv.take([128, bs]) for _ in range(2)]
            rstd = cv.take([128, bs])
            tmp = [cv.take([128, bs]) for _ in range(2)]
            for bix, (t0, n, col) in enumerate(_blocks(lo, hi, bs)):
                o32 = out_f32[bix % 2] if out_f32 is not None else None
                okey = out_key(t0) if callable(out_key) else out_key
                k32 = "nm_h32_0"
                for c in range(8):
                    P.op("act", lambda e, c=c, t0=t0, n=n: e.activation(out=sq[c % 2][:, 0:n], in_=sT[:, c, t0:t0 + n], func=AF.Square),
                         reads=[skey(c, t0)], writes=[f"nm_sq{c % 2}"])
                    P.op("pe", mm(pb[7][:, 0:n], ones_f, sq[c % 2][:, 0:n], c == 0, c == 7), reads=[f"nm_sq{c % 2}", "cf"], writes=[PB[7]])
                P.op("dve", lambda e, n=n: e.tensor_scalar(out=rstd[:, 0:n], in0=pb[7][:, 0:n], scalar1=1.0 / D, scalar2=EPS,
                                                          op0=ALU.mult, op1=ALU.add), reads=[PB[7]], writes=["nm_rstd"])
                P.op("act", lambda e, n=n: e.activation(out=rstd[:, 0:n], in_=rstd[:, 0:n], func=AF.Sqrt), reads=["nm_rstd"], writes=["nm_rstd"])
                P.op("dve", lambda e, n=n: e.reciprocal(out=rstd[:, 0:n], in_=rstd[:, 0:n]), reads=["nm_rstd"], writes=["nm_rstd"])
                for c in range(8):
                    tp = tmp[c % 2]
                    P.op("dve", lambda e, c=c, t0=t0, n=n, tp=tp: e.tensor_tensor(out=tp[:, 0:n], in0=sT[:, c, t0:t0 + n], in1=rstd[:, 0:n], op=ALU.mult),
                         reads=[skey(c, t0), "nm_rstd"], writes=[f"nm_tmp{c % 2}"])
                    src = tp[:, 0:n]
                    if out_f32 is not None:
                        dst, dkey = o32[:, c, 0:n], k32
                    else:
                        dst, dkey = out_bf[:, c, t0:t0 + n], okey
                        if perm_cols and t0 >= CTX:
                            r0, nr = (t0 - CTX) // 64, n // 64
                            dst = out_bf[:, c, CTX:NT].rearrange("p (col row) -> p row col", row=32)[:, r0:r0 + nr, :]
                            src = src.rearrange("p (r c) -> p r c", c=64)
                    P.op("dve", lambda e, c=c, col=col, dst=dst, src=src: e.tensor_scalar(
                        out=dst, in0=src, scalar1=A[l][:, c, col:col + 1], scalar2=SH(l, c, col), op0=ALU.mult, op1=ALU.add),
                        reads=[f"nm_tmp{c % 2}", f"A1_{l}", f"A2_{l}", f"modT{l}"], writes=[dkey])
                    if out_f32 is not None:
                        P.op("act", lambda e, c=c, t0=t0, n=n, o32=o32: e.copy(out=out_bf[:, c, t0:t0 + n], in_=o32[:, c, 0:n]), reads=[k32], writes=[okey])
                if after_block is not None:
                    after_block(t0, n)

        def moe(l, lo, hi):
            P.barrier()
            cv = Carver()
            h32s = [cv.take([128, 8, 256])] * 2
            rw = cv.take([128, 8, NE])
            rb = cv.take([128, NTILE, NE])
            cw_all = cv.take([128, NTILE, NE])
            NTN = NTILE * NE
            rt = [cv.take([128, NTILE, NE]) for _ in range(5)]
            r4 = [cv.take([128, NTILE * 4]) for _ in range(4)]
            r1 = [cv.take([128, NTILE]) for _ in range(2)]
            wgb = [cv.take([128, 8, DEXP], BF16) for _ in range(2)]
            wub = [cv.take([128, 8, DEXP], BF16) for _ in range(2)]
            wdb = [cv.take([128, 4, D], BF16) for _ in range(2)]
            aT = [cv.take([128, 4, 512], BF16) for _ in range(2)]
            sg = [cv.take([128, 512], BF16) for _ in range(2)]
            t1 = [cv.take([128, 512], BF16) for _ in range(2)]
            cwb = [cv.take([128, 512], BF16) for _ in range(2)]
            P.dma("sp", "rw", lambda e: e.dma_start(out=rw, in_=d_rw), writes=["moe_rw"])
            P.dma("sp", "rb", lambda e: e.dma_start(out=rb, in_=d_rb), writes=["moe_rb"])
            T0, T1 = lo // 128, hi // 128
            s_all = rt[0]
            blk_ctr = [0]

            def route(t0, n):
                hb = 0
                for sub in range(n // 128):
                    tix = (t0 + sub * 128) // 128
                    pl, kl = pb[7], PB[7]
                    for c in range(8):
                        P.op("pe", mm(pl[:, 256:256 + NE], h32s[hb][:, c, sub * 128:(sub + 1) * 128], rw[:, c, :], c == 0, c == 7),
                             reads=[f"nm_h32_{hb}", "moe_rw"], writes=[kl])
                    P.op("act", lambda e, tix=tix, pl=pl: e.activation(out=s_all[:, tix, :], in_=pl[:, 256:256 + NE], func=AF.Sigmoid), writes=[kl, f"rt_s{tix}"])

            sel_, sel2_, eq1, eq2 = rt[1:5]
            w_ = sel2_
            m1, m2, gs, geq = r4
            bm, ws = r1
            cw16 = cw_all

            def route_batch(Ta, Tb):
                TS = slice(Ta, Tb)
                nT = Tb - Ta
                K = lambda nm: f"{nm}{Ta}"
                v3 = lambda a: a[:, TS, :].rearrange("p t (g k) -> p (t g) k", k=4)
                g2 = lambda a: a[:, Ta * 4:Tb * 4]
                bc4 = lambda a: g2(a).unsqueeze(2).to_broadcast([128, nT * 4, 4])
                g3 = lambda a: g2(a).rearrange("p (t g) -> p t g", g=4)
                skeys = [f"rt_s{t_}" for t_ in range(Ta, Tb)]
                P.op("dve", lambda e: e.tensor_tensor(out=sel_[:, TS, :], in0=s_all[:, TS, :], in1=rb[:, TS, :], op=ALU.add), reads=skeys + ["moe_rb"], writes=[K("rt_sel")])
                P.op("dve", lambda e: e.tensor_reduce(out=g2(m1), in_=v3(sel_), axis=AX.X, op=ALU.max), reads=[K("rt_sel")], writes=[K("rt_m1")])
                P.op("dve", lambda e: e.tensor_tensor(out=v3(eq1), in0=v3(sel_), in1=bc4(m1), op=ALU.is_equal), reads=[K("rt_sel"), K("rt_m1")], writes=[K("rt_eq1")])
                P.op("dve", lambda e: e.scalar_tensor_tensor(out=sel2_[:, TS, :], in0=eq1[:, TS, :], scalar=-1e9, in1=sel_[:, TS, :], op0=ALU.mult, op1=ALU.add),
                     reads=[K("rt_eq1"), K("rt_sel")], writes=[K("rt_sel2")])
                P.op("dve", lambda e: e.tensor_reduce(out=g2(m2), in_=v3(sel2_), axis=AX.X, op=ALU.max), reads=[K("rt_sel2")], writes=[K("rt_m2")])
                P.op("dve", lambda e: e.tensor_tensor(out=v3(eq2), in0=v3(sel2_), in1=bc4(m2), op=ALU.is_equal), reads=[K("rt_sel2"), K("rt_m2")], writes=[K("rt_eq2")])
                P.op("dve", lambda e: e.tensor_tensor(out=g2(gs), in0=g2(m1), in1=g2(m2), op=ALU.add), reads=[K("rt_m1"), K("rt_m2")], writes=[K("rt_gs")])
                P.op("dve", lambda e: e.tensor_reduce(out=bm[:, TS], in_=g3(gs), axis=AX.X, op=ALU.max), reads=[K("rt_gs")], writes=[K("rt_bm")])
                P.op("dve", lambda e: e.tensor_tensor(out=g3(geq), in0=g3(gs), in1=bm[:, TS].unsqueeze(2).to_broadcast([128, nT, 4]), op=ALU.is_equal),
                     reads=[K("rt_gs"), K("rt_bm")], writes=[K("rt_geq")])
                P.op("dve", lambda e: e.tensor_tensor(out=eq1[:, TS, :], in0=eq1[:, TS, :], in1=eq2[:, TS, :], op=ALU.add), reads=[K("rt_eq2")], writes=[K("rt_eq1")])
                P.op("dve", lambda e: e.tensor_tensor(out=v3(eq1), in0=v3(eq1), in1=bc4(geq), op=ALU.mult), reads=[K("rt_geq")], writes=[K("rt_eq1")])
                P.op("dve", lambda e: e.tensor_tensor(out=w_[:, TS, :], in0=eq1[:, TS, :], in1=s_all[:, TS, :], op=ALU.mult),
                     reads=[K("rt_eq1"), K("rt_eq2"), K("rt_m2")] + skeys, writes=[K("rt_w"), K("rt_sel2")])
                P.op("dve", lambda e: e.tensor_reduce(out=ws[:, TS], in_=w_[:, TS, :], axis=AX.X, op=ALU.add), reads=[K("rt_w")], writes=[K("rt_ws")])
                P.op("dve", lambda e: e.reciprocal(out=ws[:, TS], in_=ws[:, TS]), writes=[K("rt_ws")])
                P.op("dve", lambda e: e.tensor_tensor(out=cw16[:, TS, :], in0=w_[:, TS, :], in1=ws[:, TS].unsqueeze(2).to_broadcast([128, nT, NE]), op=ALU.mult),
                     reads=[K("rt_w"), K("rt_ws")], writes=[f"moe_cw{t_}" for t_ in range(Ta, Tb)])

            def after_blk(t0, n):
                route(t0, n)
                t1_ = t0 + n
                for (bt0, bn, _c) in _blocks(lo, hi):
                    if bt0 + bn == t1_:
                        route_batch(bt0 // 128, (bt0 + bn) // 128)

            norm_mod(cv, A2, SH2, l, lo, hi, out_bf=hT, out_key=lambda t0: f"hT_{t0 // 256}", out_f32=h32s, after_block=after_blk, bs=256,
                     skey=lambda c, t0: f"sT{c}_{t0 // 256}")

            blocks = _blocks(lo, hi)
            items = [(e_, b_) for e_ in range(NE) for b_ in range(len(blocks))]

            def load_w(e_):
                bi = e_ % 2
                P.dma("pool", f"wg{bi}", lambda e: e.dma_start(out=wgb[bi], in_=d_wg[l, e_], max_dma_last_dim=4096), writes=[f"moe_wg{bi}"])
                P.dma("pool", f"wu{bi}", lambda e: e.dma_start(out=wub[bi], in_=d_wu[l, e_], max_dma_last_dim=4096), writes=[f"moe_wu{bi}"])
                P.dma("pool", f"wd{bi}", lambda e: e.dma_start(out=wdb[bi], in_=d_wd[l, e_], max_dma_last_dim=4096), writes=[f"moe_wd{bi}"])

            cwcol = [cv.take([128, 128], BF16) for _ in range(4)]
            cwc_ctr = [0]

            def stage1(i):
                e_, b_ = items[i]
                t0, n, col = blocks[b_]
                bi = e_ % 2
                ai = i % 2
                hkeys = [f"hT_{k_}" for k_ in range(t0 // 256, (t0 + n + 255) // 256)]
                for sub in range(n // 128):
                    tix = (t0 + sub * 128) // 128
                    ci_ = cwc_ctr[0] % 4
                    cwc_ctr[0] += 1
                    P.op("dve", lambda e, tix=tix, ci_=ci_: e.tensor_copy(out=cwcol[ci_], in_=cw16[:, tix, e_:e_ + 1].to_broadcast([128, 128])),
                         reads=[f"moe_cw{tix}"], writes=[f"moe_cwcol{ci_}"])
                    P.op("pe", mm(pb[6][:, sub * 128:(sub + 1) * 128], cwcol[ci_], ident_b, True, True),
                         reads=[f"moe_cwcol{ci_}", "cb"], writes=[PB[6]])
                P.op("act", lambda e: e.copy(out=cwb[ai][:, 0:n], in_=pb[6][:, 0:n]), reads=[PB[6]], writes=[f"moe_cwb{ai}"])
                for fc in range(4):
                    pg, pu = pb[(fc % 2) * 2], pb[(fc % 2) * 2 + 1]
                    kg, ku = PB[(fc % 2) * 2], PB[(fc % 2) * 2 + 1]
                    for c in range(8):
                        P.op("pe", mm(pg[:, 0:n], wgb[bi][:, c, fc * 128:(fc + 1) * 128], hT[:, c, t0:t0 + n], c == 0, c == 7),
                             reads=[f"moe_wg{bi}"] + hkeys, writes=[kg])
                    for c in range(8):
                        P.op("pe", mm(pu[:, 0:n], wub[bi][:, c, fc * 128:(fc + 1) * 128], hT[:, c, t0:t0 + n], c == 0, c == 7),
                             reads=[f"moe_wu{bi}"] + hkeys, writes=[ku])
                    si = fc % 2
                    P.op("act", lambda e, pg=pg, si=si: e.activation(out=sg[si][:, 0:n], in_=pg[:, 0:n], func=AF.Silu), reads=[kg], writes=[f"moe_sg{si}"])
                    P.op("dve", lambda e, pu=pu, si=si: e.tensor_tensor(out=t1[si][:, 0:n], in0=sg[si][:, 0:n], in1=pu[:, 0:n], op=ALU.mult),
                         reads=[f"moe_sg{si}", ku], writes=[f"moe_t1{si}"])
                    P.op("pool", lambda e, si=si, fc=fc: e.tensor_tensor(out=aT[ai][:, fc, 0:n], in0=t1[si][:, 0:n], in1=cwb[ai][:, 0:n], op=ALU.mult),
                         reads=[f"moe_t1{si}", f"moe_cwb{ai}"], writes=[f"moe_aT{ai}"])

            def stage2(i):
                e_, b_ = items[i]
                t0, n, col = blocks[b_]
                bi = e_ % 2
                ai = i % 2
                for dc in range(8):
                    pd, kd = pb[4 + dc % 2], PB[4 + dc % 2]
                    for fc in range(4):
                        P.op("pe", mm(pd[:, 0:n], wdb[bi][:, fc, dc * 128:(dc + 1) * 128], aT[ai][:, fc, 0:n], fc == 0, fc == 3),
                             reads=[f"moe_wd{bi}", f"moe_aT{ai}"], writes=[kd])
                    sk = [f"sT{dc}_{k_}" for k_ in range(t0 // 256, (t0 + n + 255) // 256)]
                    P.op("dve", lambda e, dc=dc, pd=pd: e.scalar_tensor_tensor(
                        out=sT[:, dc, t0:t0 + n], in0=pd[:, 0:n], scalar=G2(l, dc, col), in1=sT[:, dc, t0:t0 + n], op0=ALU.mult, op1=ALU.add),
                        reads=[kd, f"modT{l}"] + sk, writes=sk)

            load_w(0)
            for i in range(len(items)):
                e_, b_ = items[i]
                stage1(i)
                if i > 0:
                    stage2(i - 1)
                if b_ == 0 and e_ + 1 < NE:
                    load_w(e_ + 1)
            stage2(len(items) - 1)

        def mlstm(l):
            cv = Carver()
            wgt = cv.take([128, 8, 16], BF16)
            mgb = cv.take([128, 16])
            mhg = cv.take([128, 256])
            cvw = cv.take([128, 8, 4])
            G = cv.take([128, NTILE, 16])
            Lg = cv.take([128, NTILE, 8])
            EE = cv.take([128, NTILE, 24])
            EB = cv.take([128, NTILE, 8])
            arg = cv.take([128, 24])
            P.dma("pool", "m_wgt", lambda e: e.dma_start(out=wgt, in_=d_mwin[:, :, 3072:3088]), writes=["m_wgt"])
            P.dma("sp", "m_mgb", lambda e: e.dma_start(out=mgb, in_=d_mgb), writes=["m_mgb"])
            P.dma("sp", "m_cvw", lambda e: e.dma_start(out=cvw, in_=d_mconv), writes=["m_cvw"])

            norm_mod(cv, A1, SH1, l, 0, NT, out_bf=hT, out_key="hT", bs=256)

            for tl in range(NTILE):
                ts_ = slice(tl * 128, (tl + 1) * 128)
                for c in range(8):
                    P.op("pe", mm(pb[0][:, 0:16], hT[:, c, ts_], wgt[:, c, :], c == 0, c == 7), reads=["hT", "m_wgt"], writes=[PB[0]])
                P.op("dve", lambda e, tl=tl: e.tensor_tensor(out=G[:, tl, :], in0=pb[0][:, 0:16], in1=mgb, op=ALU.add), reads=[PB[0], "m_mgb"], writes=["m_G"])
                for k, src in enumerate((slice(4, 8), slice(12, 16))):
                    P.op("act", lambda e, tl=tl, k=k, src=src: e.activation(out=Lg[:, tl, 4 * k:4 * k + 4], in_=G[:, tl, src], func=AF.Exp, scale=-1.0),
                         reads=["m_G"], writes=["m_L"])
                P.op("dve", lambda e, tl=tl: e.tensor_scalar(out=Lg[:, tl, :], in0=Lg[:, tl, :], scalar1=1.0, scalar2=None, op0=ALU.add), reads=["m_L"], writes=["m_L"])
                P.op("act", lambda e, tl=tl: e.activation(out=Lg[:, tl, :], in_=Lg[:, tl, :], func=AF.Ln), reads=["m_L"], writes=["m_L"])
                q_ = pb[1]
                for j, (mat, cs) in enumerate((("tF", slice(0, 4)), ("tB", slice(4, 8)), ("sF", slice(0, 4)), ("sB", slice(4, 8)))):
                    P.op("pe", mm(q_[:, 4 * j:4 * j + 4], cf[:, CI[mat], :], Lg[:, tl, cs], True, True), reads=["cf", "m_L"], writes=[PB[1]])
                P.op("pe", mm(q_[:, 16:24], cf[:, CI["ones"], :], Lg[:, tl, :], True, True), reads=["cf", "m_L"], writes=[PB[1]])
                P.op("dve", lambda e, tl=tl: e.tensor_tensor(out=arg[:, 0:4], in0=G[:, tl, 0:4], in1=q_[:, 0:4], op=ALU.add), reads=["m_G"], writes=["m_arg", PB[1]])
                P.op("dve", lambda e, tl=tl: e.tensor_tensor(out=arg[:, 4:8], in0=G[:, tl, 0:4], in1=q_[:, 8:12], op=ALU.subtract), reads=["m_G"], writes=["m_arg", PB[1]])
                P.op("dve", lambda e, tl=tl: e.tensor_tensor(out=arg[:, 8:12], in0=G[:, tl, 8:12], in1=q_[:, 4:8], op=ALU.add), reads=["m_G"], writes=["m_arg", PB[1]])
                P.op("dve", lambda e, tl=tl: e.tensor_tensor(out=arg[:, 12:16], in0=G[:, tl, 8:12], in1=q_[:, 12:16], op=ALU.subtract), reads=["m_G"], writes=["m_arg", PB[1]])
                P.op("dve", lambda e, tl=tl: e.tensor_copy(out=arg[:, 16:24], in_=q_[:, 0:8]), writes=["m_arg", PB[1]])
                P.op("act", lambda e, tl=tl: e.activation(out=EE[:, tl, :], in_=arg, func=AF.Exp), reads=["m_arg"], writes=["m_EE"])
                P.op("act", lambda e, tl=tl: e.activation(out=EB[:, tl, :], in_=q_[:, 16:24], func=AF.Exp, scale=-1.0), writes=["m_EB", PB[1]])

            assert cv.off <= MODS_BASE, (cv.off, MODS_BASE)
            P.barrier()
            wqk = cv.take([128, 8, 2, 128], BF16)
            wv = cv.take([128, 8, 256], BF16)
            wo = cv.take([128, 8, 256], BF16)
            wout = cv.take([128, 2, D], BF16)
            qT = cv.take([128, NT], BF16)
            kT = cv.take([128, NT], BF16)
            vaug = cv.take([128, NTILE, 257], BF16)
            ktok = cv.take([128, NTILE, 128], BF16)
            hfirst = cv.take([128, NTILE, 256], BF16)
            ybuf = [cv.take([128, 512]) for _ in range(2)]
            cvs = cv
            STf = [[cvs.take([128, 128], BF16) for _ in range(2)] for _ in range(2)]
            vt = [[cvs.take([128, 257], BF16) for _ in range(2)] for _ in range(2)]
            vt2 = [[cvs.take([128, 257], BF16) for _ in range(2)] for _ in range(2)]
            X = [cvs.take([128, 257]) for _ in range(2)]
            Xb = [[cvs.take([128, 257], BF16) for _ in range(2)] for _ in range(2)]
            numS = [[cvs.take([128, 257]) for _ in range(2)] for _ in range(2)]
            hs = [cvs.take([128, 256]) for _ in range(2)]
            hn = hs
            hg = [cvs.take([128, 256], BF16) for _ in range(2)]
            sgo = [cvs.take([128, 256]) for _ in range(2)]
            tmpo = [cvs.take([128, 4, 128]) for _ in range(2)]
            sm = [[cvs.take([128, 1]) for _ in range(2)] for _ in range(2)]
            xTt = [cvs.take([128, 2, 128], BF16) for _ in range(2)]
            P.op("pool", lambda e: e.memset(vaug[:, :, 256:257], 1.0), writes=["m_vaug"])
            pb6b = pb[6].bitcast(BF16)

            for h in range(4):
                P.dma("sp", "m_mhg", lambda e, h=h: e.dma_start(out=mhg, in_=d_mhg[:, h * 256:(h + 1) * 256]), writes=["m_mhg"])
                P.dma("pool", "m_wqk", lambda e, h=h: [e.dma_start(out=wqk[:, :, 0, :], in_=d_mwin[:, :, h * 128:(h + 1) * 128]),
                                                       e.dma_start(out=wqk[:, :, 1, :], in_=d_mwin[:, :, 512 + h * 128:512 + (h + 1) * 128])],
                      writes=["m_wqk"], n=2)
                P.dma("pool", "m_wv", lambda e, h=h: e.dma_start(out=wv, in_=d_mwin[:, :, 1024 + h * 256:1024 + (h + 1) * 256]), writes=["m_wv"])
                P.dma("pool", "m_wo", lambda e, h=h: e.dma_start(out=wo, in_=d_mwin[:, :, 2048 + h * 256:2048 + (h + 1) * 256]), writes=["m_wo"])
                P.dma("pool", "m_wout", lambda e, h=h: e.dma_start(out=wout, in_=d_mwout[:, 2 * h:2 * h + 2, :], max_dma_last_dim=4096), writes=["m_wout"])
                qk_blocks = [(0, CTX, 0, CTX)] + [(CTX + 410 * j, min(CTX + 410 * (j + 1), NT), CTX, NT) for j in range(5)]
                cnt_ = [0]
                for which, dstT, dkey in ((0, qT, "m_qT"), (1, kT, "m_kT")):
                    ch = (0 if which == 0 else 4) + h
                    for (a_, b_, A_, B_) in qk_blocks:
                        n = b_ - a_
                        a2, b2 = max(a_ - 1, A_), min(b_ + 1, B_)
                        off = a_ - a2
                        N_ = b2 - a2
                        bi_ = cnt_[0] % 2
                        cnt_[0] += 1
                        pq, kq = pb[bi_], PB[bi_]
                        yb, ky = ybuf[bi_], f"m_y{bi_}"
                        for c in range(8):
                            P.op("pe", mm(pq[:, 0:N_], wqk[:, c, which, :], hT[:, c, a2:b2], c == 0, c == 7), reads=["m_wqk", "hT"], writes=[kq])
                        P.op("dve", lambda e, pq=pq, yb=yb, off=off, n=n, ch=ch: e.tensor_scalar(out=yb[:, 0:n], in0=pq[:, off:off + n], scalar1=cvw[:, ch, 1:2], scalar2=cvw[:, ch, 3:4],
                                                                                       op0=ALU.mult, op1=ALU.add), reads=["m_cvw"], writes=[kq, ky])
                        if off == 1:
                            o0, i0, m0 = 0, 0, n
                        else:
                            o0, i0, m0 = 1, 0, n - 1
                        P.op("dve", lambda e, pq=pq, yb=yb, o0=o0, i0=i0, m0=m0, ch=ch: e.scalar_tensor_tensor(
                            out=yb[:, o0:o0 + m0], in0=pq[:, i0:i0 + m0], scalar=cvw[:, ch, 0:1], in1=yb[:, o0:o0 + m0], op0=ALU.mult, op1=ALU.add),
                            reads=["m_cvw"], writes=[kq, ky])
                        m2 = n if b2 > b_ else n - 1
                        P.op("dve", lambda e, pq=pq, yb=yb, off=off, m2=m2, ch=ch: e.scalar_tensor_tensor(
                            out=yb[:, 0:m2], in0=pq[:, off + 1:off + 1 + m2], scalar=cvw[:, ch, 2:3], in1=yb[:, 0:m2], op0=ALU.mult, op1=ALU.add),
                            reads=["m_cvw"], writes=[kq, ky])
                        if which == 0:
                            P.op("act", lambda e, yb=yb, n=n: e.activation(out=yb[:, 0:n], in_=yb[:, 0:n], func=AF.Silu), writes=[ky])
                            P.op("act", lambda e, yb=yb, n=n, a_=a_, b_=b_: e.activation(out=qT[:, a_:b_], in_=yb[:, 0:n], func=AF.Copy, scale=float(128 ** -0.5)),
                                 reads=[ky], writes=[dkey])
                        else:
                            P.op("act", lambda e, yb=yb, n=n, a_=a_, b_=b_: e.activation(out=kT[:, a_:b_], in_=yb[:, 0:n], func=AF.Silu), reads=[ky], writes=[dkey])
                for tl in range(NTILE):
                    ts_ = slice(tl * 128, (tl + 1) * 128)
                    pv, kv_ = pb[2 + tl % 2], PB[2 + tl % 2]
                    for c in range(8):
                        P.op("pe", mm(pv[:, 0:256], hT[:, c, ts_], wv[:, c, :], c == 0, c == 7), reads=["hT", "m_wv"], writes=[kv_])
                    P.op("act", lambda e, tl=tl, pv=pv: e.copy(out=vaug[:, tl, 0:256], in_=pv[:, 0:256]), reads=[kv_], writes=["m_vaug"])
                    tb = pb[4 + tl % 2].bitcast(BF16)
                    P.op("pe", lambda e, ts_=ts_, tb=tb: e.transpose(out=tb[:, 0:128], in_=kT[:, ts_], identity=ident_b), reads=["m_kT", "cb"], writes=[PB[4 + tl % 2]])
                    P.op("act", lambda e, tl=tl, tb=tb: e.copy(out=ktok[:, tl, :], in_=tb[:, 0:128]), reads=[PB[4 + tl % 2]], writes=["m_ktok"])

                orders = [list(range(NTILE)), [1, 0] + list(range(NTILE - 1, 1, -1))]
                if DEBUG_NT is not None:
                    orders = [o_[:DEBUG_NT] for o_ in orders]
                visited = set()
                for d_ in range(2):
                    P.op("dve", lambda e, d_=d_: e.memset(X[d_], 0.0), writes=[f"m_X{d_}"])
                    P.op("dve", lambda e, d_=d_: e.memset(Xb[d_][0], 0.0), writes=[f"m_Xb{d_}_0"])
                ncomp = [0]

                def step(d_, it, tl):
                    ts_ = slice(tl * 128, (tl + 1) * 128)
                    pi = it % 2
                    eoff = 0 if d_ == 0 else 8
                    doff = 4 if d_ else 0
                    second = tl in visited
                    visited.add(tl)
                    sc_cols = slice(128 * d_, 128 * d_ + 128)
                    P.op("pe", mm(pb[6][:, sc_cols], kT[:, ts_], qT[:, ts_], True, True), reads=["m_kT", "m_qT"], writes=[PB[6]])
                    msk = cb[:, CI["tF"], :] if d_ == 0 else cb[:, CI["tB"], :]
                    P.op("dve", lambda e: e.tensor_tensor(out=STf[d_][pi], in0=pb[6][:, sc_cols], in1=msk, op=ALU.mult),
                         reads=["cb"], writes=[PB[6], f"m_STf{d_}{pi}"])
                    e1 = EE[:, tl, eoff + h:eoff + h + 1]
                    e2 = EE[:, tl, eoff + 4 + h:eoff + 5 + h]
                    P.op("act", lambda e: e.activation(out=vt[d_][pi], in_=vaug[:, tl, :], func=AF.Copy, scale=e1),
                         reads=["m_vaug", "m_EE"], writes=[f"m_vt{d_}{pi}"])
                    P.op("dve", lambda e: e.tensor_scalar(out=vt2[d_][pi], in0=vaug[:, tl, :], scalar1=e2, scalar2=None, op0=ALU.mult),
                         reads=["m_vaug", "m_EE"], writes=[f"m_vt2{d_}{pi}"])
                    if second:
                        for c in range(8):
                            P.op("pe", mm(pb[7][:, 0:256], hT[:, c, ts_], wo[:, c, :], c == 0, c == 7), reads=["hT", "m_wo"], writes=[PB[7]])
                        P.op("act", lambda e: e.activation(out=sgo[d_], in_=pb[7][:, 0:256], func=AF.Exp, scale=-1.0), writes=[PB[7], f"m_sgo{d_}"])
                        P.op("dve", lambda e: e.tensor_scalar(out=sgo[d_], in0=sgo[d_], scalar1=1.0, scalar2=None, op0=ALU.add), writes=[f"m_sgo{d_}"])
                        P.op("act", lambda e: e.activation(out=sgo[d_], in_=sgo[d_], func=AF.Ln), writes=[f"m_sgo{d_}"])
                        P.op("act", lambda e: e.activation(out=sgo[d_], in_=sgo[d_], func=AF.Exp, scale=-1.0), writes=[f"m_sgo{d_}"])
                    num, knum = pb[d_], PB[d_]
                    P.op("pe", mm(num[:, 0:257], STf[d_][pi], vt[d_][pi], True, False), reads=[f"m_STf{d_}{pi}", f"m_vt{d_}{pi}"], writes=[knum])
                    kv, kkv = pb[2 + d_], PB[2 + d_]
                    P.op("pe", mm(kv[:, 0:257], ktok[:, tl, :], vt2[d_][pi], True, True), reads=["m_ktok", f"m_vt2{d_}{pi}"], writes=[kkv])
                    r0, r1 = it % 2, (it + 1) % 2
                    P.op("pe", mm(num[:, 0:257], qT[:, ts_], Xb[d_][r0], False, True), reads=["m_qT", f"m_Xb{d_}_{r0}", knum], writes=[knum])
                    ebc = EB[:, tl, doff + h:doff + h + 1]
                    P.op("dve", lambda e: e.scalar_tensor_tensor(out=X[d_], in0=X[d_], scalar=ebc, in1=kv[:, 0:257], op0=ALU.mult, op1=ALU.add),
                         reads=["m_EB", f"m_X{d_}"], writes=[kkv, f"m_X{d_}"])
                    P.op("act", lambda e: e.copy(out=Xb[d_][r1], in_=X[d_]), reads=[f"m_X{d_}"], writes=[f"m_Xb{d_}_{r1}"])
                    ns, kns = numS[d_][pi], f"m_numS{d_}{pi}"
                    P.op("act", lambda e: e.copy(out=ns, in_=num[:, 0:257]), writes=[knum, kns])
                    thr = EE[:, tl, 16 + doff + h:16 + doff + h + 1]
                    s0, ks0 = sm[d_][0], f"m_sm{d_}0"
                    P.op("act", lambda e: e.activation(out=s0, in_=ns[:, 256:257], func=AF.Abs), reads=[kns], writes=[ks0])
                    P.op("dve", lambda e: e.tensor_tensor(out=s0, in0=s0, in1=thr, op=ALU.max), reads=[ks0, "m_EE"], writes=[ks0])
                    P.op("dve", lambda e: e.reciprocal(out=s0, in_=s0), reads=[ks0], writes=[ks0])
                    if not second:
                        P.op("dve", lambda e: e.tensor_scalar(out=hfirst[:, tl, :], in0=ns[:, 0:256], scalar1=s0[:, 0:1], scalar2=None, op0=ALU.mult),
                             reads=[kns, ks0], writes=[f"m_hf{tl}"])
                        return
                    s1, ks1 = sm[d_][1], f"m_sm{d_}1"
                    P.op("dve", lambda e: e.scalar_tensor_tensor(out=hs[d_], in0=ns[:, 0:256], scalar=s0[:, 0:1], in1=hfirst[:, tl, :], op0=ALU.mult, op1=ALU.add),
                         reads=[kns, ks0, f"m_hf{tl}"], writes=[f"m_hs{d_}"])
                    P.op("act", lambda e: e.activation(out=hg[d_], in_=hs[d_], func=AF.Square, accum_out=s1), reads=[f"m_hs{d_}"], writes=[f"m_hg{d_}", ks1])
                    P.op("dve", lambda e: e.tensor_scalar(out=s1, in0=s1, scalar1=1.0 / 256, scalar2=EPS, op0=ALU.mult, op1=ALU.add), reads=[ks1], writes=[ks1])
                    P.op("act", lambda e: e.activation(out=s1, in_=s1, func=AF.Ln), reads=[ks1], writes=[ks1])
                    P.op("act", lambda e: e.activation(out=s1, in_=s1, func=AF.Exp, scale=-0.5), reads=[ks1], writes=[ks1])
                    P.op("dve", lambda e: e.scalar_tensor_tensor(out=hs[d_], in0=hs[d_], scalar=s1[:, 0:1], in1=mhg, op0=ALU.mult, op1=ALU.mult),
                         reads=[f"m_hs{d_}", ks1, "m_mhg"], writes=[f"m_hs{d_}"])
                    P.op("dve", lambda e: e.tensor_tensor(out=hg[d_], in0=hs[d_], in1=sgo[d_], op=ALU.mult), reads=[f"m_hs{d_}", f"m_sgo{d_}", f"m_hg{d_}"], writes=[f"m_hg{d_}"])
                    for fc in range(2):
                        P.op("pe", lambda e, fc=fc: e.transpose(out=pb6b[:, 512 + fc * 128:512 + (fc + 1) * 128], in_=hg[d_][:, fc * 128:(fc + 1) * 128], identity=ident_b),
                             reads=[f"m_hg{d_}", "cb"], writes=[PB[6]])
                    xi = ncomp[0] % 2
                    ncomp[0] += 1
                    P.op("act", lambda e: e.copy(out=xTt[xi], in_=pb6b[:, 512:768].rearrange("p (a b) -> p a b", a=2)), writes=[PB[6], f"m_xT{xi}"])
                    col = 1 if tl < 2 else 0
                    for half in range(2):
                        po, ko = pb[4 + half], PB[4 + half]
                        for j in range(4):
                            dc = half * 4 + j
                            for fc in range(2):
                                P.op("pe", mm(po[:, j * 128:(j + 1) * 128], wout[:, fc, dc * 128:(dc + 1) * 128], xTt[xi][:, fc, :], fc == 0, fc == 1), reads=["m_wout", f"m_xT{xi}"], writes=[ko])
                        g1b = modT[l][:, 16 + half * 4:16 + half * 4 + 4, col:col + 1].to_broadcast([128, 4, 128])
                        P.op("dve", lambda e, po=po, g1b=g1b, half=half: e.tensor_tensor(out=tmpo[half], in0=po[:, :].rearrange("p (a b) -> p a b", a=4), in1=g1b, op=ALU.mult),
                             reads=[f"modT{l}"], writes=[ko, f"m_tmpo{half}"])
                        keys = SKEY[half * 4:half * 4 + 4]
                        P.op("dve", lambda e, half=half: e.tensor_tensor(out=sT[:, half * 4:half * 4 + 4, ts_], in0=sT[:, half * 4:half * 4 + 4, ts_], in1=tmpo[half], op=ALU.add),
                             reads=[f"m_tmpo{half}"] + keys, writes=keys)

                for it in range(len(orders[0])):
                    step(0, it, orders[0][it])
                    step(1, it, orders[1][it])

        def hgrn(l):
            P.barrier()
            cv = Carver()
            hlb = cv.take([128, 2, 16])
            lbT = cv.take([128, 16])
            omlb = cv.take([128, 16])
            hhg = cv.take([128, 128])
            rst = cv.take([128, 512])
            P.dma("sp", "h_hlb", lambda e: e.dma_start(out=hlb, in_=d_hlb), writes=["h_hlb"])
            P.dma("sp", "h_rst", lambda e: e.dma_start(out=rst, in_=d_rst[:, 0:512]), writes=["h_rst"])
            P.op("dve", lambda e: e.tensor_tensor(out=lbT, in0=hlb[:, 1, :], in1=hlb[:, 0, :], op=ALU.subtract), reads=["h_hlb"], writes=["h_lb"])
            P.op("act", lambda e: e.activation(out=omlb, in_=lbT, func=AF.Exp), reads=["h_lb"], writes=["h_omlb"])
            P.op("act", lambda e: e.activation(out=lbT, in_=lbT, func=AF.Exp, scale=-1.0), reads=["h_lb", "h_omlb"], writes=["h_lb"])
            for t_, k_ in ((omlb, "h_omlb"), (lbT, "h_lb")):
                P.op("dve", lambda e, t_=t_: e.tensor_scalar(out=t_, in0=t_, scalar1=1.0, scalar2=None, op0=ALU.add), writes=[k_])
                P.op("dve", lambda e, t_=t_: e.reciprocal(out=t_, in_=t_), writes=[k_])

            ptmp = [cv.take([128, SEQ]) for _ in range(2)]
            for c in range(8):
                pt = ptmp[c % 2]
                src = sT[:, c, CTX:NT].rearrange("p (row col) -> p col row", col=64)
                P.op("dve" if c % 2 == 0 else "pool", lambda e, pt=pt, src=src: e.tensor_copy(out=pt.rearrange("p (col row) -> p col row", row=32), in_=src),
                     reads=[SKEY[c]], writes=[f"h_ptmp{c % 2}"])
                P.op("act", lambda e, pt=pt, c=c: e.copy(out=sT[:, c, CTX:NT], in_=pt), reads=[f"h_ptmp{c % 2}"], writes=[SKEY[c]])
            P.barrier()
            cv.off -= 2 * SEQ * 4
            norm_base = cv.off
            norm_mod(cv, A1, SH1, l, 0, NT, out_bf=hT, out_key="hT", bs=256)
            norm_end = cv.off
            P.barrier()

            wq = cv.take([128, 8, 128], BF16)
            wz = cv.take([128, 8, 2, 128], BF16)
            wi = cv.take([128, 8, 128], BF16)
            wgg = cv.take([128, 8, 128], BF16)
            wout = cv.take([128, D], BF16)
            qs = cv.take([128, NT], BF16)
            cvn = Carver()
            cvn.off = norm_base
            fTs = [cv.take([128, 512]), cvn.take([128, 512])]
            bTs = [cv.take([128, 512]), cvn.take([128, 512])]
            t32s = [cv.take([128, 512]), cv.take([128, 512])]
            kks = [cv.take([128, 512], BF16), cvn.take([128, 512], BF16)]
            assert cvn.off <= norm_end
            totcs = [cv.take([128, 4]) for _ in range(2)]
            rcs = [cv.take([128, 4]) for _ in range(2)]
            t32 = t32s[0]
            qtl = [cv.take([128, NT], BF16) for _ in range(2)]
            ktl = [cv.take([128, NT], BF16) for _ in range(2)]
            kht = [cv.take([128, NT], BF16) for _ in range(2)]
            ebT = [cv.take([128, NTILE]) for _ in range(2)]
            erT = [cv.take([128, NTILE]) for _ in range(2)]
            itok = cv.take([128, NTILE, 128], BF16)
            sgt = cv.take([128, NTILE, 128], BF16)
            gtmp = cv.take([128, 128])
            ofirst = cv.take([128, NTILE, 128], BF16)
            AT = [[cv.take([128, 128], BF16) for _ in range(2)] for _ in range(2)]
            khtok = [[cv.take([128, 128], BF16) for _ in range(2)] for _ in range(2)]
            S = [cv.take([128, 128]) for _ in range(2)]
            Sb = [[cv.take([128, 128], BF16) for _ in range(2)] for _ in range(2)]
            os_ = [cv.take([128, 128]) for _ in range(2)]
            og = [cv.take([128, 128], BF16) for _ in range(2)]
            sm = [cv.take([128, 1]) for _ in range(2)]
            xTt = [cv.take([128, 128], BF16) for _ in range(2)]
            pb7b = pb[7].bitcast(BF16)
            g1row = cv.take([128, D], BF16)
            for dc in range(8):
                P.op("pe", mm(pb[dc % 2][:, 0:128], modT[l][:, 16 + dc, 0:1].to_broadcast([128, 128]), ident_f, True, True), reads=[f"modT{l}", "cf"], writes=[PB[dc % 2]])
                P.op("act", lambda e, dc=dc: e.copy(out=g1row[:, dc * 128:(dc + 1) * 128], in_=pb[dc % 2][:, 0:128]), writes=[PB[dc % 2], "h_g1row"])
            REF = 64
            pieces = [(0, 512), (512, 512), (1024, 512), (1536, 512), (2048, 256)]

            def v128(a):
                return a.rearrange("p (c k) -> p c k", k=128)

            for h in range(8):
                P.dma("sp", "h_hhg", lambda e, h=h: e.dma_start(out=hhg, in_=d_hhg[:, h * 128:(h + 1) * 128]), writes=["h_hhg"])
                P.dma("pool", "h_wq", lambda e, h=h: e.dma_start(out=wq, in_=d_hwin[:, :, h * 128:(h + 1) * 128]), writes=["h_wq"])
                P.dma("pool", "h_wz", lambda e, h=h: [e.dma_start(out=wz[:, :, 0, :], in_=d_hwin[:, :, 1024 + h * 128:1024 + (h + 1) * 128]),
                                                      e.dma_start(out=wz[:, :, 1, :], in_=d_hwin[:, :, 2048 + h * 128:2048 + (h + 1) * 128])], writes=["h_wz"], n=2)
                P.dma("pool", "h_wi", lambda e, h=h: e.dma_start(out=wi, in_=d_hwin[:, :, 3072 + h * 128:3072 + (h + 1) * 128]), writes=["h_wi"])
                P.dma("pool", "h_wg", lambda e, h=h: e.dma_start(out=wgg, in_=d_hwin[:, :, 4096 + h * 128:4096 + (h + 1) * 128]), writes=["h_wg"])
                P.dma("pool", "h_wout", lambda e, h=h: e.dma_start(out=wout, in_=d_hwout[:, h, :], max_dma_last_dim=4096), writes=["h_wout"])
                P.op("dve", lambda e: e.tensor_tensor(out=wout, in0=wout, in1=g1row, op=ALU.mult), reads=["h_g1row"], writes=["h_wout"])
                for bi_, (t0, n, col) in enumerate(_blocks(0, NT)):
                    pq, kq = pb[bi_ % 2], PB[bi_ % 2]
                    for c in range(8):
                        P.op("pe", mm(pq[:, 0:n], wq[:, c, :], hT[:, c, t0:t0 + n], c == 0, c == 7), reads=["h_wq", "hT"], writes=[kq])
                    P.op("act", lambda e, n=n, pq=pq: e.activation(out=t32[:, 0:n], in_=pq[:, 0:n], func=AF.Exp, scale=-1.0), writes=[kq, "h_t32_0"])
                    P.op("act", lambda e, n=n: e.activation(out=t32[:, 0:n], in_=t32[:, 0:n], func=AF.Ln, bias=1.0), writes=["h_t32_0"])
                    P.op("act", lambda e, n=n: e.activation(out=t32[:, 0:n], in_=t32[:, 0:n], func=AF.Exp, scale=-1.0), writes=["h_t32_0"])
                    P.op("dve", lambda e, t0=t0, n=n, pq=pq: e.tensor_tensor(out=qs[:, t0:t0 + n], in0=t32[:, 0:n], in1=pq[:, 0:n], op=ALU.mult),
                         reads=["h_t32_0"], writes=[kq, "h_qs"])
                for tl in range(NTILE):
                    ts_ = slice(tl * 128, (tl + 1) * 128)
                    pi_, ki_ = pb[2 + tl % 2], PB[2 + tl % 2]
                    for c in range(8):
                        P.op("pe", mm(pi_[:, 0:128], hT[:, c, ts_], wi[:, c, :], c == 0, c == 7), reads=["hT", "h_wi"], writes=[ki_])
                    if tl >= 2:
                        for c in range(8):
                            P.op("pe", mm(pi_[:, 128:256], hT[:, c, ts_], wgg[:, c, :], c == 0, c == 7), reads=["hT", "h_wg"], writes=[ki_])
                    P.op("act", lambda e, tl=tl, pi_=pi_: e.copy(out=itok[:, tl, :], in_=pi_[:, 0:128]), writes=[ki_, "h_itok"])
                    if tl >= 2:
                        P.op("act", lambda e, pi_=pi_: e.activation(out=gtmp, in_=pi_[:, 128:256], func=AF.Exp, scale=-1.0), writes=[ki_, "h_gtmp"])
                        P.op("act", lambda e: e.activation(out=gtmp, in_=gtmp, func=AF.Ln, bias=1.0), writes=["h_gtmp"])
                        P.op("act", lambda e: e.activation(out=gtmp, in_=gtmp, func=AF.Exp, scale=-1.0), writes=["h_gtmp"])
                        P.op("dve", lambda e, tl=tl, pi_=pi_: e.tensor_tensor(out=sgt[:, tl, :], in0=gtmp, in1=pi_[:, 128:256], op=ALU.mult), reads=["h_gtmp"], writes=[ki_, "h_sgt"])

                def precompute(d_, p0, pn, pk):
                    fT, bT, t32, kk, totc, rc = fTs[d_], bTs[d_], t32s[d_], kks[d_], totcs[d_], rcs[d_]
                    kx = f"_{d_}"
                    lbc = lbT[:, d_ * 8 + h:d_ * 8 + h + 1]
                    omc = omlb[:, d_ * 8 + h:d_ * 8 + h + 1]
                    nt = pn // 128
                    tsl = slice(p0 // 128, p0 // 128 + nt)
                    ps_ = slice(p0, p0 + pn)
                    pz, kz = pb[4 + d_], PB[4 + d_]
                    for c in range(8):
                        P.op("pe", mm(pz[:, 0:pn], wz[:, c, d_, :], hT[:, c, ps_], c == 0, c == 7), reads=["h_wz", "hT"], writes=[kz])
                    P.op("act", lambda e: e.activation(out=fT[:, 0:pn], in_=pz[:, 0:pn], func=AF.Exp, scale=-1.0), writes=[kz, "h_fT" + kx])
                    P.op("act", lambda e: e.activation(out=fT[:, 0:pn], in_=fT[:, 0:pn], func=AF.Ln, bias=1.0), writes=["h_fT" + kx])
                    P.op("act", lambda e: e.activation(out=fT[:, 0:pn], in_=fT[:, 0:pn], func=AF.Exp, scale=-1.0), writes=["h_fT" + kx])
                    P.op("dve", lambda e: e.tensor_scalar(out=fT[:, 0:pn], in0=fT[:, 0:pn], scalar1=omc, scalar2=lbc, op0=ALU.mult, op1=ALU.add),
                         reads=["h_lb", "h_omlb"], writes=["h_fT" + kx])
                    P.op("dve", lambda e: e.tensor_scalar(out=kk[:, 0:pn], in0=fT[:, 0:pn], scalar1=-1.0, scalar2=1.0, op0=ALU.mult, op1=ALU.add), reads=["h_fT" + kx], writes=["h_kk" + kx])
                    P.op("act", lambda e: e.activation(out=fT[:, 0:pn], in_=fT[:, 0:pn], func=AF.Ln), reads=["h_kk" + kx], writes=["h_fT" + kx])
                    P.op("dve", lambda e: e.tensor_tensor_scan(out=bT[:, 0:pn], data0=rst[:, 0:pn], data1=fT[:, 0:pn], initial=0.0, op0=ALU.mult, op1=ALU.add),
                         reads=["h_rst", "h_fT" + kx], writes=["h_bT" + kx])
                    P.op("dve", lambda e: e.tensor_copy(out=totc[:, 0:nt], in_=v128(bT[:, 0:pn])[:, :, 127]), reads=["h_bT" + kx], writes=["h_totc" + kx])
                    totb = totc[:, 0:nt].unsqueeze(2).to_broadcast([128, nt, 128])
                    P.op("act", lambda e: e.activation(out=ebT[d_][:, tsl], in_=totc[:, 0:nt], func=AF.Exp), reads=["h_totc" + kx], writes=[f"h_ebT{pk}"])
                    if d_ == 0:
                        P.op("dve", lambda e: e.tensor_tensor(out=v128(t32[:, 0:pn]), in0=totb, in1=v128(bT[:, 0:pn]), op=ALU.subtract),
                             reads=["h_bT" + kx, "h_totc" + kx], writes=["h_t32" + kx])
                    else:
                        P.op("dve", lambda e: e.tensor_tensor(out=t32[:, 0:pn], in0=bT[:, 0:pn], in1=fT[:, 0:pn], op=ALU.subtract), reads=["h_bT" + kx, "h_fT" + kx], writes=["h_t32" + kx])
                        P.op("dve", lambda e: e.tensor_tensor(out=v128(bT[:, 0:pn]), in0=totb, in1=v128(t32[:, 0:pn]), op=ALU.subtract),
                             reads=["h_totc" + kx, "h_t32" + kx], writes=["h_bT" + kx])
                    P.op("act", lambda e: e.activation(out=t32[:, 0:pn], in_=t32[:, 0:pn], func=AF.Exp), writes=["h_t32" + kx])
                    P.op("dve", lambda e: e.tensor_tensor(out=kht[d_][:, ps_], in0=t32[:, 0:pn], in1=kk[:, 0:pn], op=ALU.mult),
                         reads=["h_t32" + kx, "h_kk" + kx], writes=[f"h_kht{pk}"])
                    P.op("dve", lambda e: e.tensor_copy(out=rc[:, 0:nt], in_=v128(bT[:, 0:pn])[:, :, REF]), reads=["h_bT" + kx], writes=["h_rc" + kx])
                    rb = rc[:, 0:nt].unsqueeze(2).to_broadcast([128, nt, 128])
                    P.op("act", lambda e: e.activation(out=erT[d_][:, tsl], in_=rc[:, 0:nt], func=AF.Exp), reads=["h_rc" + kx], writes=[f"h_erT{pk}"])
                    P.op("dve", lambda e: e.tensor_tensor(out=v128(bT[:, 0:pn]), in0=v128(bT[:, 0:pn]), in1=rb, op=ALU.subtract), reads=["h_rc" + kx], writes=["h_bT" + kx])
                    P.op("act", lambda e: e.activation(out=t32[:, 0:pn], in_=bT[:, 0:pn], func=AF.Exp), reads=["h_bT" + kx, f"h_kht{pk}"], writes=["h_t32" + kx])
                    P.op("dve", lambda e: e.tensor_tensor(out=qtl[d_][:, ps_], in0=t32[:, 0:pn], in1=qs[:, ps_], op=ALU.mult),
                         reads=["h_t32" + kx, "h_qs"], writes=[f"h_qtl{pk}"])
                    P.op("act", lambda e: e.activation(out=t32[:, 0:pn], in_=bT[:, 0:pn], func=AF.Exp, scale=-1.0), reads=["h_bT" + kx, f"h_qtl{pk}"], writes=["h_t32" + kx])
                    P.op("dve", lambda e: e.tensor_tensor(out=ktl[d_][:, ps_], in0=t32[:, 0:pn], in1=kk[:, 0:pn], op=ALU.mult),
                         reads=["h_t32" + kx, "h_kk" + kx], writes=[f"h_ktl{pk}"])

                orders = [list(range(NTILE)), [1, 0] + list(range(NTILE - 1, 1, -1))]
                piecesD = [[(0, 512), (512, 512), (1024, 512), (1536, 512), (2048, 256)],
                           [(0, 256), (1792, 512), (1280, 512), (768, 512), (256, 512)]]
                pkey = {}
                for d_ in range(2):
                    for k_, (p0, pn) in enumerate(piecesD[d_]):
                        for tl in range(p0 // 128, (p0 + pn) // 128):
                            pkey[(d_, tl)] = f"{d_}_{k_}"
                visited = set()
                for d_ in range(2):
                    P.op("dve", lambda e, d_=d_: e.memset(S[d_], 0.0), writes=[f"h_S{d_}"])
                ncomp = [0]

                def step(d_, it, tl):
                    ts_ = slice(tl * 128, (tl + 1) * 128)
                    pi = it % 2
                    pk = pkey[(d_, tl)]
                    lat = tl >= 2
                    second = tl in visited
                    visited.add(tl)
                    o_, ko = pb[d_], PB[d_]
                    sc_cols = slice(128 * d_, 128 * d_ + 128)
                    if lat:
                        erc = erT[d_][:, tl:tl + 1]
                        P.op("act", lambda e: e.activation(out=Sb[d_][pi], in_=S[d_], func=AF.Copy, scale=erc), reads=[f"h_S{d_}", f"h_erT{pk}"], writes=[f"h_Sb{d_}_{pi}"])
                        P.op("pe", mm(pb[6][:, sc_cols], ktl[d_][:, ts_], qtl[d_][:, ts_], True, True), reads=[f"h_ktl{pk}", f"h_qtl{pk}"], writes=[PB[6]])
                        msk = cb[:, CI["tF"], :] if d_ == 0 else cb[:, CI["tB"], :]
                        P.op("dve", lambda e: e.tensor_tensor(out=AT[d_][pi], in0=pb[6][:, sc_cols], in1=msk, op=ALU.mult), reads=["cb"], writes=[PB[6], f"h_AT{d_}{pi}"])
                        P.op("pe", mm(o_[:, 0:128], AT[d_][pi], itok[:, tl, :], True, False), reads=[f"h_AT{d_}{pi}", "h_itok"], writes=[ko])
                    P.op("pe", lambda e: e.transpose(out=pb7b[:, sc_cols], in_=kht[d_][:, ts_], identity=ident_b), reads=[f"h_kht{pk}", "cb"], writes=[PB[7]])
                    P.op("act", lambda e: e.copy(out=khtok[d_][pi], in_=pb7b[:, sc_cols]), writes=[PB[7], f"h_khtok{d_}{pi}"])
                    kv, kkv = pb[2 + d_], PB[2 + d_]
                    P.op("pe", mm(kv[:, 0:128], khtok[d_][pi], itok[:, tl, :], True, True), reads=[f"h_khtok{d_}{pi}", "h_itok"], writes=[kkv])
                    if lat:
                        P.op("pe", mm(o_[:, 0:128], qtl[d_][:, ts_], Sb[d_][pi], False, True), reads=[f"h_qtl{pk}", f"h_Sb{d_}_{pi}", ko], writes=[ko])
                    ebc = ebT[d_][:, tl:tl + 1]
                    P.op("dve", lambda e: e.scalar_tensor_tensor(out=S[d_], in0=S[d_], scalar=ebc, in1=kv[:, 0:128], op0=ALU.mult, op1=ALU.add),
                         reads=[f"h_ebT{pk}", f"h_S{d_}"], writes=[kkv, f"h_S{d_}"])
                    if not lat:
                        return
                    if not second:
                        P.op("act", lambda e: e.copy(out=ofirst[:, tl, :], in_=o_[:, 0:128]), writes=[ko, f"h_of{tl}"])
                        return
                    s1, ks1 = sm[d_], f"h_sm{d_}"
                    P.op("dve", lambda e: e.tensor_tensor(out=os_[d_], in0=o_[:, 0:128], in1=ofirst[:, tl, :], op=ALU.add), reads=[f"h_of{tl}"], writes=[ko, f"h_os{d_}"])
                    P.op("act", lambda e: e.activation(out=og[d_], in_=os_[d_], func=AF.Square, accum_out=s1), reads=[f"h_os{d_}"], writes=[f"h_og{d_}", ks1])
                    P.op("dve", lambda e: e.tensor_scalar(out=s1, in0=s1, scalar1=1.0 / 128, scalar2=EPS, op0=ALU.mult, op1=ALU.add), writes=[ks1])
                    P.op("act", lambda e: e.activation(out=s1, in_=s1, func=AF.Ln), writes=[ks1])
                    P.op("act", lambda e: e.activation(out=s1, in_=s1, func=AF.Exp, scale=-0.5), writes=[ks1])
                    P.op("dve", lambda e: e.scalar_tensor_tensor(out=os_[d_], in0=os_[d_], scalar=s1[:, 0:1], in1=hhg, op0=ALU.mult, op1=ALU.mult),
                         reads=[ks1, "h_hhg"], writes=[f"h_os{d_}"])
                    P.op("dve", lambda e: e.tensor_tensor(out=og[d_], in0=os_[d_], in1=sgt[:, tl, :], op=ALU.mult), reads=[f"h_os{d_}", "h_sgt"], writes=[f"h_og{d_}"])
                    P.op("pe", lambda e: e.transpose(out=pb7b[:, 256:384], in_=og[d_], identity=ident_b), reads=[f"h_og{d_}", "cb"], writes=[PB[7]])
                    xi = ncomp[0] % 2
                    ncomp[0] += 1
                    P.op("act", lambda e: e.copy(out=xTt[xi], in_=pb7b[:, 256:384]), writes=[PB[7], f"h_xT{xi}"])
                    c0 = (tl * 128 - CTX) // 32
                    for half in range(2):
                        po, kpo = pb[4 + half], PB[4 + half]
                        for j in range(4):
                            dc = half * 4 + j
                            P.op("pe", mm(po[:, j * 128:(j + 1) * 128], wout[:, dc * 128:(dc + 1) * 128], xTt[xi], True, True), reads=["h_wout", f"h_xT{xi}"], writes=[kpo])
                        keys = SKEY[half * 4:half * 4 + 4]
                        dstv = sT[:, half * 4:half * 4 + 4, ts_]
                        srcv = po[:, :].rearrange("p (d t) -> p d t", d=4)
                        P.op("dve", lambda e, dstv=dstv, srcv=srcv: e.tensor_tensor(out=dstv, in0=dstv, in1=srcv, op=ALU.add),
                             reads=keys, writes=[kpo] + keys)

                done = [0, 0]
                for k_ in range(5):
                    P.interleave([lambda d_=d_: precompute(d_, piecesD[d_][k_][0], piecesD[d_][k_][1], f"{d_}_{k_}") for d_ in range(2)])
                    avail = [min(NTILE, 4 * (k_ + 1)) if k_ < 4 else NTILE, min(NTILE, 2 + 4 * k_)]
                    if DEBUG_NT is not None:
                        avail = [min(a_, DEBUG_NT) for a_ in avail]
                    while done[0] < avail[0] or done[1] < avail[1]:
                        for d_ in range(2):
                            if done[d_] < avail[d_]:
                                step(d_, done[d_], orders[d_][done[d_]])
                                done[d_] += 1

        def final():
            P.barrier()
            cv = Carver()
            ob = [cv.take([128, 512]) for _ in range(2)]
            outs = []
            cnt = [0]
            sq = [cv.take([128, 512]) for _ in range(2)]
            rstd = cv.take([128, 512])
            for (t0, n, col) in _blocks(CTX, NT):
                for c in range(8):
                    P.op("act", lambda e, c=c, t0=t0, n=n: e.activation(out=sq[c % 2][:, 0:n], in_=sT[:, c, t0:t0 + n], func=AF.Square), reads=[SKEY[c]], writes=[f"nm_sq{c % 2}"])
                    P.op("pe", mm(pb[7][:, 0:n], ones_f, sq[c % 2][:, 0:n], c == 0, c == 7), reads=[f"nm_sq{c % 2}", "cf"], writes=[PB[7]])
                P.op("dve", lambda e, n=n: e.tensor_scalar(out=rstd[:, 0:n], in0=pb[7][:, 0:n], scalar1=1.0 / D, scalar2=EPS, op0=ALU.mult, op1=ALU.add), reads=[PB[7]], writes=["nm_rstd"])
                P.op("act", lambda e, n=n: e.activation(out=rstd[:, 0:n], in_=rstd[:, 0:n], func=AF.Sqrt), reads=["nm_rstd"], writes=["nm_rstd"])
                P.op("dve", lambda e, n=n: e.reciprocal(out=rstd[:, 0:n], in_=rstd[:, 0:n]), reads=["nm_rstd"], writes=["nm_rstd"])
                for c in range(8):
                    i = cnt[0] % 2
                    cnt[0] += 1
                    P.op("dve", lambda e, c=c, t0=t0, n=n, i=i: e.scalar_tensor_tensor(out=ob[i][:, 0:n], in0=sT[:, c, t0:t0 + n], scalar=gfin[:, c:c + 1], in1=rstd[:, 0:n],
                                                                                 op0=ALU.mult, op1=ALU.mult), reads=[SKEY[c], "gfin", "nm_rstd"], writes=[f"fin_ob{i}"])
                    outs.append(P.dma("sp", f"fin_ob{i}", lambda e, c=c, t0=t0, n=n, i=i: e.dma_start(out=d_out[:, c, t0 - CTX:t0 - CTX + n], in_=ob[i][:, 0:n]),
                                      reads=[f"fin_ob{i}"]))
            return outs

        outs = []
        if "mix0" in phases:
            mlstm(0)
        if "moe0" in phases:
            moe(0, 0, NT)
        if "mix1" in phases:
            hgrn(1)
        if "moe1" in phases:
            moe(1, CTX, NT)
        if "final" in phases:
            outs = final()
        if d_dump is not None:
            P.barrier()
            for c in range(8):
                outs.append(P.dma("sp", f"dump{c}", lambda e, c=c: e.dma_start(out=d_dump[:, c, :], in_=sT[:, c, :]), reads=[SKEY[c]]))
        P.emit(final_wait_ops=outs + dbg_outs)
    return nc


def _fm(v, lead=()):
    v = np.asarray(v, np.float32)
    k = v.shape[-1] // 128
    r = v.reshape(v.shape[:-1] + (k, 128))
    return np.ascontiguousarray(np.moveaxis(r, -1, 0))


def _wl(w):
    w = np.asarray(w, np.float32)
    K, N = w.shape
    return np.ascontiguousarray(w.reshape(K // 128, 128, N).transpose(1, 0, 2))


def prep_shared(x, c, ctx, c_ctx, ada_w, ada_b, norm_mix_g, norm_ffn_g, final_g,
                m_w_in, m_conv_w, m_conv_b, m_gate_b, m_head_g, m_w_out,
                h_w_in, h_lower_bounds, h_head_g, h_w_out,
                router_w, router_bias, e_w_gate, e_w_up, e_w_down):
    sh = {}
    sh["adaw"] = np.ascontiguousarray(np.asarray(ada_w, np.float32).reshape(2, 8, 128, 12, 512).transpose(0, 3, 2, 1, 4))
    sh["adab"] = np.ascontiguousarray(_fm(ada_b))
    sh["gmix"] = _fm(norm_mix_g)
    sh["gffn"] = _fm(norm_ffn_g)
    sh["gfin"] = _fm(final_g)
    sh["mwin"] = _wl(m_w_in[0])
    cw = _fm(m_conv_w[0])
    cbias = _fm(m_conv_b[0])
    sh["mconv"] = np.ascontiguousarray(np.concatenate([cw.transpose(0, 2, 1), cbias[:, :, None]], axis=2))
    sh["mgb"] = np.ascontiguousarray(np.broadcast_to(np.asarray(m_gate_b[0], np.float32)[None, :], (128, 16)))
    sh["mhg"] = np.ascontiguousarray(np.broadcast_to(np.asarray(m_head_g[0], np.float32)[None, :], (128, D)))
    sh["mwout"] = _wl(m_w_out[0])
    sh["hwin"] = _wl(h_w_in[0])
    sh["hlb"] = np.ascontiguousarray(_fm(h_lower_bounds))
    sh["hhg"] = np.ascontiguousarray(np.broadcast_to(np.asarray(h_head_g[0], np.float32)[None, :], (128, D)))
    sh["hwout"] = _wl(h_w_out[0])
    sh["rw"] = _wl(router_w)
    sh["rb"] = np.ascontiguousarray(np.broadcast_to(np.asarray(router_bias, np.float32)[None, None, :], (128, NTILE, NE)))
    sh["ewg"] = np.ascontiguousarray(np.asarray(e_w_gate, np.float32).reshape(2, NE, 8, 128, DEXP).transpose(0, 1, 3, 2, 4))
    sh["ewu"] = np.ascontiguousarray(np.asarray(e_w_up, np.float32).reshape(2, NE, 8, 128, DEXP).transpose(0, 1, 3, 2, 4))
    sh["ewd"] = np.ascontiguousarray(np.asarray(e_w_down, np.float32).reshape(2, NE, 4, 128, D).transpose(0, 1, 3, 2, 4))
    sh["cf"] = CARR
    sh["rst"] = RST
    return sh


def prep_core(b, x, c, ctx, c_ctx, s_override=None):
    if s_override is not None:
        s = s_override
    else:
        s = np.concatenate([np.asarray(ctx[b], np.float32), np.asarray(x[b], np.float32)], axis=0)
    xT = np.ascontiguousarray(s.reshape(NT, 8, 128).transpose(2, 1, 0))
    cc = np.stack([np.asarray(c[b], np.float32), np.asarray(c_ctx, np.float32)], axis=-1)
    cT = np.ascontiguousarray(cc.reshape(8, 128, 2).transpose(1, 0, 2))
    return {"xT": xT, "cT": cT}


_NC_CACHE = {}


def kernel(**inputs):
    x = inputs["x"]
    B = x.shape[0]
    sh = prep_shared(**inputs)
    if "full" not in _NC_CACHE:
        _NC_CACHE["full"] = build_program()
    nc = _NC_CACHE["full"]
    in_maps = []
    for b in range(B):
        m = dict(sh)
        m.update(prep_core(b, inputs["x"], inputs["c"], inputs["ctx"], inputs["c_ctx"]))
        in_maps.append(m)
    res = run_bass_kernel_spmd(nc, in_maps, core_ids=list(range(B)))
    out = np.empty((B, SEQ, D), np.float32)
    for b in range(B):
        oT = np.asarray(res.results[b]["outT"])
        out[b] = oT.transpose(2, 1, 0).reshape(64, 32, D).transpose(1, 0, 2).reshape(SEQ, D)
    return out
```
